# Optimizing a Trainium2 kernel written in Bass

```python
import math
import jax, jax.numpy as jnp
from jax import lax
import numpy as np

D_MODEL = 1024
BATCH = 8
SEQ = 8192
DEPTH = 2

N_MIXERS = 2
MEM_LEN = 256
HEAD_DIM = 64
MIX_WIDTH = D_MODEL
MEM_HEADS = 4
MEM_WIDTH = MEM_HEADS * HEAD_DIM
TOK_WIDTH = MIX_WIDTH - MEM_WIDTH
LRU_BLOCKS = TOK_WIDTH // HEAD_DIM
LRU_BLOCK = TOK_WIDTH // LRU_BLOCKS
CONV_W = 4
LRU_C = 8.0
ATTN_HEADS = TOK_WIDTH // HEAD_DIM
KV_LATENT = 128
IDX_HEADS = 4
IDX_DIM = 64
TOPK_MAX = 256
Q_BLOCK = 128
REL_BUCKETS = 32
REL_MAX_DIST = 128
N_EXPERTS = 32
TOP_K = 4
D_EXPERT = D_MODEL
SWIGLU_LIMIT = 7.0
SWIGLU_ALPHA = 1.702
EXPERT_BLOCK = 128
DN_ALPHA = (2 * DEPTH) ** 0.25
DN_BETA = (8 * DEPTH) ** -0.25
N_A = (DEPTH + 1) // 2
N_B = DEPTH // 2
W_IN_A = 2 * TOK_WIDTH + MEM_WIDTH
B_SPLITS = [TOK_WIDTH, TOK_WIDTH + KV_LATENT, TOK_WIDTH + KV_LATENT + IDX_HEADS * IDX_DIM,
            TOK_WIDTH + KV_LATENT + IDX_HEADS * IDX_DIM + IDX_DIM,
            TOK_WIDTH + KV_LATENT + IDX_HEADS * IDX_DIM + IDX_DIM + IDX_HEADS]
W_IN_B = B_SPLITS[-1] + MEM_WIDTH

kernel_name = "hybrid_rglru_dsa_moe_block"


def layer_norm(x, g, b, eps=1e-5):
    xf = x.astype(jnp.float32)
    mu = jnp.mean(xf, axis=-1, keepdims=True)
    xc = xf - mu
    var = jnp.mean(xc * xc, axis=-1, keepdims=True)
    return (xc * lax.rsqrt(var + eps) * g + b).astype(x.dtype)


def rms_norm(x, g, eps=1e-6):
    xf = x.astype(jnp.float32)
    return (xf * lax.rsqrt(jnp.mean(xf * xf, axis=-1, keepdims=True) + eps) * g).astype(x.dtype)


def t5_bucket(rel):
    n = jnp.maximum(rel, 0)
    max_exact = REL_BUCKETS // 2
    large = max_exact + (jnp.log(jnp.maximum(n, 1).astype(jnp.float32) / max_exact)
                         / math.log(REL_MAX_DIST / max_exact) * (REL_BUCKETS - max_exact)).astype(jnp.int32)
    large = jnp.minimum(large, REL_BUCKETS - 1)
    return jnp.where(n < max_exact, n, large)


def causal_dwconv(x, w, b):
    C = x.shape[-1]
    y = lax.conv_general_dilated(x, w[:, None, :], window_strides=(1,), padding=[(CONV_W - 1, 0)],
                                 dimension_numbers=('NWC', 'WIO', 'NWC'), feature_group_count=C)
    return y + b


def rg_lru(xc, w_r, b_r, w_i, b_i, lam):
    B_, S_, C = xc.shape
    xb = xc.reshape(B_, S_, LRU_BLOCKS, LRU_BLOCK)
    r = jax.nn.sigmoid(jnp.einsum('bsnc,ncd->bsnd', xb, w_r).reshape(B_, S_, C) + b_r)
    i = jax.nn.sigmoid(jnp.einsum('bsnc,ncd->bsnd', xb, w_i).reshape(B_, S_, C) + b_i)
    log_a = -LRU_C * r.astype(jnp.float32) * jax.nn.softplus(-lam.astype(jnp.float32))
    a = jnp.exp(log_a)
    u = jnp.sqrt(-jnp.expm1(2.0 * log_a)) * (i * xc).astype(jnp.float32)

    def combine(e1, e2):
        return (e1[0] * e2[0], e2[0] * e1[1] + e2[1])

    _, h = lax.associative_scan(combine, (a, u), axis=1)
    return h.astype(xc.dtype)


def memory_cross_attention(q, mem, w_mem_kv):
    B_, S_ = q.shape[:2]
    k, v = jnp.split(mem @ w_mem_kv, 2, axis=-1)
    qh = q.reshape(B_, S_, MEM_HEADS, HEAD_DIM)
    kh = k.reshape(B_, -1, MEM_HEADS, HEAD_DIM)
    vh = v.reshape(B_, -1, MEM_HEADS, HEAD_DIM)
    logits = jnp.einsum('bshd,bmhd->bhsm', qh, kh).astype(jnp.float32) * HEAD_DIM ** -0.5
    p = jax.nn.softmax(logits, axis=-1).astype(vh.dtype)
    return jnp.einsum('bhsm,bmhd->bshd', p, vh).reshape(B_, S_, MEM_WIDTH)


def dsa_attention(q, c_kv, iq, ik, iw, w_uk, w_uv, rel_bias):
    B_, S_ = c_kv.shape[:2]
    k_sel = min(TOPK_MAX, S_ // 4)
    nqb = S_ // Q_BLOCK
    q_lat = jnp.einsum('bshd,rhd->bshr', q, w_uk) * HEAD_DIM ** -0.5
    iw = iw.astype(jnp.float32) * (IDX_HEADS ** -0.5 * IDX_DIM ** -0.5)
    key_pos = jnp.arange(S_, dtype=jnp.int32)

    def to_blocks(t):
        return jnp.moveaxis(t.reshape(B_, nqb, Q_BLOCK, *t.shape[2:]), 1, 0)

    def block(args):
        ql, iqb, iwb, start = args
        qpos = start + jnp.arange(Q_BLOCK, dtype=jnp.int32)
        s = jnp.einsum('bqhd,bsd->bqhs', iqb, ik).astype(jnp.float32)
        score = jnp.einsum('bqhs,bqh->bqs', jax.nn.relu(s), iwb)
        score = jnp.where(key_pos[None, None, :] <= qpos[None, :, None], score, -jnp.inf)
        _, idx = lax.top_k(score, k_sel)
        c_sel = jax.vmap(lambda cb, ib: cb[ib])(c_kv, idx)
        rel = qpos[None, :, None] - idx
        bias = jnp.moveaxis(rel_bias[t5_bucket(rel)], -1, -2)
        logits = jnp.einsum('bqhr,bqkr->bqhk', ql, c_sel).astype(jnp.float32) + bias.astype(jnp.float32)
        logits = jnp.where((rel >= 0)[:, :, None, :], logits, -jnp.inf)
        p = jax.nn.softmax(logits, axis=-1).astype(c_sel.dtype)
        o_lat = jnp.einsum('bqhk,bqkr->bqhr', p, c_sel)
        return jnp.einsum('bqhr,rhd->bqhd', o_lat, w_uv)

    starts = jnp.arange(nqb, dtype=jnp.int32) * Q_BLOCK
    out = lax.map(block, (to_blocks(q_lat), to_blocks(iq), to_blocks(iw), starts))
    return jnp.moveaxis(out, 0, 1).reshape(B_, S_, ATTN_HEADS * HEAD_DIM)


def moe(x, router_w, router_b, w1, b1, w2, b2):
    B_, S_, D = x.shape
    xt = x.reshape(-1, D)
    T = xt.shape[0]
    logits = (xt @ router_w + router_b).astype(jnp.float32)
    top_v, top_e = lax.top_k(logits, TOP_K)
    gate = jax.nn.softmax(top_v, axis=-1)
    A = T * TOP_K
    e_flat = top_e.reshape(-1)
    g_flat = gate.reshape(-1).astype(x.dtype)
    tok_flat = jnp.arange(A, dtype=jnp.int32) // TOP_K
    order = jnp.argsort(e_flat)
    se = e_flat[order]
    counts = jnp.bincount(e_flat, length=N_EXPERTS)
    padded = (counts + EXPERT_BLOCK - 1) // EXPERT_BLOCK * EXPERT_BLOCK
    pend = jnp.cumsum(padded)
    pstart = pend - padded
    ustart = jnp.cumsum(counts) - counts
    dest = pstart[se] + jnp.arange(A, dtype=jnp.int32) - ustart[se]
    nblk = -(-A // EXPERT_BLOCK) + N_EXPERTS
    P = nblk * EXPERT_BLOCK
    row_tok = jnp.zeros((P,), jnp.int32).at[dest].set(tok_flat[order])
    row_g = jnp.zeros((P,), x.dtype).at[dest].set(g_flat[order])
    blk_e = jnp.minimum(jnp.searchsorted(pend, jnp.arange(nblk, dtype=jnp.int32) * EXPERT_BLOCK, side='right'),
                        N_EXPERTS - 1)

    def expert_block(args):
        tok, g, e = args
        h = xt[tok] @ w1[e] + b1[e]
        gt = jnp.minimum(h[:, :D_EXPERT], SWIGLU_LIMIT)
        up = jnp.clip(h[:, D_EXPERT:], -SWIGLU_LIMIT, SWIGLU_LIMIT)
        act = (up + 1.0) * (gt * jax.nn.sigmoid(SWIGLU_ALPHA * gt))
        return (act @ w2[e] + b2[e]) * g[:, None]

    y = lax.map(expert_block, (row_tok.reshape(nblk, EXPERT_BLOCK), row_g.reshape(nblk, EXPERT_BLOCK), blk_e))
    y = jax.ops.segment_sum(y.reshape(P, D), row_tok, num_segments=T)
    return y.reshape(B_, S_, D)


def setup_inputs(seed: int = 0) -> dict:
    key = jax.random.key(seed)
    ks = jax.random.split(key, 32)
    f32 = jnp.float32

    def nrm(k, shape, scale):
        return jax.random.normal(k, shape, f32) * scale

    u = jax.random.uniform(ks[9], (N_A, TOK_WIDTH), f32, 0.9, 0.999)
    p = u ** (1.0 / LRU_C)
    a_lambda = jnp.log(p) - jnp.log1p(-p)
    mem_kv_scale = jnp.concatenate([jnp.ones((MEM_WIDTH,), f32), jnp.full((MEM_WIDTH,), DN_BETA, f32)])
    return {
        'x': nrm(ks[0], (BATCH, SEQ, D_MODEL), 1.0),
        'mem': nrm(ks[1], (BATCH, MEM_LEN, D_MODEL), 1.0),
        'rel_bias': nrm(ks[2], (REL_BUCKETS, ATTN_HEADS), 0.5),
        'a_w_in': nrm(ks[3], (N_A, D_MODEL, W_IN_A), D_MODEL ** -0.5),
        'a_conv_w': nrm(ks[4], (N_A, CONV_W, TOK_WIDTH), CONV_W ** -0.5),
        'a_conv_b': nrm(ks[5], (N_A, TOK_WIDTH), 0.01),
        'a_wr': nrm(ks[6], (N_A, LRU_BLOCKS, LRU_BLOCK, LRU_BLOCK), LRU_BLOCK ** -0.5),
        'a_br': nrm(ks[7], (N_A, TOK_WIDTH), 0.01),
        'a_wi': nrm(ks[8], (N_A, LRU_BLOCKS, LRU_BLOCK, LRU_BLOCK), LRU_BLOCK ** -0.5),
        'a_bi': nrm(ks[10], (N_A, TOK_WIDTH), 0.01),
        'a_lambda': a_lambda,
        'b_w_in': nrm(ks[11], (N_B, D_MODEL, W_IN_B), D_MODEL ** -0.5),
        'b_kv_norm_g': 1.0 + nrm(ks[12], (N_B, KV_LATENT), 0.01),
        'b_w_uk': nrm(ks[13], (N_B, KV_LATENT, ATTN_HEADS, HEAD_DIM), KV_LATENT ** -0.5),
        'b_w_uv': nrm(ks[14], (N_B, KV_LATENT, ATTN_HEADS, HEAD_DIM), KV_LATENT ** -0.5 * DN_BETA),
        'b_idx_norm_g': 1.0 + nrm(ks[15], (N_B, IDX_DIM), 0.01),
        'b_idx_norm_b': nrm(ks[16], (N_B, IDX_DIM), 0.01),
        'w_mem_kv': nrm(ks[17], (DEPTH, D_MODEL, 2 * MEM_WIDTH), D_MODEL ** -0.5) * mem_kv_scale,
        'w_out': nrm(ks[18], (DEPTH, MIX_WIDTH, D_MODEL), MIX_WIDTH ** -0.5 * DN_BETA),
        'ln1_g': 1.0 + nrm(ks[19], (DEPTH, D_MODEL), 0.01),
        'ln1_b': nrm(ks[20], (DEPTH, D_MODEL), 0.01),
        'router_w': nrm(ks[21], (DEPTH, D_MODEL, N_EXPERTS), D_MODEL ** -0.5),
        'router_b': nrm(ks[22], (DEPTH, N_EXPERTS), 0.01),
        'exp_w1': nrm(ks[23], (DEPTH, N_EXPERTS, D_MODEL, 2 * D_EXPERT), D_MODEL ** -0.5),
        'exp_b1': nrm(ks[24], (DEPTH, N_EXPERTS, 2 * D_EXPERT), 0.01),
        'exp_w2': nrm(ks[25], (DEPTH, N_EXPERTS, D_EXPERT, D_MODEL), D_EXPERT ** -0.5 * DN_BETA),
        'exp_b2': nrm(ks[26], (DEPTH, N_EXPERTS, D_MODEL), 0.01),
        'ln2_g': 1.0 + nrm(ks[27], (DEPTH, D_MODEL), 0.01),
        'ln2_b': nrm(ks[28], (DEPTH, D_MODEL), 0.01),
    }


def reference(x, mem, rel_bias, a_w_in, a_conv_w, a_conv_b, a_wr, a_br, a_wi, a_bi, a_lambda,
              b_w_in, b_kv_norm_g, b_w_uk, b_w_uv, b_idx_norm_g, b_idx_norm_b,
              w_mem_kv, w_out, ln1_g, ln1_b, router_w, router_b, exp_w1, exp_b1, exp_w2, exp_b2,
              ln2_g, ln2_b):
    B_, S_, _ = x.shape
    for layer in range(DEPTH):
        j = layer // N_MIXERS
        if layer % N_MIXERS == 0:
            proj = x @ a_w_in[j]
            xb, gb, mq = jnp.split(proj, [TOK_WIDTH, 2 * TOK_WIDTH], axis=-1)
            xc = causal_dwconv(xb, a_conv_w[j], a_conv_b[j])
            h = rg_lru(xc, a_wr[j], a_br[j], a_wi[j], a_bi[j], a_lambda[j])
            tok = h * jax.nn.gelu(gb)
        else:
            proj = x @ b_w_in[j]
            q, c, iq, ik, iw, mq = jnp.split(proj, B_SPLITS, axis=-1)
            q = q.reshape(B_, S_, ATTN_HEADS, HEAD_DIM)
            c = rms_norm(c, b_kv_norm_g[j])
            iq = iq.reshape(B_, S_, IDX_HEADS, IDX_DIM)
            ik = layer_norm(ik, b_idx_norm_g[j], b_idx_norm_b[j])
            tok = dsa_attention(q, c, iq, ik, iw, b_w_uk[j], b_w_uv[j], rel_bias)
        mix = jnp.concatenate([tok, memory_cross_attention(mq, mem, w_mem_kv[layer])], axis=-1)
        x = layer_norm(DN_ALPHA * x + mix @ w_out[layer], ln1_g[layer], ln1_b[layer])
        ffn = moe(x, router_w[layer], router_b[layer], exp_w1[layer], exp_b1[layer], exp_w2[layer], exp_b2[layer])
        x = layer_norm(DN_ALPHA * x + ffn, ln2_g[layer], ln2_b[layer])
    return x
```

```python
from contextlib import ExitStack
import numpy as np
import concourse.bass as bass
import concourse.mybir as mybir
from concourse.bass_utils import run_bass_kernel_spmd

F32 = mybir.dt.float32
BF16 = mybir.dt.bfloat16
I32 = mybir.dt.int32
U32 = mybir.dt.uint32
AF = mybir.ActivationFunctionType
ALU = mybir.AluOpType
AX = mybir.AxisListType

S = 8192
D = 1024
NT = S // 128
MEM = 256
TOKW = 768
NE = 32
CAP = 1536
NROW = NE * CAP
RG = 384
ALPHA = float(4 ** 0.25)
W_IN_A = 1792
W_IN_B = 1476
BIGOOB = 1.0e6


class KB:
    NS = 8
    ND = 32

    def __init__(self, nc, es):
        self.nc = nc
        self.eng = {'pe': nc.tensor, 'act': nc.scalar, 'dve': nc.vector, 'pool': nc.gpsimd, 'sp': nc.sync}
        self.esem = {e: [es.enter_context(nc.semaphore(f"s_{e}{i}")) for i in range(self.NS)]
                     for e in ('pe', 'act', 'dve', 'pool')}
        self.cnt = {e: 0 for e in ('pe', 'act', 'dve', 'pool')}
        self.dsem = [es.enter_context(nc.semaphore(f"s_d{i}")) for i in range(self.ND)]
        self.dtot = [0] * self.ND
        self.dnext = 0
        self.dnextp = 0
        self.wc = {e: {} for e in self.eng}
        self.wd = {e: {} for e in self.eng}
        self.lastw = {}
        self.readers = {}

    def _wait(self, e, tok):
        eng = self.eng[e]
        if tok[0] == 'c':
            _, e2, k = tok
            if e2 == e and e == 'pe':
                return
            if self.wc[e].get(e2, 0) >= k:
                return
            eng.wait_ge(self.esem[e2][(k - 1) % self.NS], (k - 1) // self.NS + 1)
            self.wc[e][e2] = k
        else:
            _, s, tot = tok
            if self.wd[e].get(s, 0) >= tot:
                return
            eng.wait_ge(self.dsem[s], tot)
            self.wd[e][s] = tot

    def _deps(self, e, reads, writes):
        deps = []
        for k in reads:
            t = self.lastw.get(k)
            if t is not None:
                deps.append(t)
        for k in writes:
            t = self.lastw.get(k)
            if t is not None:
                deps.append(t)
            deps.extend(self.readers.get(k, ()))
        for t in deps:
            self._wait(e, t)

    def _commit(self, tok, reads, writes):
        for k in reads:
            lst = self.readers.setdefault(k, [])
            lst[:] = [t for t in lst if not (t[0] == tok[0] and t[1] == tok[1])]
            lst.append(tok)
        for k in writes:
            self.lastw[k] = tok
            self.readers[k] = []

    def op(self, e, fn, reads=(), writes=()):
        self._deps(e, reads, writes)
        ins = fn(self.eng[e])
        self.cnt[e] += 1
        k = self.cnt[e]
        ins.then_inc(self.esem[e][(k - 1) % self.NS], 1)
        self._commit(('c', e, k), reads, writes)

    def dma(self, e, fn, reads=(), writes=()):
        half = self.ND // 2
        if e == 'pool':
            s = half + self.dnextp
            self.dnextp = (self.dnextp + 1) % 2
        else:
            s = self.dnext
            self.dnext = (self.dnext + 1) % half
        if self.dtot[s] > 0:
            self._wait(e, ('d', s, self.dtot[s]))
        self._deps(e, reads, writes)
        ins = fn(self.eng[e])
        self.dtot[s] += 16
        ins.then_inc(self.dsem[s], 16)
        self._commit(('d', s, self.dtot[s]), reads, writes)

    def barrier(self):
        for e in self.eng:
            for e2 in self.cnt:
                if self.cnt[e2] > 0:
                    self._wait(e, ('c', e2, self.cnt[e2]))
            for s in range(self.ND):
                if self.dtot[s] > 0:
                    self._wait(e, ('d', s, self.dtot[s]))
        self.lastw.clear()
        self.readers.clear()


def build_program(mode="full"):
    nc = bass.Bass("TRN2", target_bir_lowering=False)
    es = ExitStack()
    kb = KB(nc, es)

    def din(name, shape, dt=F32):
        return nc.dram_tensor(name, list(shape), dt, kind="ExternalInput").ap()

    def dscr(name, shape, dt=F32):
        return nc.dram_tensor(name, list(shape), dt, kind="Internal").ap()

    x_in = din("x", [S, D])
    mem_in = din("mem", [MEM, D])
    rel_bias = din("rel_bias", [32, 12])
    a_w_in = din("a_w_in", [D, W_IN_A])
    a_conv_w = din("a_conv_w", [4, TOKW])
    a_conv_b = din("a_conv_b", [TOKW])
    a_wr = din("a_wr", [12, 64, 64])
    a_br = din("a_br", [TOKW])
    a_wi = din("a_wi", [12, 64, 64])
    a_bi = din("a_bi", [TOKW])
    a_lambda = din("a_lambda", [TOKW])
    b_w_in = din("b_w_in", [D, W_IN_B])
    b_kv_norm_g = din("b_kv_norm_g", [128])
    b_w_uk = din("b_w_uk", [128, 12, 64])
    b_w_uv = din("b_w_uv", [128, 12, 64])
    b_idx_norm_g = din("b_idx_norm_g", [64])
    b_idx_norm_b = din("b_idx_norm_b", [64])
    w_mem_kv = din("w_mem_kv", [2, D, 512])
    w_out = din("w_out", [2, D, D])
    ln1_g = din("ln1_g", [2, D])
    ln1_b = din("ln1_b", [2, D])
    router_w = din("router_w", [2, D, NE])
    router_b = din("router_b", [2, NE])
    exp_w1 = din("exp_w1", [2, NE, D, 2 * D])
    exp_b1 = din("exp_b1", [2, NE, 2 * D])
    exp_w2 = din("exp_w2", [2, NE, D, D])
    exp_b2 = din("exp_b2", [2, NE, D])
    ln2_g = din("ln2_g", [2, D])
    ln2_b = din("ln2_b", [2, D])
    c_ident = din("c_ident", [128, 128])
    c_tri = din("c_tri", [128, 128])
    c_iota = din("c_iota", [128, NE])
    c_ecap = din("c_ecap", [128, NE])
    c_cmask = din("c_cmask", [128, 128])
    c_caus = din("c_caus", [128, 128])
    c_bkt = din("c_bkt", [128, 2, 128])

    out = nc.dram_tensor("out", [S, D], F32, kind="ExternalOutput").ap()
    x1buf = dscr("x1buf", [S, D])
    x2buf = dscr("x2buf", [S, D])
    xg = dscr("xg", [NROW, D], BF16)
    yg = dscr("yg", [NROW, D])
    qlat = dscr("qlat", [12, 128, S], BF16)
    iqd = dscr("iqd", [2, 128, S], BF16)
    memod = dscr("memod", [2, 128, S], BF16)

    def sb(name, shape, dt=F32, stack=es):
        return stack.enter_context(nc.sbuf_tensor(name, list(shape), dt))

    ps = [es.enter_context(nc.psum_tensor(f"ps{i}", [128, 512], F32)) for i in range(8)]
    psn = [0]

    def psum():
        i = psn[0]
        psn[0] = (i + 1) % 6
        return ps[i], ('ps', i)

    bc_reg = nc.gpsimd.alloc_register("bc_reg")
    nc.gpsimd.reg_mov(bc_reg, NROW - 1)
    ident_f = sb("ident_f", [128, 128])
    ident_b = sb("ident_b", [128, 128], BF16)
    tri_f = sb("tri_f", [128, 128])
    ones_f = sb("ones_f", [128, 128])
    ones_b = sb("ones_b", [128, 128], BF16)
    iota_e = sb("iota_e", [128, NE])
    ecap = sb("ecap", [128, NE])
    destall = sb("destall", [128, NT, 4], I32)
    gall = sb("gall", [128, NT, 4])

    kb.dma('sp', lambda q: q.dma_start(out=ident_f[:], in_=c_ident[:, :]), writes=['ident_f'])
    kb.dma('sp', lambda q: q.dma_start(out=tri_f[:], in_=c_tri[:, :]), writes=['tri_f'])
    kb.dma('sp', lambda q: q.dma_start(out=iota_e[:], in_=c_iota[:, :]), writes=['iota_e'])
    kb.dma('sp', lambda q: q.dma_start(out=ecap[:], in_=c_ecap[:, :]), writes=['ecap'])
    kb.op('dve', lambda v: v.tensor_copy(out=ident_b[:], in_=ident_f[:]), reads=['ident_f'], writes=['ident_b'])
    kb.op('dve', lambda v: v.memset(ones_f[:], 1.0), writes=['ones_f'])
    kb.op('dve', lambda v: v.memset(ones_b[:], 1.0), writes=['ones_b'])

    def load_cast(dst_ap, src_ap, key):
        kb.dma('pool', lambda q: q.dma_start(out=dst_ap, in_=src_ap), writes=[key])

    def bcast_rows(dst, src_row_ap, key, n=128):
        kb.dma('sp', lambda q: q.dma_start(out=dst, in_=src_row_ap.partition_broadcast(n)), writes=[key])

    def setup_mem_kv(layer, st, kT, vpad, onespad):
        memf = sb(f"memf{layer}", [128, 2, D], F32, st)
        memb = sb(f"memb{layer}", [128, 2, D], BF16, st)
        memT = sb(f"memT{layer}", [128, 8, MEM], BF16, st)
        wkv = sb(f"wkv{layer}", [128, 8, 512], BF16, st)
        kb.dma('sp', lambda q: q.dma_start(out=memf[:], in_=mem_in.rearrange("(t p) d -> p t d", p=128)), writes=['memf'])
        load_cast(wkv[:], w_mem_kv[layer].rearrange("(k p) c -> p k c", p=128), 'wkv')
        kb.op('act', lambda a: a.copy(out=memb[:], in_=memf[:]), reads=['memf'], writes=['memb'])
        for k in range(8):
            pt, pk = psum()
            ptb = pt[:].bitcast(BF16)
            for t in range(2):
                kb.op('pe', lambda p, t=t, k=k, ptb=ptb: p.transpose(out=ptb[:, t * 128:(t + 1) * 128],
                      in_=memb[:, t, k * 128:(k + 1) * 128], identity=ident_b[:]),
                      reads=['memb', 'ident_b'], writes=[pk])
            kb.op('dve', lambda v, k=k, ptb=ptb: v.tensor_copy(out=memT[:, k, :], in_=ptb[:, 0:256]),
                  reads=[pk], writes=['memT'])
        for pr in range(2):
            pt, pk = psum()
            for k in range(8):
                kb.op('pe', lambda p, k=k, pr=pr, pt=pt: p.matmul(pt[:, 0:256], lhsT=wkv[:, k, pr * 128:(pr + 1) * 128],
                      rhs=memT[:, k, :], start=(k == 0), stop=(k == 7)), reads=['wkv', 'memT'], writes=[pk])
            kb.op('dve', lambda v, pr=pr, pt=pt: v.tensor_copy(out=kT[:, pr, :], in_=pt[:, 0:256]), reads=[pk], writes=['kT'])
        kb.op('pool', lambda g: g.memset(vpad[:], 0.0), writes=['vpad'])
        kb.op('pool', lambda g: g.memset(onespad[:], 0.0), writes=['onespad'])
        for par in range(2):
            kb.op('pool', lambda g, par=par: g.memset(onespad[:, par, par * 64:(par + 1) * 64], 1.0), writes=['onespad'])
        for mc in range(2):
            pt, pk = psum()
            for k in range(8):
                kb.op('pe', lambda p, k=k, mc=mc, pt=pt: p.matmul(pt[:, 0:256], lhsT=memT[:, k, mc * 128:(mc + 1) * 128],
                      rhs=wkv[:, k, 256:512], start=(k == 0), stop=(k == 7)), reads=['wkv', 'memT'], writes=[pk])
            for h in range(4):
                par = h % 2
                kb.op('dve', lambda v, h=h, mc=mc, par=par, pt=pt: v.tensor_copy(
                    out=vpad[:, h, mc, par * 64:(par + 1) * 64], in_=pt[:, h * 64:(h + 1) * 64]),
                    reads=[pk], writes=['vpad'])

    def mem_attn(mqT, T, kT, vpad, onespad, mixT_dst, E, rden, tag):
        for pr in range(2):
            for hh in range(2):
                h = 2 * pr + hh
                for mc in range(2):
                    pt, pk = psum()
                    kb.op('pe', lambda p, pt=pt, pr=pr, hh=hh, mc=mc: p.matmul(
                        pt[:, 0:T], lhsT=kT[hh * 64:(hh + 1) * 64, pr, mc * 128:(mc + 1) * 128],
                        rhs=mqT[hh * 64:(hh + 1) * 64, pr, :], start=True, stop=True),
                        reads=['kT', 'mqT' + tag], writes=[pk])
                    kb.op('act', lambda a, pt=pt, hh=hh, mc=mc: a.activation(
                        out=E[:, hh, mc, :], in_=pt[:, 0:T], func=AF.Exp, scale=0.125),
                        reads=[pk], writes=[('E' + tag, hh, mc)])
            po, pok = psum()
            pd, pdk = psum()
            n = 0
            for hh in range(2):
                h = 2 * pr + hh
                for mc in range(2):
                    kb.op('pe', lambda p, po=po, h=h, hh=hh, mc=mc, n=n: p.matmul(
                        po[:, 0:T], lhsT=vpad[:, h, mc, :], rhs=E[:, hh, mc, :], start=(n == 0), stop=(n == 3)),
                        reads=['vpad', ('E' + tag, hh, mc)], writes=[pok])
                    n += 1
            n = 0
            for hh in range(2):
                for mc in range(2):
                    kb.op('pe', lambda p, pd=pd, hh=hh, mc=mc, n=n: p.matmul(
                        pd[:, 0:T], lhsT=onespad[:, hh, :], rhs=E[:, hh, mc, :], start=(n == 0), stop=(n == 3)),
                        reads=['onespad', ('E' + tag, hh, mc)], writes=[pdk])
                    n += 1
            kb.op('dve', lambda v, pd=pd: v.reciprocal(out=rden[:, 0:T], in_=pd[:, 0:T]), reads=[pdk], writes=['rden' + tag])
            kb.op('dve', lambda v, po=po, pr=pr: v.tensor_tensor(out=mixT_dst(pr), in0=po[:, 0:T], in1=rden[:, 0:T], op=ALU.mult),
                  reads=[pok, 'rden' + tag], writes=[('mixT' + tag, 6 + pr)])

    class Tail:
        def __init__(self, layer, st, nbuf=2):
            self.layer = layer
            self.nbuf = nbuf
            L = layer
            self.wout = sb(f"wout{L}", [128, 8, D], BF16, st)
            load_cast(self.wout[:], w_out[L].rearrange("(k p) n -> p k n", p=128), 'wout')
            self.lng = sb(f"lng{L}", [128, D], F32, st)
            self.lnb = sb(f"lnb{L}", [128, D], F32, st)
            bcast_rows(self.lng[:], ln1_g[L], 'lng')
            bcast_rows(self.lnb[:], ln1_b[L], 'lnb')
            self.rw = sb(f"rw{L}", [128, 8, NE], F32, st)
            kb.dma('sp', lambda q: q.dma_start(out=self.rw[:], in_=router_w[L].rearrange("(k p) e -> p k e", p=128)), writes=['rw'])
            self.rb = sb(f"rb{L}", [128, NE], F32, st)
            bcast_rows(self.rb[:], router_b[L], 'rb')
            self.rrun = sb(f"rrun{L}", [128, NE], F32, st)
            kb.op('dve', lambda v: v.memset(self.rrun[:], 0.0), writes=['rrun'])
            self.z = [sb(f"z{L}_{i}", [128, D], F32, st) for i in range(nbuf)]
            self.x1f = [sb(f"x1f{L}_{i}", [128, D], F32, st) for i in range(nbuf)]
            self.x1b = [sb(f"x1b{L}_{i}", [128, D], BF16, st) for i in range(nbuf)]
            self.x1T = sb(f"x1T{L}", [128, 8, 128], F32, st)
            self.sm = [sb(f"sm{L}_{i}", [128, 256], F32, st) for i in range(nbuf)]
            self.smu = [sb(f"smu{L}_{i}", [128, 8], U32, st) for i in range(nbuf)]
            self.n = 0

        def run(self, ti, mixT_ap, mix_keys, xres_ap, xres_key, x1dst):
            i = self.n % self.nbuf
            self.n += 1
            z, x1f, x1b, sm, smu = self.z[i], self.x1f[i], self.x1b[i], self.sm[i], self.smu[i]
            zk, x1fk, x1bk, smk = ('z', i), ('x1f', i), ('x1b', i), ('sm', i)
            for nh in range(2):
                pt, pk = psum()
                for k in range(8):
                    kb.op('pe', lambda p, pt=pt, k=k, nh=nh: p.matmul(pt[:, :], lhsT=mixT_ap(k), rhs=self.wout[:, k, nh * 512:(nh + 1) * 512],
                          start=(k == 0), stop=(k == 7)), reads=['wout'] + list(mix_keys), writes=[pk])
                kb.op('dve', lambda v, pt=pt, nh=nh: v.scalar_tensor_tensor(
                    out=z[:, nh * 512:(nh + 1) * 512], in0=xres_ap[:, nh * 512:(nh + 1) * 512], scalar=ALPHA,
                    in1=pt[:, :], op0=ALU.mult, op1=ALU.add), reads=[pk, xres_key], writes=[zk])
            self.layernorm(z, zk, x1f, x1fk, sm, smk, self.lng, self.lnb)
            kb.dma('sp', lambda q: q.dma_start(out=x1dst, in_=x1f[:]), reads=[x1fk], writes=[('x1d', ti)])
            kb.op('act', lambda a: a.copy(out=x1b[:], in_=x1f[:]), reads=[x1fk], writes=[x1bk])
            for half in range(2):
                pt, pk = psum()
                for kk in range(4):
                    k = half * 4 + kk
                    kb.op('pe', lambda p, pt=pt, k=k, kk=kk: p.transpose(out=pt[:, kk * 128:(kk + 1) * 128],
                          in_=x1f[:, k * 128:(k + 1) * 128], identity=ident_f[:]), reads=[x1fk, 'ident_f'], writes=[pk])
                kb.op('act', lambda a, pt=pt, half=half: a.copy(
                    out=self.x1T[:, half * 4:(half + 1) * 4, :].rearrange("p k t -> p (k t)"), in_=pt[:, :]),
                    reads=[pk], writes=['x1T'])
            pl, plk = psum()
            for k in range(8):
                kb.op('pe', lambda p, k=k: p.matmul(pl[:, 0:NE], lhsT=self.x1T[:, k, :], rhs=self.rw[:, k, :],
                      start=(k == 0), stop=(k == 7)), reads=['x1T', 'rw'], writes=[plk])
            lg = sm[:, 0:32]
            v8 = sm[:, 32:40]
            mask = sm[:, 40:72]
            slot = sm[:, 72:104]
            junk = sm[:, 104:136]
            idxf = sm[:, 136:144]
            destf = sm[:, 144:148]
            ev = sm[:, 148:152]
            nm = sm[:, 152:153]
            gsum = sm[:, 153:154]
            bad = sm[:, 160:192]
            kb.op('dve', lambda v: v.tensor_tensor(out=lg, in0=pl[:, 0:NE], in1=self.rb[:], op=ALU.add),
                  reads=[plk, 'rb'], writes=[smk])
            kb.op('dve', lambda v: v.max(out=v8, in_=lg), reads=[smk], writes=[smk])
            kb.op('dve', lambda v: v.max_index(out=smu[:], in_max=v8, in_values=lg), reads=[smk], writes=[('smu', i)])
            kb.op('dve', lambda v: v.tensor_scalar(out=mask, in0=lg, scalar1=v8[:, 3:4], scalar2=None, op0=ALU.is_ge),
                  reads=[smk], writes=[smk])
            pc, pck = psum()
            kb.op('pe', lambda p: p.matmul(pc[:, 0:NE], lhsT=tri_f[:], rhs=mask, start=True, stop=True),
                  reads=['tri_f', smk], writes=[pck])
            kb.op('pe', lambda p: p.matmul(pc[:, NE:2 * NE], lhsT=ones_f[:], rhs=mask, start=True, stop=True),
                  reads=['ones_f', smk], writes=[pck])
            kb.op('dve', lambda v: v.tensor_tensor(out=slot, in0=pc[:, 0:NE], in1=self.rrun[:], op=ALU.add),
                  reads=[pck, 'rrun'], writes=[smk])
            kb.op('dve', lambda v: v.tensor_tensor(out=self.rrun[:], in0=pc[:, NE:2 * NE], in1=self.rrun[:], op=ALU.add),
                  reads=[pck, 'rrun'], writes=['rrun'])
            kb.op('dve', lambda v: v.tensor_scalar(out=bad, in0=slot, scalar1=float(CAP), scalar2=BIGOOB, op0=ALU.is_ge, op1=ALU.mult),
                  reads=[smk], writes=[smk])
            kb.op('dve', lambda v: v.tensor_tensor(out=slot, in0=slot, in1=bad, op=ALU.add), reads=[smk], writes=[smk])
            kb.op('dve', lambda v: v.tensor_tensor(out=slot, in0=slot, in1=ecap[:], op=ALU.add), reads=[smk, 'ecap'], writes=[smk])
            kb.op('dve', lambda v: v.tensor_copy(out=idxf, in_=smu[:]), reads=[('smu', i)], writes=[smk])
            for k in range(4):
                kb.op('dve', lambda v, k=k: v.scalar_tensor_tensor(out=junk, in0=iota_e[:], scalar=idxf[:, k:k + 1], in1=slot,
                      op0=ALU.is_equal, op1=ALU.mult, accum_out=destf[:, k:k + 1]), reads=[smk, 'iota_e'], writes=[smk])
            kb.op('dve', lambda v: v.tensor_copy(out=destall[:, ti, :], in_=destf), reads=[smk], writes=[('dest', ti)])
            kb.op('dve', lambda v: v.tensor_scalar(out=nm, in0=v8[:, 0:1], scalar1=-1.0, scalar2=None, op0=ALU.mult),
                  reads=[smk], writes=[smk])
            kb.op('act', lambda a: a.activation(out=ev, in_=v8[:, 0:4], func=AF.Exp, bias=nm, scale=1.0, accum_out=gsum),
                  reads=[smk], writes=[smk])
            kb.op('dve', lambda v: v.reciprocal(out=gsum, in_=gsum), reads=[smk], writes=[smk])
            kb.op('dve', lambda v: v.tensor_scalar(out=gall[:, ti, :], in0=ev, scalar1=gsum, scalar2=None, op0=ALU.mult),
                  reads=[smk], writes=[('gate', ti)])
            for k in range(4):
                kb.dma('pool', lambda q, k=k: q.indirect_dma_start(
                    out=xg[:, :], out_offset=bass.IndirectOffsetOnAxis(ap=destall[:, ti, k:k + 1], axis=0),
                    in_=x1b[:, :], in_offset=None, bounds_check=bc_reg, oob_is_err=False),
                    reads=[x1bk, ('dest', ti)], writes=['xg'])

        def layernorm(self, z, zk, o, ok, sm, smk, g, b, gk='lng', bk='lnb'):
            st6 = sm[:, 200:212]
            mv = sm[:, 212:214]
            rstd = sm[:, 214:215]
            for c in range(2):
                kb.op('dve', lambda v, c=c: v.bn_stats(out=st6[:, c * 6:(c + 1) * 6], in_=z[:, c * 512:(c + 1) * 512]),
                      reads=[zk], writes=[smk])
            kb.op('dve', lambda v: v.bn_aggr(out=mv, in_=st6), reads=[smk], writes=[smk])
            kb.op('act', lambda a: a.activation(out=rstd, in_=mv[:, 1:2], func=AF.Sqrt, bias=1e-5, scale=1.0), reads=[smk], writes=[smk])
            kb.op('dve', lambda v: v.reciprocal(out=rstd, in_=rstd), reads=[smk], writes=[smk])
            kb.op('dve', lambda v: v.tensor_scalar(out=o[:], in0=z[:], scalar1=mv[:, 0:1], scalar2=rstd, op0=ALU.subtract, op1=ALU.mult),
                  reads=[zk, smk], writes=[ok])
            kb.op('pool', lambda p: p.tensor_tensor(out=o[:], in0=o[:], in1=g[:], op=ALU.mult), reads=[ok, gk], writes=[ok])
            kb.op('pool', lambda p: p.tensor_tensor(out=o[:], in0=o[:], in1=b[:], op=ALU.add), reads=[ok, bk], writes=[ok])

    def phase_a0(ntiles=NT):
        T = 256
        st = ExitStack()
        tail = Tail(0, st)
        kT = sb("kT0", [128, 2, MEM], BF16, st)
        vpad = sb("vpad0", [128, 4, 2, 128], BF16, st)
        onespad = sb("onespad0", [128, 2, 128], BF16, st)
        setup_mem_kv(0, st, kT, vpad, onespad)
        win = sb("win0", [128, 8, W_IN_A], BF16, st)
        load_cast(win[:], a_w_in.rearrange("(k p) c -> p k c", p=128), 'win')
        wr_bd = sb("wr_bd", [128, 6, 128], BF16, st)
        wi_bd = sb("wi_bd", [128, 6, 128], BF16, st)
        kb.op('pool', lambda g: g.memset(wr_bd[:], 0.0), writes=['wr_bd'])
        kb.op('pool', lambda g: g.memset(wi_bd[:], 0.0), writes=['wi_bd'])
        for n in range(12):
            j, par = n // 2, n % 2
            load_cast(wr_bd[par * 64:(par + 1) * 64, j, par * 64:(par + 1) * 64], a_wr[n], 'wr_bd')
            load_cast(wi_bd[par * 64:(par + 1) * 64, j, par * 64:(par + 1) * 64], a_wi[n], 'wi_bd')
        cw = sb("cw", [128, 4, 6], F32, st)
        vecs = sb("vecs", [128, 4, 6], F32, st)
        with nc.allow_non_contiguous_dma(reason="tiny per-channel vectors"):
            kb.dma('sp', lambda q: q.dma_start(out=cw[:], in_=a_conv_w.rearrange("w (j p) -> p w j", p=128)), writes=['cw'])
            for n, v_ in enumerate((a_conv_b, a_br, a_bi, a_lambda)):
                kb.dma('sp', lambda q, n=n, v_=v_: q.dma_start(out=vecs[:, n, :], in_=v_.rearrange("(j p) -> p j", p=128)), writes=['vecs'])
        coef = sb("coef", [128, 6], F32, st)
        kb.op('act', lambda a: a.activation(out=coef[:], in_=vecs[:, 3, :], func=AF.Exp, scale=-1.0), reads=['vecs'], writes=['coef'])
        kb.op('act', lambda a: a.activation(out=coef[:], in_=coef[:], func=AF.Ln, bias=1.0, scale=1.0), reads=['coef'], writes=['coef'])
        kb.op('dve', lambda v: v.tensor_scalar(out=coef[:], in0=coef[:], scalar1=-8.0, scalar2=None, op0=ALU.mult), reads=['coef'], writes=['coef'])

        xf = [sb(f"xf{i}", [128, 2, D], F32, st) for i in range(2)]
        xbf = sb("xbf", [128, 2, D], BF16, st)
        xT = sb("xT", [128, 8, T], BF16, st)
        xbh = sb("xbh", [128, 6, 3 + T], F32, st)
        gbT = sb("gbT", [128, 6, T], F32, st)
        mqT = sb("mqT", [128, 2, T], BF16, st)
        mixT = [sb(f"mixT{i}", [128, 8, T], BF16, st) for i in range(2)]
        E = sb("E0", [128, 2, 2, T], BF16, st)
        rden = sb("rden0", [128, T], F32, st)
        NTMP = 10
        tmp = [[sb(f"tmp{n}_{i}", [128, T], F32, st) for i in range(2)] for n in range(NTMP)]
        xcb = [sb(f"xcb{i}", [128, T], BF16, st) for i in range(2)]
        hbuf = [sb(f"hbuf{i}", [128, 6, T], F32, st) for i in range(2)]
        kb.op('dve', lambda v: v.memset(xbh[:], 0.0), writes=['xbh'])
        zt = sb("zt", [128, 4096], BF16, st)
        kb.op('pool', lambda g: g.memset(zt[:], 0.0), writes=['zt'])
        for r0 in range(0, NROW, 512):
            kb.dma('sp', lambda q, r0=r0: q.dma_start(out=xg[r0:r0 + 512, :].rearrange("(p t) d -> p (t d)", t=4), in_=zt[:]),
                   reads=['zt'], writes=['xg'])

        nch = ntiles * 128 // T
        for ci in range(nch):
            xi = ci % 2
            xfc, xfk = xf[xi], ('xf', xi)
            kb.dma('sp', lambda q, ci=ci, xfc=xfc: q.dma_start(
                out=xfc[:], in_=x_in[ci * T:(ci + 1) * T, :].rearrange("(t p) d -> p t d", p=128)), writes=[xfk])
            kb.op('act', lambda a, xfc=xfc: a.copy(out=xbf[:], in_=xfc[:]), reads=[xfk], writes=['xbf'])
            for k in range(8):
                pt, pk = psum()
                ptb = pt[:].bitcast(BF16)
                for t in range(2):
                    kb.op('pe', lambda p, ptb=ptb, t=t, k=k: p.transpose(out=ptb[:, t * 128:(t + 1) * 128],
                          in_=xbf[:, t, k * 128:(k + 1) * 128], identity=ident_b[:]), reads=['xbf', 'ident_b'], writes=[pk])
                kb.op('dve' if k % 2 == 0 else 'act',
                      (lambda v, ptb=ptb, k=k: v.tensor_copy(out=xT[:, k, :], in_=ptb[:, 0:T])) if k % 2 == 0 else
                      (lambda a, ptb=ptb, k=k: a.copy(out=xT[:, k, :], in_=ptb[:, 0:T])),
                      reads=[pk], writes=[('xT', k)])
            xTk = [('xT', k) for k in range(8)]
            for c in range(14):
                pt, pk = psum()
                for k in range(8):
                    kb.op('pe', lambda p, pt=pt, k=k, c=c: p.matmul(pt[:, 0:T], lhsT=win[:, k, c * 128:(c + 1) * 128],
                          rhs=xT[:, k, :], start=(k == 0), stop=(k == 7)), reads=['win'] + xTk, writes=[pk])
                if c < 6:
                    kb.op('act', lambda a, pt=pt, c=c: a.copy(out=xbh[:, c, 3:3 + T], in_=pt[:, 0:T]), reads=[pk], writes=[('xbh', c)])
                elif c < 12:
                    kb.op('act', lambda a, pt=pt, c=c: a.copy(out=gbT[:, c - 6, :], in_=pt[:, 0:T]), reads=[pk], writes=[('gbT', c - 6)])
                else:
                    kb.op('dve', lambda v, pt=pt, c=c: v.tensor_copy(out=mqT[:, c - 12, :], in_=pt[:, 0:T]), reads=[pk], writes=['mqT0'])
            mx = mixT[ci % 2]
            hb = hbuf[ci % 2]
            hprev = hbuf[(ci + 1) % 2]
            for c in range(6):
                r_ = c % 2
                xc, rr, ii, aa, ss, uu, sq, t2, sg, gl = [tmp[n][r_] for n in range(NTMP)]
                tk = [(f'tmp{n}', r_) for n in range(NTMP)]
                xck = tk[0]
                kb.op('dve', lambda v, c=c, xc=xc: v.tensor_scalar(out=xc[:], in0=xbh[:, c, 3:3 + T], scalar1=cw[:, 3, c:c + 1],
                      scalar2=vecs[:, 0, c:c + 1], op0=ALU.mult, op1=ALU.add), reads=[('xbh', c), 'cw', 'vecs'], writes=[xck])
                for j in range(3):
                    kb.op('dve', lambda v, c=c, j=j, xc=xc: v.scalar_tensor_tensor(out=xc[:], in0=xbh[:, c, j:j + T], scalar=cw[:, j, c:c + 1],
                          in1=xc[:], op0=ALU.mult, op1=ALU.add), reads=[('xbh', c), 'cw', xck], writes=[xck])
                kb.op('pool', lambda g, c=c: g.tensor_copy(out=xbh[:, c, 0:3], in_=xbh[:, c, T:T + 3]), reads=[('xbh', c)], writes=[('xbh', c)])
                kb.op('pool', lambda g, xc=xc, r_=r_: g.tensor_copy(out=xcb[r_][:], in_=xc[:]), reads=[xck], writes=[('xcb', r_)])
                pr_, prk = psum()
                kb.op('pe', lambda p, pr_=pr_, c=c, r_=r_: p.matmul(pr_[:, 0:T], lhsT=wr_bd[:, c, :], rhs=xcb[r_][:], start=True, stop=True),
                      reads=['wr_bd', ('xcb', r_)], writes=[prk])
                pi_, pik = psum()
                kb.op('pe', lambda p, pi_=pi_, c=c, r_=r_: p.matmul(pi_[:, 0:T], lhsT=wi_bd[:, c, :], rhs=xcb[r_][:], start=True, stop=True),
                      reads=['wi_bd', ('xcb', r_)], writes=[pik])
                kb.op('act', lambda a, pr_=pr_, c=c, rr=rr: a.activation(out=rr[:], in_=pr_[:, 0:T], func=AF.Sigmoid, bias=vecs[:, 1, c:c + 1], scale=1.0),
                      reads=[prk, 'vecs'], writes=[tk[1]])
                kb.op('act', lambda a, pi_=pi_, c=c, ii=ii: a.activation(out=ii[:], in_=pi_[:, 0:T], func=AF.Sigmoid, bias=vecs[:, 2, c:c + 1], scale=1.0),
                      reads=[pik, 'vecs'], writes=[tk[2]])
                gb = gbT[:, c, :]
                kb.op('pool', lambda g, gb=gb, sq=sq: g.tensor_tensor(out=sq[:], in0=gb, in1=gb, op=ALU.mult), reads=[('gbT', c)], writes=[tk[6]])
                kb.op('pool', lambda g, sq=sq: g.tensor_scalar(out=sq[:], in0=sq[:], scalar1=0.044715, scalar2=1.0, op0=ALU.mult, op1=ALU.add),
                      reads=[tk[6]], writes=[tk[6]])
                kb.op('pool', lambda g, gb=gb, sq=sq, t2=t2: g.tensor_tensor(out=t2[:], in0=sq[:], in1=gb, op=ALU.mult), reads=[tk[6], ('gbT', c)], writes=[tk[7]])
                kb.op('act', lambda a, t2=t2, sg=sg: a.activation(out=sg[:], in_=t2[:], func=AF.Sigmoid, scale=1.5957691216057308),
                      reads=[tk[7]], writes=[tk[8]])
                kb.op('act', lambda a, aa=aa, rr=rr, c=c: a.activation(out=aa[:], in_=rr[:], func=AF.Exp, scale=coef[:, c:c + 1]),
                      reads=[tk[1], 'coef'], writes=[tk[3]])
                kb.op('pool', lambda g, aa=aa, ss=ss: g.tensor_tensor(out=ss[:], in0=aa[:], in1=aa[:], op=ALU.mult), reads=[tk[3]], writes=[tk[4]])
                kb.op('act', lambda a, ss=ss: a.activation(out=ss[:], in_=ss[:], func=AF.Sqrt, bias=1.0, scale=-1.0), reads=[tk[4]], writes=[tk[4]])
                kb.op('pool', lambda g, uu=uu, ss=ss, ii=ii: g.tensor_tensor(out=uu[:], in0=ss[:], in1=ii[:], op=ALU.mult), reads=[tk[4], tk[2]], writes=[tk[5]])
                kb.op('dve', lambda v, uu=uu, xc=xc: v.tensor_tensor(out=uu[:], in0=uu[:], in1=xc[:], op=ALU.mult), reads=[tk[5], xck], writes=[tk[5]])
                init = 0.0 if ci == 0 else hprev[:, c, T - 1:T]
                kb.op('dve', lambda v, aa=aa, uu=uu, c=c, init=init, hb=hb: v.tensor_tensor_scan(out=hb[:, c, :], data0=aa[:], data1=uu[:],
                      initial=init, op0=ALU.mult, op1=ALU.add), reads=[tk[3], tk[5], ('h', (ci + 1) % 2, c)], writes=[('h', ci % 2, c)])
                kb.op('pool', lambda g, gl=gl, sg=sg, gb=gb: g.tensor_tensor(out=gl[:], in0=sg[:], in1=gb, op=ALU.mult), reads=[tk[8], ('gbT', c)], writes=[tk[9]])
                kb.op('dve', lambda v, gl=gl, c=c, hb=hb, mx=mx: v.tensor_tensor(out=mx[:, c, :], in0=hb[:, c, :], in1=gl[:], op=ALU.mult),
                      reads=[tk[9], ('h', ci % 2, c)], writes=[('mixT0', c)])
            mem_attn(mqT, T, kT, vpad, onespad, lambda pr, mx=mx: mx[:, 6 + pr, :], E, rden, '0')
            mixkeys = [('mixT0', c) for c in range(8)]
            for t in range(T // 128):
                ti = ci * (T // 128) + t
                tail.run(ti, lambda k, mx=mx, t=t: mx[:, k, t * 128:(t + 1) * 128], mixkeys, xfc[:, t, :], xfk,
                         x1buf[ti * 128:(ti + 1) * 128, :])
        kb.barrier()
        if mode.startswith("a0"):
            dbg_r = nc.dram_tensor("dbg_r", [128, NE], F32, kind="ExternalOutput").ap()
            dbg_d = nc.dram_tensor("dbg_d", [128, NT * 4], I32, kind="ExternalOutput").ap()
            kb.dma('sp', lambda q: q.dma_start(out=dbg_r[:, :], in_=tail.rrun[:]))
            kb.dma('sp', lambda q: q.dma_start(out=dbg_d[:, :], in_=destall[:].rearrange("p t k -> p (t k)")))
            kb.barrier()
        st.close()


    def phase_a1(src, ntiles=NT):
        T = 256
        KSEL = 256
        NBIS = 20
        CH = 1024
        st = ExitStack()
        tail = Tail(1, st, nbuf=1)
        cTok = sb("cTok", [128, NT, 128], BF16, st)
        cT = sb("cT", [128, S], BF16, st)
        ikT2 = sb("ikT2", [128, S], BF16, st)
        absw = sb("absw", [128, NT, 4], F32, st)
        sgnw = sb("sgnw", [128, NT, 4], F32, st)
        BT = sb("BT", [128, 2, 12, 128], BF16, st)
        wuvpad = sb("wuvpad", [128, 12, 128], BF16, st)
        i4big = sb("i4big", [128, 4, 128], BF16, st)
        for r in range(4):
            kb.op('dve', lambda v, r=r: v.tensor_scalar(out=i4big[:, r, :], in0=ident_f[:], scalar1=100.0, scalar2=None, op0=ALU.mult),
                  reads=['ident_f'], writes=['i4big'])
        kb.op('pool', lambda g: g.memset(wuvpad[:], 0.0), writes=['wuvpad'])
        for h in range(12):
            par = h % 2
            load_cast(wuvpad[:, h, par * 64:(par + 1) * 64], b_w_uv[:, h, :], 'wuvpad')
        s1 = ExitStack()
        kT = sb("kT1", [128, 2, MEM], BF16, s1)
        vpad = sb("vpad1", [128, 4, 2, 128], BF16, s1)
        onespad = sb("onespad1", [128, 2, 128], BF16, s1)
        win = sb("win1", [128, 8, W_IN_B], BF16, s1)
        load_cast(win[:], b_w_in.rearrange("(k p) c -> p k c", p=128), 'win')
        wukT = sb("wukT", [128, 6, 128], BF16, s1)
        gkv = sb("gkv", [128, 128], F32, s1)
        gik = sb("gik", [128, 64], F32, s1)
        bik = sb("bik", [128, 64], F32, s1)
        bcast_rows(gkv[:], b_kv_norm_g, 'gkv')
        bcast_rows(gik[:], b_idx_norm_g, 'gik')
        bcast_rows(bik[:], b_idx_norm_b, 'bik')
        ssetup = ExitStack()
        setup_mem_kv(1, ssetup, kT, vpad, onespad)
        wuk = sb("wuk", [128, 768], BF16, ssetup)
        load_cast(wuk[:], b_w_uk.rearrange("r h d -> r (h d)"), 'wuk')
        for j in range(6):
            pt, pk = psum()
            ptb = pt[:].bitcast(BF16)
            kb.op('pe', lambda p, ptb=ptb, j=j: p.transpose(out=ptb[:, 0:128], in_=wuk[:, j * 128:(j + 1) * 128], identity=ident_b[:]),
                  reads=['wuk', 'ident_b'], writes=[pk])
            kb.op('dve', lambda v, ptb=ptb, j=j: v.tensor_copy(out=wukT[:, j, :], in_=ptb[:, 0:128]), reads=[pk], writes=['wukT'])
        rbb = sb("rbb", [128, 32, 12], F32, ssetup)
        bkt = sb("bkt", [128, 2, 128], F32, ssetup)
        caus = sb("caus", [128, 128], F32, ssetup)
        acc = sb("bacc", [128, 12, 128], F32, ssetup)
        prod = sb("bprod", [128, 12, 128], F32, ssetup)
        oh = sb("boh", [128, 128], F32, ssetup)
        kb.dma('sp', lambda q: q.dma_start(out=rbb[:].rearrange("p b h -> p (b h)"), in_=rel_bias.rearrange("b h -> (b h)").partition_broadcast(128)), writes=['rbb'])
        kb.dma('sp', lambda q: q.dma_start(out=bkt[:], in_=c_bkt[:, :, :]), writes=['bkt'])
        kb.dma('sp', lambda q: q.dma_start(out=caus[:], in_=c_caus[:, :]), writes=['caus'])
        for dt in range(2):
            kb.op('dve', lambda v: v.memset(acc[:], 0.0), writes=['bacc'])
            for b in range(32):
                kb.op('dve', lambda v, b=b, dt=dt: v.tensor_scalar(out=oh[:], in0=bkt[:, dt, :], scalar1=float(b), scalar2=None, op0=ALU.is_equal),
                      reads=['bkt'], writes=['boh'])
                kb.op('dve', lambda v, b=b: v.tensor_tensor(out=prod[:], in0=oh[:].unsqueeze(1).to_broadcast([128, 12, 128]),
                      in1=rbb[:, b, :].unsqueeze(2).to_broadcast([128, 12, 128]), op=ALU.mult), reads=['boh', 'rbb'], writes=['bprod'])
                kb.op('dve', lambda v: v.tensor_tensor(out=acc[:], in0=acc[:], in1=prod[:], op=ALU.add), reads=['bacc', 'bprod'], writes=['bacc'])
            kb.op('dve', lambda v: v.tensor_tensor(out=acc[:], in0=acc[:], in1=rbb[:, 31, :].unsqueeze(2).to_broadcast([128, 12, 128]), op=ALU.subtract),
                  reads=['bacc', 'rbb'], writes=['bacc'])
            if dt == 0:
                kb.op('dve', lambda v: v.tensor_tensor(out=acc[:], in0=acc[:], in1=caus[:].unsqueeze(1).to_broadcast([128, 12, 128]), op=ALU.add),
                      reads=['bacc', 'caus'], writes=['bacc'])
            kb.op('dve', lambda v, dt=dt: v.tensor_copy(out=BT[:, dt, :, :], in_=acc[:]), reads=['bacc'], writes=['BT'])
        kb.barrier()
        ssetup.close()

        xf = [sb(f"xf1_{i}", [128, 2, D], F32, s1) for i in range(2)]
        xbf = sb("xbf1", [128, 2, D], BF16, s1)
        xT = sb("xT1", [128, 8, T], BF16, s1)
        qT = sb("qT1", [128, 6, T], BF16, s1)
        qlb = [sb(f"qlb{i}", [128, 12, T], BF16, s1) for i in range(2)]
        iqT = [sb(f"iqT{i}", [128, 2, T], BF16, s1) for i in range(2)]
        mqT = sb("mqT1", [128, 2, T], BF16, s1)
        memo = [sb(f"memo{i}", [128, 2, T], BF16, s1) for i in range(2)]
        E = sb("E1", [128, 2, 2, T], BF16, s1)
        rden = sb("rden1", [128, T], F32, s1)
        csb = [sb(f"csb{i}", [128, 128], F32, s1) for i in range(2)]
        cnb = [sb(f"cnb{i}", [128, 128], BF16, s1) for i in range(2)]
        iks = [sb(f"iks{i}", [128, 68], F32, s1) for i in range(2)]
        ik2 = [sb(f"ik2{i}", [128, 128], BF16, s1) for i in range(2)]
        sm1 = [sb(f"smp1_{i}", [128, 32], F32, s1) for i in range(2)]
        nch = ntiles * 128 // T
        for ci in range(nch):
            xi = ci % 2
            xfc, xfk = xf[xi], ('xf', xi)
            kb.dma('sp', lambda q, ci=ci, xfc=xfc: q.dma_start(
                out=xfc[:], in_=src[ci * T:(ci + 1) * T, :].rearrange("(t p) d -> p t d", p=128)), writes=[xfk])
            kb.op('act', lambda a, xfc=xfc: a.copy(out=xbf[:], in_=xfc[:]), reads=[xfk], writes=['xbf'])
            for k in range(8):
                pt, pk = psum()
                ptb = pt[:].bitcast(BF16)
                for t in range(2):
                    kb.op('pe', lambda p, ptb=ptb, t=t, k=k: p.transpose(out=ptb[:, t * 128:(t + 1) * 128],
                          in_=xbf[:, t, k * 128:(k + 1) * 128], identity=ident_b[:]), reads=['xbf', 'ident_b'], writes=[pk])
                if k % 2 == 0:
                    kb.op('dve', lambda v, ptb=ptb, k=k: v.tensor_copy(out=xT[:, k, :], in_=ptb[:, 0:T]), reads=[pk], writes=[('xT', k)])
                else:
                    kb.op('act', lambda a, ptb=ptb, k=k: a.copy(out=xT[:, k, :], in_=ptb[:, 0:T]), reads=[pk], writes=[('xT', k)])
            xTk = [('xT', k) for k in range(8)]

            def fm_proj(col0, dst_ap, dkey, eng):
                pt, pk = psum()
                for k in range(8):
                    kb.op('pe', lambda p, pt=pt, k=k: p.matmul(pt[:, 0:T], lhsT=win[:, k, col0:col0 + 128], rhs=xT[:, k, :],
                          start=(k == 0), stop=(k == 7)), reads=['win'] + xTk, writes=[pk])
                if eng == 'act':
                    kb.op('act', lambda a, pt=pt: a.copy(out=dst_ap, in_=pt[:, 0:T]), reads=[pk], writes=[dkey])
                else:
                    kb.op('dve', lambda v, pt=pt: v.tensor_copy(out=dst_ap, in_=pt[:, 0:T]), reads=[pk], writes=[dkey])

            for c in range(6):
                fm_proj(c * 128, qT[:, c, :], ('qT', c), 'act' if c % 2 else 'dve')
            bi = ci % 2
            for j in range(2):
                fm_proj(896 + j * 128, iqT[bi][:, j, :], ('iqT', bi), 'act')
            for j in range(2):
                fm_proj(1220 + j * 128, mqT[:, j, :], 'mqT1', 'dve')
            kb.dma('sp', lambda q, ci=ci, bi=bi: q.dma_start(out=iqd[:, :, ci * T:(ci + 1) * T].rearrange("j p t -> p j t"), in_=iqT[bi][:]),
                   reads=[('iqT', bi)], writes=['iqd'])
            for h in range(12):
                j, hh = h // 2, h % 2
                pt, pk = psum()
                kb.op('pe', lambda p, pt=pt, j=j, hh=hh: p.matmul(pt[:, 0:T], lhsT=wukT[hh * 64:(hh + 1) * 64, j, :],
                      rhs=qT[hh * 64:(hh + 1) * 64, j, :], start=True, stop=True), reads=['wukT', ('qT', j)], writes=[pk])
                if h % 2 == 0:
                    kb.op('act', lambda a, pt=pt, h=h, bi=bi: a.activation(out=qlb[bi][:, h, :], in_=pt[:, 0:T], func=AF.Copy, scale=0.125),
                          reads=[pk], writes=[('qlb', bi)])
                else:
                    kb.op('dve', lambda v, pt=pt, h=h, bi=bi: v.tensor_scalar(out=qlb[bi][:, h, :], in0=pt[:, 0:T], scalar1=0.125, scalar2=None, op0=ALU.mult),
                          reads=[pk], writes=[('qlb', bi)])
            kb.dma('sp', lambda q, ci=ci, bi=bi: q.dma_start(out=qlat[:, :, ci * T:(ci + 1) * T].rearrange("h p t -> p h t"), in_=qlb[bi][:]),
                   reads=[('qlb', bi)], writes=['qlat'])
            mem_attn(mqT, T, kT, vpad, onespad, lambda pr, bi=bi: memo[bi][:, pr, :], E, rden, '1')
            kb.dma('sp', lambda q, ci=ci, bi=bi: q.dma_start(out=memod[:, :, ci * T:(ci + 1) * T].rearrange("j p t -> p j t"), in_=memo[bi][:]),
                   reads=[('mixT1', 6), ('mixT1', 7)], writes=['memod'])
            for t in range(T // 128):
                ti = ci * (T // 128) + t
                i2 = ti % 2
                smk = ('sm1', i2)
                sm = sm1[i2]
                pc, pck = psum()
                for k in range(8):
                    kb.op('pe', lambda p, pc=pc, k=k, t=t: p.matmul(pc[:, 0:128], lhsT=xT[:, k, t * 128:(t + 1) * 128], rhs=win[:, k, 768:896],
                          start=(k == 0), stop=(k == 7)), reads=['win'] + xTk, writes=[pck])
                pi_, pik = psum()
                for k in range(8):
                    kb.op('pe', lambda p, pi_=pi_, k=k, t=t: p.matmul(pi_[:, 0:68], lhsT=xT[:, k, t * 128:(t + 1) * 128], rhs=win[:, k, 1152:1220],
                          start=(k == 0), stop=(k == 7)), reads=['win'] + xTk, writes=[pik])
                ss = sm[:, 0:1]
                kb.op('act', lambda a, pc=pc, i2=i2, ss=ss: a.activation(out=csb[i2][:], in_=pc[:, 0:128], func=AF.Square, accum_out=ss),
                      reads=[pck], writes=[('csb', i2), smk])
                kb.op('act', lambda a, ss=ss: a.activation(out=ss, in_=ss, func=AF.Sqrt, bias=1e-6, scale=1.0 / 128.0), reads=[smk], writes=[smk])
                kb.op('dve', lambda v, ss=ss: v.reciprocal(out=ss, in_=ss), reads=[smk], writes=[smk])
                kb.op('dve', lambda v, pc=pc, i2=i2, ss=ss: v.scalar_tensor_tensor(out=csb[i2][:], in0=pc[:, 0:128], scalar=ss, in1=gkv[:],
                      op0=ALU.mult, op1=ALU.mult), reads=[pck, smk, 'gkv', ('csb', i2)], writes=[('csb', i2)])
                kb.op('act', lambda a, i2=i2, ti=ti: a.copy(out=cTok[:, ti, :], in_=csb[i2][:]), reads=[('csb', i2)], writes=[('cTok', ti)])
                pt, pk = psum()
                ptb = pt[:].bitcast(BF16)
                kb.op('pe', lambda p, ptb=ptb, ti=ti: p.transpose(out=ptb[:, 0:128], in_=cTok[:, ti, :], identity=ident_b[:]),
                      reads=[('cTok', ti), 'ident_b'], writes=[pk])
                kb.op('dve', lambda v, ptb=ptb, ti=ti: v.tensor_copy(out=cT[:, ti * 128:(ti + 1) * 128], in_=ptb[:, 0:128]), reads=[pk], writes=[('cT', ti)])
                kb.op('act', lambda a, pi_=pi_, i2=i2: a.copy(out=iks[i2][:], in_=pi_[:, 0:68]), reads=[pik], writes=[('iks', i2)])
                st6 = sm[:, 8:14]
                mv = sm[:, 14:16]
                rs = sm[:, 16:17]
                kb.op('dve', lambda v, i2=i2, st6=st6: v.bn_stats(out=st6, in_=iks[i2][:, 0:64]), reads=[('iks', i2)], writes=[smk])
                kb.op('dve', lambda v, st6=st6, mv=mv: v.bn_aggr(out=mv, in_=st6), reads=[smk], writes=[smk])
                kb.op('act', lambda a, mv=mv, rs=rs: a.activation(out=rs, in_=mv[:, 1:2], func=AF.Sqrt, bias=1e-5, scale=1.0), reads=[smk], writes=[smk])
                kb.op('dve', lambda v, rs=rs: v.reciprocal(out=rs, in_=rs), reads=[smk], writes=[smk])
                kb.op('dve', lambda v, i2=i2, mv=mv, rs=rs: v.tensor_scalar(out=iks[i2][:, 0:64], in0=iks[i2][:, 0:64], scalar1=mv[:, 0:1], scalar2=rs,
                      op0=ALU.subtract, op1=ALU.mult), reads=[('iks', i2), smk], writes=[('iks', i2)])
                kb.op('dve', lambda v, i2=i2: v.tensor_tensor(out=iks[i2][:, 0:64], in0=iks[i2][:, 0:64], in1=gik[:], op=ALU.mult),
                      reads=[('iks', i2), 'gik'], writes=[('iks', i2)])
                for r in range(2):
                    kb.op('dve', lambda v, i2=i2, r=r: v.tensor_tensor(out=ik2[i2][:, r * 64:(r + 1) * 64], in0=iks[i2][:, 0:64], in1=bik[:], op=ALU.add),
                          reads=[('iks', i2), 'bik'], writes=[('ik2', i2)])
                pt, pk = psum()
                ptb = pt[:].bitcast(BF16)
                kb.op('pe', lambda p, ptb=ptb, i2=i2: p.transpose(out=ptb[:, 0:128], in_=ik2[i2][:], identity=ident_b[:]),
                      reads=[('ik2', i2), 'ident_b'], writes=[pk])
                kb.op('act', lambda a, ptb=ptb, ti=ti: a.copy(out=ikT2[:, ti * 128:(ti + 1) * 128], in_=ptb[:, 0:128]), reads=[pk], writes=[('ikT2', ti)])
                kb.op('act', lambda a, i2=i2, ti=ti: a.activation(out=absw[:, ti, :], in_=iks[i2][:, 64:68], func=AF.Abs),
                      reads=[('iks', i2)], writes=[('absw', ti)])
                kb.op('dve', lambda v, i2=i2, ti=ti: v.tensor_scalar(out=sgnw[:, ti, :], in0=iks[i2][:, 64:68], scalar1=0.0, scalar2=2.0, op0=ALU.is_ge, op1=ALU.mult),
                      reads=[('iks', i2)], writes=[('sgnw', ti)])
                kb.op('dve', lambda v, ti=ti: v.tensor_scalar(out=sgnw[:, ti, :], in0=sgnw[:, ti, :], scalar1=-1.0, scalar2=None, op0=ALU.add),
                      reads=[('sgnw', ti)], writes=[('sgnw', ti)])
        kb.barrier()
        s1.close()

        s2 = ExitStack()
        sc = sb("sc", [128, S], F32, s2)
        junk = sb("junk", [128, S], mybir.dt.uint8, s2)
        maskb = sb("maskb", [128, S], BF16, s2)
        tiec = [sb(f"tiec{i}", [128, CH], BF16, s2) for i in range(2)]
        cumc = [sb(f"cumc{i}", [128, CH], F32, s2) for i in range(2)]
        onesc = sb("onesc", [128, CH], BF16, s2)
        kb.op('pool', lambda g: g.memset(onesc[:], 1.0), writes=['onesc'])
        cmask = sb("cmask", [128, 128], F32, s2)
        kb.dma('sp', lambda q: q.dma_start(out=cmask[:], in_=c_cmask[:, :]), writes=['cmask'])
        rl = [sb(f"rl{i}", [128, 512], F32, s2) for i in range(2)]
        ql = sb("ql", [128, 12, 128], BF16, s2)
        iqb = [sb(f"iqb{i}", [128, 2, 128], BF16, s2) for i in range(2)]
        mixT = [sb(f"mixT1_{i}", [128, 8, 128], BF16, s2) for i in range(2)]
        x2t = sb("x2t", [128, D], F32, s2)
        Pt = [sb(f"Pt{i}", [128, 512], BF16, s2) for i in range(3)]
        olat = [sb(f"olat{i}", [128, 512], BF16, s2) for i in range(2)]
        rD = sb("rD", [128, 512], F32, s2)
        bs = sb("bs", [128, 16], F32, s2)
        half = sb("half", [128, 1], F32, s2)
        kb.op('dve', lambda v: v.memset(half[:], 0.5), writes=['half'])
        lo, hi, mid, cnt, ge, dd, ee, need, cgt, carry = [bs[:, i:i + 1] for i in range(10)]
        npt = [0]

        def selection_a(qb):
            n = (qb + 1) * 128
            ib = qb % 2
            kb.dma('sp', lambda q: q.dma_start(out=iqb[ib][:], in_=iqd[:, :, qb * 128:(qb + 1) * 128].rearrange("j p t -> p j t")),
                   writes=[('iqb', ib)])
            for g0 in range(0, n, 512):
                w = min(512, n - g0)
                for h in range(4):
                    j, hh = h // 2, h % 2
                    pt, pk = psum()
                    kb.op('pe', lambda p, pt=pt, j=j, hh=hh, g0=g0, w=w: p.matmul(pt[:, 0:w], lhsT=iqb[ib][hh * 64:(hh + 1) * 64, j, :],
                          rhs=ikT2[hh * 64:(hh + 1) * 64, g0:g0 + w], start=True, stop=True), reads=[('iqb', ib), 'ikT2'], writes=[pk])
                    ri = h % 2
                    kb.op('act', lambda a, pt=pt, ri=ri, w=w, h=h: a.activation(out=rl[ri][:, 0:w], in_=pt[:, 0:w], func=AF.Relu, scale=absw[:, qb, h:h + 1]),
                          reads=[pk], writes=[('rl', ri)])
                    if h == 0:
                        kb.op('dve', lambda v, ri=ri, g0=g0, w=w, h=h: v.tensor_scalar(out=sc[:, g0:g0 + w], in0=rl[ri][:, 0:w], scalar1=sgnw[:, qb, h:h + 1],
                              scalar2=None, op0=ALU.mult), reads=[('rl', ri)], writes=['sc'])
                    else:
                        kb.op('dve', lambda v, ri=ri, g0=g0, w=w, h=h: v.scalar_tensor_tensor(out=sc[:, g0:g0 + w], in0=rl[ri][:, 0:w], scalar=sgnw[:, qb, h:h + 1],
                              in1=sc[:, g0:g0 + w], op0=ALU.mult, op1=ALU.add), reads=[('rl', ri), 'sc'], writes=['sc'])
            kb.op('dve', lambda v: v.tensor_tensor(out=sc[:, n - 128:n], in0=sc[:, n - 128:n], in1=cmask[:], op=ALU.add), reads=['sc', 'cmask'], writes=['sc'])
            kb.op('dve', lambda v: v.tensor_reduce(out=hi, in_=sc[:, 0:n], axis=AX.X, op=ALU.max), reads=['sc'], writes=['bs'])
            kb.op('dve', lambda v: v.tensor_reduce(out=lo, in_=sc[:, 0:n - 128], axis=AX.X, op=ALU.min), reads=['sc'], writes=['bs'])
            kb.op('dve', lambda v: v.tensor_scalar(out=hi, in0=hi, scalar1=1.0, scalar2=None, op0=ALU.add), reads=['bs'], writes=['bs'])
            kb.op('dve', lambda v: v.tensor_scalar(out=lo, in0=lo, scalar1=-1.0, scalar2=None, op0=ALU.add), reads=['bs'], writes=['bs'])
            for it in range(NBIS):
                kb.op('dve', lambda v: v.scalar_tensor_tensor(out=mid, in0=lo, scalar=hi, in1=half[:], op0=ALU.add, op1=ALU.mult),
                      reads=['bs', 'half'], writes=['bs'])
                kb.op('dve', lambda v: v.tensor_scalar(out=junk[:, 0:n], in0=sc[:, 0:n], scalar1=mid, scalar2=None, op0=ALU.is_ge, op1=ALU.add, accum_out=cnt),
                      reads=['sc', 'bs'], writes=['junk', 'bs'])
                kb.op('dve', lambda v: v.tensor_scalar(out=ge, in0=cnt, scalar1=float(KSEL), scalar2=None, op0=ALU.is_ge), reads=['bs'], writes=['bs'])
                kb.op('dve', lambda v: v.tensor_tensor(out=dd, in0=mid, in1=lo, op=ALU.subtract), reads=['bs'], writes=['bs'])
                kb.op('dve', lambda v: v.tensor_tensor(out=ee, in0=hi, in1=mid, op=ALU.subtract), reads=['bs'], writes=['bs'])
                kb.op('dve', lambda v: v.scalar_tensor_tensor(out=lo, in0=dd, scalar=ge, in1=lo, op0=ALU.mult, op1=ALU.add), reads=['bs'], writes=['bs'])
                kb.op('dve', lambda v: v.scalar_tensor_tensor(out=hi, in0=ee, scalar=ge, in1=mid, op0=ALU.mult, op1=ALU.add), reads=['bs'], writes=['bs'])
            kb.op('dve', lambda v: v.tensor_scalar(out=junk[:, 0:n], in0=sc[:, 0:n], scalar1=hi, scalar2=None, op0=ALU.is_ge, op1=ALU.add, accum_out=cgt),
                  reads=['sc', 'bs'], writes=['junk', 'bs'])
            kb.op('dve', lambda v: v.tensor_scalar(out=need, in0=cgt, scalar1=-1.0, scalar2=float(KSEL), op0=ALU.mult, op1=ALU.add), reads=['bs'], writes=['bs'])

        def selection_b(qb):
            n = (qb + 1) * 128
            for ci_, c0 in enumerate(range(0, n, CH)):
                w = min(CH, n - c0)
                r_ = ci_ % 2
                tk, ck = ('tiec', r_), ('cumc', r_)
                kb.op('dve', lambda v, c0=c0, w=w, r_=r_: v.tensor_scalar(out=tiec[r_][:, 0:w], in0=sc[:, c0:c0 + w], scalar1=hi, scalar2=None, op0=ALU.is_lt),
                      reads=['sc', 'bs'], writes=[tk])
                kb.op('dve', lambda v, c0=c0, w=w, r_=r_: v.scalar_tensor_tensor(out=tiec[r_][:, 0:w], in0=sc[:, c0:c0 + w], scalar=lo, in1=tiec[r_][:, 0:w],
                      op0=ALU.is_ge, op1=ALU.mult), reads=['sc', 'bs', tk], writes=[tk])
                init = 0.0 if c0 == 0 else carry
                kb.op('dve', lambda v, w=w, r_=r_, init=init: v.tensor_tensor_scan(out=cumc[r_][:, 0:w], data0=onesc[:, 0:w], data1=tiec[r_][:, 0:w],
                      initial=init, op0=ALU.mult, op1=ALU.add), reads=['onesc', tk, 'bs'], writes=[ck])
                kb.op('dve', lambda v, w=w, r_=r_: v.tensor_copy(out=carry, in_=cumc[r_][:, w - 1:w]), reads=[ck], writes=['bs'])
                kb.op('dve', lambda v, w=w, r_=r_: v.scalar_tensor_tensor(out=tiec[r_][:, 0:w], in0=cumc[r_][:, 0:w], scalar=need, in1=tiec[r_][:, 0:w],
                      op0=ALU.is_le, op1=ALU.mult), reads=[ck, 'bs', tk], writes=[tk])
                kb.op('dve', lambda v, c0=c0, w=w, r_=r_: v.scalar_tensor_tensor(out=maskb[:, c0:c0 + w], in0=sc[:, c0:c0 + w], scalar=hi, in1=tiec[r_][:, 0:w],
                      op0=ALU.is_ge, op1=ALU.add), reads=['sc', 'bs', tk], writes=['maskb'])

        def attention(qb):
            mx = mixT[qb % 2]
            kb.dma('sp', lambda q: q.dma_start(out=ql[:], in_=qlat[:, :, qb * 128:(qb + 1) * 128].rearrange("h p t -> p h t")), writes=['ql'])
            kb.dma('sp', lambda q: q.dma_start(out=mx[:, 6:8, :], in_=memod[:, :, qb * 128:(qb + 1) * 128].rearrange("j p t -> p j t")),
                   writes=[('mixTm', qb % 2)])
            kb.dma('sp', lambda q: q.dma_start(out=x2t[:], in_=src[qb * 128:(qb + 1) * 128, :]), writes=['x2t'])
            for hg in range(3):
                pO, pOk = ps[6], ('ps', 6)
                pD, pDk = ps[7], ('ps', 7)
                qrhs = ql[:, hg * 4:(hg + 1) * 4, :].rearrange("p h t -> p (h t)")
                for j in range(qb + 1):
                    pL, pLk = psum()
                    dt = qb - j
                    last = 'mask' if qb >= 2 else None
                    nmm = 1 + (1 if qb >= 2 else 0) + (1 if dt <= 1 else 0)
                    m = 0
                    kb.op('pe', lambda p, pL=pL, j=j, nmm=nmm: p.matmul(pL[:, :], lhsT=cT[:, j * 128:(j + 1) * 128], rhs=qrhs, start=True, stop=(nmm == 1)),
                          reads=[('cT', j), 'ql'], writes=[pLk])
                    m += 1
                    if qb >= 2:
                        kb.op('pe', lambda p, pL=pL, j=j, m=m, nmm=nmm: p.matmul(pL[:, :], lhsT=maskb[:, j * 128:(j + 1) * 128],
                              rhs=i4big[:].rearrange("p r t -> p (r t)"), start=False, stop=(m == nmm - 1)), reads=['maskb', 'i4big'], writes=[pLk])
                        m += 1
                    if dt <= 1:
                        kb.op('pe', lambda p, pL=pL, dt=dt, m=m, nmm=nmm: p.matmul(pL[:, :], lhsT=ident_b[:],
                              rhs=BT[:, dt, hg * 4:(hg + 1) * 4, :].rearrange("p h t -> p (h t)"), start=False, stop=(m == nmm - 1)),
                              reads=['BT', 'ident_b'], writes=[pLk])
                        m += 1
                    pi = npt[0] % 3
                    npt[0] += 1
                    ebias = -100.0 if qb >= 2 else 0.0
                    kb.op('act', lambda a, pL=pL, pi=pi, ebias=ebias: a.activation(out=Pt[pi][:], in_=pL[:, :], func=AF.Exp, bias=nbias[:] if ebias else zbias[:], scale=1.0),
                          reads=[pLk, 'nbias'], writes=[('Pt', pi)])
                    kb.op('pe', lambda p, j=j, pi=pi: p.matmul(pO[:, :], lhsT=cTok[:, j, :], rhs=Pt[pi][:], start=(j == 0), stop=(j == qb)),
                          reads=[('cTok', j), ('Pt', pi)], writes=[pOk])
                    kb.op('pe', lambda p, j=j, pi=pi: p.matmul(pD[:, :], lhsT=ones_b[:], rhs=Pt[pi][:], start=(j == 0), stop=(j == qb)),
                          reads=['ones_b', ('Pt', pi)], writes=[pDk])
                kb.op('dve', lambda v: v.reciprocal(out=rD[:], in_=pD[:, :]), reads=[pDk], writes=['rD'])
                oi = hg % 2
                kb.op('dve', lambda v, oi=oi: v.tensor_tensor(out=olat[oi][:], in0=pO[:, :], in1=rD[:], op=ALU.mult), reads=[pOk, 'rD'], writes=[('olat', oi)])
                for pp in range(2):
                    pT, pTk = psum()
                    for hh in range(2):
                        hl = 2 * pp + hh
                        h = hg * 4 + hl
                        kb.op('pe', lambda p, pT=pT, h=h, hl=hl, hh=hh, oi=oi: p.matmul(pT[:, 0:128], lhsT=wuvpad[:, h, :], rhs=olat[oi][:, hl * 128:(hl + 1) * 128],
                              start=(hh == 0), stop=(hh == 1)), reads=['wuvpad', ('olat', oi)], writes=[pTk])
                    kb.op('act', lambda a, pT=pT, hg=hg, pp=pp: a.copy(out=mx[:, hg * 2 + pp, :], in_=pT[:, 0:128]), reads=[pTk], writes=[('mixTt', qb % 2, hg * 2 + pp)])

        nbias = sb("nbias", [128, 1], F32, s2)
        zbias = sb("zbias", [128, 1], F32, s2)
        kb.op('dve', lambda v: v.memset(nbias[:], -100.0), writes=['nbias'])
        kb.op('dve', lambda v: v.memset(zbias[:], 0.0), writes=['nbias'])

        nqb = ntiles
        if nqb > 2:
            selection_a(2)
        for qb in range(nqb):
            if qb >= 2:
                selection_b(qb)
            attention(qb)
            if qb + 1 < nqb and qb + 1 >= 2:
                selection_a(qb + 1)
            mx = mixT[qb % 2]
            mkeys = [('mixTt', qb % 2, k) for k in range(6)] + [('mixTm', qb % 2)]
            tail.run(qb, lambda k, mx=mx: mx[:, k, :], mkeys, x2t, 'x2t', x1buf[qb * 128:(qb + 1) * 128, :])
        kb.barrier()
        s2.close()
        st.close()

    def phase_moe(layer, dst, ntiles=NT, nexp=NE):
        L = layer
        st = ExitStack()
        w1 = [sb(f"w1_{L}_{i}", [128, 8, 2 * D], BF16, st) for i in range(2)]
        w2 = [sb(f"w2_{L}_{i}", [128, 8, D], BF16, st) for i in range(2)]
        b2r = [sb(f"b2r_{L}_{i}", [1, D], BF16, st) for i in range(2)]
        b1a = sb(f"b1a_{L}", [128, NE, 16], F32, st)
        b1u = sb(f"b1u_{L}", [128, NE, 8], F32, st)
        with nc.allow_non_contiguous_dma(reason="bias layout"):
            kb.dma('sp', lambda q: q.dma_start(out=b1a[:], in_=exp_b1[L].rearrange("e (j p) -> p e j", p=128)), writes=['b1a'])
        kb.op('dve', lambda v: v.tensor_scalar(out=b1u[:], in0=b1a[:, :, 8:16], scalar1=1.0, scalar2=None, op0=ALU.add), reads=['b1a'], writes=['b1u'])
        xgt = [sb(f"xgt_{L}_{i}", [128, 3, D], BF16, st) for i in range(2)]
        xgT = [sb(f"xgT_{L}_{i}", [128, 8, RG], BF16, st) for i in range(2)]
        actT = [sb(f"actT_{L}_{i}", [128, 8, RG], BF16, st) for i in range(2)]
        tg = [sb(f"tg_{L}_{i}", [128, RG], F32, st) for i in range(2)]
        tu = [sb(f"tu_{L}_{i}", [128, RG], F32, st) for i in range(2)]
        tsg = [sb(f"tsg_{L}_{i}", [128, RG], F32, st) for i in range(2)]
        tt = [sb(f"tt_{L}_{i}", [128, RG], F32, st) for i in range(2)]
        yev = [sb(f"yev_{L}_{i}", [128, D], F32, st) for i in range(2)]
        nyev = 0
        gcount = 0

        def load_w(e):
            i = e % 2
            load_cast(w1[i][:], exp_w1[L, e].rearrange("(k p) f -> p k f", p=128), ('w1', i))
            load_cast(w2[i][:], exp_w2[L, e].rearrange("(k p) n -> p k n", p=128), ('w2', i))
            load_cast(b2r[i][:], exp_b2[L, e:e + 1, :], ('b2r', i))

        load_w(0)
        for e in range(nexp):
            wi = e % 2
            if e + 1 < nexp:
                load_w(e + 1)
            for g in range(CAP // RG):
                gi = gcount % 2
                gcount += 1
                r0 = e * CAP + g * RG
                kb.dma('sp', lambda q, r0=r0, gi=gi: q.dma_start(out=xgt[gi][:], in_=xg[r0:r0 + RG, :].rearrange("(t p) d -> p t d", p=128)),
                       reads=['xg'], writes=[('xgt', gi)])
                for k in range(8):
                    pt, pk = psum()
                    ptb = pt[:].bitcast(BF16)
                    for t in range(3):
                        kb.op('pe', lambda p, ptb=ptb, t=t, k=k, gi=gi: p.transpose(out=ptb[:, t * 128:(t + 1) * 128],
                              in_=xgt[gi][:, t, k * 128:(k + 1) * 128], identity=ident_b[:]), reads=[('xgt', gi), 'ident_b'], writes=[pk])
                    if k % 2 == 0:
                        kb.op('dve', lambda v, ptb=ptb, k=k, gi=gi: v.tensor_copy(out=xgT[gi][:, k, :], in_=ptb[:, 0:RG]), reads=[pk], writes=[('xgT', gi, k)])
                    else:
                        kb.op('act', lambda a, ptb=ptb, k=k, gi=gi: a.copy(out=xgT[gi][:, k, :], in_=ptb[:, 0:RG]), reads=[pk], writes=[('xgT', gi, k)])
                xgTk = [('xgT', gi, k) for k in range(8)]
                for j in range(8):
                    ji = j % 2
                    pg, pgk = psum()
                    pu, puk = psum()
                    for k in range(8):
                        kb.op('pe', lambda p, pg=pg, k=k, j=j, wi=wi, gi=gi: p.matmul(pg[:, 0:RG], lhsT=w1[wi][:, k, j * 128:(j + 1) * 128],
                              rhs=xgT[gi][:, k, :], start=(k == 0), stop=(k == 7)), reads=[('w1', wi)] + xgTk, writes=[pgk])
                    for k in range(8):
                        kb.op('pe', lambda p, pu=pu, k=k, j=j, wi=wi, gi=gi: p.matmul(pu[:, 0:RG], lhsT=w1[wi][:, k, D + j * 128:D + (j + 1) * 128],
                              rhs=xgT[gi][:, k, :], start=(k == 0), stop=(k == 7)), reads=[('w1', wi)] + xgTk, writes=[puk])
                    kb.op('dve', lambda v, pg=pg, ji=ji, e=e, j=j: v.tensor_scalar(out=tg[ji][:], in0=pg[:, 0:RG], scalar1=b1a[:, e, j:j + 1],
                          scalar2=7.0, op0=ALU.add, op1=ALU.min), reads=[pgk, 'b1a'], writes=[('tg', ji)])
                    kb.op('act', lambda a, ji=ji: a.activation(out=tsg[ji][:], in_=tg[ji][:], func=AF.Sigmoid, scale=1.702),
                          reads=[('tg', ji)], writes=[('tsg', ji)])
                    kb.op('dve', lambda v, pu=pu, ji=ji, e=e, j=j: v.tensor_scalar(out=tu[ji][:], in0=pu[:, 0:RG], scalar1=b1u[:, e, j:j + 1],
                          scalar2=8.0, op0=ALU.add, op1=ALU.min), reads=[puk, 'b1u'], writes=[('tu', ji)])
                    kb.op('dve', lambda v, ji=ji: v.scalar_tensor_tensor(out=tt[ji][:], in0=tu[ji][:], scalar=-6.0, in1=tg[ji][:],
                          op0=ALU.max, op1=ALU.mult), reads=[('tu', ji), ('tg', ji)], writes=[('tt', ji)])
                    kb.op('pool', lambda g_, ji=ji, gi=gi, j=j: g_.tensor_tensor(out=actT[gi][:, j, :], in0=tt[ji][:], in1=tsg[ji][:], op=ALU.mult),
                          reads=[('tt', ji), ('tsg', ji)], writes=[('actT', gi, j)])
                actk = [('actT', gi, j) for j in range(8)]
                for t in range(3):
                    yi = nyev % 2
                    nyev += 1
                    for nh in range(2):
                        py, pyk = psum()
                        for k in range(8):
                            kb.op('pe', lambda p, py=py, k=k, t=t, nh=nh, wi=wi, gi=gi: p.matmul(py[:, :], lhsT=actT[gi][:, k, t * 128:(t + 1) * 128],
                                  rhs=w2[wi][:, k, nh * 512:(nh + 1) * 512], start=(k == 0), stop=False), reads=[('w2', wi)] + actk, writes=[pyk])
                        kb.op('pe', lambda p, py=py, nh=nh, wi=wi: p.matmul(py[:, :], lhsT=ones_b[0:1, :], rhs=b2r[wi][0:1, nh * 512:(nh + 1) * 512],
                              start=False, stop=True), reads=[('b2r', wi), 'ones_b'], writes=[pyk])
                        kb.op('act', lambda a, py=py, nh=nh, yi=yi: a.copy(out=yev[yi][:, nh * 512:(nh + 1) * 512], in_=py[:, :]),
                              reads=[pyk], writes=[('yev', yi)])
                    rr0 = r0 + t * 128
                    kb.dma('sp', lambda q, rr0=rr0, yi=yi: q.dma_start(out=yg[rr0:rr0 + 128, :], in_=yev[yi][:]), reads=[('yev', yi)], writes=['yg'])
        kb.barrier()
        st.close()
        st = ExitStack()
        lng = sb(f"ln2g{L}", [128, D], F32, st)
        lnb = sb(f"ln2b{L}", [128, D], F32, st)
        bcast_rows(lng[:], ln2_g[L], 'lng2')
        bcast_rows(lnb[:], ln2_b[L], 'lnb2')
        yk = [[sb(f"yk{L}_{i}_{k}", [128, D], F32, st) for k in range(4)] for i in range(2)]
        x1r = [sb(f"x1r{L}_{i}", [128, D], F32, st) for i in range(2)]
        zz = [sb(f"zz{L}_{i}", [128, D], F32, st) for i in range(2)]
        oo = [sb(f"oo{L}_{i}", [128, D], F32, st) for i in range(2)]
        smm = [sb(f"smm{L}_{i}", [128, 256], F32, st) for i in range(2)]
        lnh = Tail.__new__(Tail)
        for ti in range(ntiles):
            i = ti % 2
            kb.dma('sp', lambda q, ti=ti, i=i: q.dma_start(out=x1r[i][:], in_=x1buf[ti * 128:(ti + 1) * 128, :]), reads=[('x1d', ti)], writes=[('x1r', i)])
            for k in range(4):
                kb.op('pool', lambda g_, i=i, k=k: g_.memset(yk[i][k][:], 0.0), writes=[('yk', i, k)])
                kb.dma('pool', lambda q, ti=ti, i=i, k=k: q.indirect_dma_start(
                    out=yk[i][k][:, :], out_offset=None, in_=yg[:, :],
                    in_offset=bass.IndirectOffsetOnAxis(ap=destall[:, ti, k:k + 1], axis=0),
                    bounds_check=bc_reg, oob_is_err=False), reads=['yg', ('dest', ti)], writes=[('yk', i, k)])
            kb.op('dve', lambda v, i=i: v.tensor_scalar(out=zz[i][:], in0=x1r[i][:], scalar1=ALPHA, scalar2=None, op0=ALU.mult),
                  reads=[('x1r', i)], writes=[('zz', i)])
            for k in range(4):
                kb.op('dve', lambda v, ti=ti, i=i, k=k: v.scalar_tensor_tensor(out=zz[i][:], in0=yk[i][k][:], scalar=gall[:, ti, k:k + 1],
                      in1=zz[i][:], op0=ALU.mult, op1=ALU.add), reads=[('yk', i, k), ('gate', ti), ('zz', i)], writes=[('zz', i)])
            Tail.layernorm(lnh, zz[i], ('zz', i), oo[i], ('oo', i), smm[i], ('smm', i), lng, lnb, 'lng2', 'lnb2')
            kb.dma('sp', lambda q, ti=ti, i=i: q.dma_start(out=dst[ti * 128:(ti + 1) * 128, :], in_=oo[i][:]), reads=[('oo', i)], writes=[('dst', L, ti)])
        kb.barrier()
        st.close()

    if mode.startswith("a0"):
        ntl = NT if mode == "a0" else int(mode[2:])
        phase_a0(ntiles=ntl)
        st = ExitStack()
        cp = [sb(f"cp{i}", [128, D], F32, st) for i in range(2)]
        for ti in range(ntl):
            i = ti % 2
            kb.dma('sp', lambda q, ti=ti, i=i: q.dma_start(out=cp[i][:], in_=x1buf[ti * 128:(ti + 1) * 128, :]), writes=[('cp', i)])
            kb.dma('sp', lambda q, ti=ti, i=i: q.dma_start(out=out[ti * 128:(ti + 1) * 128, :], in_=cp[i][:]), reads=[('cp', i)], writes=[('o', ti)])
        kb.barrier()
        st.close()
    elif mode == "full":
        phase_a0()
        phase_moe(0, x2buf)
        phase_a1(x2buf)
        phase_moe(1, out)
    elif mode.startswith("a1"):
        ntl = int(mode[2:])
        zt = sb("zt1", [128, 4096], BF16)
        kb.op('pool', lambda g: g.memset(zt[:], 0.0), writes=['zt'])
        for r0 in range(0, NROW, 512):
            kb.dma('sp', lambda q, r0=r0: q.dma_start(out=xg[r0:r0 + 512, :].rearrange("(p t) d -> p (t d)", t=4), in_=zt[:]),
                   reads=['zt'], writes=['xg'])
        phase_a1(x_in, ntiles=ntl)
        st = ExitStack()
        cp = [sb(f"cp{i}", [128, D], F32, st) for i in range(2)]
        for ti in range(ntl):
            i = ti % 2
            kb.dma('sp', lambda q, ti=ti, i=i: q.dma_start(out=cp[i][:], in_=x1buf[ti * 128:(ti + 1) * 128, :]), writes=[('cp', i)])
            kb.dma('sp', lambda q, ti=ti, i=i: q.dma_start(out=out[ti * 128:(ti + 1) * 128, :], in_=cp[i][:]), reads=[('cp', i)], writes=[('o', ti)])
        kb.barrier()
        st.close()
    elif mode == "l0":
        phase_a0()
        phase_moe(0, out)
    es.close()
    return nc


def host_consts():
    ident = np.eye(128, dtype=np.float32)
    tri = np.triu(np.ones((128, 128), np.float32), 1)
    iota = np.tile(np.arange(NE, dtype=np.float32)[None, :], (128, 1))
    ecap = iota * CAP
    q = np.arange(128)
    cmask = np.where(q[None, :] <= q[:, None], 0.0, -2000.0).astype(np.float32)
    caus = np.where(q[:, None] <= q[None, :], 0.0, -30000.0).astype(np.float32)
    bkt = np.zeros((128, 2, 128), np.float32)
    for dt in range(2):
        rel = np.maximum(q[None, :] - q[:, None] + 128 * dt, 0)
        large = 16 + (np.log(np.maximum(rel, 1).astype(np.float32) / 16) / np.float32(np.log(128 / 16)) * 16).astype(np.int32)
        large = np.minimum(large, 31)
        bkt[:, dt, :] = np.where(rel < 16, rel, large)
    return {"c_ident": ident, "c_tri": tri, "c_iota": iota, "c_ecap": ecap, "c_cmask": cmask, "c_caus": caus, "c_bkt": bkt}


_PARAMS = ["rel_bias", "a_w_in", "a_conv_w", "a_conv_b", "a_wr", "a_br", "a_wi", "a_bi", "a_lambda", "b_w_in",
           "b_kv_norm_g", "b_w_uk", "b_w_uv", "b_idx_norm_g", "b_idx_norm_b", "w_mem_kv", "w_out", "ln1_g", "ln1_b",
           "router_w", "router_b", "exp_w1", "exp_b1", "exp_w2", "exp_b2", "ln2_g", "ln2_b"]
_SQUEEZE = {"a_w_in", "a_conv_w", "a_conv_b", "a_wr", "a_br", "a_wi", "a_bi", "a_lambda", "b_w_in", "b_kv_norm_g",
            "b_w_uk", "b_w_uv", "b_idx_norm_g", "b_idx_norm_b"}


def make_in_maps(inputs, cores):
    shared = {}
    for k in _PARAMS:
        v = np.ascontiguousarray(np.asarray(inputs[k], dtype=np.float32))
        if k in _SQUEEZE:
            v = v[0]
        shared[k] = v
    shared.update(host_consts())
    maps = []
    for c in cores:
        m = dict(shared)
        m["x"] = np.ascontiguousarray(inputs["x"][c])
        m["mem"] = np.ascontiguousarray(inputs["mem"][c])
        maps.append(m)
    return maps


def kernel(**inputs):
    nc = build_program("full")
    maps = make_in_maps(inputs, list(range(8)))
    res = run_bass_kernel_spmd(nc, maps, core_ids=list(range(8)))
    return np.stack([r["out"] for r in res.results], axis=0)
```

```python
from contextlib import ExitStack
import numpy as np
import concourse.bass as bass
import concourse.mybir as mybir
from concourse.bass_utils import run_bass_kernel_spmd

F32 = mybir.dt.float32
BF16 = mybir.dt.bfloat16
I32 = mybir.dt.int32
U32 = mybir.dt.uint32
AF = mybir.ActivationFunctionType
ALU = mybir.AluOpType
AX = mybir.AxisListType

S = 8192
D = 1024
NT = S // 128
MEM = 256
TOKW = 768
NE = 32
CAP = 1536
NROW = NE * CAP
RG = 384
ALPHA = float(4 ** 0.25)
W_IN_A = 1792
W_IN_B = 1476
BIGOOB = 1.0e6


class KB:
    NS = 8
    ND = 32

    def __init__(self, nc, es):
        self.nc = nc
        self.eng = {'pe': nc.tensor, 'act': nc.scalar, 'dve': nc.vector, 'pool': nc.gpsimd, 'sp': nc.sync}
        self.esem = {e: [es.enter_context(nc.semaphore(f"s_{e}{i}")) for i in range(self.NS)]
                     for e in ('pe', 'act', 'dve', 'pool')}
        self.cnt = {e: 0 for e in ('pe', 'act', 'dve', 'pool')}
        self.dsem = [es.enter_context(nc.semaphore(f"s_d{i}")) for i in range(self.ND)]
        self.dtot = [0] * self.ND
        self.dnext = 0
        self.dnextp = 0
        self.wc = {e: {} for e in self.eng}
        self.wd = {e: {} for e in self.eng}
        self.lastw = {}
        self.readers = {}

    def _wait(self, e, tok):
        eng = self.eng[e]
        if tok[0] == 'c':
            _, e2, k = tok
            if e2 == e and e == 'pe':
                return
            if self.wc[e].get(e2, 0) >= k:
                return
            eng.wait_ge(self.esem[e2][(k - 1) % self.NS], (k - 1) // self.NS + 1)
            self.wc[e][e2] = k
        else:
            _, s, tot = tok
            if self.wd[e].get(s, 0) >= tot:
                return
            eng.wait_ge(self.dsem[s], tot)
            self.wd[e][s] = tot

    def _deps(self, e, reads, writes):
        deps = []
        for k in reads:
            t = self.lastw.get(k)
            if t is not None:
                deps.append(t)
        for k in writes:
            t = self.lastw.get(k)
            if t is not None:
                deps.append(t)
            deps.extend(self.readers.get(k, ()))
        for t in deps:
            self._wait(e, t)

    def _commit(self, tok, reads, writes):
        for k in reads:
            lst = self.readers.setdefault(k, [])
            lst[:] = [t for t in lst if not (t[0] == tok[0] and t[1] == tok[1])]
            lst.append(tok)
        for k in writes:
            self.lastw[k] = tok
            self.readers[k] = []

    def op(self, e, fn, reads=(), writes=()):
        self._deps(e, reads, writes)
        ins = fn(self.eng[e])
        self.cnt[e] += 1
        k = self.cnt[e]
        ins.then_inc(self.esem[e][(k - 1) % self.NS], 1)
        self._commit(('c', e, k), reads, writes)

    def dma(self, e, fn, reads=(), writes=()):
        half = self.ND // 2
        if e == 'pool':
            s = half + self.dnextp
            self.dnextp = (self.dnextp + 1) % 2
        else:
            s = self.dnext
            self.dnext = (self.dnext + 1) % half
        if self.dtot[s] > 0:
            self._wait(e, ('d', s, self.dtot[s]))
        self._deps(e, reads, writes)
        ins = fn(self.eng[e])
        self.dtot[s] += 16
        ins.then_inc(self.dsem[s], 16)
        self._commit(('d', s, self.dtot[s]), reads, writes)

    def barrier(self):
        for e in self.eng:
            for e2 in self.cnt:
                if self.cnt[e2] > 0:
                    self._wait(e, ('c', e2, self.cnt[e2]))
            for s in range(self.ND):
                if self.dtot[s] > 0:
                    self._wait(e, ('d', s, self.dtot[s]))
        self.lastw.clear()
        self.readers.clear()


def interleave(streams):
    live = [[g, max(1, n), 0] for g, n in streams if g is not None]
    while live:
        live.sort(key=lambda r: r[2] / r[1])
        r = live[0]
        try:
            next(r[0])
            r[2] += 1
        except StopIteration:
            live.remove(r)


def build_program(mode="full"):
    nc = bass.Bass("TRN2", target_bir_lowering=False)
    es = ExitStack()
    kb = KB(nc, es)

    def din(name, shape, dt=F32):
        return nc.dram_tensor(name, list(shape), dt, kind="ExternalInput").ap()

    def dscr(name, shape, dt=F32):
        return nc.dram_tensor(name, list(shape), dt, kind="Internal").ap()

    x_in = din("x", [S, D])
    mem_in = din("mem", [MEM, D])
    rel_bias = din("rel_bias", [32, 12])
    a_w_in = din("a_w_in", [D, W_IN_A])
    a_conv_w = din("a_conv_w", [4, TOKW])
    a_conv_b = din("a_conv_b", [TOKW])
    a_wr = din("a_wr", [12, 64, 64])
    a_br = din("a_br", [TOKW])
    a_wi = din("a_wi", [12, 64, 64])
    a_bi = din("a_bi", [TOKW])
    a_lambda = din("a_lambda", [TOKW])
    b_w_in = din("b_w_in", [D, W_IN_B])
    b_kv_norm_g = din("b_kv_norm_g", [128])
    b_w_uk = din("b_w_uk", [128, 12, 64])
    b_w_uv = din("b_w_uv", [128, 12, 64])
    b_idx_norm_g = din("b_idx_norm_g", [64])
    b_idx_norm_b = din("b_idx_norm_b", [64])
    w_mem_kv = din("w_mem_kv", [2, D, 512])
    w_out = din("w_out", [2, D, D])
    ln1_g = din("ln1_g", [2, D])
    ln1_b = din("ln1_b", [2, D])
    router_w = din("router_w", [2, D, NE])
    router_b = din("router_b", [2, NE])
    exp_w1 = din("exp_w1", [2, NE, D, 2 * D])
    exp_b1 = din("exp_b1", [2, NE, 2 * D])
    exp_w2 = din("exp_w2", [2, NE, D, D])
    exp_b2 = din("exp_b2", [2, NE, D])
    ln2_g = din("ln2_g", [2, D])
    ln2_b = din("ln2_b", [2, D])
    c_ident = din("c_ident", [128, 128])
    c_tri = din("c_tri", [128, 128])
    c_iota = din("c_iota", [128, NE])
    c_ecap = din("c_ecap", [128, NE])
    c_cmask = din("c_cmask", [128, 128])
    c_caus = din("c_caus", [128, 128])
    c_bkt = din("c_bkt", [128, 2, 128])
    c_pow2 = din("c_pow2", [128, 24])

    out = nc.dram_tensor("out", [S, D], F32, kind="ExternalOutput").ap()
    x1buf = dscr("x1buf", [S, D])
    x2buf = dscr("x2buf", [S, D])
    xg = dscr("xg", [NROW, D], BF16)
    yg = dscr("yg", [NROW, D])
    qlat = dscr("qlat", [12, 128, S], BF16)
    iqd = dscr("iqd", [2, 128, S], BF16)
    memod = dscr("memod", [2, 128, S], BF16)

    def sb(name, shape, dt=F32, stack=es):
        return stack.enter_context(nc.sbuf_tensor(name, list(shape), dt))

    ps = [es.enter_context(nc.psum_tensor(f"ps{i}", [128, 512], F32)) for i in range(8)]
    psn = [0]

    def psum():
        i = psn[0]
        psn[0] = (i + 1) % 6
        return ps[i], ('ps', i)

    bc_reg = nc.gpsimd.alloc_register("bc_reg")
    nc.gpsimd.reg_mov(bc_reg, NROW - 1)
    ident_f = sb("ident_f", [128, 128])
    ident_b = sb("ident_b", [128, 128], BF16)
    tri_f = sb("tri_f", [128, 128])
    ones_f = sb("ones_f", [128, 128])
    ones_b = sb("ones_b", [128, 128], BF16)
    iota_e = sb("iota_e", [128, NE])
    ecap = sb("ecap", [128, NE])
    destall = sb("destall", [128, NT, 4], I32)
    gall = sb("gall", [128, NT, 4])

    kb.dma('sp', lambda q: q.dma_start(out=ident_f[:], in_=c_ident[:, :]), writes=['ident_f'])
    kb.dma('sp', lambda q: q.dma_start(out=tri_f[:], in_=c_tri[:, :]), writes=['tri_f'])
    kb.dma('sp', lambda q: q.dma_start(out=iota_e[:], in_=c_iota[:, :]), writes=['iota_e'])
    kb.dma('sp', lambda q: q.dma_start(out=ecap[:], in_=c_ecap[:, :]), writes=['ecap'])
    kb.op('dve', lambda v: v.tensor_copy(out=ident_b[:], in_=ident_f[:]), reads=['ident_f'], writes=['ident_b'])
    kb.op('dve', lambda v: v.memset(ones_f[:], 1.0), writes=['ones_f'])
    kb.op('dve', lambda v: v.memset(ones_b[:], 1.0), writes=['ones_b'])

    def load_cast(dst_ap, src_ap, key):
        kb.dma('pool', lambda q: q.dma_start(out=dst_ap, in_=src_ap), writes=[key])

    def bcast_rows(dst, src_row_ap, key, n=128):
        kb.dma('sp', lambda q: q.dma_start(out=dst, in_=src_row_ap.partition_broadcast(n)), writes=[key])

    def setup_mem_kv(layer, st, kT, vpad, onespad):
        memf = sb(f"memf{layer}", [128, 2, D], F32, st)
        memb = sb(f"memb{layer}", [128, 2, D], BF16, st)
        memT = sb(f"memT{layer}", [128, 8, MEM], BF16, st)
        wkv = sb(f"wkv{layer}", [128, 8, 512], BF16, st)
        kb.dma('sp', lambda q: q.dma_start(out=memf[:], in_=mem_in.rearrange("(t p) d -> p t d", p=128)), writes=['memf'])
        load_cast(wkv[:], w_mem_kv[layer].rearrange("(k p) c -> p k c", p=128), 'wkv')
        kb.op('act', lambda a: a.copy(out=memb[:], in_=memf[:]), reads=['memf'], writes=['memb'])
        for k in range(8):
            pt, pk = psum()
            ptb = pt[:].bitcast(BF16)
            for t in range(2):
                kb.op('pe', lambda p, t=t, k=k, ptb=ptb: p.transpose(out=ptb[:, t * 128:(t + 1) * 128],
                      in_=memb[:, t, k * 128:(k + 1) * 128], identity=ident_b[:]),
                      reads=['memb', 'ident_b'], writes=[pk])
            kb.op('dve', lambda v, k=k, ptb=ptb: v.tensor_copy(out=memT[:, k, :], in_=ptb[:, 0:256]),
                  reads=[pk], writes=['memT'])
        for pr in range(2):
            pt, pk = psum()
            for k in range(8):
                kb.op('pe', lambda p, k=k, pr=pr, pt=pt: p.matmul(pt[:, 0:256], lhsT=wkv[:, k, pr * 128:(pr + 1) * 128],
                      rhs=memT[:, k, :], start=(k == 0), stop=(k == 7)), reads=['wkv', 'memT'], writes=[pk])
            kb.op('dve', lambda v, pr=pr, pt=pt: v.tensor_copy(out=kT[:, pr, :], in_=pt[:, 0:256]), reads=[pk], writes=['kT'])
        kb.op('pool', lambda g: g.memset(vpad[:], 0.0), writes=['vpad'])
        kb.op('pool', lambda g: g.memset(onespad[:], 0.0), writes=['onespad'])
        for par in range(2):
            kb.op('pool', lambda g, par=par: g.memset(onespad[:, par, par * 64:(par + 1) * 64], 1.0), writes=['onespad'])
        for mc in range(2):
            pt, pk = psum()
            for k in range(8):
                kb.op('pe', lambda p, k=k, mc=mc, pt=pt: p.matmul(pt[:, 0:256], lhsT=memT[:, k, mc * 128:(mc + 1) * 128],
                      rhs=wkv[:, k, 256:512], start=(k == 0), stop=(k == 7)), reads=['wkv', 'memT'], writes=[pk])
            for h in range(4):
                par = h % 2
                kb.op('dve', lambda v, h=h, mc=mc, par=par, pt=pt: v.tensor_copy(
                    out=vpad[:, h, mc, par * 64:(par + 1) * 64], in_=pt[:, h * 64:(h + 1) * 64]),
                    reads=[pk], writes=['vpad'])

    def mem_attn(mqT, T, kT, vpad, onespad, mixT_dst, E, rden, tag):
        for pr in range(2):
            for hh in range(2):
                h = 2 * pr + hh
                for mc in range(2):
                    pt, pk = psum()
                    kb.op('pe', lambda p, pt=pt, pr=pr, hh=hh, mc=mc: p.matmul(
                        pt[:, 0:T], lhsT=kT[hh * 64:(hh + 1) * 64, pr, mc * 128:(mc + 1) * 128],
                        rhs=mqT[hh * 64:(hh + 1) * 64, pr, :], start=True, stop=True),
                        reads=['kT', 'mqT' + tag], writes=[pk])
                    kb.op('act', lambda a, pt=pt, hh=hh, mc=mc: a.activation(
                        out=E[:, hh, mc, :], in_=pt[:, 0:T], func=AF.Exp, scale=0.125),
                        reads=[pk], writes=[('E' + tag, hh, mc)])
            po, pok = psum()
            pd, pdk = psum()
            n = 0
            for hh in range(2):
                h = 2 * pr + hh
                for mc in range(2):
                    kb.op('pe', lambda p, po=po, h=h, hh=hh, mc=mc, n=n: p.matmul(
                        po[:, 0:T], lhsT=vpad[:, h, mc, :], rhs=E[:, hh, mc, :], start=(n == 0), stop=(n == 3)),
                        reads=['vpad', ('E' + tag, hh, mc)], writes=[pok])
                    n += 1
            n = 0
            for hh in range(2):
                for mc in range(2):
                    kb.op('pe', lambda p, pd=pd, hh=hh, mc=mc, n=n: p.matmul(
                        pd[:, 0:T], lhsT=onespad[:, hh, :], rhs=E[:, hh, mc, :], start=(n == 0), stop=(n == 3)),
                        reads=['onespad', ('E' + tag, hh, mc)], writes=[pdk])
                    n += 1
            kb.op('dve', lambda v, pd=pd: v.reciprocal(out=rden[:, 0:T], in_=pd[:, 0:T]), reads=[pdk], writes=['rden' + tag])
            kb.op('dve', lambda v, po=po, pr=pr: v.tensor_tensor(out=mixT_dst(pr), in0=po[:, 0:T], in1=rden[:, 0:T], op=ALU.mult),
                  reads=[pok, 'rden' + tag], writes=[('mixT' + tag, 6 + pr)])

    class Tail:
        def __init__(self, layer, st, nbuf=2):
            self.layer = layer
            self.nbuf = nbuf
            L = layer
            self.wout = sb(f"wout{L}", [128, 8, D], BF16, st)
            load_cast(self.wout[:], w_out[L].rearrange("(k p) n -> p k n", p=128), 'wout')
            self.lng = sb(f"lng{L}", [128, D], F32, st)
            self.lnb = sb(f"lnb{L}", [128, D], F32, st)
            bcast_rows(self.lng[:], ln1_g[L], 'lng')
            bcast_rows(self.lnb[:], ln1_b[L], 'lnb')
            self.rw = sb(f"rw{L}", [128, 8, NE], F32, st)
            kb.dma('sp', lambda q: q.dma_start(out=self.rw[:], in_=router_w[L].rearrange("(k p) e -> p k e", p=128)), writes=['rw'])
            self.rb = sb(f"rb{L}", [128, NE], F32, st)
            bcast_rows(self.rb[:], router_b[L], 'rb')
            self.rrun = sb(f"rrun{L}", [128, NE], F32, st)
            kb.op('dve', lambda v: v.memset(self.rrun[:], 0.0), writes=['rrun'])
            self.z = [sb(f"z{L}_{i}", [128, D], F32, st) for i in range(nbuf)]
            self.x1f = [sb(f"x1f{L}_{i}", [128, D], F32, st) for i in range(nbuf)]
            self.x1b = [sb(f"x1b{L}_{i}", [128, D], BF16, st) for i in range(nbuf)]
            self.x1T = sb(f"x1T{L}", [128, 8, 128], F32, st)
            self.sm = [sb(f"sm{L}_{i}", [128, 256], F32, st) for i in range(nbuf)]
            self.smu = [sb(f"smu{L}_{i}", [128, 8], U32, st) for i in range(nbuf)]
            self.n = 0

        def run(self, *a):
            for _ in self.run_gen(*a):
                pass

        def run_gen(self, ti, mixT_ap, mix_keys, xres_ap, xres_key, x1dst):
            i = self.n % self.nbuf
            self.n += 1
            z, x1f, x1b, sm, smu = self.z[i], self.x1f[i], self.x1b[i], self.sm[i], self.smu[i]
            zk, x1fk, x1bk, smk = ('z', i), ('x1f', i), ('x1b', i), ('sm', i)
            for nh in range(2):
                pt, pk = psum()
                for k in range(8):
                    kb.op('pe', lambda p, pt=pt, k=k, nh=nh: p.matmul(pt[:, :], lhsT=mixT_ap(k), rhs=self.wout[:, k, nh * 512:(nh + 1) * 512],
                          start=(k == 0), stop=(k == 7)), reads=['wout'] + list(mix_keys), writes=[pk])
                kb.op('dve', lambda v, pt=pt, nh=nh: v.scalar_tensor_tensor(
                    out=z[:, nh * 512:(nh + 1) * 512], in0=xres_ap[:, nh * 512:(nh + 1) * 512], scalar=ALPHA,
                    in1=pt[:, :], op0=ALU.mult, op1=ALU.add), reads=[pk, xres_key], writes=[zk])
                yield
            self.layernorm(z, zk, x1f, x1fk, sm, smk, self.lng, self.lnb)
            kb.dma('sp', lambda q: q.dma_start(out=x1dst, in_=x1f[:]), reads=[x1fk], writes=[('x1d', ti)])
            yield
            kb.op('act', lambda a: a.copy(out=x1b[:], in_=x1f[:]), reads=[x1fk], writes=[x1bk])
            yield
            for half in range(2):
                pt, pk = psum()
                for kk in range(4):
                    k = half * 4 + kk
                    kb.op('pe', lambda p, pt=pt, k=k, kk=kk: p.transpose(out=pt[:, kk * 128:(kk + 1) * 128],
                          in_=x1f[:, k * 128:(k + 1) * 128], identity=ident_f[:]), reads=[x1fk, 'ident_f'], writes=[pk])
                kb.op('act', lambda a, pt=pt, half=half: a.copy(
                    out=self.x1T[:, half * 4:(half + 1) * 4, :].rearrange("p k t -> p (k t)"), in_=pt[:, :]),
                    reads=[pk], writes=['x1T'])
            yield
            pl, plk = psum()
            for k in range(8):
                kb.op('pe', lambda p, k=k: p.matmul(pl[:, 0:NE], lhsT=self.x1T[:, k, :], rhs=self.rw[:, k, :],
                      start=(k == 0), stop=(k == 7)), reads=['x1T', 'rw'], writes=[plk])
            lg = sm[:, 0:32]
            v8 = sm[:, 32:40]
            mask = sm[:, 40:72]
            slot = sm[:, 72:104]
            junk = sm[:, 104:136]
            idxf = sm[:, 136:144]
            destf = sm[:, 144:148]
            ev = sm[:, 148:152]
            nm = sm[:, 152:153]
            gsum = sm[:, 153:154]
            bad = sm[:, 160:192]
            kb.op('dve', lambda v: v.tensor_tensor(out=lg, in0=pl[:, 0:NE], in1=self.rb[:], op=ALU.add),
                  reads=[plk, 'rb'], writes=[smk])
            kb.op('dve', lambda v: v.max(out=v8, in_=lg), reads=[smk], writes=[smk])
            kb.op('dve', lambda v: v.max_index(out=smu[:], in_max=v8, in_values=lg), reads=[smk], writes=[('smu', i)])
            kb.op('dve', lambda v: v.tensor_scalar(out=mask, in0=lg, scalar1=v8[:, 3:4], scalar2=None, op0=ALU.is_ge),
                  reads=[smk], writes=[smk])
            yield
            pc, pck = psum()
            kb.op('pe', lambda p: p.matmul(pc[:, 0:NE], lhsT=tri_f[:], rhs=mask, start=True, stop=True),
                  reads=['tri_f', smk], writes=[pck])
            kb.op('pe', lambda p: p.matmul(pc[:, NE:2 * NE], lhsT=ones_f[:], rhs=mask, start=True, stop=True),
                  reads=['ones_f', smk], writes=[pck])
            kb.op('dve', lambda v: v.tensor_tensor(out=slot, in0=pc[:, 0:NE], in1=self.rrun[:], op=ALU.add),
                  reads=[pck, 'rrun'], writes=[smk])
            kb.op('dve', lambda v: v.tensor_tensor(out=self.rrun[:], in0=pc[:, NE:2 * NE], in1=self.rrun[:], op=ALU.add),
                  reads=[pck, 'rrun'], writes=['rrun'])
            kb.op('dve', lambda v: v.tensor_scalar(out=bad, in0=slot, scalar1=float(CAP), scalar2=BIGOOB, op0=ALU.is_ge, op1=ALU.mult),
                  reads=[smk], writes=[smk])
            kb.op('dve', lambda v: v.tensor_tensor(out=slot, in0=slot, in1=bad, op=ALU.add), reads=[smk], writes=[smk])
            kb.op('dve', lambda v: v.tensor_tensor(out=slot, in0=slot, in1=ecap[:], op=ALU.add), reads=[smk, 'ecap'], writes=[smk])
            yield
            kb.op('dve', lambda v: v.tensor_copy(out=idxf, in_=smu[:]), reads=[('smu', i)], writes=[smk])
            for k in range(4):
                kb.op('dve', lambda v, k=k: v.scalar_tensor_tensor(out=junk, in0=iota_e[:], scalar=idxf[:, k:k + 1], in1=slot,
                      op0=ALU.is_equal, op1=ALU.mult, accum_out=destf[:, k:k + 1]), reads=[smk, 'iota_e'], writes=[smk])
            kb.op('dve', lambda v: v.tensor_copy(out=destall[:, ti, :], in_=destf), reads=[smk], writes=[('dest', ti)])
            kb.op('dve', lambda v: v.tensor_scalar(out=nm, in0=v8[:, 0:1], scalar1=-1.0, scalar2=None, op0=ALU.mult),
                  reads=[smk], writes=[smk])
            kb.op('act', lambda a: a.activation(out=ev, in_=v8[:, 0:4], func=AF.Exp, bias=nm, scale=1.0, accum_out=gsum),
                  reads=[smk], writes=[smk])
            yield
            kb.op('dve', lambda v: v.reciprocal(out=gsum, in_=gsum), reads=[smk], writes=[smk])
            kb.op('dve', lambda v: v.tensor_scalar(out=gall[:, ti, :], in0=ev, scalar1=gsum, scalar2=None, op0=ALU.mult),
                  reads=[smk], writes=[('gate', ti)])
            for k in range(4):
                kb.dma('pool', lambda q, k=k: q.indirect_dma_start(
                    out=xg[:, :], out_offset=bass.IndirectOffsetOnAxis(ap=destall[:, ti, k:k + 1], axis=0),
                    in_=x1b[:, :], in_offset=None, bounds_check=bc_reg, oob_is_err=False),
                    reads=[x1bk, ('dest', ti)], writes=['xg'])

        def layernorm(self, z, zk, o, ok, sm, smk, g, b, gk='lng', bk='lnb'):
            st6 = sm[:, 200:212]
            mv = sm[:, 212:214]
            rstd = sm[:, 214:215]
            for c in range(2):
                kb.op('dve', lambda v, c=c: v.bn_stats(out=st6[:, c * 6:(c + 1) * 6], in_=z[:, c * 512:(c + 1) * 512]),
                      reads=[zk], writes=[smk])
            kb.op('dve', lambda v: v.bn_aggr(out=mv, in_=st6), reads=[smk], writes=[smk])
            kb.op('act', lambda a: a.activation(out=rstd, in_=mv[:, 1:2], func=AF.Sqrt, bias=1e-5, scale=1.0), reads=[smk], writes=[smk])
            kb.op('dve', lambda v: v.reciprocal(out=rstd, in_=rstd), reads=[smk], writes=[smk])
            kb.op('dve', lambda v: v.tensor_scalar(out=o[:], in0=z[:], scalar1=mv[:, 0:1], scalar2=rstd, op0=ALU.subtract, op1=ALU.mult),
                  reads=[zk, smk], writes=[ok])
            kb.op('pool', lambda p: p.tensor_tensor(out=o[:], in0=o[:], in1=g[:], op=ALU.mult), reads=[ok, gk], writes=[ok])
            kb.op('pool', lambda p: p.tensor_tensor(out=o[:], in0=o[:], in1=b[:], op=ALU.add), reads=[ok, bk], writes=[ok])

    def phase_a0(ntiles=NT):
        T = 256
        st = ExitStack()
        tail = Tail(0, st)
        kT = sb("kT0", [128, 2, MEM], BF16, st)
        vpad = sb("vpad0", [128, 4, 2, 128], BF16, st)
        onespad = sb("onespad0", [128, 2, 128], BF16, st)
        setup_mem_kv(0, st, kT, vpad, onespad)
        win = sb("win0", [128, 8, W_IN_A], BF16, st)
        load_cast(win[:], a_w_in.rearrange("(k p) c -> p k c", p=128), 'win')
        wr_bd = sb("wr_bd", [128, 6, 128], BF16, st)
        wi_bd = sb("wi_bd", [128, 6, 128], BF16, st)
        kb.op('pool', lambda g: g.memset(wr_bd[:], 0.0), writes=['wr_bd'])
        kb.op('pool', lambda g: g.memset(wi_bd[:], 0.0), writes=['wi_bd'])
        for n in range(12):
            j, par = n // 2, n % 2
            load_cast(wr_bd[par * 64:(par + 1) * 64, j, par * 64:(par + 1) * 64], a_wr[n], 'wr_bd')
            load_cast(wi_bd[par * 64:(par + 1) * 64, j, par * 64:(par + 1) * 64], a_wi[n], 'wi_bd')
        cw = sb("cw", [128, 4, 6], F32, st)
        vecs = sb("vecs", [128, 4, 6], F32, st)
        with nc.allow_non_contiguous_dma(reason="tiny per-channel vectors"):
            kb.dma('sp', lambda q: q.dma_start(out=cw[:], in_=a_conv_w.rearrange("w (j p) -> p w j", p=128)), writes=['cw'])
            for n, v_ in enumerate((a_conv_b, a_br, a_bi, a_lambda)):
                kb.dma('sp', lambda q, n=n, v_=v_: q.dma_start(out=vecs[:, n, :], in_=v_.rearrange("(j p) -> p j", p=128)), writes=['vecs'])
        coef = sb("coef", [128, 6], F32, st)
        kb.op('act', lambda a: a.activation(out=coef[:], in_=vecs[:, 3, :], func=AF.Exp, scale=-1.0), reads=['vecs'], writes=['coef'])
        kb.op('act', lambda a: a.activation(out=coef[:], in_=coef[:], func=AF.Ln, bias=1.0, scale=1.0), reads=['coef'], writes=['coef'])
        kb.op('dve', lambda v: v.tensor_scalar(out=coef[:], in0=coef[:], scalar1=-8.0, scalar2=None, op0=ALU.mult), reads=['coef'], writes=['coef'])

        xf = [sb(f"xf{i}", [128, 2, D], F32, st) for i in range(2)]
        xbf = sb("xbf", [128, 2, D], BF16, st)
        xT = sb("xT", [128, 8, T], BF16, st)
        xbh = sb("xbh", [128, 6, 3 + T], F32, st)
        gbT = sb("gbT", [128, 6, T], F32, st)
        mqT = sb("mqT", [128, 2, T], BF16, st)
        mixT = [sb(f"mixT{i}", [128, 8, T], BF16, st) for i in range(2)]
        E = sb("E0", [128, 2, 2, T], BF16, st)
        rden = sb("rden0", [128, T], F32, st)
        NTMP = 10
        tmp = [[sb(f"tmp{n}_{i}", [128, T], F32, st) for i in range(2)] for n in range(NTMP)]
        xcb = [sb(f"xcb{i}", [128, T], BF16, st) for i in range(2)]
        hbuf = [sb(f"hbuf{i}", [128, 6, T], F32, st) for i in range(2)]
        kb.op('dve', lambda v: v.memset(xbh[:], 0.0), writes=['xbh'])
        zt = sb("zt", [128, 4096], BF16, st)
        kb.op('pool', lambda g: g.memset(zt[:], 0.0), writes=['zt'])
        for r0 in range(0, NROW, 512):
            kb.dma('sp', lambda q, r0=r0: q.dma_start(out=xg[r0:r0 + 512, :].rearrange("(p t) d -> p (t d)", t=4), in_=zt[:]),
                   reads=['zt'], writes=['xg'])

        nch = ntiles * 128 // T

        def mixer(ci):
            xi = ci % 2
            xfc, xfk = xf[xi], ('xf', xi)
            kb.dma('sp', lambda q, ci=ci, xfc=xfc: q.dma_start(
                out=xfc[:], in_=x_in[ci * T:(ci + 1) * T, :].rearrange("(t p) d -> p t d", p=128)), writes=[xfk])
            kb.op('act', lambda a, xfc=xfc: a.copy(out=xbf[:], in_=xfc[:]), reads=[xfk], writes=['xbf'])
            for k in range(8):
                pt, pk = psum()
                ptb = pt[:].bitcast(BF16)
                for t in range(2):
                    kb.op('pe', lambda p, ptb=ptb, t=t, k=k: p.transpose(out=ptb[:, t * 128:(t + 1) * 128],
                          in_=xbf[:, t, k * 128:(k + 1) * 128], identity=ident_b[:]), reads=['xbf', 'ident_b'], writes=[pk])
                kb.op('dve' if k % 2 == 0 else 'act',
                      (lambda v, ptb=ptb, k=k: v.tensor_copy(out=xT[:, k, :], in_=ptb[:, 0:T])) if k % 2 == 0 else
                      (lambda a, ptb=ptb, k=k: a.copy(out=xT[:, k, :], in_=ptb[:, 0:T])),
                      reads=[pk], writes=[('xT', k)])
            yield
            xTk = [('xT', k) for k in range(8)]
            for c in range(14):
                pt, pk = psum()
                for k in range(8):
                    kb.op('pe', lambda p, pt=pt, k=k, c=c: p.matmul(pt[:, 0:T], lhsT=win[:, k, c * 128:(c + 1) * 128],
                          rhs=xT[:, k, :], start=(k == 0), stop=(k == 7)), reads=['win'] + xTk, writes=[pk])
                if c < 6:
                    kb.op('act', lambda a, pt=pt, c=c: a.copy(out=xbh[:, c, 3:3 + T], in_=pt[:, 0:T]), reads=[pk], writes=[('xbh', c)])
                elif c < 12:
                    kb.op('act', lambda a, pt=pt, c=c: a.copy(out=gbT[:, c - 6, :], in_=pt[:, 0:T]), reads=[pk], writes=[('gbT', c - 6)])
                else:
                    kb.op('dve', lambda v, pt=pt, c=c: v.tensor_copy(out=mqT[:, c - 12, :], in_=pt[:, 0:T]), reads=[pk], writes=['mqT0'])
                if c % 2 == 1:
                    yield
            mx = mixT[ci % 2]
            hb = hbuf[ci % 2]
            hprev = hbuf[(ci + 1) % 2]
            for c in range(6):
                r_ = c % 2
                xc, rr, ii, aa, ss, uu, sq, t2, sg, gl = [tmp[n][r_] for n in range(NTMP)]
                tk = [(f'tmp{n}', r_) for n in range(NTMP)]
                xck = tk[0]
                kb.op('dve', lambda v, c=c, xc=xc: v.tensor_scalar(out=xc[:], in0=xbh[:, c, 3:3 + T], scalar1=cw[:, 3, c:c + 1],
                      scalar2=vecs[:, 0, c:c + 1], op0=ALU.mult, op1=ALU.add), reads=[('xbh', c), 'cw', 'vecs'], writes=[xck])
                for j in range(3):
                    kb.op('dve', lambda v, c=c, j=j, xc=xc: v.scalar_tensor_tensor(out=xc[:], in0=xbh[:, c, j:j + T], scalar=cw[:, j, c:c + 1],
                          in1=xc[:], op0=ALU.mult, op1=ALU.add), reads=[('xbh', c), 'cw', xck], writes=[xck])
                kb.op('pool', lambda g, c=c: g.tensor_copy(out=xbh[:, c, 0:3], in_=xbh[:, c, T:T + 3]), reads=[('xbh', c)], writes=[('xbh', c)])
                kb.op('pool', lambda g, xc=xc, r_=r_: g.tensor_copy(out=xcb[r_][:], in_=xc[:]), reads=[xck], writes=[('xcb', r_)])
                pr_, prk = psum()
                kb.op('pe', lambda p, pr_=pr_, c=c, r_=r_: p.matmul(pr_[:, 0:T], lhsT=wr_bd[:, c, :], rhs=xcb[r_][:], start=True, stop=True),
                      reads=['wr_bd', ('xcb', r_)], writes=[prk])
                pi_, pik = psum()
                kb.op('pe', lambda p, pi_=pi_, c=c, r_=r_: p.matmul(pi_[:, 0:T], lhsT=wi_bd[:, c, :], rhs=xcb[r_][:], start=True, stop=True),
                      reads=['wi_bd', ('xcb', r_)], writes=[pik])
                kb.op('act', lambda a, pr_=pr_, c=c, rr=rr: a.activation(out=rr[:], in_=pr_[:, 0:T], func=AF.Sigmoid, bias=vecs[:, 1, c:c + 1], scale=1.0),
                      reads=[prk, 'vecs'], writes=[tk[1]])
                kb.op('act', lambda a, pi_=pi_, c=c, ii=ii: a.activation(out=ii[:], in_=pi_[:, 0:T], func=AF.Sigmoid, bias=vecs[:, 2, c:c + 1], scale=1.0),
                      reads=[pik, 'vecs'], writes=[tk[2]])
                yield
                gb = gbT[:, c, :]
                kb.op('pool', lambda g, gb=gb, sq=sq: g.tensor_tensor(out=sq[:], in0=gb, in1=gb, op=ALU.mult), reads=[('gbT', c)], writes=[tk[6]])
                kb.op('pool', lambda g, sq=sq: g.tensor_scalar(out=sq[:], in0=sq[:], scalar1=0.044715, scalar2=1.0, op0=ALU.mult, op1=ALU.add),
                      reads=[tk[6]], writes=[tk[6]])
                kb.op('pool', lambda g, gb=gb, sq=sq, t2=t2: g.tensor_tensor(out=t2[:], in0=sq[:], in1=gb, op=ALU.mult), reads=[tk[6], ('gbT', c)], writes=[tk[7]])
                kb.op('act', lambda a, t2=t2, sg=sg: a.activation(out=sg[:], in_=t2[:], func=AF.Sigmoid, scale=1.5957691216057308),
                      reads=[tk[7]], writes=[tk[8]])
                kb.op('act', lambda a, aa=aa, rr=rr, c=c: a.activation(out=aa[:], in_=rr[:], func=AF.Exp, scale=coef[:, c:c + 1]),
                      reads=[tk[1], 'coef'], writes=[tk[3]])
                kb.op('pool', lambda g, aa=aa, ss=ss: g.tensor_tensor(out=ss[:], in0=aa[:], in1=aa[:], op=ALU.mult), reads=[tk[3]], writes=[tk[4]])
                kb.op('act', lambda a, ss=ss: a.activation(out=ss[:], in_=ss[:], func=AF.Sqrt, bias=1.0, scale=-1.0), reads=[tk[4]], writes=[tk[4]])
                kb.op('pool', lambda g, uu=uu, ss=ss, ii=ii: g.tensor_tensor(out=uu[:], in0=ss[:], in1=ii[:], op=ALU.mult), reads=[tk[4], tk[2]], writes=[tk[5]])
                kb.op('dve', lambda v, uu=uu, xc=xc: v.tensor_tensor(out=uu[:], in0=uu[:], in1=xc[:], op=ALU.mult), reads=[tk[5], xck], writes=[tk[5]])
                init = 0.0 if ci == 0 else hprev[:, c, T - 1:T]
                kb.op('dve', lambda v, aa=aa, uu=uu, c=c, init=init, hb=hb: v.tensor_tensor_scan(out=hb[:, c, :], data0=aa[:], data1=uu[:],
                      initial=init, op0=ALU.mult, op1=ALU.add), reads=[tk[3], tk[5], ('h', (ci + 1) % 2, c)], writes=[('h', ci % 2, c)])
                kb.op('pool', lambda g, gl=gl, sg=sg, gb=gb: g.tensor_tensor(out=gl[:], in0=sg[:], in1=gb, op=ALU.mult), reads=[tk[8], ('gbT', c)], writes=[tk[9]])
                kb.op('dve', lambda v, gl=gl, c=c, hb=hb, mx=mx: v.tensor_tensor(out=mx[:, c, :], in0=hb[:, c, :], in1=gl[:], op=ALU.mult),
                      reads=[tk[9], ('h', ci % 2, c)], writes=[('mixT0', c)])
                yield
            mem_attn(mqT, T, kT, vpad, onespad, lambda pr, mx=mx: mx[:, 6 + pr, :], E, rden, '0')
            yield

        mixkeys = [('mixT0', c) for c in range(8)]

        def tails(ci):
            mx = mixT[ci % 2]
            xfc, xfk = xf[ci % 2], ('xf', ci % 2)
            for t in range(T // 128):
                ti = ci * (T // 128) + t
                yield from tail.run_gen(ti, lambda k, mx=mx, t=t: mx[:, k, t * 128:(t + 1) * 128], mixkeys, xfc[:, t, :], xfk,
                                        x1buf[ti * 128:(ti + 1) * 128, :])

        for _ in mixer(0):
            pass
        for ci in range(nch):
            streams = [(tails(ci), 18)]
            if ci + 1 < nch:
                streams.append((mixer(ci + 1), 25))
            interleave(streams)
        kb.barrier()
        if mode.startswith("a0"):
            dbg_r = nc.dram_tensor("dbg_r", [128, NE], F32, kind="ExternalOutput").ap()
            dbg_d = nc.dram_tensor("dbg_d", [128, NT * 4], I32, kind="ExternalOutput").ap()
            kb.dma('sp', lambda q: q.dma_start(out=dbg_r[:, :], in_=tail.rrun[:]))
            kb.dma('sp', lambda q: q.dma_start(out=dbg_d[:, :], in_=destall[:].rearrange("p t k -> p (t k)")))
            kb.barrier()
        st.close()


    def phase_a1(src, ntiles=NT):
        T = 256
        KSEL = 256
        NBIS = 16
        CH = 512
        st = ExitStack()
        tail = Tail(1, st, nbuf=1)
        cTok = sb("cTok", [128, NT, 128], BF16, st)
        cT = sb("cT", [128, S], BF16, st)
        ikT2 = sb("ikT2", [128, S], BF16, st)
        absw = sb("absw", [128, NT, 4], F32, st)
        sgnw = sb("sgnw", [128, NT, 4], F32, st)
        BT = sb("BT", [128, 2, 12, 128], BF16, st)
        wuvpad = sb("wuvpad", [128, 12, 128], BF16, st)
        i4big = sb("i4big", [128, 4, 128], BF16, st)
        for r in range(4):
            kb.op('dve', lambda v, r=r: v.tensor_scalar(out=i4big[:, r, :], in0=ident_f[:], scalar1=100.0, scalar2=None, op0=ALU.mult),
                  reads=['ident_f'], writes=['i4big'])
        kb.op('pool', lambda g: g.memset(wuvpad[:], 0.0), writes=['wuvpad'])
        for h in range(12):
            par = h % 2
            load_cast(wuvpad[:, h, par * 64:(par + 1) * 64], b_w_uv[:, h, :], 'wuvpad')
        s1 = ExitStack()
        kT = sb("kT1", [128, 2, MEM], BF16, s1)
        vpad = sb("vpad1", [128, 4, 2, 128], BF16, s1)
        onespad = sb("onespad1", [128, 2, 128], BF16, s1)
        win = sb("win1", [128, 8, W_IN_B], BF16, s1)
        load_cast(win[:], b_w_in.rearrange("(k p) c -> p k c", p=128), 'win')
        wukT = sb("wukT", [128, 6, 128], BF16, s1)
        gkv = sb("gkv", [128, 128], F32, s1)
        gik = sb("gik", [128, 64], F32, s1)
        bik = sb("bik", [128, 64], F32, s1)
        bcast_rows(gkv[:], b_kv_norm_g, 'gkv')
        bcast_rows(gik[:], b_idx_norm_g, 'gik')
        bcast_rows(bik[:], b_idx_norm_b, 'bik')
        ssetup = ExitStack()
        setup_mem_kv(1, ssetup, kT, vpad, onespad)
        wuk = sb("wuk", [128, 768], BF16, ssetup)
        load_cast(wuk[:], b_w_uk.rearrange("r h d -> r (h d)"), 'wuk')
        for j in range(6):
            pt, pk = psum()
            ptb = pt[:].bitcast(BF16)
            kb.op('pe', lambda p, ptb=ptb, j=j: p.transpose(out=ptb[:, 0:128], in_=wuk[:, j * 128:(j + 1) * 128], identity=ident_b[:]),
                  reads=['wuk', 'ident_b'], writes=[pk])
            kb.op('dve', lambda v, ptb=ptb, j=j: v.tensor_copy(out=wukT[:, j, :], in_=ptb[:, 0:128]), reads=[pk], writes=['wukT'])
        rbb = sb("rbb", [128, 32, 12], F32, ssetup)
        bkt = sb("bkt", [128, 2, 128], F32, ssetup)
        caus = sb("caus", [128, 128], F32, ssetup)
        acc = sb("bacc", [128, 12, 128], F32, ssetup)
        prod = sb("bprod", [128, 12, 128], F32, ssetup)
        oh = sb("boh", [128, 128], F32, ssetup)
        kb.dma('sp', lambda q: q.dma_start(out=rbb[:].rearrange("p b h -> p (b h)"), in_=rel_bias.rearrange("b h -> (b h)").partition_broadcast(128)), writes=['rbb'])
        kb.dma('sp', lambda q: q.dma_start(out=bkt[:], in_=c_bkt[:, :, :]), writes=['bkt'])
        kb.dma('sp', lambda q: q.dma_start(out=caus[:], in_=c_caus[:, :]), writes=['caus'])
        for dt in range(2):
            kb.op('dve', lambda v: v.memset(acc[:], 0.0), writes=['bacc'])
            for b in range(32):
                kb.op('dve', lambda v, b=b, dt=dt: v.tensor_scalar(out=oh[:], in0=bkt[:, dt, :], scalar1=float(b), scalar2=None, op0=ALU.is_equal),
                      reads=['bkt'], writes=['boh'])
                kb.op('dve', lambda v, b=b: v.tensor_tensor(out=prod[:], in0=oh[:].unsqueeze(1).to_broadcast([128, 12, 128]),
                      in1=rbb[:, b, :].unsqueeze(2).to_broadcast([128, 12, 128]), op=ALU.mult), reads=['boh', 'rbb'], writes=['bprod'])
                kb.op('dve', lambda v: v.tensor_tensor(out=acc[:], in0=acc[:], in1=prod[:], op=ALU.add), reads=['bacc', 'bprod'], writes=['bacc'])
            kb.op('dve', lambda v: v.tensor_tensor(out=acc[:], in0=acc[:], in1=rbb[:, 31, :].unsqueeze(2).to_broadcast([128, 12, 128]), op=ALU.subtract),
                  reads=['bacc', 'rbb'], writes=['bacc'])
            if dt == 0:
                kb.op('dve', lambda v: v.tensor_tensor(out=acc[:], in0=acc[:], in1=caus[:].unsqueeze(1).to_broadcast([128, 12, 128]), op=ALU.add),
                      reads=['bacc', 'caus'], writes=['bacc'])
            kb.op('dve', lambda v, dt=dt: v.tensor_copy(out=BT[:, dt, :, :], in_=acc[:]), reads=['bacc'], writes=['BT'])
        kb.barrier()
        ssetup.close()

        xf = [sb(f"xf1_{i}", [128, 2, D], F32, s1) for i in range(2)]
        xbf = sb("xbf1", [128, 2, D], BF16, s1)
        xT = sb("xT1", [128, 8, T], BF16, s1)
        qT = sb("qT1", [128, 6, T], BF16, s1)
        qlb = [sb(f"qlb{i}", [128, 12, T], BF16, s1) for i in range(2)]
        iqT = [sb(f"iqT{i}", [128, 2, T], BF16, s1) for i in range(2)]
        mqT = sb("mqT1", [128, 2, T], BF16, s1)
        memo = [sb(f"memo{i}", [128, 2, T], BF16, s1) for i in range(2)]
        E = sb("E1", [128, 2, 2, T], BF16, s1)
        rden = sb("rden1", [128, T], F32, s1)
        csb = [sb(f"csb{i}", [128, 128], F32, s1) for i in range(2)]
        cnb = [sb(f"cnb{i}", [128, 128], BF16, s1) for i in range(2)]
        iks = [sb(f"iks{i}", [128, 68], F32, s1) for i in range(2)]
        ik2 = [sb(f"ik2{i}", [128, 128], BF16, s1) for i in range(2)]
        sm1 = [sb(f"smp1_{i}", [128, 32], F32, s1) for i in range(2)]
        nch = ntiles * 128 // T
        for ci in range(nch):
            xi = ci % 2
            xfc, xfk = xf[xi], ('xf', xi)
            kb.dma('sp', lambda q, ci=ci, xfc=xfc: q.dma_start(
                out=xfc[:], in_=src[ci * T:(ci + 1) * T, :].rearrange("(t p) d -> p t d", p=128)), writes=[xfk])
            kb.op('act', lambda a, xfc=xfc: a.copy(out=xbf[:], in_=xfc[:]), reads=[xfk], writes=['xbf'])
            for k in range(8):
                pt, pk = psum()
                ptb = pt[:].bitcast(BF16)
                for t in range(2):
                    kb.op('pe', lambda p, ptb=ptb, t=t, k=k: p.transpose(out=ptb[:, t * 128:(t + 1) * 128],
                          in_=xbf[:, t, k * 128:(k + 1) * 128], identity=ident_b[:]), reads=['xbf', 'ident_b'], writes=[pk])
                if k % 2 == 0:
                    kb.op('dve', lambda v, ptb=ptb, k=k: v.tensor_copy(out=xT[:, k, :], in_=ptb[:, 0:T]), reads=[pk], writes=[('xT', k)])
                else:
                    kb.op('act', lambda a, ptb=ptb, k=k: a.copy(out=xT[:, k, :], in_=ptb[:, 0:T]), reads=[pk], writes=[('xT', k)])
            xTk = [('xT', k) for k in range(8)]

            def fm_proj(col0, dst_ap, dkey, eng):
                pt, pk = psum()
                for k in range(8):
                    kb.op('pe', lambda p, pt=pt, k=k: p.matmul(pt[:, 0:T], lhsT=win[:, k, col0:col0 + 128], rhs=xT[:, k, :],
                          start=(k == 0), stop=(k == 7)), reads=['win'] + xTk, writes=[pk])
                if eng == 'act':
                    kb.op('act', lambda a, pt=pt: a.copy(out=dst_ap, in_=pt[:, 0:T]), reads=[pk], writes=[dkey])
                else:
                    kb.op('dve', lambda v, pt=pt: v.tensor_copy(out=dst_ap, in_=pt[:, 0:T]), reads=[pk], writes=[dkey])

            for c in range(6):
                fm_proj(c * 128, qT[:, c, :], ('qT', c), 'act' if c % 2 else 'dve')
            bi = ci % 2
            for j in range(2):
                fm_proj(896 + j * 128, iqT[bi][:, j, :], ('iqT', bi), 'act')
            for j in range(2):
                fm_proj(1220 + j * 128, mqT[:, j, :], 'mqT1', 'dve')
            kb.dma('sp', lambda q, ci=ci, bi=bi: q.dma_start(out=iqd[:, :, ci * T:(ci + 1) * T].rearrange("j p t -> p j t"), in_=iqT[bi][:]),
                   reads=[('iqT', bi)], writes=['iqd'])
            for h in range(12):
                j, hh = h // 2, h % 2
                pt, pk = psum()
                kb.op('pe', lambda p, pt=pt, j=j, hh=hh: p.matmul(pt[:, 0:T], lhsT=wukT[hh * 64:(hh + 1) * 64, j, :],
                      rhs=qT[hh * 64:(hh + 1) * 64, j, :], start=True, stop=True), reads=['wukT', ('qT', j)], writes=[pk])
                if h % 2 == 0:
                    kb.op('act', lambda a, pt=pt, h=h, bi=bi: a.activation(out=qlb[bi][:, h, :], in_=pt[:, 0:T], func=AF.Copy, scale=0.125),
                          reads=[pk], writes=[('qlb', bi)])
                else:
                    kb.op('dve', lambda v, pt=pt, h=h, bi=bi: v.tensor_scalar(out=qlb[bi][:, h, :], in0=pt[:, 0:T], scalar1=0.125, scalar2=None, op0=ALU.mult),
                          reads=[pk], writes=[('qlb', bi)])
            kb.dma('sp', lambda q, ci=ci, bi=bi: q.dma_start(out=qlat[:, :, ci * T:(ci + 1) * T].rearrange("h p t -> p h t"), in_=qlb[bi][:]),
                   reads=[('qlb', bi)], writes=['qlat'])
            mem_attn(mqT, T, kT, vpad, onespad, lambda pr, bi=bi: memo[bi][:, pr, :], E, rden, '1')
            kb.dma('sp', lambda q, ci=ci, bi=bi: q.dma_start(out=memod[:, :, ci * T:(ci + 1) * T].rearrange("j p t -> p j t"), in_=memo[bi][:]),
                   reads=[('mixT1', 6), ('mixT1', 7)], writes=['memod'])
            for t in range(T // 128):
                ti = ci * (T // 128) + t
                i2 = ti % 2
                smk = ('sm1', i2)
                sm = sm1[i2]
                pc, pck = psum()
                for k in range(8):
                    kb.op('pe', lambda p, pc=pc, k=k, t=t: p.matmul(pc[:, 0:128], lhsT=xT[:, k, t * 128:(t + 1) * 128], rhs=win[:, k, 768:896],
                          start=(k == 0), stop=(k == 7)), reads=['win'] + xTk, writes=[pck])
                pi_, pik = psum()
                for k in range(8):
                    kb.op('pe', lambda p, pi_=pi_, k=k, t=t: p.matmul(pi_[:, 0:68], lhsT=xT[:, k, t * 128:(t + 1) * 128], rhs=win[:, k, 1152:1220],
                          start=(k == 0), stop=(k == 7)), reads=['win'] + xTk, writes=[pik])
                ss = sm[:, 0:1]
                kb.op('act', lambda a, pc=pc, i2=i2, ss=ss: a.activation(out=csb[i2][:], in_=pc[:, 0:128], func=AF.Square, accum_out=ss),
                      reads=[pck], writes=[('csb', i2), smk])
                kb.op('act', lambda a, ss=ss: a.activation(out=ss, in_=ss, func=AF.Sqrt, bias=1e-6, scale=1.0 / 128.0), reads=[smk], writes=[smk])
                kb.op('dve', lambda v, ss=ss: v.reciprocal(out=ss, in_=ss), reads=[smk], writes=[smk])
                kb.op('dve', lambda v, pc=pc, i2=i2, ss=ss: v.scalar_tensor_tensor(out=csb[i2][:], in0=pc[:, 0:128], scalar=ss, in1=gkv[:],
                      op0=ALU.mult, op1=ALU.mult), reads=[pck, smk, 'gkv', ('csb', i2)], writes=[('csb', i2)])
                kb.op('act', lambda a, i2=i2, ti=ti: a.copy(out=cTok[:, ti, :], in_=csb[i2][:]), reads=[('csb', i2)], writes=[('cTok', ti)])
                pt, pk = psum()
                ptb = pt[:].bitcast(BF16)
                kb.op('pe', lambda p, ptb=ptb, ti=ti: p.transpose(out=ptb[:, 0:128], in_=cTok[:, ti, :], identity=ident_b[:]),
                      reads=[('cTok', ti), 'ident_b'], writes=[pk])
                kb.op('dve', lambda v, ptb=ptb, ti=ti: v.tensor_copy(out=cT[:, ti * 128:(ti + 1) * 128], in_=ptb[:, 0:128]), reads=[pk], writes=[('cT', ti)])
                kb.op('act', lambda a, pi_=pi_, i2=i2: a.copy(out=iks[i2][:], in_=pi_[:, 0:68]), reads=[pik], writes=[('iks', i2)])
                st6 = sm[:, 8:14]
                mv = sm[:, 14:16]
                rs = sm[:, 16:17]
                kb.op('dve', lambda v, i2=i2, st6=st6: v.bn_stats(out=st6, in_=iks[i2][:, 0:64]), reads=[('iks', i2)], writes=[smk])
                kb.op('dve', lambda v, st6=st6, mv=mv: v.bn_aggr(out=mv, in_=st6), reads=[smk], writes=[smk])
                kb.op('act', lambda a, mv=mv, rs=rs: a.activation(out=rs, in_=mv[:, 1:2], func=AF.Sqrt, bias=1e-5, scale=1.0), reads=[smk], writes=[smk])
                kb.op('dve', lambda v, rs=rs: v.reciprocal(out=rs, in_=rs), reads=[smk], writes=[smk])
                kb.op('dve', lambda v, i2=i2, mv=mv, rs=rs: v.tensor_scalar(out=iks[i2][:, 0:64], in0=iks[i2][:, 0:64], scalar1=mv[:, 0:1], scalar2=rs,
                      op0=ALU.subtract, op1=ALU.mult), reads=[('iks', i2), smk], writes=[('iks', i2)])
                kb.op('dve', lambda v, i2=i2: v.tensor_tensor(out=iks[i2][:, 0:64], in0=iks[i2][:, 0:64], in1=gik[:], op=ALU.mult),
                      reads=[('iks', i2), 'gik'], writes=[('iks', i2)])
                for r in range(2):
                    kb.op('dve', lambda v, i2=i2, r=r: v.tensor_tensor(out=ik2[i2][:, r * 64:(r + 1) * 64], in0=iks[i2][:, 0:64], in1=bik[:], op=ALU.add),
                          reads=[('iks', i2), 'bik'], writes=[('ik2', i2)])
                pt, pk = psum()
                ptb = pt[:].bitcast(BF16)
                kb.op('pe', lambda p, ptb=ptb, i2=i2: p.transpose(out=ptb[:, 0:128], in_=ik2[i2][:], identity=ident_b[:]),
                      reads=[('ik2', i2), 'ident_b'], writes=[pk])
                kb.op('act', lambda a, ptb=ptb, ti=ti: a.copy(out=ikT2[:, ti * 128:(ti + 1) * 128], in_=ptb[:, 0:128]), reads=[pk], writes=[('ikT2', ti)])
                kb.op('act', lambda a, i2=i2, ti=ti: a.activation(out=absw[:, ti, :], in_=iks[i2][:, 64:68], func=AF.Abs),
                      reads=[('iks', i2)], writes=[('absw', ti)])
                kb.op('dve', lambda v, i2=i2, ti=ti: v.tensor_scalar(out=sgnw[:, ti, :], in0=iks[i2][:, 64:68], scalar1=0.0, scalar2=2.0, op0=ALU.is_ge, op1=ALU.mult),
                      reads=[('iks', i2)], writes=[('sgnw', ti)])
                kb.op('dve', lambda v, ti=ti: v.tensor_scalar(out=sgnw[:, ti, :], in0=sgnw[:, ti, :], scalar1=-1.0, scalar2=None, op0=ALU.add),
                      reads=[('sgnw', ti)], writes=[('sgnw', ti)])
        kb.barrier()
        s1.close()

        s2 = ExitStack()
        sc = sb("sc", [128, S], F32, s2)
        junk = sb("junk", [128, S], mybir.dt.uint8, s2)
        maskb = sb("maskb", [128, S], BF16, s2)
        tiec = [sb(f"tiec{i}", [128, CH], BF16, s2) for i in range(2)]
        cumc = [sb(f"cumc{i}", [128, CH], F32, s2) for i in range(2)]
        onesc = sb("onesc", [128, CH], BF16, s2)
        kb.op('pool', lambda g: g.memset(onesc[:], 1.0), writes=['onesc'])
        cmask = sb("cmask", [128, 128], F32, s2)
        kb.dma('sp', lambda q: q.dma_start(out=cmask[:], in_=c_cmask[:, :]), writes=['cmask'])
        rl = [sb(f"rl{i}", [128, 512], F32, s2) for i in range(2)]
        ql = sb("ql", [128, 12, 128], BF16, s2)
        iqb = [sb(f"iqb{i}", [128, 2, 128], BF16, s2) for i in range(2)]
        mixT = [sb(f"mixT1_{i}", [128, 8, 128], BF16, s2) for i in range(2)]
        x2t = sb("x2t", [128, D], F32, s2)
        Pt = [sb(f"Pt{i}", [128, 512], BF16, s2) for i in range(3)]
        olat = [sb(f"olat{i}", [128, 512], BF16, s2) for i in range(2)]
        Dsb = sb("Dsb", [128, 512], F32, s2)
        Osb = sb("Osb", [128, 512], F32, s2)
        bs = sb("bs", [128, 16], F32, s2)
        steps = sb("steps", [128, 24], F32, s2)
        pow2 = sb("pow2", [128, 24], F32, s2)
        kb.dma('sp', lambda q: q.dma_start(out=pow2[:], in_=c_pow2[:, :]), writes=['pow2'])
        lo, hi, mid, cnt, ge, dd, ee, need, cgt, carry = [bs[:, i:i + 1] for i in range(10)]
        npt = [0]

        def selection_a(qb):
            n = (qb + 1) * 128
            ib = qb % 2
            kb.dma('sp', lambda q: q.dma_start(out=iqb[ib][:], in_=iqd[:, :, qb * 128:(qb + 1) * 128].rearrange("j p t -> p j t")),
                   writes=[('iqb', ib)])
            for g0 in range(0, n, 512):
                w = min(512, n - g0)
                for h in range(4):
                    j, hh = h // 2, h % 2
                    pt, pk = psum()
                    kb.op('pe', lambda p, pt=pt, j=j, hh=hh, g0=g0, w=w: p.matmul(pt[:, 0:w], lhsT=iqb[ib][hh * 64:(hh + 1) * 64, j, :],
                          rhs=ikT2[hh * 64:(hh + 1) * 64, g0:g0 + w], start=True, stop=True), reads=[('iqb', ib), 'ikT2'], writes=[pk])
                    ri = h % 2
                    kb.op('act', lambda a, pt=pt, ri=ri, w=w, h=h: a.activation(out=rl[ri][:, 0:w], in_=pt[:, 0:w], func=AF.Relu, scale=absw[:, qb, h:h + 1]),
                          reads=[pk], writes=[('rl', ri)])
                    if h == 0:
                        kb.op('dve', lambda v, ri=ri, g0=g0, w=w, h=h: v.tensor_scalar(out=sc[:, g0:g0 + w], in0=rl[ri][:, 0:w], scalar1=sgnw[:, qb, h:h + 1],
                              scalar2=None, op0=ALU.mult), reads=[('rl', ri)], writes=['sc'])
                    else:
                        kb.op('dve', lambda v, ri=ri, g0=g0, w=w, h=h: v.scalar_tensor_tensor(out=sc[:, g0:g0 + w], in0=rl[ri][:, 0:w], scalar=sgnw[:, qb, h:h + 1],
                              in1=sc[:, g0:g0 + w], op0=ALU.mult, op1=ALU.add), reads=[('rl', ri), 'sc'], writes=['sc'])
                    if h % 2 == 1:
                        yield
            kb.op('dve', lambda v: v.tensor_tensor(out=sc[:, n - 128:n], in0=sc[:, n - 128:n], in1=cmask[:], op=ALU.add), reads=['sc', 'cmask'], writes=['sc'])
            kb.op('dve', lambda v: v.tensor_reduce(out=hi, in_=sc[:, 0:n], axis=AX.X, op=ALU.max), reads=['sc'], writes=['bs'])
            kb.op('dve', lambda v: v.tensor_reduce(out=lo, in_=sc[:, 0:n - 128], axis=AX.X, op=ALU.min), reads=['sc'], writes=['bs'])
            yield
            kb.op('dve', lambda v: v.scalar_tensor_tensor(out=dd, in0=hi, scalar=2.0, in1=lo, op0=ALU.add, op1=ALU.subtract), reads=['bs'], writes=['bs'])
            kb.op('dve', lambda v: v.tensor_scalar(out=steps[:], in0=pow2[:], scalar1=dd, scalar2=None, op0=ALU.mult), reads=['bs', 'pow2'], writes=['steps'])
            kb.op('dve', lambda v: v.scalar_tensor_tensor(out=mid, in0=lo, scalar=-1.0, in1=steps[:, 0:1], op0=ALU.add, op1=ALU.add), reads=['bs', 'steps'], writes=['bs'])
            yield
            for it in range(NBIS):
                kb.op('dve', lambda v: v.tensor_scalar(out=junk[:, 0:n], in0=sc[:, 0:n], scalar1=mid, scalar2=None, op0=ALU.is_ge, op1=ALU.add, accum_out=cnt),
                      reads=['sc', 'bs'], writes=['junk', 'bs'])
                yield
                kb.op('dve', lambda v: v.tensor_scalar(out=ge, in0=cnt, scalar1=float(KSEL), scalar2=0.5, op0=ALU.is_ge, op1=ALU.subtract), reads=['bs'], writes=['bs'])
                kb.op('dve', lambda v, it=it: v.scalar_tensor_tensor(out=mid, in0=ge, scalar=steps[:, it:it + 1], in1=mid, op0=ALU.mult, op1=ALU.add),
                      reads=['bs', 'steps'], writes=['bs'])
                yield
            kb.op('dve', lambda v: v.tensor_tensor(out=lo, in0=mid, in1=steps[:, NBIS:NBIS + 1], op=ALU.subtract), reads=['bs', 'steps'], writes=['bs'])
            kb.op('dve', lambda v: v.tensor_tensor(out=hi, in0=mid, in1=steps[:, NBIS:NBIS + 1], op=ALU.add), reads=['bs', 'steps'], writes=['bs'])
            kb.op('dve', lambda v: v.tensor_scalar(out=junk[:, 0:n], in0=sc[:, 0:n], scalar1=hi, scalar2=None, op0=ALU.is_ge, op1=ALU.add, accum_out=cgt),
                  reads=['sc', 'bs'], writes=['junk', 'bs'])
            kb.op('dve', lambda v: v.tensor_scalar(out=need, in0=cgt, scalar1=-1.0, scalar2=float(KSEL), op0=ALU.mult, op1=ALU.add), reads=['bs'], writes=['bs'])
            yield

        def selection_b(qb):
            n = (qb + 1) * 128
            for ci_, c0 in enumerate(range(0, n, CH)):
                w = min(CH, n - c0)
                r_ = ci_ % 2
                tk, ck = ('tiec', r_), ('cumc', r_)
                kb.op('dve', lambda v, c0=c0, w=w, r_=r_: v.tensor_scalar(out=tiec[r_][:, 0:w], in0=sc[:, c0:c0 + w], scalar1=hi, scalar2=None, op0=ALU.is_lt),
                      reads=['sc', 'bs'], writes=[tk])
                kb.op('dve', lambda v, c0=c0, w=w, r_=r_: v.scalar_tensor_tensor(out=tiec[r_][:, 0:w], in0=sc[:, c0:c0 + w], scalar=lo, in1=tiec[r_][:, 0:w],
                      op0=ALU.is_ge, op1=ALU.mult), reads=['sc', 'bs', tk], writes=[tk])
                init = 0.0 if c0 == 0 else carry
                kb.op('dve', lambda v, w=w, r_=r_, init=init: v.tensor_tensor_scan(out=cumc[r_][:, 0:w], data0=onesc[:, 0:w], data1=tiec[r_][:, 0:w],
                      initial=init, op0=ALU.mult, op1=ALU.add), reads=['onesc', tk, 'bs'], writes=[ck])
                kb.op('dve', lambda v, w=w, r_=r_: v.tensor_copy(out=carry, in_=cumc[r_][:, w - 1:w]), reads=[ck], writes=['bs'])
                kb.op('dve', lambda v, w=w, r_=r_: v.scalar_tensor_tensor(out=tiec[r_][:, 0:w], in0=cumc[r_][:, 0:w], scalar=need, in1=tiec[r_][:, 0:w],
                      op0=ALU.is_le, op1=ALU.mult), reads=[ck, 'bs', tk], writes=[tk])
                kb.op('dve', lambda v, c0=c0, w=w, r_=r_: v.scalar_tensor_tensor(out=maskb[:, c0:c0 + w], in0=sc[:, c0:c0 + w], scalar=hi, in1=tiec[r_][:, 0:w],
                      op0=ALU.is_ge, op1=ALU.add), reads=['sc', 'bs', tk], writes=['maskb'])

        def attention(qb):
            mx = mixT[qb % 2]
            kb.dma('sp', lambda q: q.dma_start(out=ql[:], in_=qlat[:, :, qb * 128:(qb + 1) * 128].rearrange("h p t -> p h t")), writes=['ql'])
            kb.dma('sp', lambda q: q.dma_start(out=mx[:, 6:8, :], in_=memod[:, :, qb * 128:(qb + 1) * 128].rearrange("j p t -> p j t")),
                   writes=[('mixTm', qb % 2)])
            kb.dma('sp', lambda q: q.dma_start(out=x2t[:], in_=src[qb * 128:(qb + 1) * 128, :]), writes=['x2t'])
            pO, pOk = ps[6], ('ps', 6)
            pD, pDk = ps[7], ('ps', 7)
            steps_ = [(hg, j) for hg in range(3) for j in range(qb + 1)]

            def logits(hg, j):
                qrhs = ql[:, hg * 4:(hg + 1) * 4, :].rearrange("p h t -> p (h t)")
                pL, pLk = psum()
                dt = qb - j
                nmm = 1 + (1 if qb >= 2 else 0) + (1 if dt <= 1 else 0)
                m = 0
                kb.op('pe', lambda p: p.matmul(pL[:, :], lhsT=cT[:, j * 128:(j + 1) * 128], rhs=qrhs, start=True, stop=(nmm == 1)),
                      reads=[('cT', j), 'ql'], writes=[pLk])
                m += 1
                if qb >= 2:
                    kb.op('pe', lambda p, m=m: p.matmul(pL[:, :], lhsT=maskb[:, j * 128:(j + 1) * 128],
                          rhs=i4big[:].rearrange("p r t -> p (r t)"), start=False, stop=(m == nmm - 1)), reads=['maskb', 'i4big'], writes=[pLk])
                    m += 1
                if dt <= 1:
                    kb.op('pe', lambda p, m=m: p.matmul(pL[:, :], lhsT=ident_b[:],
                          rhs=BT[:, dt, hg * 4:(hg + 1) * 4, :].rearrange("p h t -> p (h t)"), start=False, stop=(m == nmm - 1)),
                          reads=['BT', 'ident_b'], writes=[pLk])
                    m += 1
                return pL, pLk

            cur = logits(*steps_[0])
            for idx, (hg, j) in enumerate(steps_):
                pL, pLk = cur
                pi = npt[0] % 3
                npt[0] += 1
                kb.op('act', lambda a, pL=pL, pi=pi: a.activation(out=Pt[pi][:], in_=pL[:, :], func=AF.Exp, bias=(nbias[:] if qb >= 2 else zbias[:]), scale=1.0),
                      reads=[pLk, 'nbias'], writes=[('Pt', pi)])
                if idx + 1 < len(steps_):
                    cur = logits(*steps_[idx + 1])
                kb.op('pe', lambda p, j=j, pi=pi: p.matmul(pO[:, :], lhsT=cTok[:, j, :], rhs=Pt[pi][:], start=(j == 0), stop=(j == qb)),
                      reads=[('cTok', j), ('Pt', pi)], writes=[pOk])
                kb.op('pe', lambda p, j=j, pi=pi: p.matmul(pD[:, :], lhsT=ones_b[:], rhs=Pt[pi][:], start=(j == 0), stop=(j == qb)),
                      reads=['ones_b', ('Pt', pi)], writes=[pDk])
                yield
                if j == qb:
                    oi = hg % 2
                    kb.op('act', lambda a: a.copy(out=Dsb[:], in_=pD[:, :]), reads=[pDk], writes=['Dsb'])
                    kb.op('act', lambda a: a.copy(out=Osb[:], in_=pO[:, :]), reads=[pOk], writes=['Osb'])
                    kb.op('dve', lambda v: v.reciprocal(out=Dsb[:], in_=Dsb[:]), reads=['Dsb'], writes=['Dsb'])
                    kb.op('pool', lambda g, oi=oi: g.tensor_tensor(out=olat[oi][:], in0=Osb[:], in1=Dsb[:], op=ALU.mult), reads=['Osb', 'Dsb'], writes=[('olat', oi)])
                    for pp in range(2):
                        pT, pTk = psum()
                        for hh in range(2):
                            hl = 2 * pp + hh
                            h = hg * 4 + hl
                            kb.op('pe', lambda p, pT=pT, h=h, hl=hl, hh=hh, oi=oi: p.matmul(pT[:, 0:128], lhsT=wuvpad[:, h, :], rhs=olat[oi][:, hl * 128:(hl + 1) * 128],
                                  start=(hh == 0), stop=(hh == 1)), reads=['wuvpad', ('olat', oi)], writes=[pTk])
                        kb.op('act', lambda a, pT=pT, hg=hg, pp=pp: a.copy(out=mx[:, hg * 2 + pp, :], in_=pT[:, 0:128]), reads=[pTk], writes=[('mixTt', qb % 2, hg * 2 + pp)])
                    yield

        nbias = sb("nbias", [128, 1], F32, s2)
        zbias = sb("zbias", [128, 1], F32, s2)
        kb.op('dve', lambda v: v.memset(nbias[:], -100.0), writes=['nbias'])
        kb.op('dve', lambda v: v.memset(zbias[:], 0.0), writes=['nbias'])

        nqb = ntiles

        def chain(*gens):
            for g in gens:
                yield from g

        def nsteps_sel(qb):
            n = (qb + 1) * 128
            return 2 * ((n + 511) // 512) + 3 + 2 * NBIS

        if nqb > 2:
            for _ in selection_a(2):
                pass
        for qb in range(nqb):
            if qb >= 2:
                selection_b(qb)
            mx = mixT[qb % 2]
            mkeys = [('mixTt', qb % 2, k) for k in range(6)] + [('mixTm', qb % 2)]
            gB = chain(attention(qb), tail.run_gen(qb, lambda k, mx=mx: mx[:, k, :], mkeys, x2t, 'x2t', x1buf[qb * 128:(qb + 1) * 128, :]))
            nB = 3 * (qb + 1) + 3 + 8
            streams = [(gB, nB)]
            if qb + 1 < nqb and qb + 1 >= 2:
                streams.append((selection_a(qb + 1), nsteps_sel(qb + 1)))
            interleave(streams)
        kb.barrier()
        s2.close()
        st.close()

    def phase_moe(layer, dst, ntiles=NT, nexp=NE):
        L = layer
        st = ExitStack()
        w1 = [sb(f"w1_{L}_{i}", [128, 8, 2 * D], BF16, st) for i in range(2)]
        w2 = [sb(f"w2_{L}_{i}", [128, 8, D], BF16, st) for i in range(2)]
        b2r = [sb(f"b2r_{L}_{i}", [1, D], BF16, st) for i in range(2)]
        b1a = sb(f"b1a_{L}", [128, NE, 16], F32, st)
        b1u = sb(f"b1u_{L}", [128, NE, 8], F32, st)
        with nc.allow_non_contiguous_dma(reason="bias layout"):
            kb.dma('sp', lambda q: q.dma_start(out=b1a[:], in_=exp_b1[L].rearrange("e (j p) -> p e j", p=128)), writes=['b1a'])
        kb.op('dve', lambda v: v.tensor_scalar(out=b1u[:], in0=b1a[:, :, 8:16], scalar1=1.0, scalar2=None, op0=ALU.add), reads=['b1a'], writes=['b1u'])
        xgt = [sb(f"xgt_{L}_{i}", [128, 3, D], BF16, st) for i in range(2)]
        xgT = [sb(f"xgT_{L}_{i}", [128, 8, RG], BF16, st) for i in range(2)]
        actT = [sb(f"actT_{L}_{i}", [128, 8, RG], BF16, st) for i in range(2)]
        tg = [sb(f"tg_{L}_{i}", [128, RG], F32, st) for i in range(2)]
        tu = [sb(f"tu_{L}_{i}", [128, RG], F32, st) for i in range(2)]
        tsg = [sb(f"tsg_{L}_{i}", [128, RG], F32, st) for i in range(2)]
        tt = [sb(f"tt_{L}_{i}", [128, RG], F32, st) for i in range(2)]
        yev = [sb(f"yev_{L}_{i}", [128, D], F32, st) for i in range(2)]
        nyev = 0

        def load_w(e):
            i = e % 2
            load_cast(w1[i][:], exp_w1[L, e].rearrange("(k p) f -> p k f", p=128), ('w1', i))
            load_cast(w2[i][:], exp_w2[L, e].rearrange("(k p) n -> p k n", p=128), ('w2', i))
            load_cast(b2r[i][:], exp_b2[L, e:e + 1, :], ('b2r', i))

        NG = CAP // RG

        def stage_a(e, g, gi):
            wi = e % 2
            r0 = e * CAP + g * RG
            kb.dma('sp', lambda q: q.dma_start(out=xgt[gi][:], in_=xg[r0:r0 + RG, :].rearrange("(t p) d -> p t d", p=128)),
                   reads=['xg'], writes=[('xgt', gi)])
            for k in range(8):
                pt, pk = psum()
                ptb = pt[:].bitcast(BF16)
                for t in range(3):
                    kb.op('pe', lambda p, ptb=ptb, t=t, k=k: p.transpose(out=ptb[:, t * 128:(t + 1) * 128],
                          in_=xgt[gi][:, t, k * 128:(k + 1) * 128], identity=ident_b[:]), reads=[('xgt', gi), 'ident_b'], writes=[pk])
                if k % 2 == 0:
                    kb.op('dve', lambda v, ptb=ptb, k=k: v.tensor_copy(out=xgT[gi][:, k, :], in_=ptb[:, 0:RG]), reads=[pk], writes=[('xgT', gi, k)])
                else:
                    kb.op('act', lambda a, ptb=ptb, k=k: a.copy(out=xgT[gi][:, k, :], in_=ptb[:, 0:RG]), reads=[pk], writes=[('xgT', gi, k)])
                if k % 2 == 1:
                    yield
            xgTk = [('xgT', gi, k) for k in range(8)]
            for j in range(8):
                ji = j % 2
                pg, pgk = psum()
                pu, puk = psum()
                for k in range(8):
                    kb.op('pe', lambda p, pg=pg, k=k, j=j: p.matmul(pg[:, 0:RG], lhsT=w1[wi][:, k, j * 128:(j + 1) * 128],
                          rhs=xgT[gi][:, k, :], start=(k == 0), stop=(k == 7)), reads=[('w1', wi)] + xgTk, writes=[pgk])
                yield
                for k in range(8):
                    kb.op('pe', lambda p, pu=pu, k=k, j=j: p.matmul(pu[:, 0:RG], lhsT=w1[wi][:, k, D + j * 128:D + (j + 1) * 128],
                          rhs=xgT[gi][:, k, :], start=(k == 0), stop=(k == 7)), reads=[('w1', wi)] + xgTk, writes=[puk])
                kb.op('dve', lambda v, pg=pg, ji=ji, j=j: v.tensor_scalar(out=tg[ji][:], in0=pg[:, 0:RG], scalar1=b1a[:, e, j:j + 1],
                      scalar2=7.0, op0=ALU.add, op1=ALU.min), reads=[pgk, 'b1a'], writes=[('tg', ji)])
                kb.op('act', lambda a, ji=ji: a.activation(out=tsg[ji][:], in_=tg[ji][:], func=AF.Sigmoid, scale=1.702),
                      reads=[('tg', ji)], writes=[('tsg', ji)])
                kb.op('dve', lambda v, pu=pu, ji=ji, j=j: v.tensor_scalar(out=tu[ji][:], in0=pu[:, 0:RG], scalar1=b1u[:, e, j:j + 1],
                      scalar2=8.0, op0=ALU.add, op1=ALU.min), reads=[puk, 'b1u'], writes=[('tu', ji)])
                kb.op('dve', lambda v, ji=ji: v.scalar_tensor_tensor(out=tt[ji][:], in0=tu[ji][:], scalar=-6.0, in1=tg[ji][:],
                      op0=ALU.max, op1=ALU.mult), reads=[('tu', ji), ('tg', ji)], writes=[('tt', ji)])
                kb.op('pool', lambda g_, ji=ji, j=j: g_.tensor_tensor(out=actT[gi][:, j, :], in0=tt[ji][:], in1=tsg[ji][:], op=ALU.mult),
                      reads=[('tt', ji), ('tsg', ji)], writes=[('actT', gi, j)])
                yield

        def stage_b(e, g, gi):
            nonlocal nyev
            wi = e % 2
            r0 = e * CAP + g * RG
            actk = [('actT', gi, j) for j in range(8)]
            for t in range(3):
                yi = nyev % 2
                nyev += 1
                for nh in range(2):
                    py, pyk = psum()
                    for k in range(8):
                        kb.op('pe', lambda p, py=py, k=k, t=t, nh=nh: p.matmul(py[:, :], lhsT=actT[gi][:, k, t * 128:(t + 1) * 128],
                              rhs=w2[wi][:, k, nh * 512:(nh + 1) * 512], start=(k == 0), stop=False), reads=[('w2', wi)] + actk, writes=[pyk])
                    kb.op('pe', lambda p, py=py, nh=nh: p.matmul(py[:, :], lhsT=ones_b[0:1, :], rhs=b2r[wi][0:1, nh * 512:(nh + 1) * 512],
                          start=False, stop=True), reads=[('b2r', wi), 'ones_b'], writes=[pyk])
                    kb.op('act', lambda a, py=py, nh=nh, yi=yi: a.copy(out=yev[yi][:, nh * 512:(nh + 1) * 512], in_=py[:, :]),
                          reads=[pyk], writes=[('yev', yi)])
                    yield
                rr0 = r0 + t * 128
                kb.dma('sp', lambda q, rr0=rr0, yi=yi: q.dma_start(out=yg[rr0:rr0 + 128, :], in_=yev[yi][:]), reads=[('yev', yi)], writes=['yg'])

        groups = [(e, g) for e in range(nexp) for g in range(NG)]
        load_w(0)
        if nexp > 1:
            load_w(1)
        for _ in stage_a(groups[0][0], groups[0][1], 0):
            pass
        for i, (e, g) in enumerate(groups):
            if g == 0 and e >= 1 and e + 1 < nexp:
                load_w(e + 1)
            streams = [(stage_b(e, g, i % 2), 6)]
            if i + 1 < len(groups):
                e2, g2 = groups[i + 1]
                streams.append((stage_a(e2, g2, (i + 1) % 2), 20))
            interleave(streams)
        kb.barrier()
        st.close()
        st = ExitStack()
        lng = sb(f"ln2g{L}", [128, D], F32, st)
        lnb = sb(f"ln2b{L}", [128, D], F32, st)
        bcast_rows(lng[:], ln2_g[L], 'lng2')
        bcast_rows(lnb[:], ln2_b[L], 'lnb2')
        yk = [[sb(f"yk{L}_{i}_{k}", [128, D], F32, st) for k in range(4)] for i in range(2)]
        x1r = [sb(f"x1r{L}_{i}", [128, D], F32, st) for i in range(2)]
        zz = [sb(f"zz{L}_{i}", [128, D], F32, st) for i in range(2)]
        oo = [sb(f"oo{L}_{i}", [128, D], F32, st) for i in range(2)]
        smm = [sb(f"smm{L}_{i}", [128, 256], F32, st) for i in range(2)]
        lnh = Tail.__new__(Tail)
        for ti in range(ntiles):
            i = ti % 2
            kb.dma('sp', lambda q, ti=ti, i=i: q.dma_start(out=x1r[i][:], in_=x1buf[ti * 128:(ti + 1) * 128, :]), reads=[('x1d', ti)], writes=[('x1r', i)])
            for k in range(4):
                kb.op('pool', lambda g_, i=i, k=k: g_.memset(yk[i][k][:], 0.0), writes=[('yk', i, k)])
                kb.dma('pool', lambda q, ti=ti, i=i, k=k: q.indirect_dma_start(
                    out=yk[i][k][:, :], out_offset=None, in_=yg[:, :],
                    in_offset=bass.IndirectOffsetOnAxis(ap=destall[:, ti, k:k + 1], axis=0),
                    bounds_check=bc_reg, oob_is_err=False), reads=['yg', ('dest', ti)], writes=[('yk', i, k)])
            kb.op('dve', lambda v, i=i: v.tensor_scalar(out=zz[i][:], in0=x1r[i][:], scalar1=ALPHA, scalar2=None, op0=ALU.mult),
                  reads=[('x1r', i)], writes=[('zz', i)])
            for k in range(4):
                kb.op('dve', lambda v, ti=ti, i=i, k=k: v.scalar_tensor_tensor(out=zz[i][:], in0=yk[i][k][:], scalar=gall[:, ti, k:k + 1],
                      in1=zz[i][:], op0=ALU.mult, op1=ALU.add), reads=[('yk', i, k), ('gate', ti), ('zz', i)], writes=[('zz', i)])
            Tail.layernorm(lnh, zz[i], ('zz', i), oo[i], ('oo', i), smm[i], ('smm', i), lng, lnb, 'lng2', 'lnb2')
            kb.dma('sp', lambda q, ti=ti, i=i: q.dma_start(out=dst[ti * 128:(ti + 1) * 128, :], in_=oo[i][:]), reads=[('oo', i)], writes=[('dst', L, ti)])
        kb.barrier()
        st.close()

    if mode.startswith("a0"):
        ntl = NT if mode == "a0" else int(mode[2:])
        phase_a0(ntiles=ntl)
        st = ExitStack()
        cp = [sb(f"cp{i}", [128, D], F32, st) for i in range(2)]
        for ti in range(ntl):
            i = ti % 2
            kb.dma('sp', lambda q, ti=ti, i=i: q.dma_start(out=cp[i][:], in_=x1buf[ti * 128:(ti + 1) * 128, :]), writes=[('cp', i)])
            kb.dma('sp', lambda q, ti=ti, i=i: q.dma_start(out=out[ti * 128:(ti + 1) * 128, :], in_=cp[i][:]), reads=[('cp', i)], writes=[('o', ti)])
        kb.barrier()
        st.close()
    elif mode == "full":
        phase_a0()
        phase_moe(0, x2buf)
        phase_a1(x2buf)
        phase_moe(1, out)
    elif mode.startswith("a1"):
        ntl = int(mode[2:])
        zt = sb("zt1", [128, 4096], BF16)
        kb.op('pool', lambda g: g.memset(zt[:], 0.0), writes=['zt'])
        for r0 in range(0, NROW, 512):
            kb.dma('sp', lambda q, r0=r0: q.dma_start(out=xg[r0:r0 + 512, :].rearrange("(p t) d -> p (t d)", t=4), in_=zt[:]),
                   reads=['zt'], writes=['xg'])
        phase_a1(x_in, ntiles=ntl)
        st = ExitStack()
        cp = [sb(f"cp{i}", [128, D], F32, st) for i in range(2)]
        for ti in range(ntl):
            i = ti % 2
            kb.dma('sp', lambda q, ti=ti, i=i: q.dma_start(out=cp[i][:], in_=x1buf[ti * 128:(ti + 1) * 128, :]), writes=[('cp', i)])
            kb.dma('sp', lambda q, ti=ti, i=i: q.dma_start(out=out[ti * 128:(ti + 1) * 128, :], in_=cp[i][:]), reads=[('cp', i)], writes=[('o', ti)])
        kb.barrier()
        st.close()
    elif mode == "l0":
        phase_a0()
        phase_moe(0, out)
    es.close()
    return nc


def host_consts():
    ident = np.eye(128, dtype=np.float32)
    tri = np.triu(np.ones((128, 128), np.float32), 1)
    iota = np.tile(np.arange(NE, dtype=np.float32)[None, :], (128, 1))
    ecap = iota * CAP
    q = np.arange(128)
    cmask = np.where(q[None, :] <= q[:, None], 0.0, -2000.0).astype(np.float32)
    caus = np.where(q[:, None] <= q[None, :], 0.0, -30000.0).astype(np.float32)
    bkt = np.zeros((128, 2, 128), np.float32)
    for dt in range(2):
        rel = np.maximum(q[None, :] - q[:, None] + 128 * dt, 0)
        large = 16 + (np.log(np.maximum(rel, 1).astype(np.float32) / 16) / np.float32(np.log(128 / 16)) * 16).astype(np.int32)
        large = np.minimum(large, 31)
        bkt[:, dt, :] = np.where(rel < 16, rel, large)
    pow2 = np.tile((2.0 ** -(np.arange(24, dtype=np.float64) + 1)).astype(np.float32)[None, :], (128, 1))
    return {"c_ident": ident, "c_tri": tri, "c_iota": iota, "c_ecap": ecap, "c_cmask": cmask, "c_caus": caus, "c_bkt": bkt,
            "c_pow2": pow2}


_PARAMS = ["rel_bias", "a_w_in", "a_conv_w", "a_conv_b", "a_wr", "a_br", "a_wi", "a_bi", "a_lambda", "b_w_in",
           "b_kv_norm_g", "b_w_uk", "b_w_uv", "b_idx_norm_g", "b_idx_norm_b", "w_mem_kv", "w_out", "ln1_g", "ln1_b",
           "router_w", "router_b", "exp_w1", "exp_b1", "exp_w2", "exp_b2", "ln2_g", "ln2_b"]
_SQUEEZE = {"a_w_in", "a_conv_w", "a_conv_b", "a_wr", "a_br", "a_wi", "a_bi", "a_lambda", "b_w_in", "b_kv_norm_g",
            "b_w_uk", "b_w_uv", "b_idx_norm_g", "b_idx_norm_b"}


def make_in_maps(inputs, cores):
    shared = {}
    for k in _PARAMS:
        v = np.ascontiguousarray(np.asarray(inputs[k], dtype=np.float32))
        if k in _SQUEEZE:
            v = v[0]
        shared[k] = v
    shared.update(host_consts())
    maps = []
    for c in cores:
        m = dict(shared)
        m["x"] = np.ascontiguousarray(inputs["x"][c])
        m["mem"] = np.ascontiguousarray(inputs["mem"][c])
        maps.append(m)
    return maps


def kernel(**inputs):
    nc = build_program("full")
    maps = make_in_maps(inputs, list(range(8)))
    res = run_bass_kernel_spmd(nc, maps, core_ids=list(range(8)))
    return np.stack([r["out"] for r in res.results], axis=0)
```

```python
from contextlib import ExitStack
import numpy as np
import concourse.bass as bass
import concourse.mybir as mybir
from concourse.bass_utils import run_bass_kernel_spmd

F32 = mybir.dt.float32
BF16 = mybir.dt.bfloat16
I32 = mybir.dt.int32
U32 = mybir.dt.uint32
AF = mybir.ActivationFunctionType
ALU = mybir.AluOpType
AX = mybir.AxisListType

S = 8192
D = 1024
NT = S // 128
MEM = 256
TOKW = 768
NE = 32
CAP = 1536
NROW = NE * CAP
RG = 384
ALPHA = float(4 ** 0.25)
W_IN_A = 1792
W_IN_B = 1476
BIGOOB = 1.0e6


class KB:
    NS = 8
    ND = 32

    def __init__(self, nc, es):
        self.nc = nc
        self.eng = {'pe': nc.tensor, 'act': nc.scalar, 'dve': nc.vector, 'pool': nc.gpsimd, 'sp': nc.sync}
        self.esem = {e: [es.enter_context(nc.semaphore(f"s_{e}{i}")) for i in range(self.NS)]
                     for e in ('pe', 'act', 'dve', 'pool')}
        self.cnt = {e: 0 for e in ('pe', 'act', 'dve', 'pool')}
        self.dsem = [es.enter_context(nc.semaphore(f"s_d{i}")) for i in range(self.ND)]
        self.dtot = [0] * self.ND
        self.dnext = 0
        self.dnextp = 0
        self.pool_depth = 2
        self.wc = {e: {} for e in self.eng}
        self.wd = {e: {} for e in self.eng}
        self.lastw = {}
        self.readers = {}

    def _wait(self, e, tok):
        eng = self.eng[e]
        if tok[0] == 'c':
            _, e2, k = tok
            if e2 == e and e == 'pe':
                return
            if self.wc[e].get(e2, 0) >= k:
                return
            eng.wait_ge(self.esem[e2][(k - 1) % self.NS], (k - 1) // self.NS + 1)
            self.wc[e][e2] = k
        else:
            _, s, tot = tok
            if self.wd[e].get(s, 0) >= tot:
                return
            eng.wait_ge(self.dsem[s], tot)
            self.wd[e][s] = tot

    def _deps(self, e, reads, writes):
        deps = []
        for k in reads:
            t = self.lastw.get(k)
            if t is not None:
                deps.append(t)
        for k in writes:
            t = self.lastw.get(k)
            if t is not None:
                deps.append(t)
            deps.extend(self.readers.get(k, ()))
        for t in deps:
            self._wait(e, t)

    def _commit(self, tok, reads, writes):
        for k in reads:
            lst = self.readers.setdefault(k, [])
            lst[:] = [t for t in lst if not (t[0] == tok[0] and t[1] == tok[1])]
            lst.append(tok)
        for k in writes:
            self.lastw[k] = tok
            self.readers[k] = []

    def op(self, e, fn, reads=(), writes=()):
        self._deps(e, reads, writes)
        ins = fn(self.eng[e])
        self.cnt[e] += 1
        k = self.cnt[e]
        ins.then_inc(self.esem[e][(k - 1) % self.NS], 1)
        self._commit(('c', e, k), reads, writes)

    def dma(self, e, fn, reads=(), writes=()):
        half = self.ND // 2
        if e == 'pool':
            s = half + self.dnextp
            self.dnextp = (self.dnextp + 1) % self.pool_depth
        else:
            s = self.dnext
            self.dnext = (self.dnext + 1) % half
        if self.dtot[s] > 0:
            self._wait(e, ('d', s, self.dtot[s]))
        self._deps(e, reads, writes)
        ins = fn(self.eng[e])
        self.dtot[s] += 16
        ins.then_inc(self.dsem[s], 16)
        self._commit(('d', s, self.dtot[s]), reads, writes)

    def barrier(self):
        for e in self.eng:
            for e2 in self.cnt:
                if self.cnt[e2] > 0:
                    self._wait(e, ('c', e2, self.cnt[e2]))
            for s in range(self.ND):
                if self.dtot[s] > 0:
                    self._wait(e, ('d', s, self.dtot[s]))
        self.lastw.clear()
        self.readers.clear()


def interleave(streams, until=None):
    live = [[g, max(1, n), 0] for g, n in streams if g is not None]
    while live:
        live.sort(key=lambda r: r[2] / r[1])
        r = live[0]
        try:
            next(r[0])
            r[2] += 1
        except StopIteration:
            live.remove(r)
            if until is not None and r[0] is until:
                return


def build_program(mode="full"):
    nc = bass.Bass("TRN2", target_bir_lowering=False)
    es = ExitStack()
    kb = KB(nc, es)

    def din(name, shape, dt=F32):
        return nc.dram_tensor(name, list(shape), dt, kind="ExternalInput").ap()

    def dscr(name, shape, dt=F32):
        return nc.dram_tensor(name, list(shape), dt, kind="Internal").ap()

    x_in = din("x", [S, D])
    mem_in = din("mem", [MEM, D])
    rel_bias = din("rel_bias", [32, 12])
    a_w_in = din("a_w_in", [D, W_IN_A])
    a_conv_w = din("a_conv_w", [4, TOKW])
    a_conv_b = din("a_conv_b", [TOKW])
    a_wr = din("a_wr", [12, 64, 64])
    a_br = din("a_br", [TOKW])
    a_wi = din("a_wi", [12, 64, 64])
    a_bi = din("a_bi", [TOKW])
    a_lambda = din("a_lambda", [TOKW])
    b_w_in = din("b_w_in", [D, W_IN_B])
    b_kv_norm_g = din("b_kv_norm_g", [128])
    b_w_uk = din("b_w_uk", [128, 12, 64])
    b_w_uv = din("b_w_uv", [128, 12, 64])
    b_idx_norm_g = din("b_idx_norm_g", [64])
    b_idx_norm_b = din("b_idx_norm_b", [64])
    w_mem_kv = din("w_mem_kv", [2, D, 512])
    w_out = din("w_out", [2, D, D])
    ln1_g = din("ln1_g", [2, D])
    ln1_b = din("ln1_b", [2, D])
    router_w = din("router_w", [2, D, NE])
    router_b = din("router_b", [2, NE])
    exp_w1 = din("exp_w1", [2, NE, D, 2 * D])
    exp_b1 = din("exp_b1", [2, NE, 2 * D])
    exp_w2 = din("exp_w2", [2, NE, D, D])
    exp_b2 = din("exp_b2", [2, NE, D])
    ln2_g = din("ln2_g", [2, D])
    ln2_b = din("ln2_b", [2, D])
    c_ident = din("c_ident", [128, 128])
    c_tri = din("c_tri", [128, 128])
    c_iota = din("c_iota", [128, NE])
    c_ecap = din("c_ecap", [128, NE])
    c_cmask = din("c_cmask", [128, 128])
    c_caus = din("c_caus", [128, 128])
    c_bkt = din("c_bkt", [128, 2, 128])
    c_pow2 = din("c_pow2", [128, 24])

    out = nc.dram_tensor("out", [S, D], F32, kind="ExternalOutput").ap()
    x1buf = dscr("x1buf", [S, D])
    x2buf = dscr("x2buf", [S, D])
    xg = dscr("xg", [NROW, D], BF16)
    yg = dscr("yg", [NROW, D])
    qlat = dscr("qlat", [12, 128, S], BF16)
    iqd = dscr("iqd", [2, 128, S], BF16)
    memod = dscr("memod", [2, 128, S], BF16)

    def sb(name, shape, dt=F32, stack=es):
        return stack.enter_context(nc.sbuf_tensor(name, list(shape), dt))

    ps = [es.enter_context(nc.psum_tensor(f"ps{i}", [128, 512], F32)) for i in range(8)]
    psn = [0]
    psrot = [6]

    def psum():
        i = psn[0] % psrot[0]
        psn[0] = (i + 1) % psrot[0]
        return ps[i], ('ps', i)

    bc_reg = nc.gpsimd.alloc_register("bc_reg")
    nc.gpsimd.reg_mov(bc_reg, NROW - 1)
    ident_f = sb("ident_f", [128, 128])
    ident_b = sb("ident_b", [128, 128], BF16)
    tri_f = sb("tri_f", [128, 128])
    ones_f = sb("ones_f", [128, 128])
    ones_b = sb("ones_b", [128, 128], BF16)
    iota_e = sb("iota_e", [128, NE])
    ecap = sb("ecap", [128, NE])
    destall = sb("destall", [128, NT, 4], I32)
    gall = sb("gall", [128, NT, 4])

    kb.dma('sp', lambda q: q.dma_start(out=ident_f[:], in_=c_ident[:, :]), writes=['ident_f'])
    kb.dma('sp', lambda q: q.dma_start(out=tri_f[:], in_=c_tri[:, :]), writes=['tri_f'])
    kb.dma('sp', lambda q: q.dma_start(out=iota_e[:], in_=c_iota[:, :]), writes=['iota_e'])
    kb.dma('sp', lambda q: q.dma_start(out=ecap[:], in_=c_ecap[:, :]), writes=['ecap'])
    kb.op('dve', lambda v: v.tensor_copy(out=ident_b[:], in_=ident_f[:]), reads=['ident_f'], writes=['ident_b'])
    kb.op('dve', lambda v: v.memset(ones_f[:], 1.0), writes=['ones_f'])
    kb.op('dve', lambda v: v.memset(ones_b[:], 1.0), writes=['ones_b'])

    def load_cast(dst_ap, src_ap, key):
        kb.dma('pool', lambda q: q.dma_start(out=dst_ap, in_=src_ap), writes=[key])

    def bcast_rows(dst, src_row_ap, key, n=128):
        kb.dma('sp', lambda q: q.dma_start(out=dst, in_=src_row_ap.partition_broadcast(n)), writes=[key])

    def setup_mem_kv(layer, st, kT, vpad, onespad):
        memf = sb(f"memf{layer}", [128, 2, D], F32, st)
        memb = sb(f"memb{layer}", [128, 2, D], BF16, st)
        memT = sb(f"memT{layer}", [128, 8, MEM], BF16, st)
        wkv = sb(f"wkv{layer}", [128, 8, 512], BF16, st)
        kb.dma('sp', lambda q: q.dma_start(out=memf[:], in_=mem_in.rearrange("(t p) d -> p t d", p=128)), writes=['memf'])
        load_cast(wkv[:], w_mem_kv[layer].rearrange("(k p) c -> p k c", p=128), 'wkv')
        kb.op('act', lambda a: a.copy(out=memb[:], in_=memf[:]), reads=['memf'], writes=['memb'])
        for k in range(8):
            pt, pk = psum()
            ptb = pt[:].bitcast(BF16)
            for t in range(2):
                kb.op('pe', lambda p, t=t, k=k, ptb=ptb: p.transpose(out=ptb[:, t * 128:(t + 1) * 128],
                      in_=memb[:, t, k * 128:(k + 1) * 128], identity=ident_b[:]),
                      reads=['memb', 'ident_b'], writes=[pk])
            kb.op('dve', lambda v, k=k, ptb=ptb: v.tensor_copy(out=memT[:, k, :], in_=ptb[:, 0:256]),
                  reads=[pk], writes=['memT'])
        for pr in range(2):
            pt, pk = psum()
            for k in range(8):
                kb.op('pe', lambda p, k=k, pr=pr, pt=pt: p.matmul(pt[:, 0:256], lhsT=wkv[:, k, pr * 128:(pr + 1) * 128],
                      rhs=memT[:, k, :], start=(k == 0), stop=(k == 7)), reads=['wkv', 'memT'], writes=[pk])
            kb.op('dve', lambda v, pr=pr, pt=pt: v.tensor_copy(out=kT[:, pr, :], in_=pt[:, 0:256]), reads=[pk], writes=['kT'])
        kb.op('pool', lambda g: g.memset(vpad[:], 0.0), writes=['vpad'])
        kb.op('pool', lambda g: g.memset(onespad[:], 0.0), writes=['onespad'])
        for par in range(2):
            kb.op('pool', lambda g, par=par: g.memset(onespad[:, par, par * 64:(par + 1) * 64], 1.0), writes=['onespad'])
        for mc in range(2):
            pt, pk = psum()
            for k in range(8):
                kb.op('pe', lambda p, k=k, mc=mc, pt=pt: p.matmul(pt[:, 0:256], lhsT=memT[:, k, mc * 128:(mc + 1) * 128],
                      rhs=wkv[:, k, 256:512], start=(k == 0), stop=(k == 7)), reads=['wkv', 'memT'], writes=[pk])
            for h in range(4):
                par = h % 2
                kb.op('dve', lambda v, h=h, mc=mc, par=par, pt=pt: v.tensor_copy(
                    out=vpad[:, h, mc, par * 64:(par + 1) * 64], in_=pt[:, h * 64:(h + 1) * 64]),
                    reads=[pk], writes=['vpad'])

    def mem_attn(mqT, T, kT, vpad, onespad, mixT_dst, E, rden, tag):
        for pr in range(2):
            for hh in range(2):
                h = 2 * pr + hh
                for mc in range(2):
                    pt, pk = psum()
                    kb.op('pe', lambda p, pt=pt, pr=pr, hh=hh, mc=mc: p.matmul(
                        pt[:, 0:T], lhsT=kT[hh * 64:(hh + 1) * 64, pr, mc * 128:(mc + 1) * 128],
                        rhs=mqT[hh * 64:(hh + 1) * 64, pr, :], start=True, stop=True),
                        reads=['kT', 'mqT' + tag], writes=[pk])
                    kb.op('act', lambda a, pt=pt, hh=hh, mc=mc: a.activation(
                        out=E[:, hh, mc, :], in_=pt[:, 0:T], func=AF.Exp, scale=0.125),
                        reads=[pk], writes=[('E' + tag, hh, mc)])
            po, pok = psum()
            pd, pdk = psum()
            n = 0
            for hh in range(2):
                h = 2 * pr + hh
                for mc in range(2):
                    kb.op('pe', lambda p, po=po, h=h, hh=hh, mc=mc, n=n: p.matmul(
                        po[:, 0:T], lhsT=vpad[:, h, mc, :], rhs=E[:, hh, mc, :], start=(n == 0), stop=(n == 3)),
                        reads=['vpad', ('E' + tag, hh, mc)], writes=[pok])
                    n += 1
            n = 0
            for hh in range(2):
                for mc in range(2):
                    kb.op('pe', lambda p, pd=pd, hh=hh, mc=mc, n=n: p.matmul(
                        pd[:, 0:T], lhsT=onespad[:, hh, :], rhs=E[:, hh, mc, :], start=(n == 0), stop=(n == 3)),
                        reads=['onespad', ('E' + tag, hh, mc)], writes=[pdk])
                    n += 1
            kb.op('dve', lambda v, pd=pd: v.reciprocal(out=rden[:, 0:T], in_=pd[:, 0:T]), reads=[pdk], writes=['rden' + tag])
            kb.op('dve', lambda v, po=po, pr=pr: v.tensor_tensor(out=mixT_dst(pr), in0=po[:, 0:T], in1=rden[:, 0:T], op=ALU.mult),
                  reads=[pok, 'rden' + tag], writes=[('mixT' + tag, 6 + pr)])

    class Tail:
        def __init__(self, layer, st, nbuf=2):
            self.layer = layer
            self.nbuf = nbuf
            L = layer
            self.wout = sb(f"wout{L}", [128, 8, D], BF16, st)
            load_cast(self.wout[:], w_out[L].rearrange("(k p) n -> p k n", p=128), 'wout')
            self.lng = sb(f"lng{L}", [128, D], F32, st)
            self.lnb = sb(f"lnb{L}", [128, D], F32, st)
            bcast_rows(self.lng[:], ln1_g[L], 'lng')
            bcast_rows(self.lnb[:], ln1_b[L], 'lnb')
            self.rw = sb(f"rw{L}", [128, 8, NE], F32, st)
            kb.dma('sp', lambda q: q.dma_start(out=self.rw[:], in_=router_w[L].rearrange("(k p) e -> p k e", p=128)), writes=['rw'])
            self.rb = sb(f"rb{L}", [128, NE], F32, st)
            bcast_rows(self.rb[:], router_b[L], 'rb')
            self.rrun = sb(f"rrun{L}", [128, NE], F32, st)
            kb.op('dve', lambda v: v.memset(self.rrun[:], 0.0), writes=['rrun'])
            self.z = [sb(f"z{L}_{i}", [128, D], F32, st) for i in range(nbuf)]
            self.x1f = [sb(f"x1f{L}_{i}", [128, D], F32, st) for i in range(nbuf)]
            self.x1b = [sb(f"x1b{L}_{i}", [128, D], BF16, st) for i in range(nbuf)]
            self.x1T = sb(f"x1T{L}", [128, 8, 128], F32, st)
            self.sm = [sb(f"sm{L}_{i}", [128, 256], F32, st) for i in range(nbuf)]
            self.smu = [sb(f"smu{L}_{i}", [128, 8], U32, st) for i in range(nbuf)]
            self.n = 0

        def run(self, *a):
            for _ in self.run_gen(*a):
                pass

        def run_gen(self, ti, mixT_ap, mix_keys, xres_ap, xres_key, x1dst):
            i = self.n % self.nbuf
            self.n += 1
            z, x1f, x1b, sm, smu = self.z[i], self.x1f[i], self.x1b[i], self.sm[i], self.smu[i]
            zk, x1fk, x1bk, smk = ('z', i), ('x1f', i), ('x1b', i), ('sm', i)
            for nh in range(2):
                pt, pk = psum()
                for k in range(8):
                    kb.op('pe', lambda p, pt=pt, k=k, nh=nh: p.matmul(pt[:, :], lhsT=mixT_ap(k), rhs=self.wout[:, k, nh * 512:(nh + 1) * 512],
                          start=(k == 0), stop=(k == 7)), reads=['wout'] + list(mix_keys), writes=[pk])
                kb.op('dve', lambda v, pt=pt, nh=nh: v.scalar_tensor_tensor(
                    out=z[:, nh * 512:(nh + 1) * 512], in0=xres_ap[:, nh * 512:(nh + 1) * 512], scalar=ALPHA,
                    in1=pt[:, :], op0=ALU.mult, op1=ALU.add), reads=[pk, xres_key], writes=[zk])
                yield
            self.layernorm(z, zk, x1f, x1fk, sm, smk, self.lng, self.lnb)
            kb.dma('sp', lambda q: q.dma_start(out=x1dst, in_=x1f[:]), reads=[x1fk], writes=[('x1d', ti)])
            yield
            kb.op('act', lambda a: a.copy(out=x1b[:], in_=x1f[:]), reads=[x1fk], writes=[x1bk])
            yield
            for half in range(2):
                pt, pk = psum()
                for kk in range(4):
                    k = half * 4 + kk
                    kb.op('pe', lambda p, pt=pt, k=k, kk=kk: p.transpose(out=pt[:, kk * 128:(kk + 1) * 128],
                          in_=x1f[:, k * 128:(k + 1) * 128], identity=ident_f[:]), reads=[x1fk, 'ident_f'], writes=[pk])
                kb.op('act', lambda a, pt=pt, half=half: a.copy(
                    out=self.x1T[:, half * 4:(half + 1) * 4, :].rearrange("p k t -> p (k t)"), in_=pt[:, :]),
                    reads=[pk], writes=['x1T'])
            yield
            pl, plk = psum()
            for k in range(8):
                kb.op('pe', lambda p, k=k: p.matmul(pl[:, 0:NE], lhsT=self.x1T[:, k, :], rhs=self.rw[:, k, :],
                      start=(k == 0), stop=(k == 7)), reads=['x1T', 'rw'], writes=[plk])
            lg = sm[:, 0:32]
            v8 = sm[:, 32:40]
            mask = sm[:, 40:72]
            slot = sm[:, 72:104]
            junk = sm[:, 104:136]
            idxf = sm[:, 136:144]
            destf = sm[:, 144:148]
            ev = sm[:, 148:152]
            nm = sm[:, 152:153]
            gsum = sm[:, 153:154]
            bad = sm[:, 160:192]
            kb.op('dve', lambda v: v.tensor_tensor(out=lg, in0=pl[:, 0:NE], in1=self.rb[:], op=ALU.add),
                  reads=[plk, 'rb'], writes=[smk])
            kb.op('dve', lambda v: v.max(out=v8, in_=lg), reads=[smk], writes=[smk])
            kb.op('dve', lambda v: v.max_index(out=smu[:], in_max=v8, in_values=lg), reads=[smk], writes=[('smu', i)])
            kb.op('dve', lambda v: v.tensor_scalar(out=mask, in0=lg, scalar1=v8[:, 3:4], scalar2=None, op0=ALU.is_ge),
                  reads=[smk], writes=[smk])
            yield
            pc, pck = psum()
            kb.op('pe', lambda p: p.matmul(pc[:, 0:NE], lhsT=tri_f[:], rhs=mask, start=True, stop=True),
                  reads=['tri_f', smk], writes=[pck])
            kb.op('pe', lambda p: p.matmul(pc[:, NE:2 * NE], lhsT=ones_f[:], rhs=mask, start=True, stop=True),
                  reads=['ones_f', smk], writes=[pck])
            kb.op('dve', lambda v: v.tensor_tensor(out=slot, in0=pc[:, 0:NE], in1=self.rrun[:], op=ALU.add),
                  reads=[pck, 'rrun'], writes=[smk])
            kb.op('dve', lambda v: v.tensor_tensor(out=self.rrun[:], in0=pc[:, NE:2 * NE], in1=self.rrun[:], op=ALU.add),
                  reads=[pck, 'rrun'], writes=['rrun'])
            kb.op('dve', lambda v: v.tensor_scalar(out=bad, in0=slot, scalar1=float(CAP), scalar2=BIGOOB, op0=ALU.is_ge, op1=ALU.mult),
                  reads=[smk], writes=[smk])
            kb.op('dve', lambda v: v.tensor_tensor(out=slot, in0=slot, in1=bad, op=ALU.add), reads=[smk], writes=[smk])
            kb.op('dve', lambda v: v.tensor_tensor(out=slot, in0=slot, in1=ecap[:], op=ALU.add), reads=[smk, 'ecap'], writes=[smk])
            yield
            kb.op('dve', lambda v: v.tensor_copy(out=idxf, in_=smu[:]), reads=[('smu', i)], writes=[smk])
            for k in range(4):
                kb.op('dve', lambda v, k=k: v.scalar_tensor_tensor(out=junk, in0=iota_e[:], scalar=idxf[:, k:k + 1], in1=slot,
                      op0=ALU.is_equal, op1=ALU.mult, accum_out=destf[:, k:k + 1]), reads=[smk, 'iota_e'], writes=[smk])
            kb.op('dve', lambda v: v.tensor_copy(out=destall[:, ti, :], in_=destf), reads=[smk], writes=[('dest', ti)])
            kb.op('dve', lambda v: v.tensor_scalar(out=nm, in0=v8[:, 0:1], scalar1=-1.0, scalar2=None, op0=ALU.mult),
                  reads=[smk], writes=[smk])
            kb.op('act', lambda a: a.activation(out=ev, in_=v8[:, 0:4], func=AF.Exp, bias=nm, scale=1.0, accum_out=gsum),
                  reads=[smk], writes=[smk])
            yield
            kb.op('dve', lambda v: v.reciprocal(out=gsum, in_=gsum), reads=[smk], writes=[smk])
            kb.op('dve', lambda v: v.tensor_scalar(out=gall[:, ti, :], in0=ev, scalar1=gsum, scalar2=None, op0=ALU.mult),
                  reads=[smk], writes=[('gate', ti)])
            for k in range(4):
                kb.dma('pool', lambda q, k=k: q.indirect_dma_start(
                    out=xg[:, :], out_offset=bass.IndirectOffsetOnAxis(ap=destall[:, ti, k:k + 1], axis=0),
                    in_=x1b[:, :], in_offset=None, bounds_check=bc_reg, oob_is_err=False),
                    reads=[x1bk, ('dest', ti)], writes=['xg'])

        def layernorm(self, z, zk, o, ok, sm, smk, g, b, gk='lng', bk='lnb'):
            st6 = sm[:, 200:212]
            mv = sm[:, 212:214]
            rstd = sm[:, 214:215]
            for c in range(2):
                kb.op('dve', lambda v, c=c: v.bn_stats(out=st6[:, c * 6:(c + 1) * 6], in_=z[:, c * 512:(c + 1) * 512]),
                      reads=[zk], writes=[smk])
            kb.op('dve', lambda v: v.bn_aggr(out=mv, in_=st6), reads=[smk], writes=[smk])
            kb.op('act', lambda a: a.activation(out=rstd, in_=mv[:, 1:2], func=AF.Sqrt, bias=1e-5, scale=1.0), reads=[smk], writes=[smk])
            kb.op('dve', lambda v: v.reciprocal(out=rstd, in_=rstd), reads=[smk], writes=[smk])
            kb.op('dve', lambda v: v.tensor_scalar(out=o[:], in0=z[:], scalar1=mv[:, 0:1], scalar2=rstd, op0=ALU.subtract, op1=ALU.mult),
                  reads=[zk, smk], writes=[ok])
            kb.op('pool', lambda p: p.tensor_tensor(out=o[:], in0=o[:], in1=g[:], op=ALU.mult), reads=[ok, gk], writes=[ok])
            kb.op('pool', lambda p: p.tensor_tensor(out=o[:], in0=o[:], in1=b[:], op=ALU.add), reads=[ok, bk], writes=[ok])

    def phase_a0(ntiles=NT):
        T = 256
        st = ExitStack()
        tail = Tail(0, st)
        kT = sb("kT0", [128, 2, MEM], BF16, st)
        vpad = sb("vpad0", [128, 4, 2, 128], BF16, st)
        onespad = sb("onespad0", [128, 2, 128], BF16, st)
        setup_mem_kv(0, st, kT, vpad, onespad)
        win = sb("win0", [128, 8, W_IN_A], BF16, st)
        load_cast(win[:], a_w_in.rearrange("(k p) c -> p k c", p=128), 'win')
        wr_bd = sb("wr_bd", [128, 6, 128], BF16, st)
        wi_bd = sb("wi_bd", [128, 6, 128], BF16, st)
        kb.op('pool', lambda g: g.memset(wr_bd[:], 0.0), writes=['wr_bd'])
        kb.op('pool', lambda g: g.memset(wi_bd[:], 0.0), writes=['wi_bd'])
        for n in range(12):
            j, par = n // 2, n % 2
            load_cast(wr_bd[par * 64:(par + 1) * 64, j, par * 64:(par + 1) * 64], a_wr[n], 'wr_bd')
            load_cast(wi_bd[par * 64:(par + 1) * 64, j, par * 64:(par + 1) * 64], a_wi[n], 'wi_bd')
        cw = sb("cw", [128, 4, 6], F32, st)
        vecs = sb("vecs", [128, 4, 6], F32, st)
        with nc.allow_non_contiguous_dma(reason="tiny per-channel vectors"):
            kb.dma('sp', lambda q: q.dma_start(out=cw[:], in_=a_conv_w.rearrange("w (j p) -> p w j", p=128)), writes=['cw'])
            for n, v_ in enumerate((a_conv_b, a_br, a_bi, a_lambda)):
                kb.dma('sp', lambda q, n=n, v_=v_: q.dma_start(out=vecs[:, n, :], in_=v_.rearrange("(j p) -> p j", p=128)), writes=['vecs'])
        coef = sb("coef", [128, 6], F32, st)
        kb.op('act', lambda a: a.activation(out=coef[:], in_=vecs[:, 3, :], func=AF.Exp, scale=-1.0), reads=['vecs'], writes=['coef'])
        kb.op('act', lambda a: a.activation(out=coef[:], in_=coef[:], func=AF.Ln, bias=1.0, scale=1.0), reads=['coef'], writes=['coef'])
        kb.op('dve', lambda v: v.tensor_scalar(out=coef[:], in0=coef[:], scalar1=-8.0, scalar2=None, op0=ALU.mult), reads=['coef'], writes=['coef'])

        xf = [sb(f"xf{i}", [128, 2, D], F32, st) for i in range(2)]
        xbf = sb("xbf", [128, 2, D], BF16, st)
        xT = sb("xT", [128, 8, T], BF16, st)
        xbh = sb("xbh", [128, 6, 3 + T], F32, st)
        gbT = sb("gbT", [128, 6, T], F32, st)
        mqT = sb("mqT", [128, 2, T], BF16, st)
        mixT = [sb(f"mixT{i}", [128, 8, T], BF16, st) for i in range(2)]
        E = sb("E0", [128, 2, 2, T], BF16, st)
        rden = sb("rden0", [128, T], F32, st)
        NTMP = 10
        tmp = [[sb(f"tmp{n}_{i}", [128, T], F32, st) for i in range(2)] for n in range(NTMP)]
        xcb = [sb(f"xcb{i}", [128, T], BF16, st) for i in range(2)]
        hbuf = [sb(f"hbuf{i}", [128, 6, T], F32, st) for i in range(2)]
        kb.op('dve', lambda v: v.memset(xbh[:], 0.0), writes=['xbh'])
        zt = sb("zt", [128, 4096], BF16, st)
        kb.op('pool', lambda g: g.memset(zt[:], 0.0), writes=['zt'])
        for r0 in range(0, NROW, 512):
            kb.dma('sp', lambda q, r0=r0: q.dma_start(out=xg[r0:r0 + 512, :].rearrange("(p t) d -> p (t d)", t=4), in_=zt[:]),
                   reads=['zt'], writes=['xg'])

        nch = ntiles * 128 // T

        def mixer(ci):
            xi = ci % 2
            xfc, xfk = xf[xi], ('xf', xi)
            kb.dma('sp', lambda q, ci=ci, xfc=xfc: q.dma_start(
                out=xfc[:], in_=x_in[ci * T:(ci + 1) * T, :].rearrange("(t p) d -> p t d", p=128)), writes=[xfk])
            kb.op('act', lambda a, xfc=xfc: a.copy(out=xbf[:], in_=xfc[:]), reads=[xfk], writes=['xbf'])
            for k in range(8):
                pt, pk = psum()
                ptb = pt[:].bitcast(BF16)
                for t in range(2):
                    kb.op('pe', lambda p, ptb=ptb, t=t, k=k: p.transpose(out=ptb[:, t * 128:(t + 1) * 128],
                          in_=xbf[:, t, k * 128:(k + 1) * 128], identity=ident_b[:]), reads=['xbf', 'ident_b'], writes=[pk])
                kb.op('dve' if k % 2 == 0 else 'act',
                      (lambda v, ptb=ptb, k=k: v.tensor_copy(out=xT[:, k, :], in_=ptb[:, 0:T])) if k % 2 == 0 else
                      (lambda a, ptb=ptb, k=k: a.copy(out=xT[:, k, :], in_=ptb[:, 0:T])),
                      reads=[pk], writes=[('xT', k)])
            yield
            xTk = [('xT', k) for k in range(8)]
            for c in range(14):
                pt, pk = psum()
                for k in range(8):
                    kb.op('pe', lambda p, pt=pt, k=k, c=c: p.matmul(pt[:, 0:T], lhsT=win[:, k, c * 128:(c + 1) * 128],
                          rhs=xT[:, k, :], start=(k == 0), stop=(k == 7)), reads=['win'] + xTk, writes=[pk])
                if c < 6:
                    kb.op('act', lambda a, pt=pt, c=c: a.copy(out=xbh[:, c, 3:3 + T], in_=pt[:, 0:T]), reads=[pk], writes=[('xbh', c)])
                elif c < 12:
                    kb.op('act', lambda a, pt=pt, c=c: a.copy(out=gbT[:, c - 6, :], in_=pt[:, 0:T]), reads=[pk], writes=[('gbT', c - 6)])
                else:
                    kb.op('dve', lambda v, pt=pt, c=c: v.tensor_copy(out=mqT[:, c - 12, :], in_=pt[:, 0:T]), reads=[pk], writes=['mqT0'])
                if c % 2 == 1:
                    yield
            mx = mixT[ci % 2]
            hb = hbuf[ci % 2]
            hprev = hbuf[(ci + 1) % 2]
            for c in range(6):
                r_ = c % 2
                xc, rr, ii, aa, ss, uu, sq, t2, sg, gl = [tmp[n][r_] for n in range(NTMP)]
                tk = [(f'tmp{n}', r_) for n in range(NTMP)]
                xck = tk[0]
                kb.op('dve', lambda v, c=c, xc=xc: v.tensor_scalar(out=xc[:], in0=xbh[:, c, 3:3 + T], scalar1=cw[:, 3, c:c + 1],
                      scalar2=vecs[:, 0, c:c + 1], op0=ALU.mult, op1=ALU.add), reads=[('xbh', c), 'cw', 'vecs'], writes=[xck])
                for j in range(3):
                    kb.op('dve', lambda v, c=c, j=j, xc=xc: v.scalar_tensor_tensor(out=xc[:], in0=xbh[:, c, j:j + T], scalar=cw[:, j, c:c + 1],
                          in1=xc[:], op0=ALU.mult, op1=ALU.add), reads=[('xbh', c), 'cw', xck], writes=[xck])
                kb.op('pool', lambda g, c=c: g.tensor_copy(out=xbh[:, c, 0:3], in_=xbh[:, c, T:T + 3]), reads=[('xbh', c)], writes=[('xbh', c)])
                kb.op('pool', lambda g, xc=xc, r_=r_: g.tensor_copy(out=xcb[r_][:], in_=xc[:]), reads=[xck], writes=[('xcb', r_)])
                pr_, prk = psum()
                kb.op('pe', lambda p, pr_=pr_, c=c, r_=r_: p.matmul(pr_[:, 0:T], lhsT=wr_bd[:, c, :], rhs=xcb[r_][:], start=True, stop=True),
                      reads=['wr_bd', ('xcb', r_)], writes=[prk])
                pi_, pik = psum()
                kb.op('pe', lambda p, pi_=pi_, c=c, r_=r_: p.matmul(pi_[:, 0:T], lhsT=wi_bd[:, c, :], rhs=xcb[r_][:], start=True, stop=True),
                      reads=['wi_bd', ('xcb', r_)], writes=[pik])
                kb.op('act', lambda a, pr_=pr_, c=c, rr=rr: a.activation(out=rr[:], in_=pr_[:, 0:T], func=AF.Sigmoid, bias=vecs[:, 1, c:c + 1], scale=1.0),
                      reads=[prk, 'vecs'], writes=[tk[1]])
                kb.op('act', lambda a, pi_=pi_, c=c, ii=ii: a.activation(out=ii[:], in_=pi_[:, 0:T], func=AF.Sigmoid, bias=vecs[:, 2, c:c + 1], scale=1.0),
                      reads=[pik, 'vecs'], writes=[tk[2]])
                yield
                gb = gbT[:, c, :]
                kb.op('pool', lambda g, gb=gb, sq=sq: g.tensor_tensor(out=sq[:], in0=gb, in1=gb, op=ALU.mult), reads=[('gbT', c)], writes=[tk[6]])
                kb.op('pool', lambda g, sq=sq: g.tensor_scalar(out=sq[:], in0=sq[:], scalar1=0.044715, scalar2=1.0, op0=ALU.mult, op1=ALU.add),
                      reads=[tk[6]], writes=[tk[6]])
                kb.op('pool', lambda g, gb=gb, sq=sq, t2=t2: g.tensor_tensor(out=t2[:], in0=sq[:], in1=gb, op=ALU.mult), reads=[tk[6], ('gbT', c)], writes=[tk[7]])
                kb.op('act', lambda a, t2=t2, sg=sg: a.activation(out=sg[:], in_=t2[:], func=AF.Sigmoid, scale=1.5957691216057308),
                      reads=[tk[7]], writes=[tk[8]])
                kb.op('act', lambda a, aa=aa, rr=rr, c=c: a.activation(out=aa[:], in_=rr[:], func=AF.Exp, scale=coef[:, c:c + 1]),
                      reads=[tk[1], 'coef'], writes=[tk[3]])
                kb.op('pool', lambda g, aa=aa, ss=ss: g.tensor_tensor(out=ss[:], in0=aa[:], in1=aa[:], op=ALU.mult), reads=[tk[3]], writes=[tk[4]])
                kb.op('act', lambda a, ss=ss: a.activation(out=ss[:], in_=ss[:], func=AF.Sqrt, bias=1.0, scale=-1.0), reads=[tk[4]], writes=[tk[4]])
                kb.op('pool', lambda g, uu=uu, ss=ss, ii=ii: g.tensor_tensor(out=uu[:], in0=ss[:], in1=ii[:], op=ALU.mult), reads=[tk[4], tk[2]], writes=[tk[5]])
                kb.op('dve', lambda v, uu=uu, xc=xc: v.tensor_tensor(out=uu[:], in0=uu[:], in1=xc[:], op=ALU.mult), reads=[tk[5], xck], writes=[tk[5]])
                init = 0.0 if ci == 0 else hprev[:, c, T - 1:T]
                kb.op('dve', lambda v, aa=aa, uu=uu, c=c, init=init, hb=hb: v.tensor_tensor_scan(out=hb[:, c, :], data0=aa[:], data1=uu[:],
                      initial=init, op0=ALU.mult, op1=ALU.add), reads=[tk[3], tk[5], ('h', (ci + 1) % 2, c)], writes=[('h', ci % 2, c)])
                kb.op('pool', lambda g, gl=gl, sg=sg, gb=gb: g.tensor_tensor(out=gl[:], in0=sg[:], in1=gb, op=ALU.mult), reads=[tk[8], ('gbT', c)], writes=[tk[9]])
                kb.op('dve', lambda v, gl=gl, c=c, hb=hb, mx=mx: v.tensor_tensor(out=mx[:, c, :], in0=hb[:, c, :], in1=gl[:], op=ALU.mult),
                      reads=[tk[9], ('h', ci % 2, c)], writes=[('mixT0', c)])
                yield
            mem_attn(mqT, T, kT, vpad, onespad, lambda pr, mx=mx: mx[:, 6 + pr, :], E, rden, '0')
            yield

        mixkeys = [('mixT0', c) for c in range(8)]

        def tails(ci):
            mx = mixT[ci % 2]
            xfc, xfk = xf[ci % 2], ('xf', ci % 2)
            for t in range(T // 128):
                ti = ci * (T // 128) + t
                yield from tail.run_gen(ti, lambda k, mx=mx, t=t: mx[:, k, t * 128:(t + 1) * 128], mixkeys, xfc[:, t, :], xfk,
                                        x1buf[ti * 128:(ti + 1) * 128, :])

        for _ in mixer(0):
            pass
        for ci in range(nch):
            streams = [(tails(ci), 18)]
            if ci + 1 < nch:
                streams.append((mixer(ci + 1), 25))
            interleave(streams)
        kb.barrier()
        if mode.startswith("a0"):
            dbg_r = nc.dram_tensor("dbg_r", [128, NE], F32, kind="ExternalOutput").ap()
            dbg_d = nc.dram_tensor("dbg_d", [128, NT * 4], I32, kind="ExternalOutput").ap()
            kb.dma('sp', lambda q: q.dma_start(out=dbg_r[:, :], in_=tail.rrun[:]))
            kb.dma('sp', lambda q: q.dma_start(out=dbg_d[:, :], in_=destall[:].rearrange("p t k -> p (t k)")))
            kb.barrier()
        st.close()


    def phase_a1(src, ntiles=NT):
        T = 256
        KSEL = 256
        NBIS = 14
        CH = 512
        st = ExitStack()
        tail = Tail(1, st, nbuf=1)
        cTok = sb("cTok", [128, NT, 128], BF16, st)
        cT = sb("cT", [128, S], BF16, st)
        ikT2 = sb("ikT2", [128, S], BF16, st)
        absw = sb("absw", [128, NT, 4], F32, st)
        sgnw = sb("sgnw", [128, NT, 4], F32, st)
        BT = sb("BT", [128, 2, 12, 128], BF16, st)
        wuvpad = sb("wuvpad", [128, 12, 128], BF16, st)
        i4big = sb("i4big", [128, 4, 128], BF16, st)
        for r in range(4):
            kb.op('dve', lambda v, r=r: v.tensor_scalar(out=i4big[:, r, :], in0=ident_f[:], scalar1=100.0, scalar2=None, op0=ALU.mult),
                  reads=['ident_f'], writes=['i4big'])
        kb.op('pool', lambda g: g.memset(wuvpad[:], 0.0), writes=['wuvpad'])
        for h in range(12):
            par = h % 2
            load_cast(wuvpad[:, h, par * 64:(par + 1) * 64], b_w_uv[:, h, :], 'wuvpad')
        s1 = ExitStack()
        kT = sb("kT1", [128, 2, MEM], BF16, s1)
        vpad = sb("vpad1", [128, 4, 2, 128], BF16, s1)
        onespad = sb("onespad1", [128, 2, 128], BF16, s1)
        win = sb("win1", [128, 8, W_IN_B], BF16, s1)
        load_cast(win[:], b_w_in.rearrange("(k p) c -> p k c", p=128), 'win')
        wukT = sb("wukT", [128, 6, 128], BF16, s1)
        gkv = sb("gkv", [128, 128], F32, s1)
        gik = sb("gik", [128, 64], F32, s1)
        bik = sb("bik", [128, 64], F32, s1)
        bcast_rows(gkv[:], b_kv_norm_g, 'gkv')
        bcast_rows(gik[:], b_idx_norm_g, 'gik')
        bcast_rows(bik[:], b_idx_norm_b, 'bik')
        ssetup = ExitStack()
        setup_mem_kv(1, ssetup, kT, vpad, onespad)
        wuk = sb("wuk", [128, 768], BF16, ssetup)
        load_cast(wuk[:], b_w_uk.rearrange("r h d -> r (h d)"), 'wuk')
        for j in range(6):
            pt, pk = psum()
            ptb = pt[:].bitcast(BF16)
            kb.op('pe', lambda p, ptb=ptb, j=j: p.transpose(out=ptb[:, 0:128], in_=wuk[:, j * 128:(j + 1) * 128], identity=ident_b[:]),
                  reads=['wuk', 'ident_b'], writes=[pk])
            kb.op('dve', lambda v, ptb=ptb, j=j: v.tensor_copy(out=wukT[:, j, :], in_=ptb[:, 0:128]), reads=[pk], writes=['wukT'])
        rbb = sb("rbb", [128, 32, 12], F32, ssetup)
        bkt = sb("bkt", [128, 2, 128], F32, ssetup)
        caus = sb("caus", [128, 128], F32, ssetup)
        acc = sb("bacc", [128, 12, 128], F32, ssetup)
        prod = sb("bprod", [128, 12, 128], F32, ssetup)
        oh = sb("boh", [128, 128], F32, ssetup)
        kb.dma('sp', lambda q: q.dma_start(out=rbb[:].rearrange("p b h -> p (b h)"), in_=rel_bias.rearrange("b h -> (b h)").partition_broadcast(128)), writes=['rbb'])
        kb.dma('sp', lambda q: q.dma_start(out=bkt[:], in_=c_bkt[:, :, :]), writes=['bkt'])
        kb.dma('sp', lambda q: q.dma_start(out=caus[:], in_=c_caus[:, :]), writes=['caus'])
        for dt in range(2):
            kb.op('dve', lambda v: v.memset(acc[:], 0.0), writes=['bacc'])
            for b in range(32):
                kb.op('dve', lambda v, b=b, dt=dt: v.tensor_scalar(out=oh[:], in0=bkt[:, dt, :], scalar1=float(b), scalar2=None, op0=ALU.is_equal),
                      reads=['bkt'], writes=['boh'])
                kb.op('dve', lambda v, b=b: v.tensor_tensor(out=prod[:], in0=oh[:].unsqueeze(1).to_broadcast([128, 12, 128]),
                      in1=rbb[:, b, :].unsqueeze(2).to_broadcast([128, 12, 128]), op=ALU.mult), reads=['boh', 'rbb'], writes=['bprod'])
                kb.op('dve', lambda v: v.tensor_tensor(out=acc[:], in0=acc[:], in1=prod[:], op=ALU.add), reads=['bacc', 'bprod'], writes=['bacc'])
            kb.op('dve', lambda v: v.tensor_tensor(out=acc[:], in0=acc[:], in1=rbb[:, 31, :].unsqueeze(2).to_broadcast([128, 12, 128]), op=ALU.subtract),
                  reads=['bacc', 'rbb'], writes=['bacc'])
            if dt == 0:
                kb.op('dve', lambda v: v.tensor_tensor(out=acc[:], in0=acc[:], in1=caus[:].unsqueeze(1).to_broadcast([128, 12, 128]), op=ALU.add),
                      reads=['bacc', 'caus'], writes=['bacc'])
            kb.op('dve', lambda v, dt=dt: v.tensor_copy(out=BT[:, dt, :, :], in_=acc[:]), reads=['bacc'], writes=['BT'])
        kb.barrier()
        ssetup.close()

        xf = [sb(f"xf1_{i}", [128, 2, D], F32, s1) for i in range(2)]
        xbf = sb("xbf1", [128, 2, D], BF16, s1)
        xT = sb("xT1", [128, 8, T], BF16, s1)
        qT = sb("qT1", [128, 6, T], BF16, s1)
        qlb = [sb(f"qlb{i}", [128, 12, T], BF16, s1) for i in range(2)]
        iqT = [sb(f"iqT{i}", [128, 2, T], BF16, s1) for i in range(2)]
        mqT = sb("mqT1", [128, 2, T], BF16, s1)
        memo = [sb(f"memo{i}", [128, 2, T], BF16, s1) for i in range(2)]
        E = sb("E1", [128, 2, 2, T], BF16, s1)
        rden = sb("rden1", [128, T], F32, s1)
        csb = [sb(f"csb{i}", [128, 128], F32, s1) for i in range(2)]
        cnb = [sb(f"cnb{i}", [128, 128], BF16, s1) for i in range(2)]
        iks = [sb(f"iks{i}", [128, 68], F32, s1) for i in range(2)]
        ik2 = [sb(f"ik2{i}", [128, 128], BF16, s1) for i in range(2)]
        sm1 = [sb(f"smp1_{i}", [128, 32], F32, s1) for i in range(2)]
        nch = ntiles * 128 // T
        for ci in range(nch):
            xi = ci % 2
            xfc, xfk = xf[xi], ('xf', xi)
            kb.dma('sp', lambda q, ci=ci, xfc=xfc: q.dma_start(
                out=xfc[:], in_=src[ci * T:(ci + 1) * T, :].rearrange("(t p) d -> p t d", p=128)), writes=[xfk])
            kb.op('act', lambda a, xfc=xfc: a.copy(out=xbf[:], in_=xfc[:]), reads=[xfk], writes=['xbf'])
            for k in range(8):
                pt, pk = psum()
                ptb = pt[:].bitcast(BF16)
                for t in range(2):
                    kb.op('pe', lambda p, ptb=ptb, t=t, k=k: p.transpose(out=ptb[:, t * 128:(t + 1) * 128],
                          in_=xbf[:, t, k * 128:(k + 1) * 128], identity=ident_b[:]), reads=['xbf', 'ident_b'], writes=[pk])
                if k % 2 == 0:
                    kb.op('dve', lambda v, ptb=ptb, k=k: v.tensor_copy(out=xT[:, k, :], in_=ptb[:, 0:T]), reads=[pk], writes=[('xT', k)])
                else:
                    kb.op('act', lambda a, ptb=ptb, k=k: a.copy(out=xT[:, k, :], in_=ptb[:, 0:T]), reads=[pk], writes=[('xT', k)])
            xTk = [('xT', k) for k in range(8)]

            def fm_proj(col0, dst_ap, dkey, eng):
                pt, pk = psum()
                for k in range(8):
                    kb.op('pe', lambda p, pt=pt, k=k: p.matmul(pt[:, 0:T], lhsT=win[:, k, col0:col0 + 128], rhs=xT[:, k, :],
                          start=(k == 0), stop=(k == 7)), reads=['win'] + xTk, writes=[pk])
                if eng == 'act':
                    kb.op('act', lambda a, pt=pt: a.copy(out=dst_ap, in_=pt[:, 0:T]), reads=[pk], writes=[dkey])
                else:
                    kb.op('dve', lambda v, pt=pt: v.tensor_copy(out=dst_ap, in_=pt[:, 0:T]), reads=[pk], writes=[dkey])

            for c in range(6):
                fm_proj(c * 128, qT[:, c, :], ('qT', c), 'act' if c % 2 else 'dve')
            bi = ci % 2
            for j in range(2):
                fm_proj(896 + j * 128, iqT[bi][:, j, :], ('iqT', bi), 'act')
            for j in range(2):
                fm_proj(1220 + j * 128, mqT[:, j, :], 'mqT1', 'dve')
            kb.dma('sp', lambda q, ci=ci, bi=bi: q.dma_start(out=iqd[:, :, ci * T:(ci + 1) * T].rearrange("j p t -> p j t"), in_=iqT[bi][:]),
                   reads=[('iqT', bi)], writes=['iqd'])
            for h in range(12):
                j, hh = h // 2, h % 2
                pt, pk = psum()
                kb.op('pe', lambda p, pt=pt, j=j, hh=hh: p.matmul(pt[:, 0:T], lhsT=wukT[hh * 64:(hh + 1) * 64, j, :],
                      rhs=qT[hh * 64:(hh + 1) * 64, j, :], start=True, stop=True), reads=['wukT', ('qT', j)], writes=[pk])
                if h % 2 == 0:
                    kb.op('act', lambda a, pt=pt, h=h, bi=bi: a.activation(out=qlb[bi][:, h, :], in_=pt[:, 0:T], func=AF.Copy, scale=0.125),
                          reads=[pk], writes=[('qlb', bi)])
                else:
                    kb.op('dve', lambda v, pt=pt, h=h, bi=bi: v.tensor_scalar(out=qlb[bi][:, h, :], in0=pt[:, 0:T], scalar1=0.125, scalar2=None, op0=ALU.mult),
                          reads=[pk], writes=[('qlb', bi)])
            kb.dma('sp', lambda q, ci=ci, bi=bi: q.dma_start(out=qlat[:, :, ci * T:(ci + 1) * T].rearrange("h p t -> p h t"), in_=qlb[bi][:]),
                   reads=[('qlb', bi)], writes=['qlat'])
            mem_attn(mqT, T, kT, vpad, onespad, lambda pr, bi=bi: memo[bi][:, pr, :], E, rden, '1')
            kb.dma('sp', lambda q, ci=ci, bi=bi: q.dma_start(out=memod[:, :, ci * T:(ci + 1) * T].rearrange("j p t -> p j t"), in_=memo[bi][:]),
                   reads=[('mixT1', 6), ('mixT1', 7)], writes=['memod'])
            for t in range(T // 128):
                ti = ci * (T // 128) + t
                i2 = ti % 2
                smk = ('sm1', i2)
                sm = sm1[i2]
                pc, pck = psum()
                for k in range(8):
                    kb.op('pe', lambda p, pc=pc, k=k, t=t: p.matmul(pc[:, 0:128], lhsT=xT[:, k, t * 128:(t + 1) * 128], rhs=win[:, k, 768:896],
                          start=(k == 0), stop=(k == 7)), reads=['win'] + xTk, writes=[pck])
                pi_, pik = psum()
                for k in range(8):
                    kb.op('pe', lambda p, pi_=pi_, k=k, t=t: p.matmul(pi_[:, 0:68], lhsT=xT[:, k, t * 128:(t + 1) * 128], rhs=win[:, k, 1152:1220],
                          start=(k == 0), stop=(k == 7)), reads=['win'] + xTk, writes=[pik])
                ss = sm[:, 0:1]
                kb.op('act', lambda a, pc=pc, i2=i2, ss=ss: a.activation(out=csb[i2][:], in_=pc[:, 0:128], func=AF.Square, accum_out=ss),
                      reads=[pck], writes=[('csb', i2), smk])
                kb.op('act', lambda a, ss=ss: a.activation(out=ss, in_=ss, func=AF.Sqrt, bias=1e-6, scale=1.0 / 128.0), reads=[smk], writes=[smk])
                kb.op('dve', lambda v, ss=ss: v.reciprocal(out=ss, in_=ss), reads=[smk], writes=[smk])
                kb.op('dve', lambda v, pc=pc, i2=i2, ss=ss: v.scalar_tensor_tensor(out=csb[i2][:], in0=pc[:, 0:128], scalar=ss, in1=gkv[:],
                      op0=ALU.mult, op1=ALU.mult), reads=[pck, smk, 'gkv', ('csb', i2)], writes=[('csb', i2)])
                kb.op('act', lambda a, i2=i2, ti=ti: a.copy(out=cTok[:, ti, :], in_=csb[i2][:]), reads=[('csb', i2)], writes=[('cTok', ti)])
                pt, pk = psum()
                ptb = pt[:].bitcast(BF16)
                kb.op('pe', lambda p, ptb=ptb, ti=ti: p.transpose(out=ptb[:, 0:128], in_=cTok[:, ti, :], identity=ident_b[:]),
                      reads=[('cTok', ti), 'ident_b'], writes=[pk])
                kb.op('dve', lambda v, ptb=ptb, ti=ti: v.tensor_copy(out=cT[:, ti * 128:(ti + 1) * 128], in_=ptb[:, 0:128]), reads=[pk], writes=[('cT', ti)])
                kb.op('act', lambda a, pi_=pi_, i2=i2: a.copy(out=iks[i2][:], in_=pi_[:, 0:68]), reads=[pik], writes=[('iks', i2)])
                st6 = sm[:, 8:14]
                mv = sm[:, 14:16]
                rs = sm[:, 16:17]
                kb.op('dve', lambda v, i2=i2, st6=st6: v.bn_stats(out=st6, in_=iks[i2][:, 0:64]), reads=[('iks', i2)], writes=[smk])
                kb.op('dve', lambda v, st6=st6, mv=mv: v.bn_aggr(out=mv, in_=st6), reads=[smk], writes=[smk])
                kb.op('act', lambda a, mv=mv, rs=rs: a.activation(out=rs, in_=mv[:, 1:2], func=AF.Sqrt, bias=1e-5, scale=1.0), reads=[smk], writes=[smk])
                kb.op('dve', lambda v, rs=rs: v.reciprocal(out=rs, in_=rs), reads=[smk], writes=[smk])
                kb.op('dve', lambda v, i2=i2, mv=mv, rs=rs: v.tensor_scalar(out=iks[i2][:, 0:64], in0=iks[i2][:, 0:64], scalar1=mv[:, 0:1], scalar2=rs,
                      op0=ALU.subtract, op1=ALU.mult), reads=[('iks', i2), smk], writes=[('iks', i2)])
                kb.op('dve', lambda v, i2=i2: v.tensor_tensor(out=iks[i2][:, 0:64], in0=iks[i2][:, 0:64], in1=gik[:], op=ALU.mult),
                      reads=[('iks', i2), 'gik'], writes=[('iks', i2)])
                for r in range(2):
                    kb.op('dve', lambda v, i2=i2, r=r: v.tensor_tensor(out=ik2[i2][:, r * 64:(r + 1) * 64], in0=iks[i2][:, 0:64], in1=bik[:], op=ALU.add),
                          reads=[('iks', i2), 'bik'], writes=[('ik2', i2)])
                pt, pk = psum()
                ptb = pt[:].bitcast(BF16)
                kb.op('pe', lambda p, ptb=ptb, i2=i2: p.transpose(out=ptb[:, 0:128], in_=ik2[i2][:], identity=ident_b[:]),
                      reads=[('ik2', i2), 'ident_b'], writes=[pk])
                kb.op('act', lambda a, ptb=ptb, ti=ti: a.copy(out=ikT2[:, ti * 128:(ti + 1) * 128], in_=ptb[:, 0:128]), reads=[pk], writes=[('ikT2', ti)])
                kb.op('act', lambda a, i2=i2, ti=ti: a.activation(out=absw[:, ti, :], in_=iks[i2][:, 64:68], func=AF.Abs),
                      reads=[('iks', i2)], writes=[('absw', ti)])
                kb.op('dve', lambda v, i2=i2, ti=ti: v.tensor_scalar(out=sgnw[:, ti, :], in0=iks[i2][:, 64:68], scalar1=0.0, scalar2=2.0, op0=ALU.is_ge, op1=ALU.mult),
                      reads=[('iks', i2)], writes=[('sgnw', ti)])
                kb.op('dve', lambda v, ti=ti: v.tensor_scalar(out=sgnw[:, ti, :], in0=sgnw[:, ti, :], scalar1=-1.0, scalar2=None, op0=ALU.add),
                      reads=[('sgnw', ti)], writes=[('sgnw', ti)])
        kb.barrier()
        s1.close()

        s2 = ExitStack()
        sc = sb("sc", [128, S], F32, s2)
        junk = sb("junk", [128, S // 2 + 128], mybir.dt.uint8, s2)
        junk2 = sb("junk2", [128, S], mybir.dt.uint8, s2) if False else None
        maskb = sb("maskb", [128, S], BF16, s2)
        tiec = [sb(f"tiec{i}", [128, CH], BF16, s2) for i in range(2)]
        cumc = [sb(f"cumc{i}", [128, CH], F32, s2) for i in range(2)]
        onesc = sb("onesc", [128, CH], BF16, s2)
        kb.op('pool', lambda g: g.memset(onesc[:], 1.0), writes=['onesc'])
        cmask = sb("cmask", [128, 128], F32, s2)
        kb.dma('sp', lambda q: q.dma_start(out=cmask[:], in_=c_cmask[:, :]), writes=['cmask'])
        rl = [sb(f"rl{i}", [128, 512], F32, s2) for i in range(4)]
        ql = sb("ql", [128, 12, 128], BF16, s2)
        iqb = [sb(f"iqb{i}", [128, 2, 128], BF16, s2) for i in range(2)]
        mixT = [sb(f"mixT1_{i}", [128, 8, 128], BF16, s2) for i in range(2)]
        x2t = sb("x2t", [128, D], F32, s2)
        Pt = [sb(f"Pt{i}", [128, 512], BF16, s2) for i in range(3)]
        olat = [sb(f"olat{i}", [128, 512], BF16, s2) for i in range(2)]
        Dsb = sb("Dsb", [128, 512], F32, s2)
        Osb = sb("Osb", [128, 512], F32, s2)
        bs = sb("bs", [128, 16], F32, s2)
        steps = sb("steps", [128, 24], F32, s2)
        nmid = sb("nmid", [128, 1], F32, s2)
        sgs = sb("sgs", [128, 1], F32, s2)
        pow2 = sb("pow2", [128, 24], F32, s2)
        kb.dma('sp', lambda q: q.dma_start(out=pow2[:], in_=c_pow2[:, :]), writes=['pow2'])
        lo, hi, mid, cnt, ge, dd, ee, need, cgt, carry = [bs[:, i:i + 1] for i in range(10)]
        npt = [0]
        natt = [0]
        psrot[0] = 4

        def selection_a(qb):
            n = (qb + 1) * 128
            ib = qb % 2
            kb.dma('sp', lambda q: q.dma_start(out=iqb[ib][:], in_=iqd[:, :, qb * 128:(qb + 1) * 128].rearrange("j p t -> p j t")),
                   writes=[('iqb', ib)])
            for g0 in range(0, n, 512):
                w = min(512, n - g0)
                pts = []
                for h in range(4):
                    j, hh = h // 2, h % 2
                    pt, pk = psum()
                    pts.append((pt, pk))
                    kb.op('pe', lambda p, pt=pt, j=j, hh=hh: p.matmul(pt[:, 0:w], lhsT=iqb[ib][hh * 64:(hh + 1) * 64, j, :],
                          rhs=ikT2[hh * 64:(hh + 1) * 64, g0:g0 + w], start=True, stop=True), reads=[('iqb', ib), 'ikT2'], writes=[pk])
                for h in range(4):
                    pt, pk = pts[h]
                    kb.op('act', lambda a, pt=pt, h=h: a.activation(out=rl[h][:, 0:w], in_=pt[:, 0:w], func=AF.Relu, scale=absw[:, qb, h:h + 1]),
                          reads=[pk], writes=[('rl', h)])
                yield
                for h in range(4):
                    if h == 0:
                        kb.op('dve', lambda v, h=h: v.tensor_scalar(out=sc[:, g0:g0 + w], in0=rl[h][:, 0:w], scalar1=sgnw[:, qb, h:h + 1],
                              scalar2=None, op0=ALU.mult), reads=[('rl', h)], writes=['sc'])
                    else:
                        kb.op('dve', lambda v, h=h: v.scalar_tensor_tensor(out=sc[:, g0:g0 + w], in0=rl[h][:, 0:w], scalar=sgnw[:, qb, h:h + 1],
                              in1=sc[:, g0:g0 + w], op0=ALU.mult, op1=ALU.add), reads=[('rl', h), 'sc'], writes=['sc'])
                yield
            kb.op('dve', lambda v: v.tensor_tensor(out=sc[:, n - 128:n], in0=sc[:, n - 128:n], in1=cmask[:], op=ALU.add), reads=['sc', 'cmask'], writes=['sc'])
            kb.op('dve', lambda v: v.tensor_reduce(out=hi, in_=sc[:, 0:n], axis=AX.X, op=ALU.max), reads=['sc'], writes=['bs'])
            kb.op('dve', lambda v: v.tensor_reduce(out=lo, in_=sc[:, 0:256], axis=AX.X, op=ALU.min), reads=['sc'], writes=['bs'])
            yield
            kb.op('dve', lambda v: v.scalar_tensor_tensor(out=dd, in0=hi, scalar=2.0, in1=lo, op0=ALU.add, op1=ALU.subtract), reads=['bs'], writes=['bs'])
            kb.op('dve', lambda v: v.tensor_scalar(out=steps[:], in0=pow2[:], scalar1=dd, scalar2=None, op0=ALU.mult), reads=['bs', 'pow2'], writes=['steps'])
            kb.op('dve', lambda v: v.scalar_tensor_tensor(out=mid, in0=lo, scalar=-1.0, in1=steps[:, 0:1], op0=ALU.add, op1=ALU.add), reads=['bs', 'steps'], writes=['bs'])
            yield
            hsp = ((n // 2) // 128) * 128
            wact = n - hsp
            for it in range(NBIS):
                kb.op('dve', lambda v: v.tensor_scalar(out=junk[:, 0:hsp], in0=sc[:, 0:hsp], scalar1=mid, scalar2=None, op0=ALU.is_ge, op1=ALU.add, accum_out=cnt),
                      reads=['sc', 'bs'], writes=['junk', 'bs'])
                kb.op('dve', lambda v: v.tensor_scalar(out=junk[:, 0:wact], in0=sc[:, hsp:n], scalar1=mid, scalar2=cnt, op0=ALU.is_ge, op1=ALU.add, accum_out=cnt),
                      reads=['sc', 'bs'], writes=['junk', 'bs'])
                yield
                kb.op('dve', lambda v: v.tensor_scalar(out=ge, in0=cnt, scalar1=float(KSEL), scalar2=0.5, op0=ALU.is_ge, op1=ALU.subtract), reads=['bs'], writes=['bs'])
                kb.op('dve', lambda v, it=it: v.scalar_tensor_tensor(out=mid, in0=ge, scalar=steps[:, it:it + 1], in1=mid, op0=ALU.mult, op1=ALU.add),
                      reads=['bs', 'steps'], writes=['bs'])
                yield
            kb.op('dve', lambda v: v.tensor_tensor(out=lo, in0=mid, in1=steps[:, NBIS:NBIS + 1], op=ALU.subtract), reads=['bs', 'steps'], writes=['bs'])
            kb.op('dve', lambda v: v.tensor_tensor(out=hi, in0=mid, in1=steps[:, NBIS:NBIS + 1], op=ALU.add), reads=['bs', 'steps'], writes=['bs'])
            kb.op('dve', lambda v: v.tensor_scalar(out=junk[:, 0:hsp], in0=sc[:, 0:hsp], scalar1=hi, scalar2=None, op0=ALU.is_ge, op1=ALU.add, accum_out=cgt),
                  reads=['sc', 'bs'], writes=['junk', 'bs'])
            kb.op('dve', lambda v: v.tensor_scalar(out=junk[:, 0:wact], in0=sc[:, hsp:n], scalar1=hi, scalar2=cgt, op0=ALU.is_ge, op1=ALU.add, accum_out=cgt),
                  reads=['sc', 'bs'], writes=['junk', 'bs'])
            kb.op('dve', lambda v: v.tensor_scalar(out=need, in0=cgt, scalar1=-1.0, scalar2=float(KSEL), op0=ALU.mult, op1=ALU.add), reads=['bs'], writes=['bs'])
            yield

        def selection_b(qb):
            n = (qb + 1) * 128
            for ci_, c0 in enumerate(range(0, n, CH)):
                w = min(CH, n - c0)
                r_ = ci_ % 2
                tk, ck = ('tiec', r_), ('cumc', r_)
                kb.op('dve', lambda v, c0=c0, w=w, r_=r_: v.tensor_scalar(out=tiec[r_][:, 0:w], in0=sc[:, c0:c0 + w], scalar1=hi, scalar2=None, op0=ALU.is_lt),
                      reads=['sc', 'bs'], writes=[tk])
                kb.op('dve', lambda v, c0=c0, w=w, r_=r_: v.scalar_tensor_tensor(out=tiec[r_][:, 0:w], in0=sc[:, c0:c0 + w], scalar=lo, in1=tiec[r_][:, 0:w],
                      op0=ALU.is_ge, op1=ALU.mult), reads=['sc', 'bs', tk], writes=[tk])
                init = 0.0 if c0 == 0 else carry
                kb.op('dve', lambda v, w=w, r_=r_, init=init: v.tensor_tensor_scan(out=cumc[r_][:, 0:w], data0=onesc[:, 0:w], data1=tiec[r_][:, 0:w],
                      initial=init, op0=ALU.mult, op1=ALU.add), reads=['onesc', tk, 'bs'], writes=[ck])
                kb.op('dve', lambda v, w=w, r_=r_: v.tensor_copy(out=carry, in_=cumc[r_][:, w - 1:w]), reads=[ck], writes=['bs'])
                kb.op('dve', lambda v, w=w, r_=r_: v.scalar_tensor_tensor(out=tiec[r_][:, 0:w], in0=cumc[r_][:, 0:w], scalar=need, in1=tiec[r_][:, 0:w],
                      op0=ALU.is_le, op1=ALU.mult), reads=[ck, 'bs', tk], writes=[tk])
                kb.op('dve', lambda v, c0=c0, w=w, r_=r_: v.scalar_tensor_tensor(out=maskb[:, c0:c0 + w], in0=sc[:, c0:c0 + w], scalar=hi, in1=tiec[r_][:, 0:w],
                      op0=ALU.is_ge, op1=ALU.add), reads=['sc', 'bs', tk], writes=['maskb'])
                yield

        def attention(qb):
            mx = mixT[qb % 2]
            kb.dma('sp', lambda q: q.dma_start(out=ql[:], in_=qlat[:, :, qb * 128:(qb + 1) * 128].rearrange("h p t -> p h t")), writes=['ql'])
            kb.dma('sp', lambda q: q.dma_start(out=mx[:, 6:8, :], in_=memod[:, :, qb * 128:(qb + 1) * 128].rearrange("j p t -> p j t")),
                   writes=[('mixTm', qb % 2)])
            pO, pOk = ps[6], ('ps', 6)
            pD, pDk = ps[7], ('ps', 7)
            steps_ = [(hg, j) for hg in range(3) for j in range(qb + 1)]

            def logits(hg, j):
                qrhs = ql[:, hg * 4:(hg + 1) * 4, :].rearrange("p h t -> p (h t)")
                li = 4 + (natt[0] % 2)
                natt[0] += 1
                pL, pLk = ps[li], ('ps', li)
                dt = qb - j
                nmm = 1 + (1 if qb >= 2 else 0) + (1 if dt <= 1 else 0)
                m = 0
                kb.op('pe', lambda p: p.matmul(pL[:, :], lhsT=cT[:, j * 128:(j + 1) * 128], rhs=qrhs, start=True, stop=(nmm == 1)),
                      reads=[('cT', j), 'ql'], writes=[pLk])
                m += 1
                if qb >= 2:
                    kb.op('pe', lambda p, m=m: p.matmul(pL[:, :], lhsT=maskb[:, j * 128:(j + 1) * 128],
                          rhs=i4big[:].rearrange("p r t -> p (r t)"), start=False, stop=(m == nmm - 1)), reads=['maskb', 'i4big'], writes=[pLk])
                    m += 1
                if dt <= 1:
                    kb.op('pe', lambda p, m=m: p.matmul(pL[:, :], lhsT=ident_b[:],
                          rhs=BT[:, dt, hg * 4:(hg + 1) * 4, :].rearrange("p h t -> p (h t)"), start=False, stop=(m == nmm - 1)),
                          reads=['BT', 'ident_b'], writes=[pLk])
                    m += 1
                return pL, pLk

            cur = logits(*steps_[0])
            for idx, (hg, j) in enumerate(steps_):
                pL, pLk = cur
                pi = npt[0] % 3
                npt[0] += 1
                kb.op('act', lambda a, pL=pL, pi=pi: a.activation(out=Pt[pi][:], in_=pL[:, :], func=AF.Exp, bias=(nbias[:] if qb >= 2 else zbias[:]), scale=1.0),
                      reads=[pLk, 'nbias'], writes=[('Pt', pi)])
                if idx + 1 < len(steps_):
                    cur = logits(*steps_[idx + 1])
                kb.op('pe', lambda p, j=j, pi=pi: p.matmul(pO[:, :], lhsT=cTok[:, j, :], rhs=Pt[pi][:], start=(j == 0), stop=(j == qb)),
                      reads=[('cTok', j), ('Pt', pi)], writes=[pOk])
                kb.op('pe', lambda p, j=j, pi=pi: p.matmul(pD[:, :], lhsT=ones_b[:], rhs=Pt[pi][:], start=(j == 0), stop=(j == qb)),
                      reads=['ones_b', ('Pt', pi)], writes=[pDk])
                yield
                if j == qb:
                    oi = hg % 2
                    kb.op('act', lambda a: a.copy(out=Dsb[:], in_=pD[:, :]), reads=[pDk], writes=['Dsb'])
                    kb.op('act', lambda a: a.copy(out=Osb[:], in_=pO[:, :]), reads=[pOk], writes=['Osb'])
                    kb.op('dve', lambda v: v.reciprocal(out=Dsb[:], in_=Dsb[:]), reads=['Dsb'], writes=['Dsb'])
                    kb.op('pool', lambda g, oi=oi: g.tensor_tensor(out=olat[oi][:], in0=Osb[:], in1=Dsb[:], op=ALU.mult), reads=['Osb', 'Dsb'], writes=[('olat', oi)])
                    for pp in range(2):
                        pT, pTk = psum()
                        for hh in range(2):
                            hl = 2 * pp + hh
                            h = hg * 4 + hl
                            kb.op('pe', lambda p, pT=pT, h=h, hl=hl, hh=hh, oi=oi: p.matmul(pT[:, 0:128], lhsT=wuvpad[:, h, :], rhs=olat[oi][:, hl * 128:(hl + 1) * 128],
                                  start=(hh == 0), stop=(hh == 1)), reads=['wuvpad', ('olat', oi)], writes=[pTk])
                        kb.op('act', lambda a, pT=pT, hg=hg, pp=pp: a.copy(out=mx[:, hg * 2 + pp, :], in_=pT[:, 0:128]), reads=[pTk], writes=[('mixTt', qb % 2, hg * 2 + pp)])
                    yield

        nbias = sb("nbias", [128, 1], F32, s2)
        zbias = sb("zbias", [128, 1], F32, s2)
        kb.op('dve', lambda v: v.memset(nbias[:], -100.0), writes=['nbias'])
        kb.op('dve', lambda v: v.memset(zbias[:], 0.0), writes=['nbias'])

        nqb = ntiles

        def chain(*gens):
            for g in gens:
                yield from g

        def nsteps_sel(qb):
            n = (qb + 1) * 128
            return 2 * ((n + 511) // 512) + 3 + 2 * NBIS

        def tail_stream(qb):
            mx = mixT[qb % 2]
            mkeys = [('mixTt', qb % 2, k) for k in range(6)] + [('mixTm', qb % 2)]
            kb.dma('sp', lambda q: q.dma_start(out=x2t[:], in_=src[qb * 128:(qb + 1) * 128, :]), writes=['x2t'])
            yield
            yield from tail.run_gen(qb, lambda k, mx=mx: mx[:, k, :], mkeys, x2t, 'x2t', x1buf[qb * 128:(qb + 1) * 128, :])

        if nqb > 2:
            for _ in selection_a(2):
                pass
        gT = None
        for qb in range(nqb):
            if qb >= 2:
                gS = selection_b(qb)
                st_ = [(gS, 2 * (qb + 1))]
                if gT is not None:
                    st_.append((gT, 40))
                interleave(st_, until=gS)
            streams = [(attention(qb), 3 * (qb + 1) + 3)]
            if gT is not None:
                streams.append((gT, 12))
            if qb + 1 < nqb and qb + 1 >= 2:
                streams.append((selection_a(qb + 1), nsteps_sel(qb + 1)))
            interleave(streams)
            gT = tail_stream(qb)
        for _ in gT:
            pass
        psrot[0] = 6
        kb.barrier()
        s2.close()
        st.close()

    def phase_moe(layer, dst, ntiles=NT, nexp=NE):
        L = layer
        st = ExitStack()
        w1 = [sb(f"w1_{L}_{i}", [128, 8, 2 * D], BF16, st) for i in range(2)]
        w2 = [sb(f"w2_{L}_{i}", [128, 8, D], BF16, st) for i in range(2)]
        b2r = [sb(f"b2r_{L}_{i}", [1, D], BF16, st) for i in range(2)]
        b1a = sb(f"b1a_{L}", [128, NE, 16], F32, st)
        b1u = sb(f"b1u_{L}", [128, NE, 8], F32, st)
        with nc.allow_non_contiguous_dma(reason="bias layout"):
            kb.dma('sp', lambda q: q.dma_start(out=b1a[:], in_=exp_b1[L].rearrange("e (j p) -> p e j", p=128)), writes=['b1a'])
        kb.op('dve', lambda v: v.tensor_scalar(out=b1u[:], in0=b1a[:, :, 8:16], scalar1=1.0, scalar2=None, op0=ALU.add), reads=['b1a'], writes=['b1u'])
        xgt = [sb(f"xgt_{L}_{i}", [128, RG // 128, D], BF16, st) for i in range(2)]
        xgT = [sb(f"xgT_{L}_{i}", [128, 8, RG], BF16, st) for i in range(2)]
        actT = [sb(f"actT_{L}_{i}", [128, 8, RG], BF16, st) for i in range(2)]
        tg = [sb(f"tg_{L}_{i}", [128, RG], F32, st) for i in range(2)]
        tu = [sb(f"tu_{L}_{i}", [128, RG], F32, st) for i in range(2)]
        tsg = [sb(f"tsg_{L}_{i}", [128, RG], F32, st) for i in range(2)]
        tt = [sb(f"tt_{L}_{i}", [128, RG], F32, st) for i in range(2)]
        yev = [sb(f"yev_{L}_{i}", [128, D], F32, st) for i in range(2)]
        nyev = 0

        def load_w(e):
            i = e % 2
            load_cast(w1[i][:], exp_w1[L, e].rearrange("(k p) f -> p k f", p=128), ('w1', i))
            load_cast(w2[i][:], exp_w2[L, e].rearrange("(k p) n -> p k n", p=128), ('w2', i))
            load_cast(b2r[i][:], exp_b2[L, e:e + 1, :], ('b2r', i))

        NG = CAP // RG

        def stage_a(e, g, gi):
            wi = e % 2
            r0 = e * CAP + g * RG
            kb.dma('sp', lambda q: q.dma_start(out=xgt[gi][:], in_=xg[r0:r0 + RG, :].rearrange("(t p) d -> p t d", p=128)),
                   reads=['xg'], writes=[('xgt', gi)])
            for k in range(8):
                pt, pk = psum()
                ptb = pt[:].bitcast(BF16)
                for t in range(RG // 128):
                    kb.op('pe', lambda p, ptb=ptb, t=t, k=k: p.transpose(out=ptb[:, t * 128:(t + 1) * 128],
                          in_=xgt[gi][:, t, k * 128:(k + 1) * 128], identity=ident_b[:]), reads=[('xgt', gi), 'ident_b'], writes=[pk])
                if k % 2 == 0:
                    kb.op('dve', lambda v, ptb=ptb, k=k: v.tensor_copy(out=xgT[gi][:, k, :], in_=ptb[:, 0:RG]), reads=[pk], writes=[('xgT', gi, k)])
                else:
                    kb.op('act', lambda a, ptb=ptb, k=k: a.copy(out=xgT[gi][:, k, :], in_=ptb[:, 0:RG]), reads=[pk], writes=[('xgT', gi, k)])
                if k % 2 == 1:
                    yield
            xgTk = [('xgT', gi, k) for k in range(8)]
            for j in range(8):
                ji = j % 2
                pg, pgk = psum()
                pu, puk = psum()
                for k in range(8):
                    kb.op('pe', lambda p, pg=pg, k=k, j=j: p.matmul(pg[:, 0:RG], lhsT=w1[wi][:, k, j * 128:(j + 1) * 128],
                          rhs=xgT[gi][:, k, :], start=(k == 0), stop=(k == 7)), reads=[('w1', wi)] + xgTk, writes=[pgk])
                for k in range(8):
                    kb.op('pe', lambda p, pu=pu, k=k, j=j: p.matmul(pu[:, 0:RG], lhsT=w1[wi][:, k, D + j * 128:D + (j + 1) * 128],
                          rhs=xgT[gi][:, k, :], start=(k == 0), stop=(k == 7)), reads=[('w1', wi)] + xgTk, writes=[puk])
                kb.op('dve', lambda v, pg=pg, ji=ji, j=j: v.tensor_scalar(out=tg[ji][:], in0=pg[:, 0:RG], scalar1=b1a[:, e, j:j + 1],
                      scalar2=7.0, op0=ALU.add, op1=ALU.min), reads=[pgk, 'b1a'], writes=[('tg', ji)])
                kb.op('act', lambda a, ji=ji: a.activation(out=tsg[ji][:], in_=tg[ji][:], func=AF.Sigmoid, scale=1.702),
                      reads=[('tg', ji)], writes=[('tsg', ji)])
                kb.op('dve', lambda v, pu=pu, ji=ji, j=j: v.tensor_scalar(out=tu[ji][:], in0=pu[:, 0:RG], scalar1=b1u[:, e, j:j + 1],
                      scalar2=8.0, op0=ALU.add, op1=ALU.min), reads=[puk, 'b1u'], writes=[('tu', ji)])
                kb.op('dve', lambda v, ji=ji: v.scalar_tensor_tensor(out=tt[ji][:], in0=tu[ji][:], scalar=-6.0, in1=tg[ji][:],
                      op0=ALU.max, op1=ALU.mult), reads=[('tu', ji), ('tg', ji)], writes=[('tt', ji)])
                kb.op('pool', lambda g_, ji=ji, j=j: g_.tensor_tensor(out=actT[gi][:, j, :], in0=tt[ji][:], in1=tsg[ji][:], op=ALU.mult),
                      reads=[('tt', ji), ('tsg', ji)], writes=[('actT', gi, j)])
                yield

        def stage_b(e, g, gi):
            nonlocal nyev
            wi = e % 2
            r0 = e * CAP + g * RG
            actk = [('actT', gi, j) for j in range(8)]
            for t in range(RG // 128):
                yi = nyev % 2
                nyev += 1
                for nh in range(2):
                    py, pyk = psum()
                    for k in range(8):
                        kb.op('pe', lambda p, py=py, k=k, t=t, nh=nh: p.matmul(py[:, :], lhsT=actT[gi][:, k, t * 128:(t + 1) * 128],
                              rhs=w2[wi][:, k, nh * 512:(nh + 1) * 512], start=(k == 0), stop=False), reads=[('w2', wi)] + actk, writes=[pyk])
                    kb.op('pe', lambda p, py=py, nh=nh: p.matmul(py[:, :], lhsT=ones_b[0:1, :], rhs=b2r[wi][0:1, nh * 512:(nh + 1) * 512],
                          start=False, stop=True), reads=[('b2r', wi), 'ones_b'], writes=[pyk])
                    kb.op('act', lambda a, py=py, nh=nh, yi=yi: a.copy(out=yev[yi][:, nh * 512:(nh + 1) * 512], in_=py[:, :]),
                          reads=[pyk], writes=[('yev', yi)])
                    yield
                rr0 = r0 + t * 128
                kb.dma('sp', lambda q, rr0=rr0, yi=yi: q.dma_start(out=yg[rr0:rr0 + 128, :], in_=yev[yi][:]), reads=[('yev', yi)], writes=['yg'])

        groups = [(e, g) for e in range(nexp) for g in range(NG)]
        load_w(0)
        if nexp > 1:
            load_w(1)
        for _ in stage_a(groups[0][0], groups[0][1], 0):
            pass
        for i, (e, g) in enumerate(groups):
            if g == 0 and e >= 1 and e + 1 < nexp:
                load_w(e + 1)
            streams = [(stage_b(e, g, i % 2), 6)]
            if i + 1 < len(groups):
                e2, g2 = groups[i + 1]
                streams.append((stage_a(e2, g2, (i + 1) % 2), 20))
            interleave(streams)
        kb.barrier()
        st.close()
        st = ExitStack()
        lng = sb(f"ln2g{L}", [128, D], F32, st)
        lnb = sb(f"ln2b{L}", [128, D], F32, st)
        bcast_rows(lng[:], ln2_g[L], 'lng2')
        bcast_rows(lnb[:], ln2_b[L], 'lnb2')
        yk = [[sb(f"yk{L}_{i}_{k}", [128, D], F32, st) for k in range(4)] for i in range(2)]
        x1r = [sb(f"x1r{L}_{i}", [128, D], F32, st) for i in range(2)]
        zz = [sb(f"zz{L}_{i}", [128, D], F32, st) for i in range(2)]
        oo = [sb(f"oo{L}_{i}", [128, D], F32, st) for i in range(2)]
        smm = [sb(f"smm{L}_{i}", [128, 256], F32, st) for i in range(2)]
        lnh = Tail.__new__(Tail)
        for ti in range(ntiles):
            i = ti % 2
            kb.dma('sp', lambda q, ti=ti, i=i: q.dma_start(out=x1r[i][:], in_=x1buf[ti * 128:(ti + 1) * 128, :]), reads=[('x1d', ti)], writes=[('x1r', i)])
            for k in range(4):
                kb.op('pool', lambda g_, i=i, k=k: g_.memset(yk[i][k][:], 0.0), writes=[('yk', i, k)])
                kb.dma('pool', lambda q, ti=ti, i=i, k=k: q.indirect_dma_start(
                    out=yk[i][k][:, :], out_offset=None, in_=yg[:, :],
                    in_offset=bass.IndirectOffsetOnAxis(ap=destall[:, ti, k:k + 1], axis=0),
                    bounds_check=bc_reg, oob_is_err=False), reads=['yg', ('dest', ti)], writes=[('yk', i, k)])
            kb.op('dve', lambda v, i=i: v.tensor_scalar(out=zz[i][:], in0=x1r[i][:], scalar1=ALPHA, scalar2=None, op0=ALU.mult),
                  reads=[('x1r', i)], writes=[('zz', i)])
            for k in range(4):
                kb.op('dve', lambda v, ti=ti, i=i, k=k: v.scalar_tensor_tensor(out=zz[i][:], in0=yk[i][k][:], scalar=gall[:, ti, k:k + 1],
                      in1=zz[i][:], op0=ALU.mult, op1=ALU.add), reads=[('yk', i, k), ('gate', ti), ('zz', i)], writes=[('zz', i)])
            Tail.layernorm(lnh, zz[i], ('zz', i), oo[i], ('oo', i), smm[i], ('smm', i), lng, lnb, 'lng2', 'lnb2')
            kb.dma('sp', lambda q, ti=ti, i=i: q.dma_start(out=dst[ti * 128:(ti + 1) * 128, :], in_=oo[i][:]), reads=[('oo', i)], writes=[('dst', L, ti)])
        kb.barrier()
        kb.pool_depth = 2
        st.close()

    if mode.startswith("a0"):
        ntl = NT if mode == "a0" else int(mode[2:])
        phase_a0(ntiles=ntl)
        st = ExitStack()
        cp = [sb(f"cp{i}", [128, D], F32, st) for i in range(2)]
        for ti in range(ntl):
            i = ti % 2
            kb.dma('sp', lambda q, ti=ti, i=i: q.dma_start(out=cp[i][:], in_=x1buf[ti * 128:(ti + 1) * 128, :]), writes=[('cp', i)])
            kb.dma('sp', lambda q, ti=ti, i=i: q.dma_start(out=out[ti * 128:(ti + 1) * 128, :], in_=cp[i][:]), reads=[('cp', i)], writes=[('o', ti)])
        kb.barrier()
        st.close()
    elif mode == "full":
        phase_a0()
        phase_moe(0, x2buf)
        phase_a1(x2buf)
        phase_moe(1, out)
    elif mode.startswith("a1"):
        ntl = int(mode[2:])
        zt = sb("zt1", [128, 4096], BF16)
        kb.op('pool', lambda g: g.memset(zt[:], 0.0), writes=['zt'])
        for r0 in range(0, NROW, 512):
            kb.dma('sp', lambda q, r0=r0: q.dma_start(out=xg[r0:r0 + 512, :].rearrange("(p t) d -> p (t d)", t=4), in_=zt[:]),
                   reads=['zt'], writes=['xg'])
        phase_a1(x_in, ntiles=ntl)
        st = ExitStack()
        cp = [sb(f"cp{i}", [128, D], F32, st) for i in range(2)]
        for ti in range(ntl):
            i = ti % 2
            kb.dma('sp', lambda q, ti=ti, i=i: q.dma_start(out=cp[i][:], in_=x1buf[ti * 128:(ti + 1) * 128, :]), writes=[('cp', i)])
            kb.dma('sp', lambda q, ti=ti, i=i: q.dma_start(out=out[ti * 128:(ti + 1) * 128, :], in_=cp[i][:]), reads=[('cp', i)], writes=[('o', ti)])
        kb.barrier()
        st.close()
    elif mode == "l0":
        phase_a0()
        phase_moe(0, out)
    es.close()
    return nc


def host_consts():
    ident = np.eye(128, dtype=np.float32)
    tri = np.triu(np.ones((128, 128), np.float32), 1)
    iota = np.tile(np.arange(NE, dtype=np.float32)[None, :], (128, 1))
    ecap = iota * CAP
    q = np.arange(128)
    cmask = np.where(q[None, :] <= q[:, None], 0.0, -2000.0).astype(np.float32)
    caus = np.where(q[:, None] <= q[None, :], 0.0, -30000.0).astype(np.float32)
    bkt = np.zeros((128, 2, 128), np.float32)
    for dt in range(2):
        rel = np.maximum(q[None, :] - q[:, None] + 128 * dt, 0)
        large = 16 + (np.log(np.maximum(rel, 1).astype(np.float32) / 16) / np.float32(np.log(128 / 16)) * 16).astype(np.int32)
        large = np.minimum(large, 31)
        bkt[:, dt, :] = np.where(rel < 16, rel, large)
    pow2 = np.tile((2.0 ** -(np.arange(24, dtype=np.float64) + 1)).astype(np.float32)[None, :], (128, 1))
    return {"c_ident": ident, "c_tri": tri, "c_iota": iota, "c_ecap": ecap, "c_cmask": cmask, "c_caus": caus, "c_bkt": bkt,
            "c_pow2": pow2}


_PARAMS = ["rel_bias", "a_w_in", "a_conv_w", "a_conv_b", "a_wr", "a_br", "a_wi", "a_bi", "a_lambda", "b_w_in",
           "b_kv_norm_g", "b_w_uk", "b_w_uv", "b_idx_norm_g", "b_idx_norm_b", "w_mem_kv", "w_out", "ln1_g", "ln1_b",
           "router_w", "router_b", "exp_w1", "exp_b1", "exp_w2", "exp_b2", "ln2_g", "ln2_b"]
_SQUEEZE = {"a_w_in", "a_conv_w", "a_conv_b", "a_wr", "a_br", "a_wi", "a_bi", "a_lambda", "b_w_in", "b_kv_norm_g",
            "b_w_uk", "b_w_uv", "b_idx_norm_g", "b_idx_norm_b"}


def make_in_maps(inputs, cores):
    shared = {}
    for k in _PARAMS:
        v = np.ascontiguousarray(np.asarray(inputs[k], dtype=np.float32))
        if k in _SQUEEZE:
            v = v[0]
        shared[k] = v
    shared.update(host_consts())
    maps = []
    for c in cores:
        m = dict(shared)
        m["x"] = np.ascontiguousarray(inputs["x"][c])
        m["mem"] = np.ascontiguousarray(inputs["mem"][c])
        maps.append(m)
    return maps


def kernel(**inputs):
    nc = build_program("full")
    maps = make_in_maps(inputs, list(range(8)))
    res = run_bass_kernel_spmd(nc, maps, core_ids=list(range(8)))
    return np.stack([r["out"] for r in res.results], axis=0)
```

```python
from contextlib import ExitStack
import numpy as np
import concourse.bass as bass
import concourse.mybir as mybir
from concourse.bass_utils import run_bass_kernel_spmd

F32 = mybir.dt.float32
BF16 = mybir.dt.bfloat16
I32 = mybir.dt.int32
U32 = mybir.dt.uint32
AF = mybir.ActivationFunctionType
ALU = mybir.AluOpType
AX = mybir.AxisListType

S = 8192
D = 1024
NT = S // 128
MEM = 256
TOKW = 768
NE = 32
CAP = 1536
NROW = NE * CAP
RG = 384
ALPHA = float(4 ** 0.25)
W_IN_A = 1792
W_IN_B = 1476
BIGOOB = 1.0e6


class KB:
    NS = 8
    ND = 32

    def __init__(self, nc, es):
        self.nc = nc
        self.eng = {'pe': nc.tensor, 'act': nc.scalar, 'dve': nc.vector, 'pool': nc.gpsimd, 'sp': nc.sync}
        self.esem = {e: [es.enter_context(nc.semaphore(f"s_{e}{i}")) for i in range(self.NS)]
                     for e in ('pe', 'act', 'dve', 'pool')}
        self.cnt = {e: 0 for e in ('pe', 'act', 'dve', 'pool')}
        self.dsem = [es.enter_context(nc.semaphore(f"s_d{i}")) for i in range(self.ND)]
        self.dtot = [0] * self.ND
        self.dnext = 0
        self.dnextp = 0
        self.pool_depth = 2
        self.wc = {e: {} for e in self.eng}
        self.wd = {e: {} for e in self.eng}
        self.lastw = {}
        self.readers = {}

    def _wait(self, e, tok):
        eng = self.eng[e]
        if tok[0] == 'c':
            _, e2, k = tok
            if e2 == e and e == 'pe':
                return
            if self.wc[e].get(e2, 0) >= k:
                return
            eng.wait_ge(self.esem[e2][(k - 1) % self.NS], (k - 1) // self.NS + 1)
            self.wc[e][e2] = k
        else:
            _, s, tot = tok
            if self.wd[e].get(s, 0) >= tot:
                return
            eng.wait_ge(self.dsem[s], tot)
            self.wd[e][s] = tot

    def _deps(self, e, reads, writes):
        deps = []
        for k in reads:
            t = self.lastw.get(k)
            if t is not None:
                deps.append(t)
        for k in writes:
            t = self.lastw.get(k)
            if t is not None:
                deps.append(t)
            deps.extend(self.readers.get(k, ()))
        for t in deps:
            self._wait(e, t)

    def _commit(self, tok, reads, writes):
        for k in reads:
            lst = self.readers.setdefault(k, [])
            lst[:] = [t for t in lst if not (t[0] == tok[0] and t[1] == tok[1])]
            lst.append(tok)
        for k in writes:
            self.lastw[k] = tok
            self.readers[k] = []

    def op(self, e, fn, reads=(), writes=()):
        self._deps(e, reads, writes)
        ins = fn(self.eng[e])
        self.cnt[e] += 1
        k = self.cnt[e]
        ins.then_inc(self.esem[e][(k - 1) % self.NS], 1)
        self._commit(('c', e, k), reads, writes)

    def dma(self, e, fn, reads=(), writes=()):
        half = self.ND // 2
        if e == 'pool':
            s = half + self.dnextp
            self.dnextp = (self.dnextp + 1) % self.pool_depth
        else:
            s = self.dnext
            self.dnext = (self.dnext + 1) % half
        if self.dtot[s] > 0:
            self._wait(e, ('d', s, self.dtot[s]))
        self._deps(e, reads, writes)
        ins = fn(self.eng[e])
        self.dtot[s] += 16
        ins.then_inc(self.dsem[s], 16)
        self._commit(('d', s, self.dtot[s]), reads, writes)

    def barrier(self):
        for e in self.eng:
            for e2 in self.cnt:
                if self.cnt[e2] > 0:
                    self._wait(e, ('c', e2, self.cnt[e2]))
            for s in range(self.ND):
                if self.dtot[s] > 0:
                    self._wait(e, ('d', s, self.dtot[s]))
        self.lastw.clear()
        self.readers.clear()


def interleave(streams, until=None):
    live = [[g, max(1, n), 0] for g, n in streams if g is not None]
    while live:
        live.sort(key=lambda r: r[2] / r[1])
        r = live[0]
        try:
            next(r[0])
            r[2] += 1
        except StopIteration:
            live.remove(r)
            if until is not None and r[0] is until:
                return


def build_program(mode="full"):
    nc = bass.Bass("TRN2", target_bir_lowering=False)
    es = ExitStack()
    kb = KB(nc, es)

    def din(name, shape, dt=F32):
        return nc.dram_tensor(name, list(shape), dt, kind="ExternalInput").ap()

    def dscr(name, shape, dt=F32):
        return nc.dram_tensor(name, list(shape), dt, kind="Internal").ap()

    x_in = din("x", [S, D])
    mem_in = din("mem", [MEM, D])
    rel_bias = din("rel_bias", [32, 12])
    a_w_in = din("a_w_in", [D, W_IN_A])
    a_conv_w = din("a_conv_w", [4, TOKW])
    a_conv_b = din("a_conv_b", [TOKW])
    a_wr = din("a_wr", [12, 64, 64])
    a_br = din("a_br", [TOKW])
    a_wi = din("a_wi", [12, 64, 64])
    a_bi = din("a_bi", [TOKW])
    a_lambda = din("a_lambda", [TOKW])
    b_w_in = din("b_w_in", [D, W_IN_B])
    b_kv_norm_g = din("b_kv_norm_g", [128])
    b_w_uk = din("b_w_uk", [128, 12, 64])
    b_w_uv = din("b_w_uv", [128, 12, 64])
    b_idx_norm_g = din("b_idx_norm_g", [64])
    b_idx_norm_b = din("b_idx_norm_b", [64])
    w_mem_kv = din("w_mem_kv", [2, D, 512])
    w_out = din("w_out", [2, D, D])
    ln1_g = din("ln1_g", [2, D])
    ln1_b = din("ln1_b", [2, D])
    router_w = din("router_w", [2, D, NE])
    router_b = din("router_b", [2, NE])
    exp_w1 = din("exp_w1", [2, NE, D, 2 * D])
    exp_b1 = din("exp_b1", [2, NE, 2 * D])
    exp_w2 = din("exp_w2", [2, NE, D, D])
    exp_b2 = din("exp_b2", [2, NE, D])
    ln2_g = din("ln2_g", [2, D])
    ln2_b = din("ln2_b", [2, D])
    c_ident = din("c_ident", [128, 128])
    c_tri = din("c_tri", [128, 128])
    c_iota = din("c_iota", [128, NE])
    c_ecap = din("c_ecap", [128, NE])
    c_cmask = din("c_cmask", [128, 128])
    c_caus = din("c_caus", [128, 128])
    c_bkt = din("c_bkt", [128, 2, 128])
    c_pow2 = din("c_pow2", [128, 24])

    out = nc.dram_tensor("out", [S, D], F32, kind="ExternalOutput").ap()
    x1buf = dscr("x1buf", [S, D])
    x2buf = dscr("x2buf", [S, D])
    xg = dscr("xg", [NROW, D], BF16)
    yg = dscr("yg", [NROW, D])
    qlat = dscr("qlat", [12, 128, S], BF16)
    iqd = dscr("iqd", [2, 128, S], BF16)
    memod = dscr("memod", [2, 128, S], BF16)

    def sb(name, shape, dt=F32, stack=es):
        return stack.enter_context(nc.sbuf_tensor(name, list(shape), dt))

    ps = [es.enter_context(nc.psum_tensor(f"ps{i}", [128, 512], F32)) for i in range(8)]
    psn = [0]
    psrot = [6]

    def psum():
        i = psn[0] % psrot[0]
        psn[0] = (i + 1) % psrot[0]
        return ps[i], ('ps', i)

    bc_reg = nc.gpsimd.alloc_register("bc_reg")
    nc.gpsimd.reg_mov(bc_reg, NROW - 1)
    ident_f = sb("ident_f", [128, 128])
    ident_b = sb("ident_b", [128, 128], BF16)
    tri_f = sb("tri_f", [128, 128])
    ones_f = sb("ones_f", [128, 128])
    ones_b = sb("ones_b", [128, 128], BF16)
    iota_e = sb("iota_e", [128, NE])
    ecap = sb("ecap", [128, NE])
    destall = sb("destall", [128, NT, 4], I32)
    gall = sb("gall", [128, NT, 4])

    kb.dma('sp', lambda q: q.dma_start(out=ident_f[:], in_=c_ident[:, :]), writes=['ident_f'])
    kb.dma('sp', lambda q: q.dma_start(out=tri_f[:], in_=c_tri[:, :]), writes=['tri_f'])
    kb.dma('sp', lambda q: q.dma_start(out=iota_e[:], in_=c_iota[:, :]), writes=['iota_e'])
    kb.dma('sp', lambda q: q.dma_start(out=ecap[:], in_=c_ecap[:, :]), writes=['ecap'])
    kb.op('dve', lambda v: v.tensor_copy(out=ident_b[:], in_=ident_f[:]), reads=['ident_f'], writes=['ident_b'])
    kb.op('dve', lambda v: v.memset(ones_f[:], 1.0), writes=['ones_f'])
    kb.op('dve', lambda v: v.memset(ones_b[:], 1.0), writes=['ones_b'])

    def load_cast(dst_ap, src_ap, key):
        kb.dma('pool', lambda q: q.dma_start(out=dst_ap, in_=src_ap), writes=[key])

    def bcast_rows(dst, src_row_ap, key, n=128):
        kb.dma('sp', lambda q: q.dma_start(out=dst, in_=src_row_ap.partition_broadcast(n)), writes=[key])

    def setup_mem_kv(layer, st, kT, vpad, onespad):
        memf = sb(f"memf{layer}", [128, 2, D], F32, st)
        memb = sb(f"memb{layer}", [128, 2, D], BF16, st)
        memT = sb(f"memT{layer}", [128, 8, MEM], BF16, st)
        wkv = sb(f"wkv{layer}", [128, 8, 512], BF16, st)
        kb.dma('sp', lambda q: q.dma_start(out=memf[:], in_=mem_in.rearrange("(t p) d -> p t d", p=128)), writes=['memf'])
        load_cast(wkv[:], w_mem_kv[layer].rearrange("(k p) c -> p k c", p=128), 'wkv')
        kb.op('act', lambda a: a.copy(out=memb[:], in_=memf[:]), reads=['memf'], writes=['memb'])
        for k in range(8):
            pt, pk = psum()
            ptb = pt[:].bitcast(BF16)
            for t in range(2):
                kb.op('pe', lambda p, t=t, k=k, ptb=ptb: p.transpose(out=ptb[:, t * 128:(t + 1) * 128],
                      in_=memb[:, t, k * 128:(k + 1) * 128], identity=ident_b[:]),
                      reads=['memb', 'ident_b'], writes=[pk])
            kb.op('dve', lambda v, k=k, ptb=ptb: v.tensor_copy(out=memT[:, k, :], in_=ptb[:, 0:256]),
                  reads=[pk], writes=['memT'])
        for pr in range(2):
            pt, pk = psum()
            for k in range(8):
                kb.op('pe', lambda p, k=k, pr=pr, pt=pt: p.matmul(pt[:, 0:256], lhsT=wkv[:, k, pr * 128:(pr + 1) * 128],
                      rhs=memT[:, k, :], start=(k == 0), stop=(k == 7)), reads=['wkv', 'memT'], writes=[pk])
            kb.op('dve', lambda v, pr=pr, pt=pt: v.tensor_copy(out=kT[:, pr, :], in_=pt[:, 0:256]), reads=[pk], writes=['kT'])
        kb.op('pool', lambda g: g.memset(vpad[:], 0.0), writes=['vpad'])
        kb.op('pool', lambda g: g.memset(onespad[:], 0.0), writes=['onespad'])
        for par in range(2):
            kb.op('pool', lambda g, par=par: g.memset(onespad[:, par, par * 64:(par + 1) * 64], 1.0), writes=['onespad'])
        for mc in range(2):
            pt, pk = psum()
            for k in range(8):
                kb.op('pe', lambda p, k=k, mc=mc, pt=pt: p.matmul(pt[:, 0:256], lhsT=memT[:, k, mc * 128:(mc + 1) * 128],
                      rhs=wkv[:, k, 256:512], start=(k == 0), stop=(k == 7)), reads=['wkv', 'memT'], writes=[pk])
            for h in range(4):
                par = h % 2
                kb.op('dve', lambda v, h=h, mc=mc, par=par, pt=pt: v.tensor_copy(
                    out=vpad[:, h, mc, par * 64:(par + 1) * 64], in_=pt[:, h * 64:(h + 1) * 64]),
                    reads=[pk], writes=['vpad'])

    def mem_attn(mqT, T, kT, vpad, onespad, mixT_dst, E, rden, tag):
        for pr in range(2):
            for hh in range(2):
                h = 2 * pr + hh
                for mc in range(2):
                    pt, pk = psum()
                    kb.op('pe', lambda p, pt=pt, pr=pr, hh=hh, mc=mc: p.matmul(
                        pt[:, 0:T], lhsT=kT[hh * 64:(hh + 1) * 64, pr, mc * 128:(mc + 1) * 128],
                        rhs=mqT[hh * 64:(hh + 1) * 64, pr, :], start=True, stop=True),
                        reads=['kT', 'mqT' + tag], writes=[pk])
                    kb.op('act', lambda a, pt=pt, hh=hh, mc=mc: a.activation(
                        out=E[:, hh, mc, :], in_=pt[:, 0:T], func=AF.Exp, scale=0.125),
                        reads=[pk], writes=[('E' + tag, hh, mc)])
            po, pok = psum()
            pd, pdk = psum()
            n = 0
            for hh in range(2):
                h = 2 * pr + hh
                for mc in range(2):
                    kb.op('pe', lambda p, po=po, h=h, hh=hh, mc=mc, n=n: p.matmul(
                        po[:, 0:T], lhsT=vpad[:, h, mc, :], rhs=E[:, hh, mc, :], start=(n == 0), stop=(n == 3)),
                        reads=['vpad', ('E' + tag, hh, mc)], writes=[pok])
                    n += 1
            n = 0
            for hh in range(2):
                for mc in range(2):
                    kb.op('pe', lambda p, pd=pd, hh=hh, mc=mc, n=n: p.matmul(
                        pd[:, 0:T], lhsT=onespad[:, hh, :], rhs=E[:, hh, mc, :], start=(n == 0), stop=(n == 3)),
                        reads=['onespad', ('E' + tag, hh, mc)], writes=[pdk])
                    n += 1
            kb.op('dve', lambda v, pd=pd: v.reciprocal(out=rden[:, 0:T], in_=pd[:, 0:T]), reads=[pdk], writes=['rden' + tag])
            kb.op('dve', lambda v, po=po, pr=pr: v.tensor_tensor(out=mixT_dst(pr), in0=po[:, 0:T], in1=rden[:, 0:T], op=ALU.mult),
                  reads=[pok, 'rden' + tag], writes=[('mixT' + tag, 6 + pr)])

    class Tail:
        def __init__(self, layer, st, nbuf=2):
            self.layer = layer
            self.nbuf = nbuf
            L = layer
            self.wout = sb(f"wout{L}", [128, 8, D], BF16, st)
            load_cast(self.wout[:], w_out[L].rearrange("(k p) n -> p k n", p=128), 'wout')
            self.lng = sb(f"lng{L}", [128, D], F32, st)
            self.lnb = sb(f"lnb{L}", [128, D], F32, st)
            bcast_rows(self.lng[:], ln1_g[L], 'lng')
            bcast_rows(self.lnb[:], ln1_b[L], 'lnb')
            self.rw = sb(f"rw{L}", [128, 8, NE], F32, st)
            kb.dma('sp', lambda q: q.dma_start(out=self.rw[:], in_=router_w[L].rearrange("(k p) e -> p k e", p=128)), writes=['rw'])
            self.rb = sb(f"rb{L}", [128, NE], F32, st)
            bcast_rows(self.rb[:], router_b[L], 'rb')
            self.rrun = sb(f"rrun{L}", [128, NE], F32, st)
            kb.op('dve', lambda v: v.memset(self.rrun[:], 0.0), writes=['rrun'])
            self.z = [sb(f"z{L}_{i}", [128, D], F32, st) for i in range(nbuf)]
            self.x1f = [sb(f"x1f{L}_{i}", [128, D], F32, st) for i in range(nbuf)]
            self.x1b = [sb(f"x1b{L}_{i}", [128, D], BF16, st) for i in range(nbuf)]
            self.x1T = sb(f"x1T{L}", [128, 8, 128], F32, st)
            self.sm = [sb(f"sm{L}_{i}", [128, 256], F32, st) for i in range(nbuf)]
            self.smu = [sb(f"smu{L}_{i}", [128, 8], U32, st) for i in range(nbuf)]
            self.n = 0

        def run(self, *a):
            for _ in self.run_gen(*a):
                pass

        def run_gen(self, ti, mixT_ap, mix_keys, xres_ap, xres_key, x1dst):
            i = self.n % self.nbuf
            self.n += 1
            z, x1f, x1b, sm, smu = self.z[i], self.x1f[i], self.x1b[i], self.sm[i], self.smu[i]
            zk, x1fk, x1bk, smk = ('z', i), ('x1f', i), ('x1b', i), ('sm', i)
            for nh in range(2):
                pt, pk = psum()
                for k in range(8):
                    kb.op('pe', lambda p, pt=pt, k=k, nh=nh: p.matmul(pt[:, :], lhsT=mixT_ap(k), rhs=self.wout[:, k, nh * 512:(nh + 1) * 512],
                          start=(k == 0), stop=(k == 7)), reads=['wout'] + list(mix_keys), writes=[pk])
                kb.op('dve', lambda v, pt=pt, nh=nh: v.scalar_tensor_tensor(
                    out=z[:, nh * 512:(nh + 1) * 512], in0=xres_ap[:, nh * 512:(nh + 1) * 512], scalar=ALPHA,
                    in1=pt[:, :], op0=ALU.mult, op1=ALU.add), reads=[pk, xres_key], writes=[zk])
                yield
            self.layernorm(z, zk, x1f, x1fk, sm, smk, self.lng, self.lnb)
            kb.dma('sp', lambda q: q.dma_start(out=x1dst, in_=x1f[:]), reads=[x1fk], writes=[('x1d', ti)])
            yield
            kb.op('act', lambda a: a.copy(out=x1b[:], in_=x1f[:]), reads=[x1fk], writes=[x1bk])
            yield
            for half in range(2):
                pt, pk = psum()
                for kk in range(4):
                    k = half * 4 + kk
                    kb.op('pe', lambda p, pt=pt, k=k, kk=kk: p.transpose(out=pt[:, kk * 128:(kk + 1) * 128],
                          in_=x1f[:, k * 128:(k + 1) * 128], identity=ident_f[:]), reads=[x1fk, 'ident_f'], writes=[pk])
                kb.op('act', lambda a, pt=pt, half=half: a.copy(
                    out=self.x1T[:, half * 4:(half + 1) * 4, :].rearrange("p k t -> p (k t)"), in_=pt[:, :]),
                    reads=[pk], writes=['x1T'])
            yield
            pl, plk = psum()
            for k in range(8):
                kb.op('pe', lambda p, k=k: p.matmul(pl[:, 0:NE], lhsT=self.x1T[:, k, :], rhs=self.rw[:, k, :],
                      start=(k == 0), stop=(k == 7)), reads=['x1T', 'rw'], writes=[plk])
            lg = sm[:, 0:32]
            v8 = sm[:, 32:40]
            mask = sm[:, 40:72]
            slot = sm[:, 72:104]
            junk = sm[:, 104:136]
            idxf = sm[:, 136:144]
            destf = sm[:, 144:148]
            ev = sm[:, 148:152]
            nm = sm[:, 152:153]
            gsum = sm[:, 153:154]
            bad = sm[:, 160:192]
            kb.op('dve', lambda v: v.tensor_tensor(out=lg, in0=pl[:, 0:NE], in1=self.rb[:], op=ALU.add),
                  reads=[plk, 'rb'], writes=[smk])
            kb.op('dve', lambda v: v.max(out=v8, in_=lg), reads=[smk], writes=[smk])
            kb.op('dve', lambda v: v.max_index(out=smu[:], in_max=v8, in_values=lg), reads=[smk], writes=[('smu', i)])
            kb.op('dve', lambda v: v.tensor_scalar(out=mask, in0=lg, scalar1=v8[:, 3:4], scalar2=None, op0=ALU.is_ge),
                  reads=[smk], writes=[smk])
            yield
            pc, pck = psum()
            kb.op('pe', lambda p: p.matmul(pc[:, 0:NE], lhsT=tri_f[:], rhs=mask, start=True, stop=True),
                  reads=['tri_f', smk], writes=[pck])
            kb.op('pe', lambda p: p.matmul(pc[:, NE:2 * NE], lhsT=ones_f[:], rhs=mask, start=True, stop=True),
                  reads=['ones_f', smk], writes=[pck])
            kb.op('dve', lambda v: v.tensor_tensor(out=slot, in0=pc[:, 0:NE], in1=self.rrun[:], op=ALU.add),
                  reads=[pck, 'rrun'], writes=[smk])
            kb.op('dve', lambda v: v.tensor_tensor(out=self.rrun[:], in0=pc[:, NE:2 * NE], in1=self.rrun[:], op=ALU.add),
                  reads=[pck, 'rrun'], writes=['rrun'])
            kb.op('dve', lambda v: v.tensor_scalar(out=bad, in0=slot, scalar1=float(CAP), scalar2=BIGOOB, op0=ALU.is_ge, op1=ALU.mult),
                  reads=[smk], writes=[smk])
            kb.op('dve', lambda v: v.tensor_tensor(out=slot, in0=slot, in1=bad, op=ALU.add), reads=[smk], writes=[smk])
            kb.op('dve', lambda v: v.tensor_tensor(out=slot, in0=slot, in1=ecap[:], op=ALU.add), reads=[smk, 'ecap'], writes=[smk])
            yield
            kb.op('dve', lambda v: v.tensor_copy(out=idxf, in_=smu[:]), reads=[('smu', i)], writes=[smk])
            for k in range(4):
                kb.op('dve', lambda v, k=k: v.scalar_tensor_tensor(out=junk, in0=iota_e[:], scalar=idxf[:, k:k + 1], in1=slot,
                      op0=ALU.is_equal, op1=ALU.mult, accum_out=destf[:, k:k + 1]), reads=[smk, 'iota_e'], writes=[smk])
            kb.op('dve', lambda v: v.tensor_copy(out=destall[:, ti, :], in_=destf), reads=[smk], writes=[('dest', ti)])
            kb.op('dve', lambda v: v.tensor_scalar(out=nm, in0=v8[:, 0:1], scalar1=-1.0, scalar2=None, op0=ALU.mult),
                  reads=[smk], writes=[smk])
            kb.op('act', lambda a: a.activation(out=ev, in_=v8[:, 0:4], func=AF.Exp, bias=nm, scale=1.0, accum_out=gsum),
                  reads=[smk], writes=[smk])
            yield
            kb.op('dve', lambda v: v.reciprocal(out=gsum, in_=gsum), reads=[smk], writes=[smk])
            kb.op('dve', lambda v: v.tensor_scalar(out=gall[:, ti, :], in0=ev, scalar1=gsum, scalar2=None, op0=ALU.mult),
                  reads=[smk], writes=[('gate', ti)])
            for k in range(4):
                kb.dma('pool', lambda q, k=k: q.indirect_dma_start(
                    out=xg[:, :], out_offset=bass.IndirectOffsetOnAxis(ap=destall[:, ti, k:k + 1], axis=0),
                    in_=x1b[:, :], in_offset=None, bounds_check=bc_reg, oob_is_err=False),
                    reads=[x1bk, ('dest', ti)], writes=['xg'])

        def layernorm(self, z, zk, o, ok, sm, smk, g, b, gk='lng', bk='lnb'):
            st6 = sm[:, 200:212]
            mv = sm[:, 212:214]
            rstd = sm[:, 214:215]
            for c in range(2):
                kb.op('dve', lambda v, c=c: v.bn_stats(out=st6[:, c * 6:(c + 1) * 6], in_=z[:, c * 512:(c + 1) * 512]),
                      reads=[zk], writes=[smk])
            kb.op('dve', lambda v: v.bn_aggr(out=mv, in_=st6), reads=[smk], writes=[smk])
            kb.op('act', lambda a: a.activation(out=rstd, in_=mv[:, 1:2], func=AF.Sqrt, bias=1e-5, scale=1.0), reads=[smk], writes=[smk])
            kb.op('dve', lambda v: v.reciprocal(out=rstd, in_=rstd), reads=[smk], writes=[smk])
            kb.op('dve', lambda v: v.tensor_scalar(out=o[:], in0=z[:], scalar1=mv[:, 0:1], scalar2=rstd, op0=ALU.subtract, op1=ALU.mult),
                  reads=[zk, smk], writes=[ok])
            kb.op('pool', lambda p: p.tensor_tensor(out=o[:], in0=o[:], in1=g[:], op=ALU.mult), reads=[ok, gk], writes=[ok])
            kb.op('pool', lambda p: p.tensor_tensor(out=o[:], in0=o[:], in1=b[:], op=ALU.add), reads=[ok, bk], writes=[ok])

    def phase_a0(ntiles=NT):
        T = 256
        st = ExitStack()
        tail = Tail(0, st)
        kT = sb("kT0", [128, 2, MEM], BF16, st)
        vpad = sb("vpad0", [128, 4, 2, 128], BF16, st)
        onespad = sb("onespad0", [128, 2, 128], BF16, st)
        setup_mem_kv(0, st, kT, vpad, onespad)
        win = sb("win0", [128, 8, W_IN_A], BF16, st)
        load_cast(win[:], a_w_in.rearrange("(k p) c -> p k c", p=128), 'win')
        wr_bd = sb("wr_bd", [128, 6, 128], BF16, st)
        wi_bd = sb("wi_bd", [128, 6, 128], BF16, st)
        kb.op('pool', lambda g: g.memset(wr_bd[:], 0.0), writes=['wr_bd'])
        kb.op('pool', lambda g: g.memset(wi_bd[:], 0.0), writes=['wi_bd'])
        for n in range(12):
            j, par = n // 2, n % 2
            load_cast(wr_bd[par * 64:(par + 1) * 64, j, par * 64:(par + 1) * 64], a_wr[n], 'wr_bd')
            load_cast(wi_bd[par * 64:(par + 1) * 64, j, par * 64:(par + 1) * 64], a_wi[n], 'wi_bd')
        cw = sb("cw", [128, 4, 6], F32, st)
        vecs = sb("vecs", [128, 4, 6], F32, st)
        with nc.allow_non_contiguous_dma(reason="tiny per-channel vectors"):
            kb.dma('sp', lambda q: q.dma_start(out=cw[:], in_=a_conv_w.rearrange("w (j p) -> p w j", p=128)), writes=['cw'])
            for n, v_ in enumerate((a_conv_b, a_br, a_bi, a_lambda)):
                kb.dma('sp', lambda q, n=n, v_=v_: q.dma_start(out=vecs[:, n, :], in_=v_.rearrange("(j p) -> p j", p=128)), writes=['vecs'])
        coef = sb("coef", [128, 6], F32, st)
        kb.op('act', lambda a: a.activation(out=coef[:], in_=vecs[:, 3, :], func=AF.Exp, scale=-1.0), reads=['vecs'], writes=['coef'])
        kb.op('act', lambda a: a.activation(out=coef[:], in_=coef[:], func=AF.Ln, bias=1.0, scale=1.0), reads=['coef'], writes=['coef'])
        kb.op('dve', lambda v: v.tensor_scalar(out=coef[:], in0=coef[:], scalar1=-8.0, scalar2=None, op0=ALU.mult), reads=['coef'], writes=['coef'])

        xf = [sb(f"xf{i}", [128, 2, D], F32, st) for i in range(2)]
        xbf = sb("xbf", [128, 2, D], BF16, st)
        xT = sb("xT", [128, 8, T], BF16, st)
        xbh = sb("xbh", [128, 6, 3 + T], F32, st)
        gbT = sb("gbT", [128, 6, T], F32, st)
        mqT = sb("mqT", [128, 2, T], BF16, st)
        mixT = [sb(f"mixT{i}", [128, 8, T], BF16, st) for i in range(2)]
        E = sb("E0", [128, 2, 2, T], BF16, st)
        rden = sb("rden0", [128, T], F32, st)
        NTMP = 10
        tmp = [[sb(f"tmp{n}_{i}", [128, T], F32, st) for i in range(2)] for n in range(NTMP)]
        xcb = [sb(f"xcb{i}", [128, T], BF16, st) for i in range(2)]
        hbuf = [sb(f"hbuf{i}", [128, 6, T], F32, st) for i in range(2)]
        kb.op('dve', lambda v: v.memset(xbh[:], 0.0), writes=['xbh'])
        zt = sb("zt", [128, 4096], BF16, st)
        kb.op('pool', lambda g: g.memset(zt[:], 0.0), writes=['zt'])
        for r0 in range(0, NROW, 512):
            kb.dma('sp', lambda q, r0=r0: q.dma_start(out=xg[r0:r0 + 512, :].rearrange("(p t) d -> p (t d)", t=4), in_=zt[:]),
                   reads=['zt'], writes=['xg'])

        nch = ntiles * 128 // T

        def mixer(ci):
            xi = ci % 2
            xfc, xfk = xf[xi], ('xf', xi)
            kb.dma('sp', lambda q, ci=ci, xfc=xfc: q.dma_start(
                out=xfc[:], in_=x_in[ci * T:(ci + 1) * T, :].rearrange("(t p) d -> p t d", p=128)), writes=[xfk])
            kb.op('act', lambda a, xfc=xfc: a.copy(out=xbf[:], in_=xfc[:]), reads=[xfk], writes=['xbf'])
            for k in range(8):
                pt, pk = psum()
                ptb = pt[:].bitcast(BF16)
                for t in range(2):
                    kb.op('pe', lambda p, ptb=ptb, t=t, k=k: p.transpose(out=ptb[:, t * 128:(t + 1) * 128],
                          in_=xbf[:, t, k * 128:(k + 1) * 128], identity=ident_b[:]), reads=['xbf', 'ident_b'], writes=[pk])
                kb.op('dve' if k % 2 == 0 else 'act',
                      (lambda v, ptb=ptb, k=k: v.tensor_copy(out=xT[:, k, :], in_=ptb[:, 0:T])) if k % 2 == 0 else
                      (lambda a, ptb=ptb, k=k: a.copy(out=xT[:, k, :], in_=ptb[:, 0:T])),
                      reads=[pk], writes=[('xT', k)])
            yield
            xTk = [('xT', k) for k in range(8)]
            for c in range(14):
                pt, pk = psum()
                for k in range(8):
                    kb.op('pe', lambda p, pt=pt, k=k, c=c: p.matmul(pt[:, 0:T], lhsT=win[:, k, c * 128:(c + 1) * 128],
                          rhs=xT[:, k, :], start=(k == 0), stop=(k == 7)), reads=['win'] + xTk, writes=[pk])
                if c < 6:
                    kb.op('act', lambda a, pt=pt, c=c: a.copy(out=xbh[:, c, 3:3 + T], in_=pt[:, 0:T]), reads=[pk], writes=[('xbh', c)])
                elif c < 12:
                    kb.op('act', lambda a, pt=pt, c=c: a.copy(out=gbT[:, c - 6, :], in_=pt[:, 0:T]), reads=[pk], writes=[('gbT', c - 6)])
                else:
                    kb.op('dve', lambda v, pt=pt, c=c: v.tensor_copy(out=mqT[:, c - 12, :], in_=pt[:, 0:T]), reads=[pk], writes=['mqT0'])
                if c % 2 == 1:
                    yield
            mx = mixT[ci % 2]
            hb = hbuf[ci % 2]
            hprev = hbuf[(ci + 1) % 2]
            for c in range(6):
                r_ = c % 2
                xc, rr, ii, aa, ss, uu, sq, t2, sg, gl = [tmp[n][r_] for n in range(NTMP)]
                tk = [(f'tmp{n}', r_) for n in range(NTMP)]
                xck = tk[0]
                kb.op('dve', lambda v, c=c, xc=xc: v.tensor_scalar(out=xc[:], in0=xbh[:, c, 3:3 + T], scalar1=cw[:, 3, c:c + 1],
                      scalar2=vecs[:, 0, c:c + 1], op0=ALU.mult, op1=ALU.add), reads=[('xbh', c), 'cw', 'vecs'], writes=[xck])
                for j in range(3):
                    kb.op('dve', lambda v, c=c, j=j, xc=xc: v.scalar_tensor_tensor(out=xc[:], in0=xbh[:, c, j:j + T], scalar=cw[:, j, c:c + 1],
                          in1=xc[:], op0=ALU.mult, op1=ALU.add), reads=[('xbh', c), 'cw', xck], writes=[xck])
                kb.op('pool', lambda g, c=c: g.tensor_copy(out=xbh[:, c, 0:3], in_=xbh[:, c, T:T + 3]), reads=[('xbh', c)], writes=[('xbh', c)])
                kb.op('pool', lambda g, xc=xc, r_=r_: g.tensor_copy(out=xcb[r_][:], in_=xc[:]), reads=[xck], writes=[('xcb', r_)])
                pr_, prk = psum()
                kb.op('pe', lambda p, pr_=pr_, c=c, r_=r_: p.matmul(pr_[:, 0:T], lhsT=wr_bd[:, c, :], rhs=xcb[r_][:], start=True, stop=True),
                      reads=['wr_bd', ('xcb', r_)], writes=[prk])
                pi_, pik = psum()
                kb.op('pe', lambda p, pi_=pi_, c=c, r_=r_: p.matmul(pi_[:, 0:T], lhsT=wi_bd[:, c, :], rhs=xcb[r_][:], start=True, stop=True),
                      reads=['wi_bd', ('xcb', r_)], writes=[pik])
                kb.op('act', lambda a, pr_=pr_, c=c, rr=rr: a.activation(out=rr[:], in_=pr_[:, 0:T], func=AF.Sigmoid, bias=vecs[:, 1, c:c + 1], scale=1.0),
                      reads=[prk, 'vecs'], writes=[tk[1]])
                kb.op('act', lambda a, pi_=pi_, c=c, ii=ii: a.activation(out=ii[:], in_=pi_[:, 0:T], func=AF.Sigmoid, bias=vecs[:, 2, c:c + 1], scale=1.0),
                      reads=[pik, 'vecs'], writes=[tk[2]])
                yield
                gb = gbT[:, c, :]
                kb.op('pool', lambda g, gb=gb, sq=sq: g.tensor_tensor(out=sq[:], in0=gb, in1=gb, op=ALU.mult), reads=[('gbT', c)], writes=[tk[6]])
                kb.op('pool', lambda g, sq=sq: g.tensor_scalar(out=sq[:], in0=sq[:], scalar1=0.044715, scalar2=1.0, op0=ALU.mult, op1=ALU.add),
                      reads=[tk[6]], writes=[tk[6]])
                kb.op('pool', lambda g, gb=gb, sq=sq, t2=t2: g.tensor_tensor(out=t2[:], in0=sq[:], in1=gb, op=ALU.mult), reads=[tk[6], ('gbT', c)], writes=[tk[7]])
                kb.op('act', lambda a, t2=t2, sg=sg: a.activation(out=sg[:], in_=t2[:], func=AF.Sigmoid, scale=1.5957691216057308),
                      reads=[tk[7]], writes=[tk[8]])
                kb.op('act', lambda a, aa=aa, rr=rr, c=c: a.activation(out=aa[:], in_=rr[:], func=AF.Exp, scale=coef[:, c:c + 1]),
                      reads=[tk[1], 'coef'], writes=[tk[3]])
                kb.op('pool', lambda g, aa=aa, ss=ss: g.tensor_tensor(out=ss[:], in0=aa[:], in1=aa[:], op=ALU.mult), reads=[tk[3]], writes=[tk[4]])
                kb.op('act', lambda a, ss=ss: a.activation(out=ss[:], in_=ss[:], func=AF.Sqrt, bias=1.0, scale=-1.0), reads=[tk[4]], writes=[tk[4]])
                kb.op('pool', lambda g, uu=uu, ss=ss, ii=ii: g.tensor_tensor(out=uu[:], in0=ss[:], in1=ii[:], op=ALU.mult), reads=[tk[4], tk[2]], writes=[tk[5]])
                kb.op('dve', lambda v, uu=uu, xc=xc: v.tensor_tensor(out=uu[:], in0=uu[:], in1=xc[:], op=ALU.mult), reads=[tk[5], xck], writes=[tk[5]])
                init = 0.0 if ci == 0 else hprev[:, c, T - 1:T]
                kb.op('dve', lambda v, aa=aa, uu=uu, c=c, init=init, hb=hb: v.tensor_tensor_scan(out=hb[:, c, :], data0=aa[:], data1=uu[:],
                      initial=init, op0=ALU.mult, op1=ALU.add), reads=[tk[3], tk[5], ('h', (ci + 1) % 2, c)], writes=[('h', ci % 2, c)])
                kb.op('pool', lambda g, gl=gl, sg=sg, gb=gb: g.tensor_tensor(out=gl[:], in0=sg[:], in1=gb, op=ALU.mult), reads=[tk[8], ('gbT', c)], writes=[tk[9]])
                kb.op('dve', lambda v, gl=gl, c=c, hb=hb, mx=mx: v.tensor_tensor(out=mx[:, c, :], in0=hb[:, c, :], in1=gl[:], op=ALU.mult),
                      reads=[tk[9], ('h', ci % 2, c)], writes=[('mixT0', c)])
                yield
            mem_attn(mqT, T, kT, vpad, onespad, lambda pr, mx=mx: mx[:, 6 + pr, :], E, rden, '0')
            yield

        mixkeys = [('mixT0', c) for c in range(8)]

        def tails(ci):
            mx = mixT[ci % 2]
            xfc, xfk = xf[ci % 2], ('xf', ci % 2)
            for t in range(T // 128):
                ti = ci * (T // 128) + t
                yield from tail.run_gen(ti, lambda k, mx=mx, t=t: mx[:, k, t * 128:(t + 1) * 128], mixkeys, xfc[:, t, :], xfk,
                                        x1buf[ti * 128:(ti + 1) * 128, :])

        for _ in mixer(0):
            pass
        for ci in range(nch):
            streams = [(tails(ci), 18)]
            if ci + 1 < nch:
                streams.append((mixer(ci + 1), 25))
            interleave(streams)
        kb.barrier()
        if mode.startswith("a0"):
            dbg_r = nc.dram_tensor("dbg_r", [128, NE], F32, kind="ExternalOutput").ap()
            dbg_d = nc.dram_tensor("dbg_d", [128, NT * 4], I32, kind="ExternalOutput").ap()
            kb.dma('sp', lambda q: q.dma_start(out=dbg_r[:, :], in_=tail.rrun[:]))
            kb.dma('sp', lambda q: q.dma_start(out=dbg_d[:, :], in_=destall[:].rearrange("p t k -> p (t k)")))
            kb.barrier()
        st.close()


    def phase_a1(src, ntiles=NT):
        T = 256
        KSEL = 256
        NBIS = 14
        CH = 512
        st = ExitStack()
        tail = Tail(1, st, nbuf=1)
        cTok = sb("cTok", [128, NT, 128], BF16, st)
        cT = sb("cT", [128, S], BF16, st)
        ikT2 = sb("ikT2", [128, S], BF16, st)
        absw = sb("absw", [128, NT, 4], F32, st)
        sgnw = sb("sgnw", [128, NT, 4], F32, st)
        BT = sb("BT", [128, 2, 12, 128], BF16, st)
        wuvpad = sb("wuvpad", [128, 12, 128], BF16, st)
        i4big = sb("i4big", [128, 4, 128], BF16, st)
        for r in range(4):
            kb.op('dve', lambda v, r=r: v.tensor_scalar(out=i4big[:, r, :], in0=ident_f[:], scalar1=100.0, scalar2=None, op0=ALU.mult),
                  reads=['ident_f'], writes=['i4big'])
        kb.op('pool', lambda g: g.memset(wuvpad[:], 0.0), writes=['wuvpad'])
        for h in range(12):
            par = h % 2
            load_cast(wuvpad[:, h, par * 64:(par + 1) * 64], b_w_uv[:, h, :], 'wuvpad')
        s1 = ExitStack()
        kT = sb("kT1", [128, 2, MEM], BF16, s1)
        vpad = sb("vpad1", [128, 4, 2, 128], BF16, s1)
        onespad = sb("onespad1", [128, 2, 128], BF16, s1)
        win = sb("win1", [128, 8, W_IN_B], BF16, s1)
        load_cast(win[:], b_w_in.rearrange("(k p) c -> p k c", p=128), 'win')
        wukT = sb("wukT", [128, 6, 128], BF16, s1)
        gkv = sb("gkv", [128, 128], F32, s1)
        gik = sb("gik", [128, 64], F32, s1)
        bik = sb("bik", [128, 64], F32, s1)
        bcast_rows(gkv[:], b_kv_norm_g, 'gkv')
        bcast_rows(gik[:], b_idx_norm_g, 'gik')
        bcast_rows(bik[:], b_idx_norm_b, 'bik')
        ssetup = ExitStack()
        setup_mem_kv(1, ssetup, kT, vpad, onespad)
        wuk = sb("wuk", [128, 768], BF16, ssetup)
        load_cast(wuk[:], b_w_uk.rearrange("r h d -> r (h d)"), 'wuk')
        for j in range(6):
            pt, pk = psum()
            ptb = pt[:].bitcast(BF16)
            kb.op('pe', lambda p, ptb=ptb, j=j: p.transpose(out=ptb[:, 0:128], in_=wuk[:, j * 128:(j + 1) * 128], identity=ident_b[:]),
                  reads=['wuk', 'ident_b'], writes=[pk])
            kb.op('dve', lambda v, ptb=ptb, j=j: v.tensor_copy(out=wukT[:, j, :], in_=ptb[:, 0:128]), reads=[pk], writes=['wukT'])
        rbb = sb("rbb", [128, 32, 12], F32, ssetup)
        bkt = sb("bkt", [128, 2, 128], F32, ssetup)
        caus = sb("caus", [128, 128], F32, ssetup)
        acc = sb("bacc", [128, 12, 128], F32, ssetup)
        prod = sb("bprod", [128, 12, 128], F32, ssetup)
        oh = sb("boh", [128, 128], F32, ssetup)
        kb.dma('sp', lambda q: q.dma_start(out=rbb[:].rearrange("p b h -> p (b h)"), in_=rel_bias.rearrange("b h -> (b h)").partition_broadcast(128)), writes=['rbb'])
        kb.dma('sp', lambda q: q.dma_start(out=bkt[:], in_=c_bkt[:, :, :]), writes=['bkt'])
        kb.dma('sp', lambda q: q.dma_start(out=caus[:], in_=c_caus[:, :]), writes=['caus'])
        for dt in range(2):
            kb.op('dve', lambda v: v.memset(acc[:], 0.0), writes=['bacc'])
            for b in range(32):
                kb.op('dve', lambda v, b=b, dt=dt: v.tensor_scalar(out=oh[:], in0=bkt[:, dt, :], scalar1=float(b), scalar2=None, op0=ALU.is_equal),
                      reads=['bkt'], writes=['boh'])
                kb.op('dve', lambda v, b=b: v.tensor_tensor(out=prod[:], in0=oh[:].unsqueeze(1).to_broadcast([128, 12, 128]),
                      in1=rbb[:, b, :].unsqueeze(2).to_broadcast([128, 12, 128]), op=ALU.mult), reads=['boh', 'rbb'], writes=['bprod'])
                kb.op('dve', lambda v: v.tensor_tensor(out=acc[:], in0=acc[:], in1=prod[:], op=ALU.add), reads=['bacc', 'bprod'], writes=['bacc'])
            kb.op('dve', lambda v: v.tensor_tensor(out=acc[:], in0=acc[:], in1=rbb[:, 31, :].unsqueeze(2).to_broadcast([128, 12, 128]), op=ALU.subtract),
                  reads=['bacc', 'rbb'], writes=['bacc'])
            if dt == 0:
                kb.op('dve', lambda v: v.tensor_tensor(out=acc[:], in0=acc[:], in1=caus[:].unsqueeze(1).to_broadcast([128, 12, 128]), op=ALU.add),
                      reads=['bacc', 'caus'], writes=['bacc'])
            kb.op('dve', lambda v, dt=dt: v.tensor_copy(out=BT[:, dt, :, :], in_=acc[:]), reads=['bacc'], writes=['BT'])
        kb.barrier()
        ssetup.close()

        xf = [sb(f"xf1_{i}", [128, 2, D], F32, s1) for i in range(2)]
        xbf = sb("xbf1", [128, 2, D], BF16, s1)
        xT = sb("xT1", [128, 8, T], BF16, s1)
        qT = sb("qT1", [128, 6, T], BF16, s1)
        qlb = [sb(f"qlb{i}", [128, 12, T], BF16, s1) for i in range(2)]
        iqT = [sb(f"iqT{i}", [128, 2, T], BF16, s1) for i in range(2)]
        mqT = sb("mqT1", [128, 2, T], BF16, s1)
        memo = [sb(f"memo{i}", [128, 2, T], BF16, s1) for i in range(2)]
        E = sb("E1", [128, 2, 2, T], BF16, s1)
        rden = sb("rden1", [128, T], F32, s1)
        csb = [sb(f"csb{i}", [128, 128], F32, s1) for i in range(2)]
        cnb = [sb(f"cnb{i}", [128, 128], BF16, s1) for i in range(2)]
        iks = [sb(f"iks{i}", [128, 68], F32, s1) for i in range(2)]
        ik2 = [sb(f"ik2{i}", [128, 128], BF16, s1) for i in range(2)]
        sm1 = [sb(f"smp1_{i}", [128, 32], F32, s1) for i in range(2)]
        nch = ntiles * 128 // T
        for ci in range(nch):
            xi = ci % 2
            xfc, xfk = xf[xi], ('xf', xi)
            kb.dma('sp', lambda q, ci=ci, xfc=xfc: q.dma_start(
                out=xfc[:], in_=src[ci * T:(ci + 1) * T, :].rearrange("(t p) d -> p t d", p=128)), writes=[xfk])
            kb.op('act', lambda a, xfc=xfc: a.copy(out=xbf[:], in_=xfc[:]), reads=[xfk], writes=['xbf'])
            for k in range(8):
                pt, pk = psum()
                ptb = pt[:].bitcast(BF16)
                for t in range(2):
                    kb.op('pe', lambda p, ptb=ptb, t=t, k=k: p.transpose(out=ptb[:, t * 128:(t + 1) * 128],
                          in_=xbf[:, t, k * 128:(k + 1) * 128], identity=ident_b[:]), reads=['xbf', 'ident_b'], writes=[pk])
                if k % 2 == 0:
                    kb.op('dve', lambda v, ptb=ptb, k=k: v.tensor_copy(out=xT[:, k, :], in_=ptb[:, 0:T]), reads=[pk], writes=[('xT', k)])
                else:
                    kb.op('act', lambda a, ptb=ptb, k=k: a.copy(out=xT[:, k, :], in_=ptb[:, 0:T]), reads=[pk], writes=[('xT', k)])
            xTk = [('xT', k) for k in range(8)]

            def fm_proj(col0, dst_ap, dkey, eng):
                pt, pk = psum()
                for k in range(8):
                    kb.op('pe', lambda p, pt=pt, k=k: p.matmul(pt[:, 0:T], lhsT=win[:, k, col0:col0 + 128], rhs=xT[:, k, :],
                          start=(k == 0), stop=(k == 7)), reads=['win'] + xTk, writes=[pk])
                if eng == 'act':
                    kb.op('act', lambda a, pt=pt: a.copy(out=dst_ap, in_=pt[:, 0:T]), reads=[pk], writes=[dkey])
                else:
                    kb.op('dve', lambda v, pt=pt: v.tensor_copy(out=dst_ap, in_=pt[:, 0:T]), reads=[pk], writes=[dkey])

            for c in range(6):
                fm_proj(c * 128, qT[:, c, :], ('qT', c), 'act' if c % 2 else 'dve')
            bi = ci % 2
            for j in range(2):
                fm_proj(896 + j * 128, iqT[bi][:, j, :], ('iqT', bi), 'act')
            for j in range(2):
                fm_proj(1220 + j * 128, mqT[:, j, :], 'mqT1', 'dve')
            kb.dma('sp', lambda q, ci=ci, bi=bi: q.dma_start(out=iqd[:, :, ci * T:(ci + 1) * T].rearrange("j p t -> p j t"), in_=iqT[bi][:]),
                   reads=[('iqT', bi)], writes=['iqd'])
            for h in range(12):
                j, hh = h // 2, h % 2
                pt, pk = psum()
                kb.op('pe', lambda p, pt=pt, j=j, hh=hh: p.matmul(pt[:, 0:T], lhsT=wukT[hh * 64:(hh + 1) * 64, j, :],
                      rhs=qT[hh * 64:(hh + 1) * 64, j, :], start=True, stop=True), reads=['wukT', ('qT', j)], writes=[pk])
                if h % 2 == 0:
                    kb.op('act', lambda a, pt=pt, h=h, bi=bi: a.activation(out=qlb[bi][:, h, :], in_=pt[:, 0:T], func=AF.Copy, scale=0.125),
                          reads=[pk], writes=[('qlb', bi)])
                else:
                    kb.op('dve', lambda v, pt=pt, h=h, bi=bi: v.tensor_scalar(out=qlb[bi][:, h, :], in0=pt[:, 0:T], scalar1=0.125, scalar2=None, op0=ALU.mult),
                          reads=[pk], writes=[('qlb', bi)])
            kb.dma('sp', lambda q, ci=ci, bi=bi: q.dma_start(out=qlat[:, :, ci * T:(ci + 1) * T].rearrange("h p t -> p h t"), in_=qlb[bi][:]),
                   reads=[('qlb', bi)], writes=['qlat'])
            mem_attn(mqT, T, kT, vpad, onespad, lambda pr, bi=bi: memo[bi][:, pr, :], E, rden, '1')
            kb.dma('sp', lambda q, ci=ci, bi=bi: q.dma_start(out=memod[:, :, ci * T:(ci + 1) * T].rearrange("j p t -> p j t"), in_=memo[bi][:]),
                   reads=[('mixT1', 6), ('mixT1', 7)], writes=['memod'])
            for t in range(T // 128):
                ti = ci * (T // 128) + t
                i2 = ti % 2
                smk = ('sm1', i2)
                sm = sm1[i2]
                pc, pck = psum()
                for k in range(8):
                    kb.op('pe', lambda p, pc=pc, k=k, t=t: p.matmul(pc[:, 0:128], lhsT=xT[:, k, t * 128:(t + 1) * 128], rhs=win[:, k, 768:896],
                          start=(k == 0), stop=(k == 7)), reads=['win'] + xTk, writes=[pck])
                pi_, pik = psum()
                for k in range(8):
                    kb.op('pe', lambda p, pi_=pi_, k=k, t=t: p.matmul(pi_[:, 0:68], lhsT=xT[:, k, t * 128:(t + 1) * 128], rhs=win[:, k, 1152:1220],
                          start=(k == 0), stop=(k == 7)), reads=['win'] + xTk, writes=[pik])
                ss = sm[:, 0:1]
                kb.op('act', lambda a, pc=pc, i2=i2, ss=ss: a.activation(out=csb[i2][:], in_=pc[:, 0:128], func=AF.Square, accum_out=ss),
                      reads=[pck], writes=[('csb', i2), smk])
                kb.op('act', lambda a, ss=ss: a.activation(out=ss, in_=ss, func=AF.Sqrt, bias=1e-6, scale=1.0 / 128.0), reads=[smk], writes=[smk])
                kb.op('dve', lambda v, ss=ss: v.reciprocal(out=ss, in_=ss), reads=[smk], writes=[smk])
                kb.op('dve', lambda v, pc=pc, i2=i2, ss=ss: v.scalar_tensor_tensor(out=csb[i2][:], in0=pc[:, 0:128], scalar=ss, in1=gkv[:],
                      op0=ALU.mult, op1=ALU.mult), reads=[pck, smk, 'gkv', ('csb', i2)], writes=[('csb', i2)])
                kb.op('act', lambda a, i2=i2, ti=ti: a.copy(out=cTok[:, ti, :], in_=csb[i2][:]), reads=[('csb', i2)], writes=[('cTok', ti)])
                pt, pk = psum()
                ptb = pt[:].bitcast(BF16)
                kb.op('pe', lambda p, ptb=ptb, ti=ti: p.transpose(out=ptb[:, 0:128], in_=cTok[:, ti, :], identity=ident_b[:]),
                      reads=[('cTok', ti), 'ident_b'], writes=[pk])
                kb.op('dve', lambda v, ptb=ptb, ti=ti: v.tensor_copy(out=cT[:, ti * 128:(ti + 1) * 128], in_=ptb[:, 0:128]), reads=[pk], writes=[('cT', ti)])
                kb.op('act', lambda a, pi_=pi_, i2=i2: a.copy(out=iks[i2][:], in_=pi_[:, 0:68]), reads=[pik], writes=[('iks', i2)])
                st6 = sm[:, 8:14]
                mv = sm[:, 14:16]
                rs = sm[:, 16:17]
                kb.op('dve', lambda v, i2=i2, st6=st6: v.bn_stats(out=st6, in_=iks[i2][:, 0:64]), reads=[('iks', i2)], writes=[smk])
                kb.op('dve', lambda v, st6=st6, mv=mv: v.bn_aggr(out=mv, in_=st6), reads=[smk], writes=[smk])
                kb.op('act', lambda a, mv=mv, rs=rs: a.activation(out=rs, in_=mv[:, 1:2], func=AF.Sqrt, bias=1e-5, scale=1.0), reads=[smk], writes=[smk])
                kb.op('dve', lambda v, rs=rs: v.reciprocal(out=rs, in_=rs), reads=[smk], writes=[smk])
                kb.op('dve', lambda v, i2=i2, mv=mv, rs=rs: v.tensor_scalar(out=iks[i2][:, 0:64], in0=iks[i2][:, 0:64], scalar1=mv[:, 0:1], scalar2=rs,
                      op0=ALU.subtract, op1=ALU.mult), reads=[('iks', i2), smk], writes=[('iks', i2)])
                kb.op('dve', lambda v, i2=i2: v.tensor_tensor(out=iks[i2][:, 0:64], in0=iks[i2][:, 0:64], in1=gik[:], op=ALU.mult),
                      reads=[('iks', i2), 'gik'], writes=[('iks', i2)])
                for r in range(2):
                    kb.op('dve', lambda v, i2=i2, r=r: v.tensor_tensor(out=ik2[i2][:, r * 64:(r + 1) * 64], in0=iks[i2][:, 0:64], in1=bik[:], op=ALU.add),
                          reads=[('iks', i2), 'bik'], writes=[('ik2', i2)])
                pt, pk = psum()
                ptb = pt[:].bitcast(BF16)
                kb.op('pe', lambda p, ptb=ptb, i2=i2: p.transpose(out=ptb[:, 0:128], in_=ik2[i2][:], identity=ident_b[:]),
                      reads=[('ik2', i2), 'ident_b'], writes=[pk])
                kb.op('act', lambda a, ptb=ptb, ti=ti: a.copy(out=ikT2[:, ti * 128:(ti + 1) * 128], in_=ptb[:, 0:128]), reads=[pk], writes=[('ikT2', ti)])
                kb.op('act', lambda a, i2=i2, ti=ti: a.activation(out=absw[:, ti, :], in_=iks[i2][:, 64:68], func=AF.Abs),
                      reads=[('iks', i2)], writes=[('absw', ti)])
                kb.op('dve', lambda v, i2=i2, ti=ti: v.tensor_scalar(out=sgnw[:, ti, :], in0=iks[i2][:, 64:68], scalar1=0.0, scalar2=2.0, op0=ALU.is_ge, op1=ALU.mult),
                      reads=[('iks', i2)], writes=[('sgnw', ti)])
                kb.op('dve', lambda v, ti=ti: v.tensor_scalar(out=sgnw[:, ti, :], in0=sgnw[:, ti, :], scalar1=-1.0, scalar2=None, op0=ALU.add),
                      reads=[('sgnw', ti)], writes=[('sgnw', ti)])
        kb.barrier()
        s1.close()

        s2 = ExitStack()
        sc = sb("sc", [128, S], F32, s2)
        junk = sb("junk", [128, S // 2 + 128], mybir.dt.uint8, s2)
        junk2 = sb("junk2", [128, S], mybir.dt.uint8, s2) if False else None
        maskb = sb("maskb", [128, S], BF16, s2)
        tiec = [sb(f"tiec{i}", [128, CH], BF16, s2) for i in range(2)]
        cumc = [sb(f"cumc{i}", [128, CH], F32, s2) for i in range(2)]
        onesc = sb("onesc", [128, CH], BF16, s2)
        kb.op('pool', lambda g: g.memset(onesc[:], 1.0), writes=['onesc'])
        cmask = sb("cmask", [128, 128], F32, s2)
        kb.dma('sp', lambda q: q.dma_start(out=cmask[:], in_=c_cmask[:, :]), writes=['cmask'])
        rl = [sb(f"rl{i}", [128, 512], F32, s2) for i in range(4)]
        ql = sb("ql", [128, 12, 128], BF16, s2)
        iqb = [sb(f"iqb{i}", [128, 2, 128], BF16, s2) for i in range(2)]
        mixT = [sb(f"mixT1_{i}", [128, 8, 128], BF16, s2) for i in range(2)]
        x2t = sb("x2t", [128, D], F32, s2)
        Pt = [sb(f"Pt{i}", [128, 512], BF16, s2) for i in range(3)]
        olat = [sb(f"olat{i}", [128, 512], BF16, s2) for i in range(2)]
        Dsb = sb("Dsb", [128, 512], F32, s2)
        Osb = sb("Osb", [128, 512], F32, s2)
        bs = sb("bs", [128, 16], F32, s2)
        steps = sb("steps", [128, 24], F32, s2)
        nmid = sb("nmid", [128, 1], F32, s2)
        sgs = sb("sgs", [128, 1], F32, s2)
        pow2 = sb("pow2", [128, 24], F32, s2)
        kb.dma('sp', lambda q: q.dma_start(out=pow2[:], in_=c_pow2[:, :]), writes=['pow2'])
        lo, hi, mid, cnt, ge, dd, ee, need, cgt, carry = [bs[:, i:i + 1] for i in range(10)]
        npt = [0]
        natt = [0]
        psrot[0] = 4

        def selection_a(qb):
            n = (qb + 1) * 128
            ib = qb % 2
            kb.dma('sp', lambda q: q.dma_start(out=iqb[ib][:], in_=iqd[:, :, qb * 128:(qb + 1) * 128].rearrange("j p t -> p j t")),
                   writes=[('iqb', ib)])
            for g0 in range(0, n, 512):
                w = min(512, n - g0)
                pts = []
                for h in range(4):
                    j, hh = h // 2, h % 2
                    pt, pk = psum()
                    pts.append((pt, pk))
                    kb.op('pe', lambda p, pt=pt, j=j, hh=hh: p.matmul(pt[:, 0:w], lhsT=iqb[ib][hh * 64:(hh + 1) * 64, j, :],
                          rhs=ikT2[hh * 64:(hh + 1) * 64, g0:g0 + w], start=True, stop=True), reads=[('iqb', ib), 'ikT2'], writes=[pk])
                for h in range(4):
                    pt, pk = pts[h]
                    kb.op('act', lambda a, pt=pt, h=h: a.activation(out=rl[h][:, 0:w], in_=pt[:, 0:w], func=AF.Relu, scale=absw[:, qb, h:h + 1]),
                          reads=[pk], writes=[('rl', h)])
                yield
                for h in range(4):
                    if h == 0:
                        kb.op('dve', lambda v, h=h: v.tensor_scalar(out=sc[:, g0:g0 + w], in0=rl[h][:, 0:w], scalar1=sgnw[:, qb, h:h + 1],
                              scalar2=None, op0=ALU.mult), reads=[('rl', h)], writes=['sc'])
                    else:
                        kb.op('dve', lambda v, h=h: v.scalar_tensor_tensor(out=sc[:, g0:g0 + w], in0=rl[h][:, 0:w], scalar=sgnw[:, qb, h:h + 1],
                              in1=sc[:, g0:g0 + w], op0=ALU.mult, op1=ALU.add), reads=[('rl', h), 'sc'], writes=['sc'])
                yield
            kb.op('dve', lambda v: v.tensor_tensor(out=sc[:, n - 128:n], in0=sc[:, n - 128:n], in1=cmask[:], op=ALU.add), reads=['sc', 'cmask'], writes=['sc'])
            kb.op('dve', lambda v: v.tensor_reduce(out=hi, in_=sc[:, 0:n], axis=AX.X, op=ALU.max), reads=['sc'], writes=['bs'])
            kb.op('dve', lambda v: v.tensor_reduce(out=lo, in_=sc[:, 0:256], axis=AX.X, op=ALU.min), reads=['sc'], writes=['bs'])
            yield
            kb.op('dve', lambda v: v.scalar_tensor_tensor(out=dd, in0=hi, scalar=2.0, in1=lo, op0=ALU.add, op1=ALU.subtract), reads=['bs'], writes=['bs'])
            kb.op('dve', lambda v: v.tensor_scalar(out=steps[:], in0=pow2[:], scalar1=dd, scalar2=None, op0=ALU.mult), reads=['bs', 'pow2'], writes=['steps'])
            kb.op('dve', lambda v: v.scalar_tensor_tensor(out=mid, in0=lo, scalar=-1.0, in1=steps[:, 0:1], op0=ALU.add, op1=ALU.add), reads=['bs', 'steps'], writes=['bs'])
            yield
            hsp = ((n // 2) // 128) * 128
            wact = n - hsp
            for it in range(NBIS):
                kb.op('dve', lambda v: v.tensor_scalar(out=junk[:, 0:hsp], in0=sc[:, 0:hsp], scalar1=mid, scalar2=None, op0=ALU.is_ge, op1=ALU.add, accum_out=cnt),
                      reads=['sc', 'bs'], writes=['junk', 'bs'])
                kb.op('dve', lambda v: v.tensor_scalar(out=junk[:, 0:wact], in0=sc[:, hsp:n], scalar1=mid, scalar2=cnt, op0=ALU.is_ge, op1=ALU.add, accum_out=cnt),
                      reads=['sc', 'bs'], writes=['junk', 'bs'])
                yield
                kb.op('dve', lambda v: v.tensor_scalar(out=ge, in0=cnt, scalar1=float(KSEL), scalar2=0.5, op0=ALU.is_ge, op1=ALU.subtract), reads=['bs'], writes=['bs'])
                kb.op('dve', lambda v, it=it: v.scalar_tensor_tensor(out=mid, in0=ge, scalar=steps[:, it:it + 1], in1=mid, op0=ALU.mult, op1=ALU.add),
                      reads=['bs', 'steps'], writes=['bs'])
                yield
            kb.op('dve', lambda v: v.tensor_tensor(out=lo, in0=mid, in1=steps[:, NBIS:NBIS + 1], op=ALU.subtract), reads=['bs', 'steps'], writes=['bs'])
            kb.op('dve', lambda v: v.tensor_tensor(out=hi, in0=mid, in1=steps[:, NBIS:NBIS + 1], op=ALU.add), reads=['bs', 'steps'], writes=['bs'])
            kb.op('dve', lambda v: v.tensor_scalar(out=junk[:, 0:hsp], in0=sc[:, 0:hsp], scalar1=hi, scalar2=None, op0=ALU.is_ge, op1=ALU.add, accum_out=cgt),
                  reads=['sc', 'bs'], writes=['junk', 'bs'])
            kb.op('dve', lambda v: v.tensor_scalar(out=junk[:, 0:wact], in0=sc[:, hsp:n], scalar1=hi, scalar2=cgt, op0=ALU.is_ge, op1=ALU.add, accum_out=cgt),
                  reads=['sc', 'bs'], writes=['junk', 'bs'])
            kb.op('dve', lambda v: v.tensor_scalar(out=need, in0=cgt, scalar1=-1.0, scalar2=float(KSEL), op0=ALU.mult, op1=ALU.add), reads=['bs'], writes=['bs'])
            yield

        def selection_b(qb):
            n = (qb + 1) * 128
            for ci_, c0 in enumerate(range(0, n, CH)):
                w = min(CH, n - c0)
                r_ = ci_ % 2
                tk, ck = ('tiec', r_), ('cumc', r_)
                kb.op('dve', lambda v, c0=c0, w=w, r_=r_: v.tensor_scalar(out=tiec[r_][:, 0:w], in0=sc[:, c0:c0 + w], scalar1=hi, scalar2=None, op0=ALU.is_lt),
                      reads=['sc', 'bs'], writes=[tk])
                kb.op('dve', lambda v, c0=c0, w=w, r_=r_: v.scalar_tensor_tensor(out=tiec[r_][:, 0:w], in0=sc[:, c0:c0 + w], scalar=lo, in1=tiec[r_][:, 0:w],
                      op0=ALU.is_ge, op1=ALU.mult), reads=['sc', 'bs', tk], writes=[tk])
                init = 0.0 if c0 == 0 else carry
                kb.op('dve', lambda v, w=w, r_=r_, init=init: v.tensor_tensor_scan(out=cumc[r_][:, 0:w], data0=onesc[:, 0:w], data1=tiec[r_][:, 0:w],
                      initial=init, op0=ALU.mult, op1=ALU.add), reads=['onesc', tk, 'bs'], writes=[ck])
                kb.op('dve', lambda v, w=w, r_=r_: v.tensor_copy(out=carry, in_=cumc[r_][:, w - 1:w]), reads=[ck], writes=['bs'])
                kb.op('dve', lambda v, w=w, r_=r_: v.scalar_tensor_tensor(out=tiec[r_][:, 0:w], in0=cumc[r_][:, 0:w], scalar=need, in1=tiec[r_][:, 0:w],
                      op0=ALU.is_le, op1=ALU.mult), reads=[ck, 'bs', tk], writes=[tk])
                kb.op('dve', lambda v, c0=c0, w=w, r_=r_: v.scalar_tensor_tensor(out=maskb[:, c0:c0 + w], in0=sc[:, c0:c0 + w], scalar=hi, in1=tiec[r_][:, 0:w],
                      op0=ALU.is_ge, op1=ALU.add), reads=['sc', 'bs', tk], writes=['maskb'])
                yield

        def attention(qb):
            mx = mixT[qb % 2]
            kb.dma('sp', lambda q: q.dma_start(out=ql[:], in_=qlat[:, :, qb * 128:(qb + 1) * 128].rearrange("h p t -> p h t")), writes=['ql'])
            kb.dma('sp', lambda q: q.dma_start(out=mx[:, 6:8, :], in_=memod[:, :, qb * 128:(qb + 1) * 128].rearrange("j p t -> p j t")),
                   writes=[('mixTm', qb % 2)])
            pO, pOk = ps[6], ('ps', 6)
            pD, pDk = ps[7], ('ps', 7)
            steps_ = [(hg, j) for hg in range(3) for j in range(qb + 1)]

            def logits(hg, j):
                qrhs = ql[:, hg * 4:(hg + 1) * 4, :].rearrange("p h t -> p (h t)")
                li = 4 + (natt[0] % 2)
                natt[0] += 1
                pL, pLk = ps[li], ('ps', li)
                dt = qb - j
                nmm = 1 + (1 if qb >= 2 else 0) + (1 if dt <= 1 else 0)
                m = 0
                kb.op('pe', lambda p: p.matmul(pL[:, :], lhsT=cT[:, j * 128:(j + 1) * 128], rhs=qrhs, start=True, stop=(nmm == 1)),
                      reads=[('cT', j), 'ql'], writes=[pLk])
                m += 1
                if qb >= 2:
                    kb.op('pe', lambda p, m=m: p.matmul(pL[:, :], lhsT=maskb[:, j * 128:(j + 1) * 128],
                          rhs=i4big[:].rearrange("p r t -> p (r t)"), start=False, stop=(m == nmm - 1)), reads=['maskb', 'i4big'], writes=[pLk])
                    m += 1
                if dt <= 1:
                    kb.op('pe', lambda p, m=m: p.matmul(pL[:, :], lhsT=ident_b[:],
                          rhs=BT[:, dt, hg * 4:(hg + 1) * 4, :].rearrange("p h t -> p (h t)"), start=False, stop=(m == nmm - 1)),
                          reads=['BT', 'ident_b'], writes=[pLk])
                    m += 1
                return pL, pLk

            cur = logits(*steps_[0])
            for idx, (hg, j) in enumerate(steps_):
                pL, pLk = cur
                pi = npt[0] % 3
                npt[0] += 1
                kb.op('act', lambda a, pL=pL, pi=pi: a.activation(out=Pt[pi][:], in_=pL[:, :], func=AF.Exp, bias=(nbias[:] if qb >= 2 else zbias[:]), scale=1.0),
                      reads=[pLk, 'nbias'], writes=[('Pt', pi)])
                if idx + 1 < len(steps_):
                    cur = logits(*steps_[idx + 1])
                kb.op('pe', lambda p, j=j, pi=pi: p.matmul(pO[:, :], lhsT=cTok[:, j, :], rhs=Pt[pi][:], start=(j == 0), stop=(j == qb)),
                      reads=[('cTok', j), ('Pt', pi)], writes=[pOk])
                kb.op('pe', lambda p, j=j, pi=pi: p.matmul(pD[:, :], lhsT=ones_b[:], rhs=Pt[pi][:], start=(j == 0), stop=(j == qb)),
                      reads=['ones_b', ('Pt', pi)], writes=[pDk])
                yield
                if j == qb:
                    oi = hg % 2
                    kb.op('act', lambda a: a.copy(out=Dsb[:], in_=pD[:, :]), reads=[pDk], writes=['Dsb'])
                    kb.op('act', lambda a: a.copy(out=Osb[:], in_=pO[:, :]), reads=[pOk], writes=['Osb'])
                    kb.op('dve', lambda v: v.reciprocal(out=Dsb[:], in_=Dsb[:]), reads=['Dsb'], writes=['Dsb'])
                    kb.op('pool', lambda g, oi=oi: g.tensor_tensor(out=olat[oi][:], in0=Osb[:], in1=Dsb[:], op=ALU.mult), reads=['Osb', 'Dsb'], writes=[('olat', oi)])
                    for pp in range(2):
                        pT, pTk = psum()
                        for hh in range(2):
                            hl = 2 * pp + hh
                            h = hg * 4 + hl
                            kb.op('pe', lambda p, pT=pT, h=h, hl=hl, hh=hh, oi=oi: p.matmul(pT[:, 0:128], lhsT=wuvpad[:, h, :], rhs=olat[oi][:, hl * 128:(hl + 1) * 128],
                                  start=(hh == 0), stop=(hh == 1)), reads=['wuvpad', ('olat', oi)], writes=[pTk])
                        kb.op('act', lambda a, pT=pT, hg=hg, pp=pp: a.copy(out=mx[:, hg * 2 + pp, :], in_=pT[:, 0:128]), reads=[pTk], writes=[('mixTt', qb % 2, hg * 2 + pp)])
                    yield

        nbias = sb("nbias", [128, 1], F32, s2)
        zbias = sb("zbias", [128, 1], F32, s2)
        kb.op('dve', lambda v: v.memset(nbias[:], -100.0), writes=['nbias'])
        kb.op('dve', lambda v: v.memset(zbias[:], 0.0), writes=['nbias'])

        nqb = ntiles

        def chain(*gens):
            for g in gens:
                yield from g

        def nsteps_sel(qb):
            n = (qb + 1) * 128
            return 2 * ((n + 511) // 512) + 3 + 2 * NBIS

        def tail_stream(qb):
            mx = mixT[qb % 2]
            mkeys = [('mixTt', qb % 2, k) for k in range(6)] + [('mixTm', qb % 2)]
            kb.dma('sp', lambda q: q.dma_start(out=x2t[:], in_=src[qb * 128:(qb + 1) * 128, :]), writes=['x2t'])
            yield
            yield from tail.run_gen(qb, lambda k, mx=mx: mx[:, k, :], mkeys, x2t, 'x2t', x1buf[qb * 128:(qb + 1) * 128, :])

        if nqb > 2:
            for _ in selection_a(2):
                pass
        gT = None
        for qb in range(nqb):
            if qb >= 2:
                gS = selection_b(qb)
                st_ = [(gS, 2 * (qb + 1))]
                if gT is not None:
                    st_.append((gT, 40))
                interleave(st_, until=gS)
            streams = [(attention(qb), 3 * (qb + 1) + 3)]
            if gT is not None:
                streams.append((gT, 12))
            if qb + 1 < nqb and qb + 1 >= 2:
                streams.append((selection_a(qb + 1), nsteps_sel(qb + 1)))
            interleave(streams)
            gT = tail_stream(qb)
        for _ in gT:
            pass
        psrot[0] = 6
        kb.barrier()
        s2.close()
        st.close()

    def phase_moe(layer, dst, ntiles=NT, nexp=NE):
        L = layer
        st = ExitStack()
        w1 = [sb(f"w1_{L}_{i}", [128, 8, 2 * D], BF16, st) for i in range(2)]
        w2 = [sb(f"w2_{L}_{i}", [128, 8, D], BF16, st) for i in range(2)]
        b2r = [sb(f"b2r_{L}_{i}", [1, D], BF16, st) for i in range(2)]
        b1a = sb(f"b1a_{L}", [128, NE, 16], F32, st)
        b1u = sb(f"b1u_{L}", [128, NE, 8], F32, st)
        with nc.allow_non_contiguous_dma(reason="bias layout"):
            kb.dma('sp', lambda q: q.dma_start(out=b1a[:], in_=exp_b1[L].rearrange("e (j p) -> p e j", p=128)), writes=['b1a'])
        kb.op('dve', lambda v: v.tensor_scalar(out=b1u[:], in0=b1a[:, :, 8:16], scalar1=1.0, scalar2=None, op0=ALU.add), reads=['b1a'], writes=['b1u'])
        xgt = [sb(f"xgt_{L}_{i}", [128, RG // 128, D], BF16, st) for i in range(2)]
        xgT = [sb(f"xgT_{L}_{i}", [128, 8, RG], BF16, st) for i in range(2)]
        actT = [sb(f"actT_{L}_{i}", [128, 8, RG], BF16, st) for i in range(2)]
        tg = [sb(f"tg_{L}_{i}", [128, RG], F32, st) for i in range(2)]
        tu = [sb(f"tu_{L}_{i}", [128, RG], F32, st) for i in range(2)]
        tsg = [sb(f"tsg_{L}_{i}", [128, RG], F32, st) for i in range(2)]
        tt = [sb(f"tt_{L}_{i}", [128, RG], F32, st) for i in range(2)]
        yev = [sb(f"yev_{L}_{i}", [128, D], F32, st) for i in range(2)]
        nyev = 0

        def load_w(e):
            i = e % 2
            load_cast(w1[i][:], exp_w1[L, e].rearrange("(k p) f -> p k f", p=128), ('w1', i))
            load_cast(w2[i][:], exp_w2[L, e].rearrange("(k p) n -> p k n", p=128), ('w2', i))
            load_cast(b2r[i][:], exp_b2[L, e:e + 1, :], ('b2r', i))

        NG = CAP // RG

        def stage_a(e, g, gi):
            wi = e % 2
            for k in range(8):
                pt, pk = psum()
                ptb = pt[:].bitcast(BF16)
                for t in range(RG // 128):
                    kb.op('pe', lambda p, ptb=ptb, t=t, k=k: p.transpose(out=ptb[:, t * 128:(t + 1) * 128],
                          in_=xgt[gi][:, t, k * 128:(k + 1) * 128], identity=ident_b[:]), reads=[('xgt', gi), 'ident_b'], writes=[pk])
                if k % 2 == 0:
                    kb.op('dve', lambda v, ptb=ptb, k=k: v.tensor_copy(out=xgT[gi][:, k, :], in_=ptb[:, 0:RG]), reads=[pk], writes=[('xgT', gi, k)])
                else:
                    kb.op('act', lambda a, ptb=ptb, k=k: a.copy(out=xgT[gi][:, k, :], in_=ptb[:, 0:RG]), reads=[pk], writes=[('xgT', gi, k)])
                if k % 2 == 1:
                    yield
            xgTk = [('xgT', gi, k) for k in range(8)]
            for j in range(8):
                ji = j % 2
                pg, pgk = psum()
                pu, puk = psum()
                for k in range(8):
                    kb.op('pe', lambda p, pg=pg, k=k, j=j: p.matmul(pg[:, 0:RG], lhsT=w1[wi][:, k, j * 128:(j + 1) * 128],
                          rhs=xgT[gi][:, k, :], start=(k == 0), stop=(k == 7)), reads=[('w1', wi)] + xgTk, writes=[pgk])
                for k in range(8):
                    kb.op('pe', lambda p, pu=pu, k=k, j=j: p.matmul(pu[:, 0:RG], lhsT=w1[wi][:, k, D + j * 128:D + (j + 1) * 128],
                          rhs=xgT[gi][:, k, :], start=(k == 0), stop=(k == 7)), reads=[('w1', wi)] + xgTk, writes=[puk])
                kb.op('dve', lambda v, pg=pg, ji=ji, j=j: v.tensor_scalar(out=tg[ji][:], in0=pg[:, 0:RG], scalar1=b1a[:, e, j:j + 1],
                      scalar2=7.0, op0=ALU.add, op1=ALU.min), reads=[pgk, 'b1a'], writes=[('tg', ji)])
                kb.op('act', lambda a, ji=ji: a.activation(out=tsg[ji][:], in_=tg[ji][:], func=AF.Sigmoid, scale=1.702),
                      reads=[('tg', ji)], writes=[('tsg', ji)])
                kb.op('dve', lambda v, pu=pu, ji=ji, j=j: v.tensor_scalar(out=tu[ji][:], in0=pu[:, 0:RG], scalar1=b1u[:, e, j:j + 1],
                      scalar2=8.0, op0=ALU.add, op1=ALU.min), reads=[puk, 'b1u'], writes=[('tu', ji)])
                kb.op('dve', lambda v, ji=ji: v.scalar_tensor_tensor(out=tt[ji][:], in0=tu[ji][:], scalar=-6.0, in1=tg[ji][:],
                      op0=ALU.max, op1=ALU.mult), reads=[('tu', ji), ('tg', ji)], writes=[('tt', ji)])
                kb.op('pool', lambda g_, ji=ji, j=j: g_.tensor_tensor(out=actT[gi][:, j, :], in0=tt[ji][:], in1=tsg[ji][:], op=ALU.mult),
                      reads=[('tt', ji), ('tsg', ji)], writes=[('actT', gi, j)])
                yield

        def stage_b(e, g, gi):
            nonlocal nyev
            wi = e % 2
            r0 = e * CAP + g * RG
            actk = [('actT', gi, j) for j in range(8)]
            for t in range(RG // 128):
                yi = nyev % 2
                nyev += 1
                for nh in range(2):
                    py, pyk = psum()
                    for k in range(8):
                        kb.op('pe', lambda p, py=py, k=k, t=t, nh=nh: p.matmul(py[:, :], lhsT=actT[gi][:, k, t * 128:(t + 1) * 128],
                              rhs=w2[wi][:, k, nh * 512:(nh + 1) * 512], start=(k == 0), stop=False), reads=[('w2', wi)] + actk, writes=[pyk])
                    kb.op('pe', lambda p, py=py, nh=nh: p.matmul(py[:, :], lhsT=ones_b[0:1, :], rhs=b2r[wi][0:1, nh * 512:(nh + 1) * 512],
                          start=False, stop=True), reads=[('b2r', wi), 'ones_b'], writes=[pyk])
                    kb.op('act', lambda a, py=py, nh=nh, yi=yi: a.copy(out=yev[yi][:, nh * 512:(nh + 1) * 512], in_=py[:, :]),
                          reads=[pyk], writes=[('yev', yi)])
                    yield
                rr0 = r0 + t * 128
                kb.dma('sp', lambda q, rr0=rr0, yi=yi: q.dma_start(out=yg[rr0:rr0 + 128, :], in_=yev[yi][:]), reads=[('yev', yi)], writes=['yg'])

        groups = [(e, g) for e in range(nexp) for g in range(NG)]

        def load_x(i):
            e_, g_ = groups[i]
            r0 = e_ * CAP + g_ * RG
            kb.dma('sp', lambda q: q.dma_start(out=xgt[i % 2][:], in_=xg[r0:r0 + RG, :].rearrange("(t p) d -> p t d", p=128)),
                   reads=['xg'], writes=[('xgt', i % 2)])

        load_x(0)
        if len(groups) > 1:
            load_x(1)
        load_w(0)
        if nexp > 1:
            load_w(1)
        for _ in stage_a(groups[0][0], groups[0][1], 0):
            pass
        for i, (e, g) in enumerate(groups):
            if i + 2 < len(groups):
                load_x(i + 2)
            if g == 0 and e >= 1 and e + 1 < nexp:
                load_w(e + 1)
            streams = [(stage_b(e, g, i % 2), 6)]
            if i + 1 < len(groups):
                e2, g2 = groups[i + 1]
                streams.append((stage_a(e2, g2, (i + 1) % 2), 20))
            interleave(streams)
        kb.barrier()
        st.close()
        st = ExitStack()
        lng = sb(f"ln2g{L}", [128, D], F32, st)
        lnb = sb(f"ln2b{L}", [128, D], F32, st)
        bcast_rows(lng[:], ln2_g[L], 'lng2')
        bcast_rows(lnb[:], ln2_b[L], 'lnb2')
        yk = [[sb(f"yk{L}_{i}_{k}", [128, D], F32, st) for k in range(4)] for i in range(2)]
        x1r = [sb(f"x1r{L}_{i}", [128, D], F32, st) for i in range(2)]
        zz = [sb(f"zz{L}_{i}", [128, D], F32, st) for i in range(2)]
        oo = [sb(f"oo{L}_{i}", [128, D], F32, st) for i in range(2)]
        smm = [sb(f"smm{L}_{i}", [128, 256], F32, st) for i in range(2)]
        lnh = Tail.__new__(Tail)
        for ti in range(ntiles):
            i = ti % 2
            kb.dma('sp', lambda q, ti=ti, i=i: q.dma_start(out=x1r[i][:], in_=x1buf[ti * 128:(ti + 1) * 128, :]), reads=[('x1d', ti)], writes=[('x1r', i)])
            for k in range(4):
                kb.op('pool', lambda g_, i=i, k=k: g_.memset(yk[i][k][:], 0.0), writes=[('yk', i, k)])
                kb.dma('pool', lambda q, ti=ti, i=i, k=k: q.indirect_dma_start(
                    out=yk[i][k][:, :], out_offset=None, in_=yg[:, :],
                    in_offset=bass.IndirectOffsetOnAxis(ap=destall[:, ti, k:k + 1], axis=0),
                    bounds_check=bc_reg, oob_is_err=False), reads=['yg', ('dest', ti)], writes=[('yk', i, k)])
            kb.op('dve', lambda v, i=i: v.tensor_scalar(out=zz[i][:], in0=x1r[i][:], scalar1=ALPHA, scalar2=None, op0=ALU.mult),
                  reads=[('x1r', i)], writes=[('zz', i)])
            for k in range(4):
                kb.op('dve', lambda v, ti=ti, i=i, k=k: v.scalar_tensor_tensor(out=zz[i][:], in0=yk[i][k][:], scalar=gall[:, ti, k:k + 1],
                      in1=zz[i][:], op0=ALU.mult, op1=ALU.add), reads=[('yk', i, k), ('gate', ti), ('zz', i)], writes=[('zz', i)])
            Tail.layernorm(lnh, zz[i], ('zz', i), oo[i], ('oo', i), smm[i], ('smm', i), lng, lnb, 'lng2', 'lnb2')
            kb.dma('sp', lambda q, ti=ti, i=i: q.dma_start(out=dst[ti * 128:(ti + 1) * 128, :], in_=oo[i][:]), reads=[('oo', i)], writes=[('dst', L, ti)])
        kb.barrier()
        kb.pool_depth = 2
        st.close()

    if mode.startswith("a0"):
        ntl = NT if mode == "a0" else int(mode[2:])
        phase_a0(ntiles=ntl)
        st = ExitStack()
        cp = [sb(f"cp{i}", [128, D], F32, st) for i in range(2)]
        for ti in range(ntl):
            i = ti % 2
            kb.dma('sp', lambda q, ti=ti, i=i: q.dma_start(out=cp[i][:], in_=x1buf[ti * 128:(ti + 1) * 128, :]), writes=[('cp', i)])
            kb.dma('sp', lambda q, ti=ti, i=i: q.dma_start(out=out[ti * 128:(ti + 1) * 128, :], in_=cp[i][:]), reads=[('cp', i)], writes=[('o', ti)])
        kb.barrier()
        st.close()
    elif mode == "full":
        phase_a0()
        phase_moe(0, x2buf)
        phase_a1(x2buf)
        phase_moe(1, out)
    elif mode.startswith("a1"):
        ntl = int(mode[2:])
        zt = sb("zt1", [128, 4096], BF16)
        kb.op('pool', lambda g: g.memset(zt[:], 0.0), writes=['zt'])
        for r0 in range(0, NROW, 512):
            kb.dma('sp', lambda q, r0=r0: q.dma_start(out=xg[r0:r0 + 512, :].rearrange("(p t) d -> p (t d)", t=4), in_=zt[:]),
                   reads=['zt'], writes=['xg'])
        phase_a1(x_in, ntiles=ntl)
        st = ExitStack()
        cp = [sb(f"cp{i}", [128, D], F32, st) for i in range(2)]
        for ti in range(ntl):
            i = ti % 2
            kb.dma('sp', lambda q, ti=ti, i=i: q.dma_start(out=cp[i][:], in_=x1buf[ti * 128:(ti + 1) * 128, :]), writes=[('cp', i)])
            kb.dma('sp', lambda q, ti=ti, i=i: q.dma_start(out=out[ti * 128:(ti + 1) * 128, :], in_=cp[i][:]), reads=[('cp', i)], writes=[('o', ti)])
        kb.barrier()
        st.close()
    elif mode == "l0":
        phase_a0()
        phase_moe(0, out)
    es.close()
    return nc


def host_consts():
    ident = np.eye(128, dtype=np.float32)
    tri = np.triu(np.ones((128, 128), np.float32), 1)
    iota = np.tile(np.arange(NE, dtype=np.float32)[None, :], (128, 1))
    ecap = iota * CAP
    q = np.arange(128)
    cmask = np.where(q[None, :] <= q[:, None], 0.0, -2000.0).astype(np.float32)
    caus = np.where(q[:, None] <= q[None, :], 0.0, -30000.0).astype(np.float32)
    bkt = np.zeros((128, 2, 128), np.float32)
    for dt in range(2):
        rel = np.maximum(q[None, :] - q[:, None] + 128 * dt, 0)
        large = 16 + (np.log(np.maximum(rel, 1).astype(np.float32) / 16) / np.float32(np.log(128 / 16)) * 16).astype(np.int32)
        large = np.minimum(large, 31)
        bkt[:, dt, :] = np.where(rel < 16, rel, large)
    pow2 = np.tile((2.0 ** -(np.arange(24, dtype=np.float64) + 1)).astype(np.float32)[None, :], (128, 1))
    return {"c_ident": ident, "c_tri": tri, "c_iota": iota, "c_ecap": ecap, "c_cmask": cmask, "c_caus": caus, "c_bkt": bkt,
            "c_pow2": pow2}


_PARAMS = ["rel_bias", "a_w_in", "a_conv_w", "a_conv_b", "a_wr", "a_br", "a_wi", "a_bi", "a_lambda", "b_w_in",
           "b_kv_norm_g", "b_w_uk", "b_w_uv", "b_idx_norm_g", "b_idx_norm_b", "w_mem_kv", "w_out", "ln1_g", "ln1_b",
           "router_w", "router_b", "exp_w1", "exp_b1", "exp_w2", "exp_b2", "ln2_g", "ln2_b"]
_SQUEEZE = {"a_w_in", "a_conv_w", "a_conv_b", "a_wr", "a_br", "a_wi", "a_bi", "a_lambda", "b_w_in", "b_kv_norm_g",
            "b_w_uk", "b_w_uv", "b_idx_norm_g", "b_idx_norm_b"}


def make_in_maps(inputs, cores):
    shared = {}
    for k in _PARAMS:
        v = np.ascontiguousarray(np.asarray(inputs[k], dtype=np.float32))
        if k in _SQUEEZE:
            v = v[0]
        shared[k] = v
    shared.update(host_consts())
    maps = []
    for c in cores:
        m = dict(shared)
        m["x"] = np.ascontiguousarray(inputs["x"][c])
        m["mem"] = np.ascontiguousarray(inputs["mem"][c])
        maps.append(m)
    return maps


def kernel(**inputs):
    nc = build_program("full")
    maps = make_in_maps(inputs, list(range(8)))
    res = run_bass_kernel_spmd(nc, maps, core_ids=list(range(8)))
    return np.stack([r["out"] for r in res.results], axis=0)
```

```python
from contextlib import ExitStack
import numpy as np
import concourse.bass as bass
import concourse.mybir as mybir
from concourse.bass_utils import run_bass_kernel_spmd

F32 = mybir.dt.float32
BF16 = mybir.dt.bfloat16
I32 = mybir.dt.int32
U32 = mybir.dt.uint32
AF = mybir.ActivationFunctionType
ALU = mybir.AluOpType
AX = mybir.AxisListType

S = 8192
D = 1024
NT = S // 128
MEM = 256
TOKW = 768
NE = 32
CAP = 1536
NROW = NE * CAP
RG = 384
ALPHA = float(4 ** 0.25)
W_IN_A = 1792
W_IN_B = 1476
BIGOOB = 1.0e6


class KB:
    NS = 8
    ND = 32

    def __init__(self, nc, es):
        self.nc = nc
        self.eng = {'pe': nc.tensor, 'act': nc.scalar, 'dve': nc.vector, 'pool': nc.gpsimd, 'sp': nc.sync}
        self.esem = {e: [es.enter_context(nc.semaphore(f"s_{e}{i}")) for i in range(self.NS)]
                     for e in ('pe', 'act', 'dve', 'pool')}
        self.cnt = {e: 0 for e in ('pe', 'act', 'dve', 'pool')}
        self.dsem = [es.enter_context(nc.semaphore(f"s_d{i}")) for i in range(self.ND)]
        self.dtot = [0] * self.ND
        self.dnext = 0
        self.dnextp = 0
        self.pool_depth = 2
        self.wc = {e: {} for e in self.eng}
        self.wd = {e: {} for e in self.eng}
        self.lastw = {}
        self.readers = {}

    def _wait(self, e, tok):
        eng = self.eng[e]
        if tok[0] == 'c':
            _, e2, k = tok
            if e2 == e and e == 'pe':
                return
            if self.wc[e].get(e2, 0) >= k:
                return
            eng.wait_ge(self.esem[e2][(k - 1) % self.NS], (k - 1) // self.NS + 1)
            self.wc[e][e2] = k
        else:
            _, s, tot = tok
            if self.wd[e].get(s, 0) >= tot:
                return
            eng.wait_ge(self.dsem[s], tot)
            self.wd[e][s] = tot

    def _deps(self, e, reads, writes):
        deps = []
        for k in reads:
            t = self.lastw.get(k)
            if t is not None:
                deps.append(t)
        for k in writes:
            t = self.lastw.get(k)
            if t is not None:
                deps.append(t)
            deps.extend(self.readers.get(k, ()))
        for t in deps:
            self._wait(e, t)

    def _commit(self, tok, reads, writes):
        for k in reads:
            lst = self.readers.setdefault(k, [])
            lst[:] = [t for t in lst if not (t[0] == tok[0] and t[1] == tok[1])]
            lst.append(tok)
        for k in writes:
            self.lastw[k] = tok
            self.readers[k] = []

    def op(self, e, fn, reads=(), writes=()):
        self._deps(e, reads, writes)
        ins = fn(self.eng[e])
        self.cnt[e] += 1
        k = self.cnt[e]
        ins.then_inc(self.esem[e][(k - 1) % self.NS], 1)
        self._commit(('c', e, k), reads, writes)

    def dma(self, e, fn, reads=(), writes=()):
        half = self.ND // 2
        if e == 'pool':
            s = half + self.dnextp
            self.dnextp = (self.dnextp + 1) % self.pool_depth
        else:
            s = self.dnext
            self.dnext = (self.dnext + 1) % half
        if self.dtot[s] > 0:
            self._wait(e, ('d', s, self.dtot[s]))
        self._deps(e, reads, writes)
        ins = fn(self.eng[e])
        self.dtot[s] += 16
        ins.then_inc(self.dsem[s], 16)
        self._commit(('d', s, self.dtot[s]), reads, writes)

    def barrier(self):
        for e in self.eng:
            for e2 in self.cnt:
                if self.cnt[e2] > 0:
                    self._wait(e, ('c', e2, self.cnt[e2]))
            for s in range(self.ND):
                if self.dtot[s] > 0:
                    self._wait(e, ('d', s, self.dtot[s]))
        self.lastw.clear()
        self.readers.clear()


def interleave(streams, until=None):
    live = [[g, max(1, n), 0] for g, n in streams if g is not None]
    while live:
        live.sort(key=lambda r: r[2] / r[1])
        r = live[0]
        try:
            next(r[0])
            r[2] += 1
        except StopIteration:
            live.remove(r)
            if until is not None and r[0] is until:
                return


def build_program(mode="full"):
    nc = bass.Bass("TRN2", target_bir_lowering=False)
    es = ExitStack()
    kb = KB(nc, es)

    def din(name, shape, dt=F32):
        return nc.dram_tensor(name, list(shape), dt, kind="ExternalInput").ap()

    def dscr(name, shape, dt=F32):
        return nc.dram_tensor(name, list(shape), dt, kind="Internal").ap()

    x_in = din("x", [S, D])
    mem_in = din("mem", [MEM, D])
    rel_bias = din("rel_bias", [32, 12])
    a_w_in = din("a_w_in", [D, W_IN_A])
    a_conv_w = din("a_conv_w", [4, TOKW])
    a_conv_b = din("a_conv_b", [TOKW])
    a_wr = din("a_wr", [12, 64, 64])
    a_br = din("a_br", [TOKW])
    a_wi = din("a_wi", [12, 64, 64])
    a_bi = din("a_bi", [TOKW])
    a_lambda = din("a_lambda", [TOKW])
    b_w_in = din("b_w_in", [D, W_IN_B])
    b_kv_norm_g = din("b_kv_norm_g", [128])
    b_w_uk = din("b_w_uk", [128, 12, 64])
    b_w_uv = din("b_w_uv", [128, 12, 64])
    b_idx_norm_g = din("b_idx_norm_g", [64])
    b_idx_norm_b = din("b_idx_norm_b", [64])
    w_mem_kv = din("w_mem_kv", [2, D, 512])
    w_out = din("w_out", [2, D, D])
    ln1_g = din("ln1_g", [2, D])
    ln1_b = din("ln1_b", [2, D])
    router_w = din("router_w", [2, D, NE])
    router_b = din("router_b", [2, NE])
    exp_w1 = din("exp_w1", [2, NE, D, 2 * D])
    exp_b1 = din("exp_b1", [2, NE, 2 * D])
    exp_w2 = din("exp_w2", [2, NE, D, D])
    exp_b2 = din("exp_b2", [2, NE, D])
    ln2_g = din("ln2_g", [2, D])
    ln2_b = din("ln2_b", [2, D])
    c_ident = din("c_ident", [128, 128])
    c_tri = din("c_tri", [128, 128])
    c_iota = din("c_iota", [128, NE])
    c_ecap = din("c_ecap", [128, NE])
    c_cmask = din("c_cmask", [128, 128])
    c_caus = din("c_caus", [128, 128])
    c_bkt = din("c_bkt", [128, 2, 128])
    c_pow2 = din("c_pow2", [128, 24])

    out = nc.dram_tensor("out", [S, D], F32, kind="ExternalOutput").ap()
    x1buf = dscr("x1buf", [S, D])
    x2buf = dscr("x2buf", [S, D])
    xg = dscr("xg", [NROW, D], BF16)
    yg = dscr("yg", [NROW, D])
    qlat = dscr("qlat", [12, 128, S], BF16)
    iqd = dscr("iqd", [2, 128, S], BF16)
    memod = dscr("memod", [2, 128, S], BF16)

    def sb(name, shape, dt=F32, stack=es):
        return stack.enter_context(nc.sbuf_tensor(name, list(shape), dt))

    ps = [es.enter_context(nc.psum_tensor(f"ps{i}", [128, 512], F32)) for i in range(8)]
    psn = [0]
    psrot = [6]

    def psum():
        i = psn[0] % psrot[0]
        psn[0] = (i + 1) % psrot[0]
        return ps[i], ('ps', i)

    bc_reg = nc.gpsimd.alloc_register("bc_reg")
    nc.gpsimd.reg_mov(bc_reg, NROW - 1)
    ident_f = sb("ident_f", [128, 128])
    ident_b = sb("ident_b", [128, 128], BF16)
    tri_f = sb("tri_f", [128, 128])
    ones_f = sb("ones_f", [128, 128])
    ones_b = sb("ones_b", [128, 128], BF16)
    iota_e = sb("iota_e", [128, NE])
    ecap = sb("ecap", [128, NE])
    destall = sb("destall", [128, NT, 4], I32)
    gall = sb("gall", [128, NT, 4])

    kb.dma('sp', lambda q: q.dma_start(out=ident_f[:], in_=c_ident[:, :]), writes=['ident_f'])
    kb.dma('sp', lambda q: q.dma_start(out=tri_f[:], in_=c_tri[:, :]), writes=['tri_f'])
    kb.dma('sp', lambda q: q.dma_start(out=iota_e[:], in_=c_iota[:, :]), writes=['iota_e'])
    kb.dma('sp', lambda q: q.dma_start(out=ecap[:], in_=c_ecap[:, :]), writes=['ecap'])
    kb.op('dve', lambda v: v.tensor_copy(out=ident_b[:], in_=ident_f[:]), reads=['ident_f'], writes=['ident_b'])
    kb.op('dve', lambda v: v.memset(ones_f[:], 1.0), writes=['ones_f'])
    kb.op('dve', lambda v: v.memset(ones_b[:], 1.0), writes=['ones_b'])

    def load_cast(dst_ap, src_ap, key):
        kb.dma('pool', lambda q: q.dma_start(out=dst_ap, in_=src_ap), writes=[key])

    def bcast_rows(dst, src_row_ap, key, n=128):
        kb.dma('sp', lambda q: q.dma_start(out=dst, in_=src_row_ap.partition_broadcast(n)), writes=[key])

    def setup_mem_kv(layer, st, kT, vpad, onespad):
        memf = sb(f"memf{layer}", [128, 2, D], F32, st)
        memb = sb(f"memb{layer}", [128, 2, D], BF16, st)
        memT = sb(f"memT{layer}", [128, 8, MEM], BF16, st)
        wkv = sb(f"wkv{layer}", [128, 8, 512], BF16, st)
        kb.dma('sp', lambda q: q.dma_start(out=memf[:], in_=mem_in.rearrange("(t p) d -> p t d", p=128)), writes=['memf'])
        load_cast(wkv[:], w_mem_kv[layer].rearrange("(k p) c -> p k c", p=128), 'wkv')
        kb.op('act', lambda a: a.copy(out=memb[:], in_=memf[:]), reads=['memf'], writes=['memb'])
        for k in range(8):
            pt, pk = psum()
            ptb = pt[:].bitcast(BF16)
            for t in range(2):
                kb.op('pe', lambda p, t=t, k=k, ptb=ptb: p.transpose(out=ptb[:, t * 128:(t + 1) * 128],
                      in_=memb[:, t, k * 128:(k + 1) * 128], identity=ident_b[:]),
                      reads=['memb', 'ident_b'], writes=[pk])
            kb.op('dve', lambda v, k=k, ptb=ptb: v.tensor_copy(out=memT[:, k, :], in_=ptb[:, 0:256]),
                  reads=[pk], writes=['memT'])
        for pr in range(2):
            pt, pk = psum()
            for k in range(8):
                kb.op('pe', lambda p, k=k, pr=pr, pt=pt: p.matmul(pt[:, 0:256], lhsT=wkv[:, k, pr * 128:(pr + 1) * 128],
                      rhs=memT[:, k, :], start=(k == 0), stop=(k == 7)), reads=['wkv', 'memT'], writes=[pk])
            kb.op('dve', lambda v, pr=pr, pt=pt: v.tensor_copy(out=kT[:, pr, :], in_=pt[:, 0:256]), reads=[pk], writes=['kT'])
        kb.op('pool', lambda g: g.memset(vpad[:], 0.0), writes=['vpad'])
        kb.op('pool', lambda g: g.memset(onespad[:], 0.0), writes=['onespad'])
        for par in range(2):
            kb.op('pool', lambda g, par=par: g.memset(onespad[:, par, par * 64:(par + 1) * 64], 1.0), writes=['onespad'])
        for mc in range(2):
            pt, pk = psum()
            for k in range(8):
                kb.op('pe', lambda p, k=k, mc=mc, pt=pt: p.matmul(pt[:, 0:256], lhsT=memT[:, k, mc * 128:(mc + 1) * 128],
                      rhs=wkv[:, k, 256:512], start=(k == 0), stop=(k == 7)), reads=['wkv', 'memT'], writes=[pk])
            for h in range(4):
                par = h % 2
                kb.op('dve', lambda v, h=h, mc=mc, par=par, pt=pt: v.tensor_copy(
                    out=vpad[:, h, mc, par * 64:(par + 1) * 64], in_=pt[:, h * 64:(h + 1) * 64]),
                    reads=[pk], writes=['vpad'])

    def mem_attn(mqT, T, kT, vpad, onespad, mixT_dst, E, rden, tag):
        for pr in range(2):
            for hh in range(2):
                h = 2 * pr + hh
                for mc in range(2):
                    pt, pk = psum()
                    kb.op('pe', lambda p, pt=pt, pr=pr, hh=hh, mc=mc: p.matmul(
                        pt[:, 0:T], lhsT=kT[hh * 64:(hh + 1) * 64, pr, mc * 128:(mc + 1) * 128],
                        rhs=mqT[hh * 64:(hh + 1) * 64, pr, :], start=True, stop=True),
                        reads=['kT', 'mqT' + tag], writes=[pk])
                    kb.op('act', lambda a, pt=pt, hh=hh, mc=mc: a.activation(
                        out=E[:, hh, mc, :], in_=pt[:, 0:T], func=AF.Exp, scale=0.125),
                        reads=[pk], writes=[('E' + tag, hh, mc)])
            po, pok = psum()
            pd, pdk = psum()
            n = 0
            for hh in range(2):
                h = 2 * pr + hh
                for mc in range(2):
                    kb.op('pe', lambda p, po=po, h=h, hh=hh, mc=mc, n=n: p.matmul(
                        po[:, 0:T], lhsT=vpad[:, h, mc, :], rhs=E[:, hh, mc, :], start=(n == 0), stop=(n == 3)),
                        reads=['vpad', ('E' + tag, hh, mc)], writes=[pok])
                    n += 1
            n = 0
            for hh in range(2):
                for mc in range(2):
                    kb.op('pe', lambda p, pd=pd, hh=hh, mc=mc, n=n: p.matmul(
                        pd[:, 0:T], lhsT=onespad[:, hh, :], rhs=E[:, hh, mc, :], start=(n == 0), stop=(n == 3)),
                        reads=['onespad', ('E' + tag, hh, mc)], writes=[pdk])
                    n += 1
            kb.op('dve', lambda v, pd=pd: v.reciprocal(out=rden[:, 0:T], in_=pd[:, 0:T]), reads=[pdk], writes=['rden' + tag])
            kb.op('dve', lambda v, po=po, pr=pr: v.tensor_tensor(out=mixT_dst(pr), in0=po[:, 0:T], in1=rden[:, 0:T], op=ALU.mult),
                  reads=[pok, 'rden' + tag], writes=[('mixT' + tag, 6 + pr)])

    class Tail:
        def __init__(self, layer, st, nbuf=2):
            self.layer = layer
            self.nbuf = nbuf
            L = layer
            self.wout = sb(f"wout{L}", [128, 8, D], BF16, st)
            load_cast(self.wout[:], w_out[L].rearrange("(k p) n -> p k n", p=128), 'wout')
            self.lng = sb(f"lng{L}", [128, D], F32, st)
            self.lnb = sb(f"lnb{L}", [128, D], F32, st)
            bcast_rows(self.lng[:], ln1_g[L], 'lng')
            bcast_rows(self.lnb[:], ln1_b[L], 'lnb')
            self.rw = sb(f"rw{L}", [128, 8, NE], F32, st)
            kb.dma('sp', lambda q: q.dma_start(out=self.rw[:], in_=router_w[L].rearrange("(k p) e -> p k e", p=128)), writes=['rw'])
            self.rb = sb(f"rb{L}", [128, NE], F32, st)
            bcast_rows(self.rb[:], router_b[L], 'rb')
            self.rrun = sb(f"rrun{L}", [128, NE], F32, st)
            kb.op('dve', lambda v: v.memset(self.rrun[:], 0.0), writes=['rrun'])
            self.z = [sb(f"z{L}_{i}", [128, D], F32, st) for i in range(nbuf)]
            self.x1f = [sb(f"x1f{L}_{i}", [128, D], F32, st) for i in range(nbuf)]
            self.x1b = [sb(f"x1b{L}_{i}", [128, D], BF16, st) for i in range(nbuf)]
            self.x1T = sb(f"x1T{L}", [128, 8, 128], F32, st)
            self.sm = [sb(f"sm{L}_{i}", [128, 256], F32, st) for i in range(nbuf)]
            self.smu = [sb(f"smu{L}_{i}", [128, 8], U32, st) for i in range(nbuf)]
            self.n = 0

        def run(self, *a):
            for _ in self.run_gen(*a):
                pass

        def run_gen(self, ti, mixT_ap, mix_keys, xres_ap, xres_key, x1dst):
            i = self.n % self.nbuf
            self.n += 1
            z, x1f, x1b, sm, smu = self.z[i], self.x1f[i], self.x1b[i], self.sm[i], self.smu[i]
            zk, x1fk, x1bk, smk = ('z', i), ('x1f', i), ('x1b', i), ('sm', i)
            for nh in range(2):
                pt, pk = psum()
                for k in range(8):
                    kb.op('pe', lambda p, pt=pt, k=k, nh=nh: p.matmul(pt[:, :], lhsT=mixT_ap(k), rhs=self.wout[:, k, nh * 512:(nh + 1) * 512],
                          start=(k == 0), stop=(k == 7)), reads=['wout'] + list(mix_keys), writes=[pk])
                kb.op('dve', lambda v, pt=pt, nh=nh: v.scalar_tensor_tensor(
                    out=z[:, nh * 512:(nh + 1) * 512], in0=xres_ap[:, nh * 512:(nh + 1) * 512], scalar=ALPHA,
                    in1=pt[:, :], op0=ALU.mult, op1=ALU.add), reads=[pk, xres_key], writes=[zk])
                yield
            self.layernorm(z, zk, x1f, x1fk, sm, smk, self.lng, self.lnb)
            kb.dma('sp', lambda q: q.dma_start(out=x1dst, in_=x1f[:]), reads=[x1fk], writes=[('x1d', ti)])
            yield
            kb.op('act', lambda a: a.copy(out=x1b[:], in_=x1f[:]), reads=[x1fk], writes=[x1bk])
            yield
            for half in range(2):
                pt, pk = psum()
                for kk in range(4):
                    k = half * 4 + kk
                    kb.op('pe', lambda p, pt=pt, k=k, kk=kk: p.transpose(out=pt[:, kk * 128:(kk + 1) * 128],
                          in_=x1f[:, k * 128:(k + 1) * 128], identity=ident_f[:]), reads=[x1fk, 'ident_f'], writes=[pk])
                kb.op('act', lambda a, pt=pt, half=half: a.copy(
                    out=self.x1T[:, half * 4:(half + 1) * 4, :].rearrange("p k t -> p (k t)"), in_=pt[:, :]),
                    reads=[pk], writes=['x1T'])
            yield
            pl, plk = psum()
            for k in range(8):
                kb.op('pe', lambda p, k=k: p.matmul(pl[:, 0:NE], lhsT=self.x1T[:, k, :], rhs=self.rw[:, k, :],
                      start=(k == 0), stop=(k == 7)), reads=['x1T', 'rw'], writes=[plk])
            lg = sm[:, 0:32]
            v8 = sm[:, 32:40]
            mask = sm[:, 40:72]
            slot = sm[:, 72:104]
            junk = sm[:, 104:136]
            idxf = sm[:, 136:144]
            destf = sm[:, 144:148]
            ev = sm[:, 148:152]
            nm = sm[:, 152:153]
            gsum = sm[:, 153:154]
            bad = sm[:, 160:192]
            kb.op('dve', lambda v: v.tensor_tensor(out=lg, in0=pl[:, 0:NE], in1=self.rb[:], op=ALU.add),
                  reads=[plk, 'rb'], writes=[smk])
            kb.op('dve', lambda v: v.max(out=v8, in_=lg), reads=[smk], writes=[smk])
            kb.op('dve', lambda v: v.max_index(out=smu[:], in_max=v8, in_values=lg), reads=[smk], writes=[('smu', i)])
            kb.op('dve', lambda v: v.tensor_scalar(out=mask, in0=lg, scalar1=v8[:, 3:4], scalar2=None, op0=ALU.is_ge),
                  reads=[smk], writes=[smk])
            yield
            pc, pck = psum()
            kb.op('pe', lambda p: p.matmul(pc[:, 0:NE], lhsT=tri_f[:], rhs=mask, start=True, stop=True),
                  reads=['tri_f', smk], writes=[pck])
            kb.op('pe', lambda p: p.matmul(pc[:, NE:2 * NE], lhsT=ones_f[:], rhs=mask, start=True, stop=True),
                  reads=['ones_f', smk], writes=[pck])
            kb.op('dve', lambda v: v.tensor_tensor(out=slot, in0=pc[:, 0:NE], in1=self.rrun[:], op=ALU.add),
                  reads=[pck, 'rrun'], writes=[smk])
            kb.op('dve', lambda v: v.tensor_tensor(out=self.rrun[:], in0=pc[:, NE:2 * NE], in1=self.rrun[:], op=ALU.add),
                  reads=[pck, 'rrun'], writes=['rrun'])
            kb.op('dve', lambda v: v.tensor_scalar(out=bad, in0=slot, scalar1=float(CAP), scalar2=BIGOOB, op0=ALU.is_ge, op1=ALU.mult),
                  reads=[smk], writes=[smk])
            kb.op('dve', lambda v: v.tensor_tensor(out=slot, in0=slot, in1=bad, op=ALU.add), reads=[smk], writes=[smk])
            kb.op('dve', lambda v: v.tensor_tensor(out=slot, in0=slot, in1=ecap[:], op=ALU.add), reads=[smk, 'ecap'], writes=[smk])
            yield
            kb.op('dve', lambda v: v.tensor_copy(out=idxf, in_=smu[:]), reads=[('smu', i)], writes=[smk])
            for k in range(4):
                kb.op('dve', lambda v, k=k: v.scalar_tensor_tensor(out=junk, in0=iota_e[:], scalar=idxf[:, k:k + 1], in1=slot,
                      op0=ALU.is_equal, op1=ALU.mult, accum_out=destf[:, k:k + 1]), reads=[smk, 'iota_e'], writes=[smk])
            kb.op('dve', lambda v: v.tensor_copy(out=destall[:, ti, :], in_=destf), reads=[smk], writes=[('dest', ti)])
            kb.op('dve', lambda v: v.tensor_scalar(out=nm, in0=v8[:, 0:1], scalar1=-1.0, scalar2=None, op0=ALU.mult),
                  reads=[smk], writes=[smk])
            kb.op('act', lambda a: a.activation(out=ev, in_=v8[:, 0:4], func=AF.Exp, bias=nm, scale=1.0, accum_out=gsum),
                  reads=[smk], writes=[smk])
            yield
            kb.op('dve', lambda v: v.reciprocal(out=gsum, in_=gsum), reads=[smk], writes=[smk])
            kb.op('dve', lambda v: v.tensor_scalar(out=gall[:, ti, :], in0=ev, scalar1=gsum, scalar2=None, op0=ALU.mult),
                  reads=[smk], writes=[('gate', ti)])
            for k in range(4):
                kb.dma('pool', lambda q, k=k: q.indirect_dma_start(
                    out=xg[:, :], out_offset=bass.IndirectOffsetOnAxis(ap=destall[:, ti, k:k + 1], axis=0),
                    in_=x1b[:, :], in_offset=None, bounds_check=bc_reg, oob_is_err=False),
                    reads=[x1bk, ('dest', ti)], writes=['xg'])

        def layernorm(self, z, zk, o, ok, sm, smk, g, b, gk='lng', bk='lnb'):
            st6 = sm[:, 200:212]
            mv = sm[:, 212:214]
            rstd = sm[:, 214:215]
            for c in range(2):
                kb.op('dve', lambda v, c=c: v.bn_stats(out=st6[:, c * 6:(c + 1) * 6], in_=z[:, c * 512:(c + 1) * 512]),
                      reads=[zk], writes=[smk])
            kb.op('dve', lambda v: v.bn_aggr(out=mv, in_=st6), reads=[smk], writes=[smk])
            kb.op('act', lambda a: a.activation(out=rstd, in_=mv[:, 1:2], func=AF.Sqrt, bias=1e-5, scale=1.0), reads=[smk], writes=[smk])
            kb.op('dve', lambda v: v.reciprocal(out=rstd, in_=rstd), reads=[smk], writes=[smk])
            kb.op('dve', lambda v: v.tensor_scalar(out=o[:], in0=z[:], scalar1=mv[:, 0:1], scalar2=rstd, op0=ALU.subtract, op1=ALU.mult),
                  reads=[zk, smk], writes=[ok])
            kb.op('pool', lambda p: p.tensor_tensor(out=o[:], in0=o[:], in1=g[:], op=ALU.mult), reads=[ok, gk], writes=[ok])
            kb.op('pool', lambda p: p.tensor_tensor(out=o[:], in0=o[:], in1=b[:], op=ALU.add), reads=[ok, bk], writes=[ok])

    def phase_a0(ntiles=NT):
        T = 256
        st = ExitStack()
        tail = Tail(0, st)
        kT = sb("kT0", [128, 2, MEM], BF16, st)
        vpad = sb("vpad0", [128, 4, 2, 128], BF16, st)
        onespad = sb("onespad0", [128, 2, 128], BF16, st)
        setup_mem_kv(0, st, kT, vpad, onespad)
        win = sb("win0", [128, 8, W_IN_A], BF16, st)
        load_cast(win[:], a_w_in.rearrange("(k p) c -> p k c", p=128), 'win')
        wr_bd = sb("wr_bd", [128, 6, 128], BF16, st)
        wi_bd = sb("wi_bd", [128, 6, 128], BF16, st)
        kb.op('pool', lambda g: g.memset(wr_bd[:], 0.0), writes=['wr_bd'])
        kb.op('pool', lambda g: g.memset(wi_bd[:], 0.0), writes=['wi_bd'])
        for n in range(12):
            j, par = n // 2, n % 2
            load_cast(wr_bd[par * 64:(par + 1) * 64, j, par * 64:(par + 1) * 64], a_wr[n], 'wr_bd')
            load_cast(wi_bd[par * 64:(par + 1) * 64, j, par * 64:(par + 1) * 64], a_wi[n], 'wi_bd')
        cw = sb("cw", [128, 4, 6], F32, st)
        vecs = sb("vecs", [128, 4, 6], F32, st)
        with nc.allow_non_contiguous_dma(reason="tiny per-channel vectors"):
            kb.dma('sp', lambda q: q.dma_start(out=cw[:], in_=a_conv_w.rearrange("w (j p) -> p w j", p=128)), writes=['cw'])
            for n, v_ in enumerate((a_conv_b, a_br, a_bi, a_lambda)):
                kb.dma('sp', lambda q, n=n, v_=v_: q.dma_start(out=vecs[:, n, :], in_=v_.rearrange("(j p) -> p j", p=128)), writes=['vecs'])
        coef = sb("coef", [128, 6], F32, st)
        kb.op('act', lambda a: a.activation(out=coef[:], in_=vecs[:, 3, :], func=AF.Exp, scale=-1.0), reads=['vecs'], writes=['coef'])
        kb.op('act', lambda a: a.activation(out=coef[:], in_=coef[:], func=AF.Ln, bias=1.0, scale=1.0), reads=['coef'], writes=['coef'])
        kb.op('dve', lambda v: v.tensor_scalar(out=coef[:], in0=coef[:], scalar1=-8.0, scalar2=None, op0=ALU.mult), reads=['coef'], writes=['coef'])

        xf = [sb(f"xf{i}", [128, 2, D], F32, st) for i in range(2)]
        xbf = sb("xbf", [128, 2, D], BF16, st)
        xT = sb("xT", [128, 8, T], BF16, st)
        xbh = sb("xbh", [128, 6, 3 + T], F32, st)
        gbT = sb("gbT", [128, 6, T], F32, st)
        mqT = sb("mqT", [128, 2, T], BF16, st)
        mixT = [sb(f"mixT{i}", [128, 8, T], BF16, st) for i in range(2)]
        E = sb("E0", [128, 2, 2, T], BF16, st)
        rden = sb("rden0", [128, T], F32, st)
        NTMP = 10
        tmp = [[sb(f"tmp{n}_{i}", [128, T], F32, st) for i in range(2)] for n in range(NTMP)]
        xcb = [sb(f"xcb{i}", [128, T], BF16, st) for i in range(2)]
        hbuf = [sb(f"hbuf{i}", [128, 6, T], F32, st) for i in range(2)]
        kb.op('dve', lambda v: v.memset(xbh[:], 0.0), writes=['xbh'])
        zt = sb("zt", [128, 4096], BF16, st)
        kb.op('pool', lambda g: g.memset(zt[:], 0.0), writes=['zt'])
        for r0 in range(0, NROW, 512):
            kb.dma('sp', lambda q, r0=r0: q.dma_start(out=xg[r0:r0 + 512, :].rearrange("(p t) d -> p (t d)", t=4), in_=zt[:]),
                   reads=['zt'], writes=['xg'])

        nch = ntiles * 128 // T

        def mixer(ci):
            xi = ci % 2
            xfc, xfk = xf[xi], ('xf', xi)
            kb.dma('sp', lambda q, ci=ci, xfc=xfc: q.dma_start(
                out=xfc[:], in_=x_in[ci * T:(ci + 1) * T, :].rearrange("(t p) d -> p t d", p=128)), writes=[xfk])
            kb.op('act', lambda a, xfc=xfc: a.copy(out=xbf[:], in_=xfc[:]), reads=[xfk], writes=['xbf'])
            for k in range(8):
                pt, pk = psum()
                ptb = pt[:].bitcast(BF16)
                for t in range(2):
                    kb.op('pe', lambda p, ptb=ptb, t=t, k=k: p.transpose(out=ptb[:, t * 128:(t + 1) * 128],
                          in_=xbf[:, t, k * 128:(k + 1) * 128], identity=ident_b[:]), reads=['xbf', 'ident_b'], writes=[pk])
                kb.op('dve' if k % 2 == 0 else 'act',
                      (lambda v, ptb=ptb, k=k: v.tensor_copy(out=xT[:, k, :], in_=ptb[:, 0:T])) if k % 2 == 0 else
                      (lambda a, ptb=ptb, k=k: a.copy(out=xT[:, k, :], in_=ptb[:, 0:T])),
                      reads=[pk], writes=[('xT', k)])
            yield
            xTk = [('xT', k) for k in range(8)]
            for c in range(14):
                pt, pk = psum()
                for k in range(8):
                    kb.op('pe', lambda p, pt=pt, k=k, c=c: p.matmul(pt[:, 0:T], lhsT=win[:, k, c * 128:(c + 1) * 128],
                          rhs=xT[:, k, :], start=(k == 0), stop=(k == 7)), reads=['win'] + xTk, writes=[pk])
                if c < 6:
                    kb.op('act', lambda a, pt=pt, c=c: a.copy(out=xbh[:, c, 3:3 + T], in_=pt[:, 0:T]), reads=[pk], writes=[('xbh', c)])
                elif c < 12:
                    kb.op('act', lambda a, pt=pt, c=c: a.copy(out=gbT[:, c - 6, :], in_=pt[:, 0:T]), reads=[pk], writes=[('gbT', c - 6)])
                else:
                    kb.op('dve', lambda v, pt=pt, c=c: v.tensor_copy(out=mqT[:, c - 12, :], in_=pt[:, 0:T]), reads=[pk], writes=['mqT0'])
                if c % 2 == 1:
                    yield
            mx = mixT[ci % 2]
            hb = hbuf[ci % 2]
            hprev = hbuf[(ci + 1) % 2]
            for c in range(6):
                r_ = c % 2
                xc, rr, ii, aa, ss, uu, sq, t2, sg, gl = [tmp[n][r_] for n in range(NTMP)]
                tk = [(f'tmp{n}', r_) for n in range(NTMP)]
                xck = tk[0]
                kb.op('dve', lambda v, c=c, xc=xc: v.tensor_scalar(out=xc[:], in0=xbh[:, c, 3:3 + T], scalar1=cw[:, 3, c:c + 1],
                      scalar2=vecs[:, 0, c:c + 1], op0=ALU.mult, op1=ALU.add), reads=[('xbh', c), 'cw', 'vecs'], writes=[xck])
                for j in range(3):
                    kb.op('dve', lambda v, c=c, j=j, xc=xc: v.scalar_tensor_tensor(out=xc[:], in0=xbh[:, c, j:j + T], scalar=cw[:, j, c:c + 1],
                          in1=xc[:], op0=ALU.mult, op1=ALU.add), reads=[('xbh', c), 'cw', xck], writes=[xck])
                kb.op('pool', lambda g, c=c: g.tensor_copy(out=xbh[:, c, 0:3], in_=xbh[:, c, T:T + 3]), reads=[('xbh', c)], writes=[('xbh', c)])
                kb.op('pool', lambda g, xc=xc, r_=r_: g.tensor_copy(out=xcb[r_][:], in_=xc[:]), reads=[xck], writes=[('xcb', r_)])
                pr_, prk = psum()
                kb.op('pe', lambda p, pr_=pr_, c=c, r_=r_: p.matmul(pr_[:, 0:T], lhsT=wr_bd[:, c, :], rhs=xcb[r_][:], start=True, stop=True),
                      reads=['wr_bd', ('xcb', r_)], writes=[prk])
                pi_, pik = psum()
                kb.op('pe', lambda p, pi_=pi_, c=c, r_=r_: p.matmul(pi_[:, 0:T], lhsT=wi_bd[:, c, :], rhs=xcb[r_][:], start=True, stop=True),
                      reads=['wi_bd', ('xcb', r_)], writes=[pik])
                kb.op('act', lambda a, pr_=pr_, c=c, rr=rr: a.activation(out=rr[:], in_=pr_[:, 0:T], func=AF.Sigmoid, bias=vecs[:, 1, c:c + 1], scale=1.0),
                      reads=[prk, 'vecs'], writes=[tk[1]])
                kb.op('act', lambda a, pi_=pi_, c=c, ii=ii: a.activation(out=ii[:], in_=pi_[:, 0:T], func=AF.Sigmoid, bias=vecs[:, 2, c:c + 1], scale=1.0),
                      reads=[pik, 'vecs'], writes=[tk[2]])
                yield
                gb = gbT[:, c, :]
                kb.op('pool', lambda g, gb=gb, sq=sq: g.tensor_tensor(out=sq[:], in0=gb, in1=gb, op=ALU.mult), reads=[('gbT', c)], writes=[tk[6]])
                kb.op('pool', lambda g, sq=sq: g.tensor_scalar(out=sq[:], in0=sq[:], scalar1=0.044715, scalar2=1.0, op0=ALU.mult, op1=ALU.add),
                      reads=[tk[6]], writes=[tk[6]])
                kb.op('pool', lambda g, gb=gb, sq=sq, t2=t2: g.tensor_tensor(out=t2[:], in0=sq[:], in1=gb, op=ALU.mult), reads=[tk[6], ('gbT', c)], writes=[tk[7]])
                kb.op('act', lambda a, t2=t2, sg=sg: a.activation(out=sg[:], in_=t2[:], func=AF.Sigmoid, scale=1.5957691216057308),
                      reads=[tk[7]], writes=[tk[8]])
                kb.op('act', lambda a, aa=aa, rr=rr, c=c: a.activation(out=aa[:], in_=rr[:], func=AF.Exp, scale=coef[:, c:c + 1]),
                      reads=[tk[1], 'coef'], writes=[tk[3]])
                kb.op('pool', lambda g, aa=aa, ss=ss: g.tensor_tensor(out=ss[:], in0=aa[:], in1=aa[:], op=ALU.mult), reads=[tk[3]], writes=[tk[4]])
                kb.op('act', lambda a, ss=ss: a.activation(out=ss[:], in_=ss[:], func=AF.Sqrt, bias=1.0, scale=-1.0), reads=[tk[4]], writes=[tk[4]])
                kb.op('pool', lambda g, uu=uu, ss=ss, ii=ii: g.tensor_tensor(out=uu[:], in0=ss[:], in1=ii[:], op=ALU.mult), reads=[tk[4], tk[2]], writes=[tk[5]])
                kb.op('dve', lambda v, uu=uu, xc=xc: v.tensor_tensor(out=uu[:], in0=uu[:], in1=xc[:], op=ALU.mult), reads=[tk[5], xck], writes=[tk[5]])
                init = 0.0 if ci == 0 else hprev[:, c, T - 1:T]
                kb.op('dve', lambda v, aa=aa, uu=uu, c=c, init=init, hb=hb: v.tensor_tensor_scan(out=hb[:, c, :], data0=aa[:], data1=uu[:],
                      initial=init, op0=ALU.mult, op1=ALU.add), reads=[tk[3], tk[5], ('h', (ci + 1) % 2, c)], writes=[('h', ci % 2, c)])
                kb.op('pool', lambda g, gl=gl, sg=sg, gb=gb: g.tensor_tensor(out=gl[:], in0=sg[:], in1=gb, op=ALU.mult), reads=[tk[8], ('gbT', c)], writes=[tk[9]])
                kb.op('dve', lambda v, gl=gl, c=c, hb=hb, mx=mx: v.tensor_tensor(out=mx[:, c, :], in0=hb[:, c, :], in1=gl[:], op=ALU.mult),
                      reads=[tk[9], ('h', ci % 2, c)], writes=[('mixT0', c)])
                yield
            mem_attn(mqT, T, kT, vpad, onespad, lambda pr, mx=mx: mx[:, 6 + pr, :], E, rden, '0')
            yield

        mixkeys = [('mixT0', c) for c in range(8)]

        def tails(ci):
            mx = mixT[ci % 2]
            xfc, xfk = xf[ci % 2], ('xf', ci % 2)
            for t in range(T // 128):
                ti = ci * (T // 128) + t
                yield from tail.run_gen(ti, lambda k, mx=mx, t=t: mx[:, k, t * 128:(t + 1) * 128], mixkeys, xfc[:, t, :], xfk,
                                        x1buf[ti * 128:(ti + 1) * 128, :])

        for _ in mixer(0):
            pass
        for ci in range(nch):
            streams = [(tails(ci), 18)]
            if ci + 1 < nch:
                streams.append((mixer(ci + 1), 25))
            interleave(streams)
        kb.barrier()
        if mode.startswith("a0"):
            dbg_r = nc.dram_tensor("dbg_r", [128, NE], F32, kind="ExternalOutput").ap()
            dbg_d = nc.dram_tensor("dbg_d", [128, NT * 4], I32, kind="ExternalOutput").ap()
            kb.dma('sp', lambda q: q.dma_start(out=dbg_r[:, :], in_=tail.rrun[:]))
            kb.dma('sp', lambda q: q.dma_start(out=dbg_d[:, :], in_=destall[:].rearrange("p t k -> p (t k)")))
            kb.barrier()
        st.close()


    def phase_a1(src, ntiles=NT):
        T = 256
        KSEL = 256
        NBIS = 14
        CH = 512
        st = ExitStack()
        tail = Tail(1, st, nbuf=1)
        cTok = sb("cTok", [128, NT, 128], BF16, st)
        cT = sb("cT", [128, S], BF16, st)
        ikT2 = sb("ikT2", [128, S], BF16, st)
        absw = sb("absw", [128, NT, 4], F32, st)
        sgnw = sb("sgnw", [128, NT, 4], F32, st)
        BT = sb("BT", [128, 2, 12, 128], BF16, st)
        wuvpad = sb("wuvpad", [128, 12, 128], BF16, st)
        i4big = sb("i4big", [128, 4, 128], BF16, st)
        for r in range(4):
            kb.op('dve', lambda v, r=r: v.tensor_scalar(out=i4big[:, r, :], in0=ident_f[:], scalar1=100.0, scalar2=None, op0=ALU.mult),
                  reads=['ident_f'], writes=['i4big'])
        kb.op('pool', lambda g: g.memset(wuvpad[:], 0.0), writes=['wuvpad'])
        for h in range(12):
            par = h % 2
            load_cast(wuvpad[:, h, par * 64:(par + 1) * 64], b_w_uv[:, h, :], 'wuvpad')
        s1 = ExitStack()
        kT = sb("kT1", [128, 2, MEM], BF16, s1)
        vpad = sb("vpad1", [128, 4, 2, 128], BF16, s1)
        onespad = sb("onespad1", [128, 2, 128], BF16, s1)
        win = sb("win1", [128, 8, W_IN_B], BF16, s1)
        load_cast(win[:], b_w_in.rearrange("(k p) c -> p k c", p=128), 'win')
        wukT = sb("wukT", [128, 6, 128], BF16, s1)
        gkv = sb("gkv", [128, 128], F32, s1)
        gik = sb("gik", [128, 64], F32, s1)
        bik = sb("bik", [128, 64], F32, s1)
        bcast_rows(gkv[:], b_kv_norm_g, 'gkv')
        bcast_rows(gik[:], b_idx_norm_g, 'gik')
        bcast_rows(bik[:], b_idx_norm_b, 'bik')
        ssetup = ExitStack()
        setup_mem_kv(1, ssetup, kT, vpad, onespad)
        wuk = sb("wuk", [128, 768], BF16, ssetup)
        load_cast(wuk[:], b_w_uk.rearrange("r h d -> r (h d)"), 'wuk')
        for j in range(6):
            pt, pk = psum()
            ptb = pt[:].bitcast(BF16)
            kb.op('pe', lambda p, ptb=ptb, j=j: p.transpose(out=ptb[:, 0:128], in_=wuk[:, j * 128:(j + 1) * 128], identity=ident_b[:]),
                  reads=['wuk', 'ident_b'], writes=[pk])
            kb.op('dve', lambda v, ptb=ptb, j=j: v.tensor_copy(out=wukT[:, j, :], in_=ptb[:, 0:128]), reads=[pk], writes=['wukT'])
        rbb = sb("rbb", [128, 32, 12], F32, ssetup)
        bkt = sb("bkt", [128, 2, 128], F32, ssetup)
        caus = sb("caus", [128, 128], F32, ssetup)
        acc = sb("bacc", [128, 12, 128], F32, ssetup)
        prod = sb("bprod", [128, 12, 128], F32, ssetup)
        oh = sb("boh", [128, 128], F32, ssetup)
        kb.dma('sp', lambda q: q.dma_start(out=rbb[:].rearrange("p b h -> p (b h)"), in_=rel_bias.rearrange("b h -> (b h)").partition_broadcast(128)), writes=['rbb'])
        kb.dma('sp', lambda q: q.dma_start(out=bkt[:], in_=c_bkt[:, :, :]), writes=['bkt'])
        kb.dma('sp', lambda q: q.dma_start(out=caus[:], in_=c_caus[:, :]), writes=['caus'])
        for dt in range(2):
            kb.op('dve', lambda v: v.memset(acc[:], 0.0), writes=['bacc'])
            for b in range(32):
                kb.op('dve', lambda v, b=b, dt=dt: v.tensor_scalar(out=oh[:], in0=bkt[:, dt, :], scalar1=float(b), scalar2=None, op0=ALU.is_equal),
                      reads=['bkt'], writes=['boh'])
                kb.op('dve', lambda v, b=b: v.tensor_tensor(out=prod[:], in0=oh[:].unsqueeze(1).to_broadcast([128, 12, 128]),
                      in1=rbb[:, b, :].unsqueeze(2).to_broadcast([128, 12, 128]), op=ALU.mult), reads=['boh', 'rbb'], writes=['bprod'])
                kb.op('dve', lambda v: v.tensor_tensor(out=acc[:], in0=acc[:], in1=prod[:], op=ALU.add), reads=['bacc', 'bprod'], writes=['bacc'])
            kb.op('dve', lambda v: v.tensor_tensor(out=acc[:], in0=acc[:], in1=rbb[:, 31, :].unsqueeze(2).to_broadcast([128, 12, 128]), op=ALU.subtract),
                  reads=['bacc', 'rbb'], writes=['bacc'])
            if dt == 0:
                kb.op('dve', lambda v: v.tensor_tensor(out=acc[:], in0=acc[:], in1=caus[:].unsqueeze(1).to_broadcast([128, 12, 128]), op=ALU.add),
                      reads=['bacc', 'caus'], writes=['bacc'])
            kb.op('dve', lambda v, dt=dt: v.tensor_copy(out=BT[:, dt, :, :], in_=acc[:]), reads=['bacc'], writes=['BT'])
        kb.barrier()
        ssetup.close()

        xf = [sb(f"xf1_{i}", [128, 2, D], F32, s1) for i in range(2)]
        xbf = sb("xbf1", [128, 2, D], BF16, s1)
        xT = sb("xT1", [128, 8, T], BF16, s1)
        qT = sb("qT1", [128, 6, T], BF16, s1)
        qlb = [sb(f"qlb{i}", [128, 12, T], BF16, s1) for i in range(2)]
        iqT = [sb(f"iqT{i}", [128, 2, T], BF16, s1) for i in range(2)]
        mqT = sb("mqT1", [128, 2, T], BF16, s1)
        memo = [sb(f"memo{i}", [128, 2, T], BF16, s1) for i in range(2)]
        E = sb("E1", [128, 2, 2, T], BF16, s1)
        rden = sb("rden1", [128, T], F32, s1)
        csb = [sb(f"csb{i}", [128, 128], F32, s1) for i in range(2)]
        cnb = [sb(f"cnb{i}", [128, 128], BF16, s1) for i in range(2)]
        iks = [sb(f"iks{i}", [128, 68], F32, s1) for i in range(2)]
        ik2 = [sb(f"ik2{i}", [128, 128], BF16, s1) for i in range(2)]
        sm1 = [sb(f"smp1_{i}", [128, 32], F32, s1) for i in range(2)]
        nch = ntiles * 128 // T
        for ci in range(nch):
            xi = ci % 2
            xfc, xfk = xf[xi], ('xf', xi)
            kb.dma('sp', lambda q, ci=ci, xfc=xfc: q.dma_start(
                out=xfc[:], in_=src[ci * T:(ci + 1) * T, :].rearrange("(t p) d -> p t d", p=128)), writes=[xfk])
            kb.op('act', lambda a, xfc=xfc: a.copy(out=xbf[:], in_=xfc[:]), reads=[xfk], writes=['xbf'])
            for k in range(8):
                pt, pk = psum()
                ptb = pt[:].bitcast(BF16)
                for t in range(2):
                    kb.op('pe', lambda p, ptb=ptb, t=t, k=k: p.transpose(out=ptb[:, t * 128:(t + 1) * 128],
                          in_=xbf[:, t, k * 128:(k + 1) * 128], identity=ident_b[:]), reads=['xbf', 'ident_b'], writes=[pk])
                if k % 2 == 0:
                    kb.op('dve', lambda v, ptb=ptb, k=k: v.tensor_copy(out=xT[:, k, :], in_=ptb[:, 0:T]), reads=[pk], writes=[('xT', k)])
                else:
                    kb.op('act', lambda a, ptb=ptb, k=k: a.copy(out=xT[:, k, :], in_=ptb[:, 0:T]), reads=[pk], writes=[('xT', k)])
            xTk = [('xT', k) for k in range(8)]

            def fm_proj(col0, dst_ap, dkey, eng):
                pt, pk = psum()
                for k in range(8):
                    kb.op('pe', lambda p, pt=pt, k=k: p.matmul(pt[:, 0:T], lhsT=win[:, k, col0:col0 + 128], rhs=xT[:, k, :],
                          start=(k == 0), stop=(k == 7)), reads=['win'] + xTk, writes=[pk])
                if eng == 'act':
                    kb.op('act', lambda a, pt=pt: a.copy(out=dst_ap, in_=pt[:, 0:T]), reads=[pk], writes=[dkey])
                else:
                    kb.op('dve', lambda v, pt=pt: v.tensor_copy(out=dst_ap, in_=pt[:, 0:T]), reads=[pk], writes=[dkey])

            for c in range(6):
                fm_proj(c * 128, qT[:, c, :], ('qT', c), 'act' if c % 2 else 'dve')
            bi = ci % 2
            for j in range(2):
                fm_proj(896 + j * 128, iqT[bi][:, j, :], ('iqT', bi), 'act')
            for j in range(2):
                fm_proj(1220 + j * 128, mqT[:, j, :], 'mqT1', 'dve')
            kb.dma('sp', lambda q, ci=ci, bi=bi: q.dma_start(out=iqd[:, :, ci * T:(ci + 1) * T].rearrange("j p t -> p j t"), in_=iqT[bi][:]),
                   reads=[('iqT', bi)], writes=['iqd'])
            for h in range(12):
                j, hh = h // 2, h % 2
                pt, pk = psum()
                kb.op('pe', lambda p, pt=pt, j=j, hh=hh: p.matmul(pt[:, 0:T], lhsT=wukT[hh * 64:(hh + 1) * 64, j, :],
                      rhs=qT[hh * 64:(hh + 1) * 64, j, :], start=True, stop=True), reads=['wukT', ('qT', j)], writes=[pk])
                if h % 2 == 0:
                    kb.op('act', lambda a, pt=pt, h=h, bi=bi: a.activation(out=qlb[bi][:, h, :], in_=pt[:, 0:T], func=AF.Copy, scale=0.125),
                          reads=[pk], writes=[('qlb', bi)])
                else:
                    kb.op('dve', lambda v, pt=pt, h=h, bi=bi: v.tensor_scalar(out=qlb[bi][:, h, :], in0=pt[:, 0:T], scalar1=0.125, scalar2=None, op0=ALU.mult),
                          reads=[pk], writes=[('qlb', bi)])
            kb.dma('sp', lambda q, ci=ci, bi=bi: q.dma_start(out=qlat[:, :, ci * T:(ci + 1) * T].rearrange("h p t -> p h t"), in_=qlb[bi][:]),
                   reads=[('qlb', bi)], writes=['qlat'])
            mem_attn(mqT, T, kT, vpad, onespad, lambda pr, bi=bi: memo[bi][:, pr, :], E, rden, '1')
            kb.dma('sp', lambda q, ci=ci, bi=bi: q.dma_start(out=memod[:, :, ci * T:(ci + 1) * T].rearrange("j p t -> p j t"), in_=memo[bi][:]),
                   reads=[('mixT1', 6), ('mixT1', 7)], writes=['memod'])
            for t in range(T // 128):
                ti = ci * (T // 128) + t
                i2 = ti % 2
                smk = ('sm1', i2)
                sm = sm1[i2]
                pc, pck = psum()
                for k in range(8):
                    kb.op('pe', lambda p, pc=pc, k=k, t=t: p.matmul(pc[:, 0:128], lhsT=xT[:, k, t * 128:(t + 1) * 128], rhs=win[:, k, 768:896],
                          start=(k == 0), stop=(k == 7)), reads=['win'] + xTk, writes=[pck])
                pi_, pik = psum()
                for k in range(8):
                    kb.op('pe', lambda p, pi_=pi_, k=k, t=t: p.matmul(pi_[:, 0:68], lhsT=xT[:, k, t * 128:(t + 1) * 128], rhs=win[:, k, 1152:1220],
                          start=(k == 0), stop=(k == 7)), reads=['win'] + xTk, writes=[pik])
                ss = sm[:, 0:1]
                kb.op('act', lambda a, pc=pc, i2=i2, ss=ss: a.activation(out=csb[i2][:], in_=pc[:, 0:128], func=AF.Square, accum_out=ss),
                      reads=[pck], writes=[('csb', i2), smk])
                kb.op('act', lambda a, ss=ss: a.activation(out=ss, in_=ss, func=AF.Sqrt, bias=1e-6, scale=1.0 / 128.0), reads=[smk], writes=[smk])
                kb.op('dve', lambda v, ss=ss: v.reciprocal(out=ss, in_=ss), reads=[smk], writes=[smk])
                kb.op('dve', lambda v, pc=pc, i2=i2, ss=ss: v.scalar_tensor_tensor(out=csb[i2][:], in0=pc[:, 0:128], scalar=ss, in1=gkv[:],
                      op0=ALU.mult, op1=ALU.mult), reads=[pck, smk, 'gkv', ('csb', i2)], writes=[('csb', i2)])
                kb.op('act', lambda a, i2=i2, ti=ti: a.copy(out=cTok[:, ti, :], in_=csb[i2][:]), reads=[('csb', i2)], writes=[('cTok', ti)])
                pt, pk = psum()
                ptb = pt[:].bitcast(BF16)
                kb.op('pe', lambda p, ptb=ptb, ti=ti: p.transpose(out=ptb[:, 0:128], in_=cTok[:, ti, :], identity=ident_b[:]),
                      reads=[('cTok', ti), 'ident_b'], writes=[pk])
                kb.op('dve', lambda v, ptb=ptb, ti=ti: v.tensor_copy(out=cT[:, ti * 128:(ti + 1) * 128], in_=ptb[:, 0:128]), reads=[pk], writes=[('cT', ti)])
                kb.op('act', lambda a, pi_=pi_, i2=i2: a.copy(out=iks[i2][:], in_=pi_[:, 0:68]), reads=[pik], writes=[('iks', i2)])
                st6 = sm[:, 8:14]
                mv = sm[:, 14:16]
                rs = sm[:, 16:17]
                kb.op('dve', lambda v, i2=i2, st6=st6: v.bn_stats(out=st6, in_=iks[i2][:, 0:64]), reads=[('iks', i2)], writes=[smk])
                kb.op('dve', lambda v, st6=st6, mv=mv: v.bn_aggr(out=mv, in_=st6), reads=[smk], writes=[smk])
                kb.op('act', lambda a, mv=mv, rs=rs: a.activation(out=rs, in_=mv[:, 1:2], func=AF.Sqrt, bias=1e-5, scale=1.0), reads=[smk], writes=[smk])
                kb.op('dve', lambda v, rs=rs: v.reciprocal(out=rs, in_=rs), reads=[smk], writes=[smk])
                kb.op('dve', lambda v, i2=i2, mv=mv, rs=rs: v.tensor_scalar(out=iks[i2][:, 0:64], in0=iks[i2][:, 0:64], scalar1=mv[:, 0:1], scalar2=rs,
                      op0=ALU.subtract, op1=ALU.mult), reads=[('iks', i2), smk], writes=[('iks', i2)])
                kb.op('dve', lambda v, i2=i2: v.tensor_tensor(out=iks[i2][:, 0:64], in0=iks[i2][:, 0:64], in1=gik[:], op=ALU.mult),
                      reads=[('iks', i2), 'gik'], writes=[('iks', i2)])
                for r in range(2):
                    kb.op('dve', lambda v, i2=i2, r=r: v.tensor_tensor(out=ik2[i2][:, r * 64:(r + 1) * 64], in0=iks[i2][:, 0:64], in1=bik[:], op=ALU.add),
                          reads=[('iks', i2), 'bik'], writes=[('ik2', i2)])
                pt, pk = psum()
                ptb = pt[:].bitcast(BF16)
                kb.op('pe', lambda p, ptb=ptb, i2=i2: p.transpose(out=ptb[:, 0:128], in_=ik2[i2][:], identity=ident_b[:]),
                      reads=[('ik2', i2), 'ident_b'], writes=[pk])
                kb.op('act', lambda a, ptb=ptb, ti=ti: a.copy(out=ikT2[:, ti * 128:(ti + 1) * 128], in_=ptb[:, 0:128]), reads=[pk], writes=[('ikT2', ti)])
                kb.op('act', lambda a, i2=i2, ti=ti: a.activation(out=absw[:, ti, :], in_=iks[i2][:, 64:68], func=AF.Abs),
                      reads=[('iks', i2)], writes=[('absw', ti)])
                kb.op('dve', lambda v, i2=i2, ti=ti: v.tensor_scalar(out=sgnw[:, ti, :], in0=iks[i2][:, 64:68], scalar1=0.0, scalar2=2.0, op0=ALU.is_ge, op1=ALU.mult),
                      reads=[('iks', i2)], writes=[('sgnw', ti)])
                kb.op('dve', lambda v, ti=ti: v.tensor_scalar(out=sgnw[:, ti, :], in0=sgnw[:, ti, :], scalar1=-1.0, scalar2=None, op0=ALU.add),
                      reads=[('sgnw', ti)], writes=[('sgnw', ti)])
        kb.barrier()
        s1.close()

        s2 = ExitStack()
        sc = sb("sc", [128, S], F32, s2)
        junk = sb("junk", [128, S // 2 + 128], mybir.dt.uint8, s2)
        junk2 = sb("junk2", [128, S], mybir.dt.uint8, s2) if False else None
        maskb = sb("maskb", [128, S], BF16, s2)
        tiec = [sb(f"tiec{i}", [128, CH], BF16, s2) for i in range(2)]
        cumc = [sb(f"cumc{i}", [128, CH], F32, s2) for i in range(2)]
        onesc = sb("onesc", [128, CH], BF16, s2)
        kb.op('pool', lambda g: g.memset(onesc[:], 1.0), writes=['onesc'])
        cmask = sb("cmask", [128, 128], F32, s2)
        kb.dma('sp', lambda q: q.dma_start(out=cmask[:], in_=c_cmask[:, :]), writes=['cmask'])
        rl = [sb(f"rl{i}", [128, 512], F32, s2) for i in range(4)]
        ql = sb("ql", [128, 12, 128], BF16, s2)
        iqb = [sb(f"iqb{i}", [128, 2, 128], BF16, s2) for i in range(2)]
        mixT = [sb(f"mixT1_{i}", [128, 8, 128], BF16, s2) for i in range(2)]
        x2t = sb("x2t", [128, D], F32, s2)
        Pt = [sb(f"Pt{i}", [128, 512], BF16, s2) for i in range(3)]
        olat = [sb(f"olat{i}", [128, 512], BF16, s2) for i in range(2)]
        Dsb = sb("Dsb", [128, 512], F32, s2)
        Osb = sb("Osb", [128, 512], F32, s2)
        bs = sb("bs", [128, 16], F32, s2)
        steps = sb("steps", [128, 24], F32, s2)
        nmid = sb("nmid", [128, 1], F32, s2)
        sgs = sb("sgs", [128, 1], F32, s2)
        pow2 = sb("pow2", [128, 24], F32, s2)
        kb.dma('sp', lambda q: q.dma_start(out=pow2[:], in_=c_pow2[:, :]), writes=['pow2'])
        lo, hi, mid, cnt, ge, dd, ee, need, cgt, carry = [bs[:, i:i + 1] for i in range(10)]
        npt = [0]
        natt = [0]
        psrot[0] = 4

        def selection_a(qb):
            n = (qb + 1) * 128
            ib = qb % 2
            kb.dma('sp', lambda q: q.dma_start(out=iqb[ib][:], in_=iqd[:, :, qb * 128:(qb + 1) * 128].rearrange("j p t -> p j t")),
                   writes=[('iqb', ib)])
            for g0 in range(0, n, 512):
                w = min(512, n - g0)
                pts = []
                for h in range(4):
                    j, hh = h // 2, h % 2
                    pt, pk = psum()
                    pts.append((pt, pk))
                    kb.op('pe', lambda p, pt=pt, j=j, hh=hh: p.matmul(pt[:, 0:w], lhsT=iqb[ib][hh * 64:(hh + 1) * 64, j, :],
                          rhs=ikT2[hh * 64:(hh + 1) * 64, g0:g0 + w], start=True, stop=True), reads=[('iqb', ib), 'ikT2'], writes=[pk])
                for h in range(4):
                    pt, pk = pts[h]
                    kb.op('act', lambda a, pt=pt, h=h: a.activation(out=rl[h][:, 0:w], in_=pt[:, 0:w], func=AF.Relu, scale=absw[:, qb, h:h + 1]),
                          reads=[pk], writes=[('rl', h)])
                yield
                for h in range(4):
                    if h == 0:
                        kb.op('dve', lambda v, h=h: v.tensor_scalar(out=sc[:, g0:g0 + w], in0=rl[h][:, 0:w], scalar1=sgnw[:, qb, h:h + 1],
                              scalar2=None, op0=ALU.mult), reads=[('rl', h)], writes=['sc'])
                    else:
                        kb.op('dve', lambda v, h=h: v.scalar_tensor_tensor(out=sc[:, g0:g0 + w], in0=rl[h][:, 0:w], scalar=sgnw[:, qb, h:h + 1],
                              in1=sc[:, g0:g0 + w], op0=ALU.mult, op1=ALU.add), reads=[('rl', h), 'sc'], writes=['sc'])
                yield
            kb.op('dve', lambda v: v.tensor_tensor(out=sc[:, n - 128:n], in0=sc[:, n - 128:n], in1=cmask[:], op=ALU.add), reads=['sc', 'cmask'], writes=['sc'])
            kb.op('dve', lambda v: v.tensor_reduce(out=hi, in_=sc[:, 0:n], axis=AX.X, op=ALU.max), reads=['sc'], writes=['bs'])
            kb.op('dve', lambda v: v.tensor_reduce(out=lo, in_=sc[:, 0:256], axis=AX.X, op=ALU.min), reads=['sc'], writes=['bs'])
            yield
            kb.op('dve', lambda v: v.scalar_tensor_tensor(out=dd, in0=hi, scalar=2.0, in1=lo, op0=ALU.add, op1=ALU.subtract), reads=['bs'], writes=['bs'])
            kb.op('dve', lambda v: v.tensor_scalar(out=steps[:], in0=pow2[:], scalar1=dd, scalar2=None, op0=ALU.mult), reads=['bs', 'pow2'], writes=['steps'])
            kb.op('dve', lambda v: v.scalar_tensor_tensor(out=mid, in0=lo, scalar=-1.0, in1=steps[:, 0:1], op0=ALU.add, op1=ALU.add), reads=['bs', 'steps'], writes=['bs'])
            yield
            hsp = ((n // 2) // 128) * 128
            wact = n - hsp
            for it in range(NBIS):
                kb.op('dve', lambda v: v.tensor_scalar(out=junk[:, 0:hsp], in0=sc[:, 0:hsp], scalar1=mid, scalar2=None, op0=ALU.is_ge, op1=ALU.add, accum_out=cnt),
                      reads=['sc', 'bs'], writes=['junk', 'bs'])
                kb.op('dve', lambda v: v.tensor_scalar(out=junk[:, 0:wact], in0=sc[:, hsp:n], scalar1=mid, scalar2=cnt, op0=ALU.is_ge, op1=ALU.add, accum_out=cnt),
                      reads=['sc', 'bs'], writes=['junk', 'bs'])
                yield
                kb.op('dve', lambda v: v.tensor_scalar(out=ge, in0=cnt, scalar1=float(KSEL), scalar2=0.5, op0=ALU.is_ge, op1=ALU.subtract), reads=['bs'], writes=['bs'])
                kb.op('dve', lambda v, it=it: v.scalar_tensor_tensor(out=mid, in0=ge, scalar=steps[:, it:it + 1], in1=mid, op0=ALU.mult, op1=ALU.add),
                      reads=['bs', 'steps'], writes=['bs'])
                yield
            kb.op('dve', lambda v: v.tensor_tensor(out=lo, in0=mid, in1=steps[:, NBIS:NBIS + 1], op=ALU.subtract), reads=['bs', 'steps'], writes=['bs'])
            kb.op('dve', lambda v: v.tensor_tensor(out=hi, in0=mid, in1=steps[:, NBIS:NBIS + 1], op=ALU.add), reads=['bs', 'steps'], writes=['bs'])
            kb.op('dve', lambda v: v.tensor_scalar(out=junk[:, 0:hsp], in0=sc[:, 0:hsp], scalar1=hi, scalar2=None, op0=ALU.is_ge, op1=ALU.add, accum_out=cgt),
                  reads=['sc', 'bs'], writes=['junk', 'bs'])
            kb.op('dve', lambda v: v.tensor_scalar(out=junk[:, 0:wact], in0=sc[:, hsp:n], scalar1=hi, scalar2=cgt, op0=ALU.is_ge, op1=ALU.add, accum_out=cgt),
                  reads=['sc', 'bs'], writes=['junk', 'bs'])
            kb.op('dve', lambda v: v.tensor_scalar(out=need, in0=cgt, scalar1=-1.0, scalar2=float(KSEL), op0=ALU.mult, op1=ALU.add), reads=['bs'], writes=['bs'])
            yield

        def selection_b(qb):
            n = (qb + 1) * 128
            for ci_, c0 in enumerate(range(0, n, CH)):
                w = min(CH, n - c0)
                r_ = ci_ % 2
                tk, ck = ('tiec', r_), ('cumc', r_)
                kb.op('dve', lambda v, c0=c0, w=w, r_=r_: v.tensor_scalar(out=tiec[r_][:, 0:w], in0=sc[:, c0:c0 + w], scalar1=hi, scalar2=None, op0=ALU.is_lt),
                      reads=['sc', 'bs'], writes=[tk])
                kb.op('dve', lambda v, c0=c0, w=w, r_=r_: v.scalar_tensor_tensor(out=tiec[r_][:, 0:w], in0=sc[:, c0:c0 + w], scalar=lo, in1=tiec[r_][:, 0:w],
                      op0=ALU.is_ge, op1=ALU.mult), reads=['sc', 'bs', tk], writes=[tk])
                init = 0.0 if c0 == 0 else carry
                kb.op('dve', lambda v, w=w, r_=r_, init=init: v.tensor_tensor_scan(out=cumc[r_][:, 0:w], data0=onesc[:, 0:w], data1=tiec[r_][:, 0:w],
                      initial=init, op0=ALU.mult, op1=ALU.add), reads=['onesc', tk, 'bs'], writes=[ck])
                kb.op('dve', lambda v, w=w, r_=r_: v.tensor_copy(out=carry, in_=cumc[r_][:, w - 1:w]), reads=[ck], writes=['bs'])
                kb.op('dve', lambda v, w=w, r_=r_: v.scalar_tensor_tensor(out=tiec[r_][:, 0:w], in0=cumc[r_][:, 0:w], scalar=need, in1=tiec[r_][:, 0:w],
                      op0=ALU.is_le, op1=ALU.mult), reads=[ck, 'bs', tk], writes=[tk])
                kb.op('dve', lambda v, c0=c0, w=w, r_=r_: v.scalar_tensor_tensor(out=maskb[:, c0:c0 + w], in0=sc[:, c0:c0 + w], scalar=hi, in1=tiec[r_][:, 0:w],
                      op0=ALU.is_ge, op1=ALU.add), reads=['sc', 'bs', tk], writes=['maskb'])
                yield

        def attention(qb):
            mx = mixT[qb % 2]
            kb.dma('sp', lambda q: q.dma_start(out=ql[:], in_=qlat[:, :, qb * 128:(qb + 1) * 128].rearrange("h p t -> p h t")), writes=['ql'])
            kb.dma('sp', lambda q: q.dma_start(out=mx[:, 6:8, :], in_=memod[:, :, qb * 128:(qb + 1) * 128].rearrange("j p t -> p j t")),
                   writes=[('mixTm', qb % 2)])
            pO, pOk = ps[6], ('ps', 6)
            pD, pDk = ps[7], ('ps', 7)
            steps_ = [(hg, j) for hg in range(3) for j in range(qb + 1)]

            def logits(hg, j):
                qrhs = ql[:, hg * 4:(hg + 1) * 4, :].rearrange("p h t -> p (h t)")
                li = 4 + (natt[0] % 2)
                natt[0] += 1
                pL, pLk = ps[li], ('ps', li)
                dt = qb - j
                nmm = 1 + (1 if qb >= 2 else 0) + (1 if dt <= 1 else 0)
                m = 0
                kb.op('pe', lambda p: p.matmul(pL[:, :], lhsT=cT[:, j * 128:(j + 1) * 128], rhs=qrhs, start=True, stop=(nmm == 1)),
                      reads=[('cT', j), 'ql'], writes=[pLk])
                m += 1
                if qb >= 2:
                    kb.op('pe', lambda p, m=m: p.matmul(pL[:, :], lhsT=maskb[:, j * 128:(j + 1) * 128],
                          rhs=i4big[:].rearrange("p r t -> p (r t)"), start=False, stop=(m == nmm - 1)), reads=['maskb', 'i4big'], writes=[pLk])
                    m += 1
                if dt <= 1:
                    kb.op('pe', lambda p, m=m: p.matmul(pL[:, :], lhsT=ident_b[:],
                          rhs=BT[:, dt, hg * 4:(hg + 1) * 4, :].rearrange("p h t -> p (h t)"), start=False, stop=(m == nmm - 1)),
                          reads=['BT', 'ident_b'], writes=[pLk])
                    m += 1
                return pL, pLk

            cur = logits(*steps_[0])
            for idx, (hg, j) in enumerate(steps_):
                pL, pLk = cur
                pi = npt[0] % 3
                npt[0] += 1
                kb.op('act', lambda a, pL=pL, pi=pi: a.activation(out=Pt[pi][:], in_=pL[:, :], func=AF.Exp, bias=(nbias[:] if qb >= 2 else zbias[:]), scale=1.0),
                      reads=[pLk, 'nbias'], writes=[('Pt', pi)])
                if idx + 1 < len(steps_):
                    cur = logits(*steps_[idx + 1])
                kb.op('pe', lambda p, j=j, pi=pi: p.matmul(pO[:, :], lhsT=cTok[:, j, :], rhs=Pt[pi][:], start=(j == 0), stop=(j == qb)),
                      reads=[('cTok', j), ('Pt', pi)], writes=[pOk])
                kb.op('pe', lambda p, j=j, pi=pi: p.matmul(pD[:, :], lhsT=ones_b[:], rhs=Pt[pi][:], start=(j == 0), stop=(j == qb)),
                      reads=['ones_b', ('Pt', pi)], writes=[pDk])
                yield
                if j == qb:
                    oi = hg % 2
                    kb.op('act', lambda a: a.copy(out=Dsb[:], in_=pD[:, :]), reads=[pDk], writes=['Dsb'])
                    kb.op('act', lambda a: a.copy(out=Osb[:], in_=pO[:, :]), reads=[pOk], writes=['Osb'])
                    kb.op('dve', lambda v: v.reciprocal(out=Dsb[:], in_=Dsb[:]), reads=['Dsb'], writes=['Dsb'])
                    kb.op('pool', lambda g, oi=oi: g.tensor_tensor(out=olat[oi][:], in0=Osb[:], in1=Dsb[:], op=ALU.mult), reads=['Osb', 'Dsb'], writes=[('olat', oi)])
                    for pp in range(2):
                        pT, pTk = psum()
                        for hh in range(2):
                            hl = 2 * pp + hh
                            h = hg * 4 + hl
                            kb.op('pe', lambda p, pT=pT, h=h, hl=hl, hh=hh, oi=oi: p.matmul(pT[:, 0:128], lhsT=wuvpad[:, h, :], rhs=olat[oi][:, hl * 128:(hl + 1) * 128],
                                  start=(hh == 0), stop=(hh == 1)), reads=['wuvpad', ('olat', oi)], writes=[pTk])
                        kb.op('act', lambda a, pT=pT, hg=hg, pp=pp: a.copy(out=mx[:, hg * 2 + pp, :], in_=pT[:, 0:128]), reads=[pTk], writes=[('mixTt', qb % 2, hg * 2 + pp)])
                    yield

        nbias = sb("nbias", [128, 1], F32, s2)
        zbias = sb("zbias", [128, 1], F32, s2)
        kb.op('dve', lambda v: v.memset(nbias[:], -100.0), writes=['nbias'])
        kb.op('dve', lambda v: v.memset(zbias[:], 0.0), writes=['nbias'])

        nqb = ntiles

        def chain(*gens):
            for g in gens:
                yield from g

        def nsteps_sel(qb):
            n = (qb + 1) * 128
            return 2 * ((n + 511) // 512) + 3 + 2 * NBIS

        def tail_stream(qb):
            mx = mixT[qb % 2]
            mkeys = [('mixTt', qb % 2, k) for k in range(6)] + [('mixTm', qb % 2)]
            kb.dma('sp', lambda q: q.dma_start(out=x2t[:], in_=src[qb * 128:(qb + 1) * 128, :]), writes=['x2t'])
            yield
            yield from tail.run_gen(qb, lambda k, mx=mx: mx[:, k, :], mkeys, x2t, 'x2t', x1buf[qb * 128:(qb + 1) * 128, :])

        if nqb > 2:
            for _ in selection_a(2):
                pass
        gT = None
        for qb in range(nqb):
            if qb >= 2:
                gS = selection_b(qb)
                st_ = [(gS, 2 * (qb + 1))]
                if gT is not None:
                    st_.append((gT, 40))
                interleave(st_, until=gS)
            streams = [(attention(qb), 3 * (qb + 1) + 3)]
            if gT is not None:
                streams.append((gT, 12))
            if qb + 1 < nqb and qb + 1 >= 2:
                streams.append((selection_a(qb + 1), nsteps_sel(qb + 1)))
            interleave(streams)
            gT = tail_stream(qb)
        for _ in gT:
            pass
        psrot[0] = 6
        kb.barrier()
        s2.close()
        st.close()

    def phase_moe(layer, dst, ntiles=NT, nexp=NE):
        L = layer
        st = ExitStack()
        w1 = [sb(f"w1_{L}_{i}", [128, 8, 2 * D], BF16, st) for i in range(2)]
        w2 = [sb(f"w2_{L}_{i}", [128, 8, D], BF16, st) for i in range(2)]
        b2r = [sb(f"b2r_{L}_{i}", [1, D], BF16, st) for i in range(2)]
        b1a = sb(f"b1a_{L}", [128, NE, 16], F32, st)
        b1u = sb(f"b1u_{L}", [128, NE, 8], F32, st)
        with nc.allow_non_contiguous_dma(reason="bias layout"):
            kb.dma('sp', lambda q: q.dma_start(out=b1a[:], in_=exp_b1[L].rearrange("e (j p) -> p e j", p=128)), writes=['b1a'])
        kb.op('dve', lambda v: v.tensor_scalar(out=b1u[:], in0=b1a[:, :, 8:16], scalar1=1.0, scalar2=None, op0=ALU.add), reads=['b1a'], writes=['b1u'])
        xgt = [sb(f"xgt_{L}_{i}", [128, RG // 128, D], BF16, st) for i in range(2)]
        xgT = [sb(f"xgT_{L}_{i}", [128, 8, RG], BF16, st) for i in range(2)]
        actT = [sb(f"actT_{L}_{i}", [128, 8, RG], BF16, st) for i in range(2)]
        tg = [sb(f"tg_{L}_{i}", [128, RG], F32, st) for i in range(2)]
        tu = [sb(f"tu_{L}_{i}", [128, RG], F32, st) for i in range(2)]
        tsg = [sb(f"tsg_{L}_{i}", [128, RG], F32, st) for i in range(2)]
        tt = [sb(f"tt_{L}_{i}", [128, RG], F32, st) for i in range(2)]
        yev = [sb(f"yev_{L}_{i}", [128, D], F32, st) for i in range(2)]
        nyev = 0

        def load_w(e):
            i = e % 2
            load_cast(b2r[i][:], exp_b2[L, e:e + 1, :], ('b2r', i))
            load_cast(w1[i][:], exp_w1[L, e].rearrange("(k p) f -> p k f", p=128), ('w1', i))
            load_cast(w2[i][:], exp_w2[L, e].rearrange("(k p) n -> p k n", p=128), ('w2', i))

        NG = CAP // RG

        def stage_a(e, g, gi):
            wi = e % 2
            for k in range(8):
                pt, pk = psum()
                ptb = pt[:].bitcast(BF16)
                for t in range(RG // 128):
                    kb.op('pe', lambda p, ptb=ptb, t=t, k=k: p.transpose(out=ptb[:, t * 128:(t + 1) * 128],
                          in_=xgt[gi][:, t, k * 128:(k + 1) * 128], identity=ident_b[:]), reads=[('xgt', gi), 'ident_b'], writes=[pk])
                if k % 2 == 0:
                    kb.op('dve', lambda v, ptb=ptb, k=k: v.tensor_copy(out=xgT[gi][:, k, :], in_=ptb[:, 0:RG]), reads=[pk], writes=[('xgT', gi, k)])
                else:
                    kb.op('act', lambda a, ptb=ptb, k=k: a.copy(out=xgT[gi][:, k, :], in_=ptb[:, 0:RG]), reads=[pk], writes=[('xgT', gi, k)])
                if k % 2 == 1:
                    yield
            xgTk = [('xgT', gi, k) for k in range(8)]
            for j in range(8):
                ji = j % 2
                pg, pgk = psum()
                pu, puk = psum()
                for k in range(8):
                    kb.op('pe', lambda p, pg=pg, k=k, j=j: p.matmul(pg[:, 0:RG], lhsT=w1[wi][:, k, j * 128:(j + 1) * 128],
                          rhs=xgT[gi][:, k, :], start=(k == 0), stop=(k == 7)), reads=[('w1', wi)] + xgTk, writes=[pgk])
                for k in range(8):
                    kb.op('pe', lambda p, pu=pu, k=k, j=j: p.matmul(pu[:, 0:RG], lhsT=w1[wi][:, k, D + j * 128:D + (j + 1) * 128],
                          rhs=xgT[gi][:, k, :], start=(k == 0), stop=(k == 7)), reads=[('w1', wi)] + xgTk, writes=[puk])
                kb.op('dve', lambda v, pg=pg, ji=ji, j=j: v.tensor_scalar(out=tg[ji][:], in0=pg[:, 0:RG], scalar1=b1a[:, e, j:j + 1],
                      scalar2=7.0, op0=ALU.add, op1=ALU.min), reads=[pgk, 'b1a'], writes=[('tg', ji)])
                kb.op('act', lambda a, ji=ji: a.activation(out=tsg[ji][:], in_=tg[ji][:], func=AF.Sigmoid, scale=1.702),
                      reads=[('tg', ji)], writes=[('tsg', ji)])
                kb.op('dve', lambda v, pu=pu, ji=ji, j=j: v.tensor_scalar(out=tu[ji][:], in0=pu[:, 0:RG], scalar1=b1u[:, e, j:j + 1],
                      scalar2=8.0, op0=ALU.add, op1=ALU.min), reads=[puk, 'b1u'], writes=[('tu', ji)])
                kb.op('dve', lambda v, ji=ji: v.scalar_tensor_tensor(out=tt[ji][:], in0=tu[ji][:], scalar=-6.0, in1=tg[ji][:],
                      op0=ALU.max, op1=ALU.mult), reads=[('tu', ji), ('tg', ji)], writes=[('tt', ji)])
                kb.op('dve', lambda v, ji=ji, j=j: v.tensor_tensor(out=actT[gi][:, j, :], in0=tt[ji][:], in1=tsg[ji][:], op=ALU.mult),
                      reads=[('tt', ji), ('tsg', ji)], writes=[('actT', gi, j)])
                yield

        def stage_b(e, g, gi):
            nonlocal nyev
            wi = e % 2
            r0 = e * CAP + g * RG
            actk = [('actT', gi, j) for j in range(8)]
            for t in range(RG // 128):
                yi = nyev % 2
                nyev += 1
                for nh in range(2):
                    py, pyk = psum()
                    for k in range(8):
                        kb.op('pe', lambda p, py=py, k=k, t=t, nh=nh: p.matmul(py[:, :], lhsT=actT[gi][:, k, t * 128:(t + 1) * 128],
                              rhs=w2[wi][:, k, nh * 512:(nh + 1) * 512], start=(k == 0), stop=False), reads=[('w2', wi)] + actk, writes=[pyk])
                    kb.op('pe', lambda p, py=py, nh=nh: p.matmul(py[:, :], lhsT=ones_b[0:1, :], rhs=b2r[wi][0:1, nh * 512:(nh + 1) * 512],
                          start=False, stop=True), reads=[('b2r', wi), 'ones_b'], writes=[pyk])
                    kb.op('act', lambda a, py=py, nh=nh, yi=yi: a.copy(out=yev[yi][:, nh * 512:(nh + 1) * 512], in_=py[:, :]),
                          reads=[pyk], writes=[('yev', yi)])
                    yield
                rr0 = r0 + t * 128
                kb.dma('sp', lambda q, rr0=rr0, yi=yi: q.dma_start(out=yg[rr0:rr0 + 128, :], in_=yev[yi][:]), reads=[('yev', yi)], writes=['yg'])

        groups = [(e, g) for e in range(nexp) for g in range(NG)]

        def load_x(i):
            e_, g_ = groups[i]
            r0 = e_ * CAP + g_ * RG
            kb.dma('sp', lambda q: q.dma_start(out=xgt[i % 2][:], in_=xg[r0:r0 + RG, :].rearrange("(t p) d -> p t d", p=128)),
                   reads=['xg'], writes=[('xgt', i % 2)])

        load_x(0)
        if len(groups) > 1:
            load_x(1)
        load_w(0)
        if nexp > 1:
            load_w(1)
        for _ in stage_a(groups[0][0], groups[0][1], 0):
            pass
        for i, (e, g) in enumerate(groups):
            if i + 2 < len(groups):
                load_x(i + 2)
            if g == 0 and e >= 1 and e + 1 < nexp:
                load_w(e + 1)
            streams = [(stage_b(e, g, i % 2), 6)]
            if i + 1 < len(groups):
                e2, g2 = groups[i + 1]
                streams.append((stage_a(e2, g2, (i + 1) % 2), 20))
            interleave(streams)
        kb.barrier()
        st.close()
        st = ExitStack()
        lng = sb(f"ln2g{L}", [128, D], F32, st)
        lnb = sb(f"ln2b{L}", [128, D], F32, st)
        bcast_rows(lng[:], ln2_g[L], 'lng2')
        bcast_rows(lnb[:], ln2_b[L], 'lnb2')
        yk = [[sb(f"yk{L}_{i}_{k}", [128, D], F32, st) for k in range(4)] for i in range(2)]
        x1r = [sb(f"x1r{L}_{i}", [128, D], F32, st) for i in range(2)]
        zz = [sb(f"zz{L}_{i}", [128, D], F32, st) for i in range(2)]
        oo = [sb(f"oo{L}_{i}", [128, D], F32, st) for i in range(2)]
        smm = [sb(f"smm{L}_{i}", [128, 256], F32, st) for i in range(2)]
        lnh = Tail.__new__(Tail)
        for ti in range(ntiles):
            i = ti % 2
            kb.dma('sp', lambda q, ti=ti, i=i: q.dma_start(out=x1r[i][:], in_=x1buf[ti * 128:(ti + 1) * 128, :]), reads=[('x1d', ti)], writes=[('x1r', i)])
            for k in range(4):
                kb.op('act', lambda a, i=i, k=k: a.memzero(yk[i][k][:]), writes=[('yk', i, k)])
                kb.dma('pool', lambda q, ti=ti, i=i, k=k: q.indirect_dma_start(
                    out=yk[i][k][:, :], out_offset=None, in_=yg[:, :],
                    in_offset=bass.IndirectOffsetOnAxis(ap=destall[:, ti, k:k + 1], axis=0),
                    bounds_check=bc_reg, oob_is_err=False), reads=['yg', ('dest', ti)], writes=[('yk', i, k)])
            kb.op('dve', lambda v, i=i: v.tensor_scalar(out=zz[i][:], in0=x1r[i][:], scalar1=ALPHA, scalar2=None, op0=ALU.mult),
                  reads=[('x1r', i)], writes=[('zz', i)])
            for k in range(4):
                kb.op('dve', lambda v, ti=ti, i=i, k=k: v.scalar_tensor_tensor(out=zz[i][:], in0=yk[i][k][:], scalar=gall[:, ti, k:k + 1],
                      in1=zz[i][:], op0=ALU.mult, op1=ALU.add), reads=[('yk', i, k), ('gate', ti), ('zz', i)], writes=[('zz', i)])
            Tail.layernorm(lnh, zz[i], ('zz', i), oo[i], ('oo', i), smm[i], ('smm', i), lng, lnb, 'lng2', 'lnb2')
            kb.dma('sp', lambda q, ti=ti, i=i: q.dma_start(out=dst[ti * 128:(ti + 1) * 128, :], in_=oo[i][:]), reads=[('oo', i)], writes=[('dst', L, ti)])
        kb.barrier()
        kb.pool_depth = 2
        st.close()

    if mode.startswith("a0"):
        ntl = NT if mode == "a0" else int(mode[2:])
        phase_a0(ntiles=ntl)
        st = ExitStack()
        cp = [sb(f"cp{i}", [128, D], F32, st) for i in range(2)]
        for ti in range(ntl):
            i = ti % 2
            kb.dma('sp', lambda q, ti=ti, i=i: q.dma_start(out=cp[i][:], in_=x1buf[ti * 128:(ti + 1) * 128, :]), writes=[('cp', i)])
            kb.dma('sp', lambda q, ti=ti, i=i: q.dma_start(out=out[ti * 128:(ti + 1) * 128, :], in_=cp[i][:]), reads=[('cp', i)], writes=[('o', ti)])
        kb.barrier()
        st.close()
    elif mode == "full":
        phase_a0()
        phase_moe(0, x2buf)
        phase_a1(x2buf)
        phase_moe(1, out)
    elif mode.startswith("a1"):
        ntl = int(mode[2:])
        zt = sb("zt1", [128, 4096], BF16)
        kb.op('pool', lambda g: g.memset(zt[:], 0.0), writes=['zt'])
        for r0 in range(0, NROW, 512):
            kb.dma('sp', lambda q, r0=r0: q.dma_start(out=xg[r0:r0 + 512, :].rearrange("(p t) d -> p (t d)", t=4), in_=zt[:]),
                   reads=['zt'], writes=['xg'])
        phase_a1(x_in, ntiles=ntl)
        st = ExitStack()
        cp = [sb(f"cp{i}", [128, D], F32, st) for i in range(2)]
        for ti in range(ntl):
            i = ti % 2
            kb.dma('sp', lambda q, ti=ti, i=i: q.dma_start(out=cp[i][:], in_=x1buf[ti * 128:(ti + 1) * 128, :]), writes=[('cp', i)])
            kb.dma('sp', lambda q, ti=ti, i=i: q.dma_start(out=out[ti * 128:(ti + 1) * 128, :], in_=cp[i][:]), reads=[('cp', i)], writes=[('o', ti)])
        kb.barrier()
        st.close()
    elif mode == "l0":
        phase_a0()
        phase_moe(0, out)
    es.close()
    return nc


def host_consts():
    ident = np.eye(128, dtype=np.float32)
    tri = np.triu(np.ones((128, 128), np.float32), 1)
    iota = np.tile(np.arange(NE, dtype=np.float32)[None, :], (128, 1))
    ecap = iota * CAP
    q = np.arange(128)
    cmask = np.where(q[None, :] <= q[:, None], 0.0, -2000.0).astype(np.float32)
    caus = np.where(q[:, None] <= q[None, :], 0.0, -30000.0).astype(np.float32)
    bkt = np.zeros((128, 2, 128), np.float32)
    for dt in range(2):
        rel = np.maximum(q[None, :] - q[:, None] + 128 * dt, 0)
        large = 16 + (np.log(np.maximum(rel, 1).astype(np.float32) / 16) / np.float32(np.log(128 / 16)) * 16).astype(np.int32)
        large = np.minimum(large, 31)
        bkt[:, dt, :] = np.where(rel < 16, rel, large)
    pow2 = np.tile((2.0 ** -(np.arange(24, dtype=np.float64) + 1)).astype(np.float32)[None, :], (128, 1))
    return {"c_ident": ident, "c_tri": tri, "c_iota": iota, "c_ecap": ecap, "c_cmask": cmask, "c_caus": caus, "c_bkt": bkt,
            "c_pow2": pow2}


_PARAMS = ["rel_bias", "a_w_in", "a_conv_w", "a_conv_b", "a_wr", "a_br", "a_wi", "a_bi", "a_lambda", "b_w_in",
           "b_kv_norm_g", "b_w_uk", "b_w_uv", "b_idx_norm_g", "b_idx_norm_b", "w_mem_kv", "w_out", "ln1_g", "ln1_b",
           "router_w", "router_b", "exp_w1", "exp_b1", "exp_w2", "exp_b2", "ln2_g", "ln2_b"]
_SQUEEZE = {"a_w_in", "a_conv_w", "a_conv_b", "a_wr", "a_br", "a_wi", "a_bi", "a_lambda", "b_w_in", "b_kv_norm_g",
            "b_w_uk", "b_w_uv", "b_idx_norm_g", "b_idx_norm_b"}


def make_in_maps(inputs, cores):
    shared = {}
    for k in _PARAMS:
        v = np.ascontiguousarray(np.asarray(inputs[k], dtype=np.float32))
        if k in _SQUEEZE:
            v = v[0]
        shared[k] = v
    shared.update(host_consts())
    maps = []
    for c in cores:
        m = dict(shared)
        m["x"] = np.ascontiguousarray(inputs["x"][c])
        m["mem"] = np.ascontiguousarray(inputs["mem"][c])
        maps.append(m)
    return maps


def kernel(**inputs):
    nc = build_program("full")
    maps = make_in_maps(inputs, list(range(8)))
    res = run_bass_kernel_spmd(nc, maps, core_ids=list(range(8)))
    return np.stack([r["out"] for r in res.results], axis=0)
```

```python
from contextlib import ExitStack
import numpy as np
import concourse.bass as bass
import concourse.mybir as mybir
from concourse.bass_utils import run_bass_kernel_spmd

F32 = mybir.dt.float32
BF16 = mybir.dt.bfloat16
I32 = mybir.dt.int32
U32 = mybir.dt.uint32
AF = mybir.ActivationFunctionType
ALU = mybir.AluOpType
AX = mybir.AxisListType

S = 8192
D = 1024
NT = S // 128
MEM = 256
TOKW = 768
NE = 32
CAP = 1536
NROW = NE * CAP
RG = 384
ALPHA = float(4 ** 0.25)
W_IN_A = 1792
W_IN_B = 1476
BIGOOB = 1.0e6


class KB:
    NS = 8
    ND = 32

    def __init__(self, nc, es):
        self.nc = nc
        self.eng = {'pe': nc.tensor, 'act': nc.scalar, 'dve': nc.vector, 'pool': nc.gpsimd, 'sp': nc.sync}
        self.esem = {e: [es.enter_context(nc.semaphore(f"s_{e}{i}")) for i in range(self.NS)]
                     for e in ('pe', 'act', 'dve', 'pool')}
        self.cnt = {e: 0 for e in ('pe', 'act', 'dve', 'pool')}
        self.dsem = [es.enter_context(nc.semaphore(f"s_d{i}")) for i in range(self.ND)]
        self.dtot = [0] * self.ND
        self.dnext = 0
        self.dnextp = 0
        self.pool_depth = 2
        self.wc = {e: {} for e in self.eng}
        self.wd = {e: {} for e in self.eng}
        self.lastw = {}
        self.readers = {}

    def _wait(self, e, tok):
        eng = self.eng[e]
        if tok[0] == 'c':
            _, e2, k = tok
            if e2 == e and e == 'pe':
                return
            if self.wc[e].get(e2, 0) >= k:
                return
            eng.wait_ge(self.esem[e2][(k - 1) % self.NS], (k - 1) // self.NS + 1)
            self.wc[e][e2] = k
        else:
            _, s, tot = tok
            if self.wd[e].get(s, 0) >= tot:
                return
            eng.wait_ge(self.dsem[s], tot)
            self.wd[e][s] = tot

    def _deps(self, e, reads, writes):
        deps = []
        for k in reads:
            t = self.lastw.get(k)
            if t is not None:
                deps.append(t)
        for k in writes:
            t = self.lastw.get(k)
            if t is not None:
                deps.append(t)
            deps.extend(self.readers.get(k, ()))
        for t in deps:
            self._wait(e, t)

    def _commit(self, tok, reads, writes):
        for k in reads:
            lst = self.readers.setdefault(k, [])
            lst[:] = [t for t in lst if not (t[0] == tok[0] and t[1] == tok[1])]
            lst.append(tok)
        for k in writes:
            self.lastw[k] = tok
            self.readers[k] = []

    def op(self, e, fn, reads=(), writes=()):
        self._deps(e, reads, writes)
        ins = fn(self.eng[e])
        self.cnt[e] += 1
        k = self.cnt[e]
        ins.then_inc(self.esem[e][(k - 1) % self.NS], 1)
        self._commit(('c', e, k), reads, writes)

    def dma(self, e, fn, reads=(), writes=()):
        half = self.ND // 2
        if e == 'pool':
            s = half + self.dnextp
            self.dnextp = (self.dnextp + 1) % self.pool_depth
        else:
            s = self.dnext
            self.dnext = (self.dnext + 1) % half
        if self.dtot[s] > 0:
            self._wait(e, ('d', s, self.dtot[s]))
        self._deps(e, reads, writes)
        ins = fn(self.eng[e])
        self.dtot[s] += 16
        ins.then_inc(self.dsem[s], 16)
        self._commit(('d', s, self.dtot[s]), reads, writes)

    def barrier(self):
        for e in self.eng:
            for e2 in self.cnt:
                if self.cnt[e2] > 0:
                    self._wait(e, ('c', e2, self.cnt[e2]))
            for s in range(self.ND):
                if self.dtot[s] > 0:
                    self._wait(e, ('d', s, self.dtot[s]))
        self.lastw.clear()
        self.readers.clear()


def interleave(streams, until=None):
    live = [[g, max(1, n), 0] for g, n in streams if g is not None]
    while live:
        live.sort(key=lambda r: r[2] / r[1])
        r = live[0]
        try:
            next(r[0])
            r[2] += 1
        except StopIteration:
            live.remove(r)
            if until is not None and r[0] is until:
                return


def build_program(mode="full"):
    nc = bass.Bass("TRN2", target_bir_lowering=False)
    es = ExitStack()
    kb = KB(nc, es)

    def din(name, shape, dt=F32):
        return nc.dram_tensor(name, list(shape), dt, kind="ExternalInput").ap()

    def dscr(name, shape, dt=F32):
        return nc.dram_tensor(name, list(shape), dt, kind="Internal").ap()

    x_in = din("x", [S, D])
    mem_in = din("mem", [MEM, D])
    rel_bias = din("rel_bias", [32, 12])
    a_w_in = din("a_w_in", [D, W_IN_A])
    a_conv_w = din("a_conv_w", [4, TOKW])
    a_conv_b = din("a_conv_b", [TOKW])
    a_wr = din("a_wr", [12, 64, 64])
    a_br = din("a_br", [TOKW])
    a_wi = din("a_wi", [12, 64, 64])
    a_bi = din("a_bi", [TOKW])
    a_lambda = din("a_lambda", [TOKW])
    b_w_in = din("b_w_in", [D, W_IN_B])
    b_kv_norm_g = din("b_kv_norm_g", [128])
    b_w_uk = din("b_w_uk", [128, 12, 64])
    b_w_uv = din("b_w_uv", [128, 12, 64])
    b_idx_norm_g = din("b_idx_norm_g", [64])
    b_idx_norm_b = din("b_idx_norm_b", [64])
    w_mem_kv = din("w_mem_kv", [2, D, 512])
    w_out = din("w_out", [2, D, D])
    ln1_g = din("ln1_g", [2, D])
    ln1_b = din("ln1_b", [2, D])
    router_w = din("router_w", [2, D, NE])
    router_b = din("router_b", [2, NE])
    exp_w1 = din("exp_w1", [2, NE, D, 2 * D])
    exp_b1 = din("exp_b1", [2, NE, 2 * D])
    exp_w2 = din("exp_w2", [2, NE, D, D])
    exp_b2 = din("exp_b2", [2, NE, D])
    ln2_g = din("ln2_g", [2, D])
    ln2_b = din("ln2_b", [2, D])
    c_ident = din("c_ident", [128, 128])
    c_tri = din("c_tri", [128, 128])
    c_iota = din("c_iota", [128, NE])
    c_ecap = din("c_ecap", [128, NE])
    c_cmask = din("c_cmask", [128, 128])
    c_caus = din("c_caus", [128, 128])
    c_bkt = din("c_bkt", [128, 2, 128])
    c_pow2 = din("c_pow2", [128, 24])

    out = nc.dram_tensor("out", [S, D], F32, kind="ExternalOutput").ap()
    x1buf = dscr("x1buf", [S, D])
    x2buf = dscr("x2buf", [S, D])
    xg = dscr("xg", [NROW, D], BF16)
    yg = dscr("yg", [NROW, D])
    qlat = dscr("qlat", [12, 128, S], BF16)
    iqd = dscr("iqd", [2, 128, S], BF16)
    memod = dscr("memod", [2, 128, S], BF16)

    def sb(name, shape, dt=F32, stack=es):
        return stack.enter_context(nc.sbuf_tensor(name, list(shape), dt))

    ps = [es.enter_context(nc.psum_tensor(f"ps{i}", [128, 512], F32)) for i in range(8)]
    psn = [0]
    psrot = [6]

    def psum():
        i = psn[0] % psrot[0]
        psn[0] = (i + 1) % psrot[0]
        return ps[i], ('ps', i)

    bc_reg = nc.gpsimd.alloc_register("bc_reg")
    nc.gpsimd.reg_mov(bc_reg, NROW - 1)
    ident_f = sb("ident_f", [128, 128])
    ident_b = sb("ident_b", [128, 128], BF16)
    tri_f = sb("tri_f", [128, 128])
    ones_f = sb("ones_f", [128, 128])
    ones_b = sb("ones_b", [128, 128], BF16)
    iota_e = sb("iota_e", [128, NE])
    ecap = sb("ecap", [128, NE])
    destall = sb("destall", [128, NT, 4], I32)
    gall = sb("gall", [128, NT, 4])

    kb.dma('sp', lambda q: q.dma_start(out=ident_f[:], in_=c_ident[:, :]), writes=['ident_f'])
    kb.dma('sp', lambda q: q.dma_start(out=tri_f[:], in_=c_tri[:, :]), writes=['tri_f'])
    kb.dma('sp', lambda q: q.dma_start(out=iota_e[:], in_=c_iota[:, :]), writes=['iota_e'])
    kb.dma('sp', lambda q: q.dma_start(out=ecap[:], in_=c_ecap[:, :]), writes=['ecap'])
    kb.op('dve', lambda v: v.tensor_copy(out=ident_b[:], in_=ident_f[:]), reads=['ident_f'], writes=['ident_b'])
    kb.op('dve', lambda v: v.memset(ones_f[:], 1.0), writes=['ones_f'])
    kb.op('dve', lambda v: v.memset(ones_b[:], 1.0), writes=['ones_b'])

    def load_cast(dst_ap, src_ap, key):
        kb.dma('pool', lambda q: q.dma_start(out=dst_ap, in_=src_ap), writes=[key])

    def bcast_rows(dst, src_row_ap, key, n=128):
        kb.dma('sp', lambda q: q.dma_start(out=dst, in_=src_row_ap.partition_broadcast(n)), writes=[key])

    def setup_mem_kv(layer, st, kT, vpad, onespad):
        memf = sb(f"memf{layer}", [128, 2, D], F32, st)
        memb = sb(f"memb{layer}", [128, 2, D], BF16, st)
        memT = sb(f"memT{layer}", [128, 8, MEM], BF16, st)
        wkv = sb(f"wkv{layer}", [128, 8, 512], BF16, st)
        kb.dma('sp', lambda q: q.dma_start(out=memf[:], in_=mem_in.rearrange("(t p) d -> p t d", p=128)), writes=['memf'])
        load_cast(wkv[:], w_mem_kv[layer].rearrange("(k p) c -> p k c", p=128), 'wkv')
        kb.op('act', lambda a: a.copy(out=memb[:], in_=memf[:]), reads=['memf'], writes=['memb'])
        for k in range(8):
            pt, pk = psum()
            ptb = pt[:].bitcast(BF16)
            for t in range(2):
                kb.op('pe', lambda p, t=t, k=k, ptb=ptb: p.transpose(out=ptb[:, t * 128:(t + 1) * 128],
                      in_=memb[:, t, k * 128:(k + 1) * 128], identity=ident_b[:]),
                      reads=['memb', 'ident_b'], writes=[pk])
            kb.op('dve', lambda v, k=k, ptb=ptb: v.tensor_copy(out=memT[:, k, :], in_=ptb[:, 0:256]),
                  reads=[pk], writes=['memT'])
        for pr in range(2):
            pt, pk = psum()
            for k in range(8):
                kb.op('pe', lambda p, k=k, pr=pr, pt=pt: p.matmul(pt[:, 0:256], lhsT=wkv[:, k, pr * 128:(pr + 1) * 128],
                      rhs=memT[:, k, :], start=(k == 0), stop=(k == 7)), reads=['wkv', 'memT'], writes=[pk])
            kb.op('dve', lambda v, pr=pr, pt=pt: v.tensor_copy(out=kT[:, pr, :], in_=pt[:, 0:256]), reads=[pk], writes=['kT'])
        kb.op('pool', lambda g: g.memset(vpad[:], 0.0), writes=['vpad'])
        kb.op('pool', lambda g: g.memset(onespad[:], 0.0), writes=['onespad'])
        for par in range(2):
            kb.op('pool', lambda g, par=par: g.memset(onespad[:, par, par * 64:(par + 1) * 64], 1.0), writes=['onespad'])
        for mc in range(2):
            pt, pk = psum()
            for k in range(8):
                kb.op('pe', lambda p, k=k, mc=mc, pt=pt: p.matmul(pt[:, 0:256], lhsT=memT[:, k, mc * 128:(mc + 1) * 128],
                      rhs=wkv[:, k, 256:512], start=(k == 0), stop=(k == 7)), reads=['wkv', 'memT'], writes=[pk])
            for h in range(4):
                par = h % 2
                kb.op('dve', lambda v, h=h, mc=mc, par=par, pt=pt: v.tensor_copy(
                    out=vpad[:, h, mc, par * 64:(par + 1) * 64], in_=pt[:, h * 64:(h + 1) * 64]),
                    reads=[pk], writes=['vpad'])

    def mem_attn(mqT, T, kT, vpad, onespad, mixT_dst, E, rden, tag):
        for pr in range(2):
            for hh in range(2):
                h = 2 * pr + hh
                for mc in range(2):
                    pt, pk = psum()
                    kb.op('pe', lambda p, pt=pt, pr=pr, hh=hh, mc=mc: p.matmul(
                        pt[:, 0:T], lhsT=kT[hh * 64:(hh + 1) * 64, pr, mc * 128:(mc + 1) * 128],
                        rhs=mqT[hh * 64:(hh + 1) * 64, pr, :], start=True, stop=True),
                        reads=['kT', 'mqT' + tag], writes=[pk])
                    kb.op('act', lambda a, pt=pt, hh=hh, mc=mc: a.activation(
                        out=E[:, hh, mc, :], in_=pt[:, 0:T], func=AF.Exp, scale=0.125),
                        reads=[pk], writes=[('E' + tag, hh, mc)])
            po, pok = psum()
            pd, pdk = psum()
            n = 0
            for hh in range(2):
                h = 2 * pr + hh
                for mc in range(2):
                    kb.op('pe', lambda p, po=po, h=h, hh=hh, mc=mc, n=n: p.matmul(
                        po[:, 0:T], lhsT=vpad[:, h, mc, :], rhs=E[:, hh, mc, :], start=(n == 0), stop=(n == 3)),
                        reads=['vpad', ('E' + tag, hh, mc)], writes=[pok])
                    n += 1
            n = 0
            for hh in range(2):
                for mc in range(2):
                    kb.op('pe', lambda p, pd=pd, hh=hh, mc=mc, n=n: p.matmul(
                        pd[:, 0:T], lhsT=onespad[:, hh, :], rhs=E[:, hh, mc, :], start=(n == 0), stop=(n == 3)),
                        reads=['onespad', ('E' + tag, hh, mc)], writes=[pdk])
                    n += 1
            kb.op('dve', lambda v, pd=pd: v.reciprocal(out=rden[:, 0:T], in_=pd[:, 0:T]), reads=[pdk], writes=['rden' + tag])
            kb.op('dve', lambda v, po=po, pr=pr: v.tensor_tensor(out=mixT_dst(pr), in0=po[:, 0:T], in1=rden[:, 0:T], op=ALU.mult),
                  reads=[pok, 'rden' + tag], writes=[('mixT' + tag, 6 + pr)])

    class Tail:
        def __init__(self, layer, st, nbuf=2):
            self.layer = layer
            self.nbuf = nbuf
            L = layer
            self.wout = sb(f"wout{L}", [128, 8, D], BF16, st)
            load_cast(self.wout[:], w_out[L].rearrange("(k p) n -> p k n", p=128), 'wout')
            self.lng = sb(f"lng{L}", [128, D], F32, st)
            self.lnb = sb(f"lnb{L}", [128, D], F32, st)
            bcast_rows(self.lng[:], ln1_g[L], 'lng')
            bcast_rows(self.lnb[:], ln1_b[L], 'lnb')
            self.rw = sb(f"rw{L}", [128, 8, NE], F32, st)
            kb.dma('sp', lambda q: q.dma_start(out=self.rw[:], in_=router_w[L].rearrange("(k p) e -> p k e", p=128)), writes=['rw'])
            self.rb = sb(f"rb{L}", [128, NE], F32, st)
            bcast_rows(self.rb[:], router_b[L], 'rb')
            self.rrun = sb(f"rrun{L}", [128, NE], F32, st)
            kb.op('dve', lambda v: v.memset(self.rrun[:], 0.0), writes=['rrun'])
            self.z = [sb(f"z{L}_{i}", [128, D], F32, st) for i in range(nbuf)]
            self.x1f = [sb(f"x1f{L}_{i}", [128, D], F32, st) for i in range(nbuf)]
            self.x1b = [sb(f"x1b{L}_{i}", [128, D], BF16, st) for i in range(nbuf)]
            self.x1T = sb(f"x1T{L}", [128, 8, 128], F32, st)
            self.sm = [sb(f"sm{L}_{i}", [128, 256], F32, st) for i in range(nbuf)]
            self.smu = [sb(f"smu{L}_{i}", [128, 8], U32, st) for i in range(nbuf)]
            self.n = 0

        def run(self, *a):
            for _ in self.run_gen(*a):
                pass

        def run_gen(self, ti, mixT_ap, mix_keys, xres_ap, xres_key, x1dst):
            i = self.n % self.nbuf
            self.n += 1
            z, x1f, x1b, sm, smu = self.z[i], self.x1f[i], self.x1b[i], self.sm[i], self.smu[i]
            zk, x1fk, x1bk, smk = ('z', i), ('x1f', i), ('x1b', i), ('sm', i)
            for nh in range(2):
                pt, pk = psum()
                for k in range(8):
                    kb.op('pe', lambda p, pt=pt, k=k, nh=nh: p.matmul(pt[:, :], lhsT=mixT_ap(k), rhs=self.wout[:, k, nh * 512:(nh + 1) * 512],
                          start=(k == 0), stop=(k == 7)), reads=['wout'] + list(mix_keys), writes=[pk])
                kb.op('dve', lambda v, pt=pt, nh=nh: v.scalar_tensor_tensor(
                    out=z[:, nh * 512:(nh + 1) * 512], in0=xres_ap[:, nh * 512:(nh + 1) * 512], scalar=ALPHA,
                    in1=pt[:, :], op0=ALU.mult, op1=ALU.add), reads=[pk, xres_key], writes=[zk])
                yield
            self.layernorm(z, zk, x1f, x1fk, sm, smk, self.lng, self.lnb)
            kb.dma('sp', lambda q: q.dma_start(out=x1dst, in_=x1f[:]), reads=[x1fk], writes=[('x1d', ti)])
            yield
            kb.op('act', lambda a: a.copy(out=x1b[:], in_=x1f[:]), reads=[x1fk], writes=[x1bk])
            yield
            for half in range(2):
                pt, pk = psum()
                for kk in range(4):
                    k = half * 4 + kk
                    kb.op('pe', lambda p, pt=pt, k=k, kk=kk: p.transpose(out=pt[:, kk * 128:(kk + 1) * 128],
                          in_=x1f[:, k * 128:(k + 1) * 128], identity=ident_f[:]), reads=[x1fk, 'ident_f'], writes=[pk])
                kb.op('act', lambda a, pt=pt, half=half: a.copy(
                    out=self.x1T[:, half * 4:(half + 1) * 4, :].rearrange("p k t -> p (k t)"), in_=pt[:, :]),
                    reads=[pk], writes=['x1T'])
            yield
            pl, plk = psum()
            for k in range(8):
                kb.op('pe', lambda p, k=k: p.matmul(pl[:, 0:NE], lhsT=self.x1T[:, k, :], rhs=self.rw[:, k, :],
                      start=(k == 0), stop=(k == 7)), reads=['x1T', 'rw'], writes=[plk])
            lg = sm[:, 0:32]
            v8 = sm[:, 32:40]
            mask = sm[:, 40:72]
            slot = sm[:, 72:104]
            junk = sm[:, 104:136]
            idxf = sm[:, 136:144]
            destf = sm[:, 144:148]
            ev = sm[:, 148:152]
            nm = sm[:, 152:153]
            gsum = sm[:, 153:154]
            bad = sm[:, 160:192]
            kb.op('dve', lambda v: v.tensor_tensor(out=lg, in0=pl[:, 0:NE], in1=self.rb[:], op=ALU.add),
                  reads=[plk, 'rb'], writes=[smk])
            kb.op('dve', lambda v: v.max(out=v8, in_=lg), reads=[smk], writes=[smk])
            kb.op('dve', lambda v: v.max_index(out=smu[:], in_max=v8, in_values=lg), reads=[smk], writes=[('smu', i)])
            kb.op('dve', lambda v: v.tensor_scalar(out=mask, in0=lg, scalar1=v8[:, 3:4], scalar2=None, op0=ALU.is_ge),
                  reads=[smk], writes=[smk])
            yield
            pc, pck = psum()
            kb.op('pe', lambda p: p.matmul(pc[:, 0:NE], lhsT=tri_f[:], rhs=mask, start=True, stop=True),
                  reads=['tri_f', smk], writes=[pck])
            kb.op('pe', lambda p: p.matmul(pc[:, NE:2 * NE], lhsT=ones_f[:], rhs=mask, start=True, stop=True),
                  reads=['ones_f', smk], writes=[pck])
            kb.op('dve', lambda v: v.tensor_tensor(out=slot, in0=pc[:, 0:NE], in1=self.rrun[:], op=ALU.add),
                  reads=[pck, 'rrun'], writes=[smk])
            kb.op('dve', lambda v: v.tensor_tensor(out=self.rrun[:], in0=pc[:, NE:2 * NE], in1=self.rrun[:], op=ALU.add),
                  reads=[pck, 'rrun'], writes=['rrun'])
            kb.op('dve', lambda v: v.tensor_scalar(out=bad, in0=slot, scalar1=float(CAP), scalar2=BIGOOB, op0=ALU.is_ge, op1=ALU.mult),
                  reads=[smk], writes=[smk])
            kb.op('dve', lambda v: v.tensor_tensor(out=slot, in0=slot, in1=bad, op=ALU.add), reads=[smk], writes=[smk])
            kb.op('dve', lambda v: v.tensor_tensor(out=slot, in0=slot, in1=ecap[:], op=ALU.add), reads=[smk, 'ecap'], writes=[smk])
            yield
            kb.op('dve', lambda v: v.tensor_copy(out=idxf, in_=smu[:]), reads=[('smu', i)], writes=[smk])
            for k in range(4):
                kb.op('dve', lambda v, k=k: v.scalar_tensor_tensor(out=junk, in0=iota_e[:], scalar=idxf[:, k:k + 1], in1=slot,
                      op0=ALU.is_equal, op1=ALU.mult, accum_out=destf[:, k:k + 1]), reads=[smk, 'iota_e'], writes=[smk])
            kb.op('dve', lambda v: v.tensor_copy(out=destall[:, ti, :], in_=destf), reads=[smk], writes=[('dest', ti)])
            kb.op('dve', lambda v: v.tensor_scalar(out=nm, in0=v8[:, 0:1], scalar1=-1.0, scalar2=None, op0=ALU.mult),
                  reads=[smk], writes=[smk])
            kb.op('act', lambda a: a.activation(out=ev, in_=v8[:, 0:4], func=AF.Exp, bias=nm, scale=1.0, accum_out=gsum),
                  reads=[smk], writes=[smk])
            yield
            kb.op('dve', lambda v: v.reciprocal(out=gsum, in_=gsum), reads=[smk], writes=[smk])
            kb.op('dve', lambda v: v.tensor_scalar(out=gall[:, ti, :], in0=ev, scalar1=gsum, scalar2=None, op0=ALU.mult),
                  reads=[smk], writes=[('gate', ti)])
            for k in range(4):
                kb.dma('pool', lambda q, k=k: q.indirect_dma_start(
                    out=xg[:, :], out_offset=bass.IndirectOffsetOnAxis(ap=destall[:, ti, k:k + 1], axis=0),
                    in_=x1b[:, :], in_offset=None, bounds_check=bc_reg, oob_is_err=False),
                    reads=[x1bk, ('dest', ti)], writes=['xg'])

        def layernorm(self, z, zk, o, ok, sm, smk, g, b, gk='lng', bk='lnb', e2='pool'):
            st6 = sm[:, 200:212]
            mv = sm[:, 212:214]
            rstd = sm[:, 214:215]
            for c in range(2):
                kb.op('dve', lambda v, c=c: v.bn_stats(out=st6[:, c * 6:(c + 1) * 6], in_=z[:, c * 512:(c + 1) * 512]),
                      reads=[zk], writes=[smk])
            kb.op('dve', lambda v: v.bn_aggr(out=mv, in_=st6), reads=[smk], writes=[smk])
            kb.op('act', lambda a: a.activation(out=rstd, in_=mv[:, 1:2], func=AF.Sqrt, bias=1e-5, scale=1.0), reads=[smk], writes=[smk])
            kb.op('dve', lambda v: v.reciprocal(out=rstd, in_=rstd), reads=[smk], writes=[smk])
            kb.op('dve', lambda v: v.tensor_scalar(out=o[:], in0=z[:], scalar1=mv[:, 0:1], scalar2=rstd, op0=ALU.subtract, op1=ALU.mult),
                  reads=[zk, smk], writes=[ok])
            kb.op(e2, lambda p: p.tensor_tensor(out=o[:], in0=o[:], in1=g[:], op=ALU.mult), reads=[ok, gk], writes=[ok])
            kb.op(e2, lambda p: p.tensor_tensor(out=o[:], in0=o[:], in1=b[:], op=ALU.add), reads=[ok, bk], writes=[ok])

    def phase_a0(ntiles=NT):
        T = 256
        st = ExitStack()
        tail = Tail(0, st)
        kT = sb("kT0", [128, 2, MEM], BF16, st)
        vpad = sb("vpad0", [128, 4, 2, 128], BF16, st)
        onespad = sb("onespad0", [128, 2, 128], BF16, st)
        setup_mem_kv(0, st, kT, vpad, onespad)
        win = sb("win0", [128, 8, W_IN_A], BF16, st)
        load_cast(win[:], a_w_in.rearrange("(k p) c -> p k c", p=128), 'win')
        wr_bd = sb("wr_bd", [128, 6, 128], BF16, st)
        wi_bd = sb("wi_bd", [128, 6, 128], BF16, st)
        kb.op('pool', lambda g: g.memset(wr_bd[:], 0.0), writes=['wr_bd'])
        kb.op('pool', lambda g: g.memset(wi_bd[:], 0.0), writes=['wi_bd'])
        for n in range(12):
            j, par = n // 2, n % 2
            load_cast(wr_bd[par * 64:(par + 1) * 64, j, par * 64:(par + 1) * 64], a_wr[n], 'wr_bd')
            load_cast(wi_bd[par * 64:(par + 1) * 64, j, par * 64:(par + 1) * 64], a_wi[n], 'wi_bd')
        cw = sb("cw", [128, 4, 6], F32, st)
        vecs = sb("vecs", [128, 4, 6], F32, st)
        with nc.allow_non_contiguous_dma(reason="tiny per-channel vectors"):
            kb.dma('sp', lambda q: q.dma_start(out=cw[:], in_=a_conv_w.rearrange("w (j p) -> p w j", p=128)), writes=['cw'])
            for n, v_ in enumerate((a_conv_b, a_br, a_bi, a_lambda)):
                kb.dma('sp', lambda q, n=n, v_=v_: q.dma_start(out=vecs[:, n, :], in_=v_.rearrange("(j p) -> p j", p=128)), writes=['vecs'])
        coef = sb("coef", [128, 6], F32, st)
        kb.op('act', lambda a: a.activation(out=coef[:], in_=vecs[:, 3, :], func=AF.Exp, scale=-1.0), reads=['vecs'], writes=['coef'])
        kb.op('act', lambda a: a.activation(out=coef[:], in_=coef[:], func=AF.Ln, bias=1.0, scale=1.0), reads=['coef'], writes=['coef'])
        kb.op('dve', lambda v: v.tensor_scalar(out=coef[:], in0=coef[:], scalar1=-8.0, scalar2=None, op0=ALU.mult), reads=['coef'], writes=['coef'])

        xf = [sb(f"xf{i}", [128, 2, D], F32, st) for i in range(2)]
        xbf = sb("xbf", [128, 2, D], BF16, st)
        xT = sb("xT", [128, 8, T], BF16, st)
        xbh = sb("xbh", [128, 6, 3 + T], F32, st)
        gbT = sb("gbT", [128, 6, T], F32, st)
        mqT = sb("mqT", [128, 2, T], BF16, st)
        mixT = [sb(f"mixT{i}", [128, 8, T], BF16, st) for i in range(2)]
        E = sb("E0", [128, 2, 2, T], BF16, st)
        rden = sb("rden0", [128, T], F32, st)
        NTMP = 10
        tmp = [[sb(f"tmp{n}_{i}", [128, T], F32, st) for i in range(2)] for n in range(NTMP)]
        xcb = [sb(f"xcb{i}", [128, T], BF16, st) for i in range(2)]
        hbuf = [sb(f"hbuf{i}", [128, 6, T], F32, st) for i in range(2)]
        kb.op('dve', lambda v: v.memset(xbh[:], 0.0), writes=['xbh'])
        zt = sb("zt", [128, 4096], BF16, st)
        kb.op('pool', lambda g: g.memset(zt[:], 0.0), writes=['zt'])
        for r0 in range(0, NROW, 512):
            kb.dma('sp', lambda q, r0=r0: q.dma_start(out=xg[r0:r0 + 512, :].rearrange("(p t) d -> p (t d)", t=4), in_=zt[:]),
                   reads=['zt'], writes=['xg'])

        nch = ntiles * 128 // T

        def mixer(ci):
            xi = ci % 2
            xfc, xfk = xf[xi], ('xf', xi)
            kb.dma('sp', lambda q, ci=ci, xfc=xfc: q.dma_start(
                out=xfc[:], in_=x_in[ci * T:(ci + 1) * T, :].rearrange("(t p) d -> p t d", p=128)), writes=[xfk])
            kb.op('act', lambda a, xfc=xfc: a.copy(out=xbf[:], in_=xfc[:]), reads=[xfk], writes=['xbf'])
            for k in range(8):
                pt, pk = psum()
                ptb = pt[:].bitcast(BF16)
                for t in range(2):
                    kb.op('pe', lambda p, ptb=ptb, t=t, k=k: p.transpose(out=ptb[:, t * 128:(t + 1) * 128],
                          in_=xbf[:, t, k * 128:(k + 1) * 128], identity=ident_b[:]), reads=['xbf', 'ident_b'], writes=[pk])
                kb.op('dve' if k % 2 == 0 else 'act',
                      (lambda v, ptb=ptb, k=k: v.tensor_copy(out=xT[:, k, :], in_=ptb[:, 0:T])) if k % 2 == 0 else
                      (lambda a, ptb=ptb, k=k: a.copy(out=xT[:, k, :], in_=ptb[:, 0:T])),
                      reads=[pk], writes=[('xT', k)])
            yield
            xTk = [('xT', k) for k in range(8)]
            for c in range(14):
                pt, pk = psum()
                for k in range(8):
                    kb.op('pe', lambda p, pt=pt, k=k, c=c: p.matmul(pt[:, 0:T], lhsT=win[:, k, c * 128:(c + 1) * 128],
                          rhs=xT[:, k, :], start=(k == 0), stop=(k == 7)), reads=['win'] + xTk, writes=[pk])
                if c < 6:
                    kb.op('act', lambda a, pt=pt, c=c: a.copy(out=xbh[:, c, 3:3 + T], in_=pt[:, 0:T]), reads=[pk], writes=[('xbh', c)])
                elif c < 12:
                    kb.op('act', lambda a, pt=pt, c=c: a.copy(out=gbT[:, c - 6, :], in_=pt[:, 0:T]), reads=[pk], writes=[('gbT', c - 6)])
                else:
                    kb.op('dve', lambda v, pt=pt, c=c: v.tensor_copy(out=mqT[:, c - 12, :], in_=pt[:, 0:T]), reads=[pk], writes=['mqT0'])
                if c % 2 == 1:
                    yield
            mx = mixT[ci % 2]
            hb = hbuf[ci % 2]
            hprev = hbuf[(ci + 1) % 2]
            for c in range(6):
                r_ = c % 2
                xc, rr, ii, aa, ss, uu, sq, t2, sg, gl = [tmp[n][r_] for n in range(NTMP)]
                tk = [(f'tmp{n}', r_) for n in range(NTMP)]
                xck = tk[0]
                kb.op('dve', lambda v, c=c, xc=xc: v.tensor_scalar(out=xc[:], in0=xbh[:, c, 3:3 + T], scalar1=cw[:, 3, c:c + 1],
                      scalar2=vecs[:, 0, c:c + 1], op0=ALU.mult, op1=ALU.add), reads=[('xbh', c), 'cw', 'vecs'], writes=[xck])
                for j in range(3):
                    kb.op('dve', lambda v, c=c, j=j, xc=xc: v.scalar_tensor_tensor(out=xc[:], in0=xbh[:, c, j:j + T], scalar=cw[:, j, c:c + 1],
                          in1=xc[:], op0=ALU.mult, op1=ALU.add), reads=[('xbh', c), 'cw', xck], writes=[xck])
                kb.op('pool', lambda g, c=c: g.tensor_copy(out=xbh[:, c, 0:3], in_=xbh[:, c, T:T + 3]), reads=[('xbh', c)], writes=[('xbh', c)])
                kb.op('pool', lambda g, xc=xc, r_=r_: g.tensor_copy(out=xcb[r_][:], in_=xc[:]), reads=[xck], writes=[('xcb', r_)])
                pr_, prk = psum()
                kb.op('pe', lambda p, pr_=pr_, c=c, r_=r_: p.matmul(pr_[:, 0:T], lhsT=wr_bd[:, c, :], rhs=xcb[r_][:], start=True, stop=True),
                      reads=['wr_bd', ('xcb', r_)], writes=[prk])
                pi_, pik = psum()
                kb.op('pe', lambda p, pi_=pi_, c=c, r_=r_: p.matmul(pi_[:, 0:T], lhsT=wi_bd[:, c, :], rhs=xcb[r_][:], start=True, stop=True),
                      reads=['wi_bd', ('xcb', r_)], writes=[pik])
                kb.op('act', lambda a, pr_=pr_, c=c, rr=rr: a.activation(out=rr[:], in_=pr_[:, 0:T], func=AF.Sigmoid, bias=vecs[:, 1, c:c + 1], scale=1.0),
                      reads=[prk, 'vecs'], writes=[tk[1]])
                kb.op('act', lambda a, pi_=pi_, c=c, ii=ii: a.activation(out=ii[:], in_=pi_[:, 0:T], func=AF.Sigmoid, bias=vecs[:, 2, c:c + 1], scale=1.0),
                      reads=[pik, 'vecs'], writes=[tk[2]])
                yield
                gb = gbT[:, c, :]
                kb.op('pool', lambda g, gb=gb, sq=sq: g.tensor_tensor(out=sq[:], in0=gb, in1=gb, op=ALU.mult), reads=[('gbT', c)], writes=[tk[6]])
                kb.op('pool', lambda g, sq=sq: g.tensor_scalar(out=sq[:], in0=sq[:], scalar1=0.044715, scalar2=1.0, op0=ALU.mult, op1=ALU.add),
                      reads=[tk[6]], writes=[tk[6]])
                kb.op('pool', lambda g, gb=gb, sq=sq, t2=t2: g.tensor_tensor(out=t2[:], in0=sq[:], in1=gb, op=ALU.mult), reads=[tk[6], ('gbT', c)], writes=[tk[7]])
                kb.op('act', lambda a, t2=t2, sg=sg: a.activation(out=sg[:], in_=t2[:], func=AF.Sigmoid, scale=1.5957691216057308),
                      reads=[tk[7]], writes=[tk[8]])
                kb.op('act', lambda a, aa=aa, rr=rr, c=c: a.activation(out=aa[:], in_=rr[:], func=AF.Exp, scale=coef[:, c:c + 1]),
                      reads=[tk[1], 'coef'], writes=[tk[3]])
                kb.op('pool', lambda g, aa=aa, ss=ss: g.tensor_tensor(out=ss[:], in0=aa[:], in1=aa[:], op=ALU.mult), reads=[tk[3]], writes=[tk[4]])
                kb.op('act', lambda a, ss=ss: a.activation(out=ss[:], in_=ss[:], func=AF.Sqrt, bias=1.0, scale=-1.0), reads=[tk[4]], writes=[tk[4]])
                kb.op('pool', lambda g, uu=uu, ss=ss, ii=ii: g.tensor_tensor(out=uu[:], in0=ss[:], in1=ii[:], op=ALU.mult), reads=[tk[4], tk[2]], writes=[tk[5]])
                kb.op('dve', lambda v, uu=uu, xc=xc: v.tensor_tensor(out=uu[:], in0=uu[:], in1=xc[:], op=ALU.mult), reads=[tk[5], xck], writes=[tk[5]])
                init = 0.0 if ci == 0 else hprev[:, c, T - 1:T]
                kb.op('dve', lambda v, aa=aa, uu=uu, c=c, init=init, hb=hb: v.tensor_tensor_scan(out=hb[:, c, :], data0=aa[:], data1=uu[:],
                      initial=init, op0=ALU.mult, op1=ALU.add), reads=[tk[3], tk[5], ('h', (ci + 1) % 2, c)], writes=[('h', ci % 2, c)])
                kb.op('pool', lambda g, gl=gl, sg=sg, gb=gb: g.tensor_tensor(out=gl[:], in0=sg[:], in1=gb, op=ALU.mult), reads=[tk[8], ('gbT', c)], writes=[tk[9]])
                kb.op('dve', lambda v, gl=gl, c=c, hb=hb, mx=mx: v.tensor_tensor(out=mx[:, c, :], in0=hb[:, c, :], in1=gl[:], op=ALU.mult),
                      reads=[tk[9], ('h', ci % 2, c)], writes=[('mixT0', c)])
                yield
            mem_attn(mqT, T, kT, vpad, onespad, lambda pr, mx=mx: mx[:, 6 + pr, :], E, rden, '0')
            yield

        mixkeys = [('mixT0', c) for c in range(8)]

        def tails(ci):
            mx = mixT[ci % 2]
            xfc, xfk = xf[ci % 2], ('xf', ci % 2)
            for t in range(T // 128):
                ti = ci * (T // 128) + t
                yield from tail.run_gen(ti, lambda k, mx=mx, t=t: mx[:, k, t * 128:(t + 1) * 128], mixkeys, xfc[:, t, :], xfk,
                                        x1buf[ti * 128:(ti + 1) * 128, :])

        for _ in mixer(0):
            pass
        for ci in range(nch):
            streams = [(tails(ci), 18)]
            if ci + 1 < nch:
                streams.append((mixer(ci + 1), 25))
            interleave(streams)
        kb.barrier()
        if mode.startswith("a0"):
            dbg_r = nc.dram_tensor("dbg_r", [128, NE], F32, kind="ExternalOutput").ap()
            dbg_d = nc.dram_tensor("dbg_d", [128, NT * 4], I32, kind="ExternalOutput").ap()
            kb.dma('sp', lambda q: q.dma_start(out=dbg_r[:, :], in_=tail.rrun[:]))
            kb.dma('sp', lambda q: q.dma_start(out=dbg_d[:, :], in_=destall[:].rearrange("p t k -> p (t k)")))
            kb.barrier()
        st.close()


    def phase_a1(src, ntiles=NT):
        T = 256
        KSEL = 256
        NBIS = 14
        CH = 512
        st = ExitStack()
        tail = Tail(1, st, nbuf=1)
        cTok = sb("cTok", [128, NT, 128], BF16, st)
        cT = sb("cT", [128, S], BF16, st)
        ikT2 = sb("ikT2", [128, S], BF16, st)
        absw = sb("absw", [128, NT, 4], F32, st)
        sgnw = sb("sgnw", [128, NT, 4], F32, st)
        BT = sb("BT", [128, 2, 12, 128], BF16, st)
        wuvpad = sb("wuvpad", [128, 12, 128], BF16, st)
        i4big = sb("i4big", [128, 4, 128], BF16, st)
        for r in range(4):
            kb.op('dve', lambda v, r=r: v.tensor_scalar(out=i4big[:, r, :], in0=ident_f[:], scalar1=100.0, scalar2=None, op0=ALU.mult),
                  reads=['ident_f'], writes=['i4big'])
        kb.op('pool', lambda g: g.memset(wuvpad[:], 0.0), writes=['wuvpad'])
        for h in range(12):
            par = h % 2
            load_cast(wuvpad[:, h, par * 64:(par + 1) * 64], b_w_uv[:, h, :], 'wuvpad')
        s1 = ExitStack()
        kT = sb("kT1", [128, 2, MEM], BF16, s1)
        vpad = sb("vpad1", [128, 4, 2, 128], BF16, s1)
        onespad = sb("onespad1", [128, 2, 128], BF16, s1)
        win = sb("win1", [128, 8, W_IN_B], BF16, s1)
        load_cast(win[:], b_w_in.rearrange("(k p) c -> p k c", p=128), 'win')
        wukT = sb("wukT", [128, 6, 128], BF16, s1)
        gkv = sb("gkv", [128, 128], F32, s1)
        gik = sb("gik", [128, 64], F32, s1)
        bik = sb("bik", [128, 64], F32, s1)
        bcast_rows(gkv[:], b_kv_norm_g, 'gkv')
        bcast_rows(gik[:], b_idx_norm_g, 'gik')
        bcast_rows(bik[:], b_idx_norm_b, 'bik')
        ssetup = ExitStack()
        setup_mem_kv(1, ssetup, kT, vpad, onespad)
        wuk = sb("wuk", [128, 768], BF16, ssetup)
        load_cast(wuk[:], b_w_uk.rearrange("r h d -> r (h d)"), 'wuk')
        for j in range(6):
            pt, pk = psum()
            ptb = pt[:].bitcast(BF16)
            kb.op('pe', lambda p, ptb=ptb, j=j: p.transpose(out=ptb[:, 0:128], in_=wuk[:, j * 128:(j + 1) * 128], identity=ident_b[:]),
                  reads=['wuk', 'ident_b'], writes=[pk])
            kb.op('dve', lambda v, ptb=ptb, j=j: v.tensor_copy(out=wukT[:, j, :], in_=ptb[:, 0:128]), reads=[pk], writes=['wukT'])
        rbb = sb("rbb", [128, 32, 12], F32, ssetup)
        bkt = sb("bkt", [128, 2, 128], F32, ssetup)
        caus = sb("caus", [128, 128], F32, ssetup)
        acc = sb("bacc", [128, 12, 128], F32, ssetup)
        prod = sb("bprod", [128, 12, 128], F32, ssetup)
        oh = sb("boh", [128, 128], F32, ssetup)
        kb.dma('sp', lambda q: q.dma_start(out=rbb[:].rearrange("p b h -> p (b h)"), in_=rel_bias.rearrange("b h -> (b h)").partition_broadcast(128)), writes=['rbb'])
        kb.dma('sp', lambda q: q.dma_start(out=bkt[:], in_=c_bkt[:, :, :]), writes=['bkt'])
        kb.dma('sp', lambda q: q.dma_start(out=caus[:], in_=c_caus[:, :]), writes=['caus'])
        for dt in range(2):
            kb.op('dve', lambda v: v.memset(acc[:], 0.0), writes=['bacc'])
            for b in range(32):
                kb.op('dve', lambda v, b=b, dt=dt: v.tensor_scalar(out=oh[:], in0=bkt[:, dt, :], scalar1=float(b), scalar2=None, op0=ALU.is_equal),
                      reads=['bkt'], writes=['boh'])
                kb.op('dve', lambda v, b=b: v.tensor_tensor(out=prod[:], in0=oh[:].unsqueeze(1).to_broadcast([128, 12, 128]),
                      in1=rbb[:, b, :].unsqueeze(2).to_broadcast([128, 12, 128]), op=ALU.mult), reads=['boh', 'rbb'], writes=['bprod'])
                kb.op('dve', lambda v: v.tensor_tensor(out=acc[:], in0=acc[:], in1=prod[:], op=ALU.add), reads=['bacc', 'bprod'], writes=['bacc'])
            kb.op('dve', lambda v: v.tensor_tensor(out=acc[:], in0=acc[:], in1=rbb[:, 31, :].unsqueeze(2).to_broadcast([128, 12, 128]), op=ALU.subtract),
                  reads=['bacc', 'rbb'], writes=['bacc'])
            if dt == 0:
                kb.op('dve', lambda v: v.tensor_tensor(out=acc[:], in0=acc[:], in1=caus[:].unsqueeze(1).to_broadcast([128, 12, 128]), op=ALU.add),
                      reads=['bacc', 'caus'], writes=['bacc'])
            kb.op('dve', lambda v, dt=dt: v.tensor_copy(out=BT[:, dt, :, :], in_=acc[:]), reads=['bacc'], writes=['BT'])
        kb.barrier()
        ssetup.close()

        xf = [sb(f"xf1_{i}", [128, 2, D], F32, s1) for i in range(2)]
        xbf = sb("xbf1", [128, 2, D], BF16, s1)
        xT = sb("xT1", [128, 8, T], BF16, s1)
        qT = sb("qT1", [128, 6, T], BF16, s1)
        qlb = [sb(f"qlb{i}", [128, 12, T], BF16, s1) for i in range(2)]
        iqT = [sb(f"iqT{i}", [128, 2, T], BF16, s1) for i in range(2)]
        mqT = sb("mqT1", [128, 2, T], BF16, s1)
        memo = [sb(f"memo{i}", [128, 2, T], BF16, s1) for i in range(2)]
        E = sb("E1", [128, 2, 2, T], BF16, s1)
        rden = sb("rden1", [128, T], F32, s1)
        csb = [sb(f"csb{i}", [128, 128], F32, s1) for i in range(2)]
        cnb = [sb(f"cnb{i}", [128, 128], BF16, s1) for i in range(2)]
        iks = [sb(f"iks{i}", [128, 68], F32, s1) for i in range(2)]
        ik2 = [sb(f"ik2{i}", [128, 128], BF16, s1) for i in range(2)]
        sm1 = [sb(f"smp1_{i}", [128, 32], F32, s1) for i in range(2)]
        nch = ntiles * 128 // T
        for ci in range(nch):
            xi = ci % 2
            xfc, xfk = xf[xi], ('xf', xi)
            kb.dma('sp', lambda q, ci=ci, xfc=xfc: q.dma_start(
                out=xfc[:], in_=src[ci * T:(ci + 1) * T, :].rearrange("(t p) d -> p t d", p=128)), writes=[xfk])
            kb.op('act', lambda a, xfc=xfc: a.copy(out=xbf[:], in_=xfc[:]), reads=[xfk], writes=['xbf'])
            for k in range(8):
                pt, pk = psum()
                ptb = pt[:].bitcast(BF16)
                for t in range(2):
                    kb.op('pe', lambda p, ptb=ptb, t=t, k=k: p.transpose(out=ptb[:, t * 128:(t + 1) * 128],
                          in_=xbf[:, t, k * 128:(k + 1) * 128], identity=ident_b[:]), reads=['xbf', 'ident_b'], writes=[pk])
                if k % 2 == 0:
                    kb.op('dve', lambda v, ptb=ptb, k=k: v.tensor_copy(out=xT[:, k, :], in_=ptb[:, 0:T]), reads=[pk], writes=[('xT', k)])
                else:
                    kb.op('act', lambda a, ptb=ptb, k=k: a.copy(out=xT[:, k, :], in_=ptb[:, 0:T]), reads=[pk], writes=[('xT', k)])
            xTk = [('xT', k) for k in range(8)]

            def fm_proj(col0, dst_ap, dkey, eng):
                pt, pk = psum()
                for k in range(8):
                    kb.op('pe', lambda p, pt=pt, k=k: p.matmul(pt[:, 0:T], lhsT=win[:, k, col0:col0 + 128], rhs=xT[:, k, :],
                          start=(k == 0), stop=(k == 7)), reads=['win'] + xTk, writes=[pk])
                if eng == 'act':
                    kb.op('act', lambda a, pt=pt: a.copy(out=dst_ap, in_=pt[:, 0:T]), reads=[pk], writes=[dkey])
                else:
                    kb.op('dve', lambda v, pt=pt: v.tensor_copy(out=dst_ap, in_=pt[:, 0:T]), reads=[pk], writes=[dkey])

            for c in range(6):
                fm_proj(c * 128, qT[:, c, :], ('qT', c), 'act' if c % 2 else 'dve')
            bi = ci % 2
            for j in range(2):
                fm_proj(896 + j * 128, iqT[bi][:, j, :], ('iqT', bi), 'act')
            for j in range(2):
                fm_proj(1220 + j * 128, mqT[:, j, :], 'mqT1', 'dve')
            kb.dma('sp', lambda q, ci=ci, bi=bi: q.dma_start(out=iqd[:, :, ci * T:(ci + 1) * T].rearrange("j p t -> p j t"), in_=iqT[bi][:]),
                   reads=[('iqT', bi)], writes=['iqd'])
            for h in range(12):
                j, hh = h // 2, h % 2
                pt, pk = psum()
                kb.op('pe', lambda p, pt=pt, j=j, hh=hh: p.matmul(pt[:, 0:T], lhsT=wukT[hh * 64:(hh + 1) * 64, j, :],
                      rhs=qT[hh * 64:(hh + 1) * 64, j, :], start=True, stop=True), reads=['wukT', ('qT', j)], writes=[pk])
                if h % 2 == 0:
                    kb.op('act', lambda a, pt=pt, h=h, bi=bi: a.activation(out=qlb[bi][:, h, :], in_=pt[:, 0:T], func=AF.Copy, scale=0.125),
                          reads=[pk], writes=[('qlb', bi)])
                else:
                    kb.op('dve', lambda v, pt=pt, h=h, bi=bi: v.tensor_scalar(out=qlb[bi][:, h, :], in0=pt[:, 0:T], scalar1=0.125, scalar2=None, op0=ALU.mult),
                          reads=[pk], writes=[('qlb', bi)])
            kb.dma('sp', lambda q, ci=ci, bi=bi: q.dma_start(out=qlat[:, :, ci * T:(ci + 1) * T].rearrange("h p t -> p h t"), in_=qlb[bi][:]),
                   reads=[('qlb', bi)], writes=['qlat'])
            mem_attn(mqT, T, kT, vpad, onespad, lambda pr, bi=bi: memo[bi][:, pr, :], E, rden, '1')
            kb.dma('sp', lambda q, ci=ci, bi=bi: q.dma_start(out=memod[:, :, ci * T:(ci + 1) * T].rearrange("j p t -> p j t"), in_=memo[bi][:]),
                   reads=[('mixT1', 6), ('mixT1', 7)], writes=['memod'])
            for t in range(T // 128):
                ti = ci * (T // 128) + t
                i2 = ti % 2
                smk = ('sm1', i2)
                sm = sm1[i2]
                pc, pck = psum()
                for k in range(8):
                    kb.op('pe', lambda p, pc=pc, k=k, t=t: p.matmul(pc[:, 0:128], lhsT=xT[:, k, t * 128:(t + 1) * 128], rhs=win[:, k, 768:896],
                          start=(k == 0), stop=(k == 7)), reads=['win'] + xTk, writes=[pck])
                pi_, pik = psum()
                for k in range(8):
                    kb.op('pe', lambda p, pi_=pi_, k=k, t=t: p.matmul(pi_[:, 0:68], lhsT=xT[:, k, t * 128:(t + 1) * 128], rhs=win[:, k, 1152:1220],
                          start=(k == 0), stop=(k == 7)), reads=['win'] + xTk, writes=[pik])
                ss = sm[:, 0:1]
                kb.op('act', lambda a, pc=pc, i2=i2, ss=ss: a.activation(out=csb[i2][:], in_=pc[:, 0:128], func=AF.Square, accum_out=ss),
                      reads=[pck], writes=[('csb', i2), smk])
                kb.op('act', lambda a, ss=ss: a.activation(out=ss, in_=ss, func=AF.Sqrt, bias=1e-6, scale=1.0 / 128.0), reads=[smk], writes=[smk])
                kb.op('dve', lambda v, ss=ss: v.reciprocal(out=ss, in_=ss), reads=[smk], writes=[smk])
                kb.op('dve', lambda v, pc=pc, i2=i2, ss=ss: v.scalar_tensor_tensor(out=csb[i2][:], in0=pc[:, 0:128], scalar=ss, in1=gkv[:],
                      op0=ALU.mult, op1=ALU.mult), reads=[pck, smk, 'gkv', ('csb', i2)], writes=[('csb', i2)])
                kb.op('act', lambda a, i2=i2, ti=ti: a.copy(out=cTok[:, ti, :], in_=csb[i2][:]), reads=[('csb', i2)], writes=[('cTok', ti)])
                pt, pk = psum()
                ptb = pt[:].bitcast(BF16)
                kb.op('pe', lambda p, ptb=ptb, ti=ti: p.transpose(out=ptb[:, 0:128], in_=cTok[:, ti, :], identity=ident_b[:]),
                      reads=[('cTok', ti), 'ident_b'], writes=[pk])
                kb.op('dve', lambda v, ptb=ptb, ti=ti: v.tensor_copy(out=cT[:, ti * 128:(ti + 1) * 128], in_=ptb[:, 0:128]), reads=[pk], writes=[('cT', ti)])
                kb.op('act', lambda a, pi_=pi_, i2=i2: a.copy(out=iks[i2][:], in_=pi_[:, 0:68]), reads=[pik], writes=[('iks', i2)])
                st6 = sm[:, 8:14]
                mv = sm[:, 14:16]
                rs = sm[:, 16:17]
                kb.op('dve', lambda v, i2=i2, st6=st6: v.bn_stats(out=st6, in_=iks[i2][:, 0:64]), reads=[('iks', i2)], writes=[smk])
                kb.op('dve', lambda v, st6=st6, mv=mv: v.bn_aggr(out=mv, in_=st6), reads=[smk], writes=[smk])
                kb.op('act', lambda a, mv=mv, rs=rs: a.activation(out=rs, in_=mv[:, 1:2], func=AF.Sqrt, bias=1e-5, scale=1.0), reads=[smk], writes=[smk])
                kb.op('dve', lambda v, rs=rs: v.reciprocal(out=rs, in_=rs), reads=[smk], writes=[smk])
                kb.op('dve', lambda v, i2=i2, mv=mv, rs=rs: v.tensor_scalar(out=iks[i2][:, 0:64], in0=iks[i2][:, 0:64], scalar1=mv[:, 0:1], scalar2=rs,
                      op0=ALU.subtract, op1=ALU.mult), reads=[('iks', i2), smk], writes=[('iks', i2)])
                kb.op('dve', lambda v, i2=i2: v.tensor_tensor(out=iks[i2][:, 0:64], in0=iks[i2][:, 0:64], in1=gik[:], op=ALU.mult),
                      reads=[('iks', i2), 'gik'], writes=[('iks', i2)])
                for r in range(2):
                    kb.op('dve', lambda v, i2=i2, r=r: v.tensor_tensor(out=ik2[i2][:, r * 64:(r + 1) * 64], in0=iks[i2][:, 0:64], in1=bik[:], op=ALU.add),
                          reads=[('iks', i2), 'bik'], writes=[('ik2', i2)])
                pt, pk = psum()
                ptb = pt[:].bitcast(BF16)
                kb.op('pe', lambda p, ptb=ptb, i2=i2: p.transpose(out=ptb[:, 0:128], in_=ik2[i2][:], identity=ident_b[:]),
                      reads=[('ik2', i2), 'ident_b'], writes=[pk])
                kb.op('act', lambda a, ptb=ptb, ti=ti: a.copy(out=ikT2[:, ti * 128:(ti + 1) * 128], in_=ptb[:, 0:128]), reads=[pk], writes=[('ikT2', ti)])
                kb.op('act', lambda a, i2=i2, ti=ti: a.activation(out=absw[:, ti, :], in_=iks[i2][:, 64:68], func=AF.Abs),
                      reads=[('iks', i2)], writes=[('absw', ti)])
                kb.op('dve', lambda v, i2=i2, ti=ti: v.tensor_scalar(out=sgnw[:, ti, :], in0=iks[i2][:, 64:68], scalar1=0.0, scalar2=2.0, op0=ALU.is_ge, op1=ALU.mult),
                      reads=[('iks', i2)], writes=[('sgnw', ti)])
                kb.op('dve', lambda v, ti=ti: v.tensor_scalar(out=sgnw[:, ti, :], in0=sgnw[:, ti, :], scalar1=-1.0, scalar2=None, op0=ALU.add),
                      reads=[('sgnw', ti)], writes=[('sgnw', ti)])
        kb.barrier()
        s1.close()

        s2 = ExitStack()
        sc = sb("sc", [128, S], F32, s2)
        junk = sb("junk", [128, S // 2 + 128], mybir.dt.uint8, s2)
        junk2 = sb("junk2", [128, S], mybir.dt.uint8, s2) if False else None
        maskb = sb("maskb", [128, S], BF16, s2)
        tiec = [sb(f"tiec{i}", [128, CH], BF16, s2) for i in range(2)]
        cumc = [sb(f"cumc{i}", [128, CH], F32, s2) for i in range(2)]
        onesc = sb("onesc", [128, CH], BF16, s2)
        kb.op('pool', lambda g: g.memset(onesc[:], 1.0), writes=['onesc'])
        cmask = sb("cmask", [128, 128], F32, s2)
        kb.dma('sp', lambda q: q.dma_start(out=cmask[:], in_=c_cmask[:, :]), writes=['cmask'])
        rl = [sb(f"rl{i}", [128, 512], F32, s2) for i in range(4)]
        ql = sb("ql", [128, 12, 128], BF16, s2)
        iqb = [sb(f"iqb{i}", [128, 2, 128], BF16, s2) for i in range(2)]
        mixT = [sb(f"mixT1_{i}", [128, 8, 128], BF16, s2) for i in range(2)]
        x2t = sb("x2t", [128, D], F32, s2)
        Pt = [sb(f"Pt{i}", [128, 512], BF16, s2) for i in range(3)]
        olat = [sb(f"olat{i}", [128, 512], BF16, s2) for i in range(2)]
        Dsb = sb("Dsb", [128, 512], F32, s2)
        Osb = sb("Osb", [128, 512], F32, s2)
        bs = sb("bs", [128, 16], F32, s2)
        steps = sb("steps", [128, 24], F32, s2)
        nmid = sb("nmid", [128, 1], F32, s2)
        sgs = sb("sgs", [128, 1], F32, s2)
        pow2 = sb("pow2", [128, 24], F32, s2)
        kb.dma('sp', lambda q: q.dma_start(out=pow2[:], in_=c_pow2[:, :]), writes=['pow2'])
        lo, hi, mid, cnt, ge, dd, ee, need, cgt, carry = [bs[:, i:i + 1] for i in range(10)]
        npt = [0]
        natt = [0]
        psrot[0] = 4

        def selection_a(qb):
            n = (qb + 1) * 128
            ib = qb % 2
            kb.dma('sp', lambda q: q.dma_start(out=iqb[ib][:], in_=iqd[:, :, qb * 128:(qb + 1) * 128].rearrange("j p t -> p j t")),
                   writes=[('iqb', ib)])
            for g0 in range(0, n, 512):
                w = min(512, n - g0)
                pts = []
                for h in range(4):
                    j, hh = h // 2, h % 2
                    pt, pk = psum()
                    pts.append((pt, pk))
                    kb.op('pe', lambda p, pt=pt, j=j, hh=hh: p.matmul(pt[:, 0:w], lhsT=iqb[ib][hh * 64:(hh + 1) * 64, j, :],
                          rhs=ikT2[hh * 64:(hh + 1) * 64, g0:g0 + w], start=True, stop=True), reads=[('iqb', ib), 'ikT2'], writes=[pk])
                for h in range(4):
                    pt, pk = pts[h]
                    kb.op('act', lambda a, pt=pt, h=h: a.activation(out=rl[h][:, 0:w], in_=pt[:, 0:w], func=AF.Relu, scale=absw[:, qb, h:h + 1]),
                          reads=[pk], writes=[('rl', h)])
                yield
                for h in range(4):
                    if h == 0:
                        kb.op('dve', lambda v, h=h: v.tensor_scalar(out=sc[:, g0:g0 + w], in0=rl[h][:, 0:w], scalar1=sgnw[:, qb, h:h + 1],
                              scalar2=None, op0=ALU.mult), reads=[('rl', h)], writes=['sc'])
                    else:
                        kb.op('dve', lambda v, h=h: v.scalar_tensor_tensor(out=sc[:, g0:g0 + w], in0=rl[h][:, 0:w], scalar=sgnw[:, qb, h:h + 1],
                              in1=sc[:, g0:g0 + w], op0=ALU.mult, op1=ALU.add), reads=[('rl', h), 'sc'], writes=['sc'])
                yield
            kb.op('dve', lambda v: v.tensor_tensor(out=sc[:, n - 128:n], in0=sc[:, n - 128:n], in1=cmask[:], op=ALU.add), reads=['sc', 'cmask'], writes=['sc'])
            kb.op('dve', lambda v: v.tensor_reduce(out=hi, in_=sc[:, 0:n], axis=AX.X, op=ALU.max), reads=['sc'], writes=['bs'])
            kb.op('dve', lambda v: v.tensor_reduce(out=lo, in_=sc[:, 0:256], axis=AX.X, op=ALU.min), reads=['sc'], writes=['bs'])
            yield
            kb.op('dve', lambda v: v.scalar_tensor_tensor(out=dd, in0=hi, scalar=2.0, in1=lo, op0=ALU.add, op1=ALU.subtract), reads=['bs'], writes=['bs'])
            kb.op('dve', lambda v: v.tensor_scalar(out=steps[:], in0=pow2[:], scalar1=dd, scalar2=None, op0=ALU.mult), reads=['bs', 'pow2'], writes=['steps'])
            kb.op('dve', lambda v: v.scalar_tensor_tensor(out=mid, in0=lo, scalar=-1.0, in1=steps[:, 0:1], op0=ALU.add, op1=ALU.add), reads=['bs', 'steps'], writes=['bs'])
            yield
            hsp = ((n // 2) // 128) * 128
            wact = n - hsp
            for it in range(NBIS):
                kb.op('dve', lambda v: v.tensor_scalar(out=junk[:, 0:hsp], in0=sc[:, 0:hsp], scalar1=mid, scalar2=None, op0=ALU.is_ge, op1=ALU.add, accum_out=cnt),
                      reads=['sc', 'bs'], writes=['junk', 'bs'])
                kb.op('dve', lambda v: v.tensor_scalar(out=junk[:, 0:wact], in0=sc[:, hsp:n], scalar1=mid, scalar2=cnt, op0=ALU.is_ge, op1=ALU.add, accum_out=cnt),
                      reads=['sc', 'bs'], writes=['junk', 'bs'])
                yield
                kb.op('dve', lambda v: v.tensor_scalar(out=ge, in0=cnt, scalar1=float(KSEL), scalar2=0.5, op0=ALU.is_ge, op1=ALU.subtract), reads=['bs'], writes=['bs'])
                kb.op('dve', lambda v, it=it: v.scalar_tensor_tensor(out=mid, in0=ge, scalar=steps[:, it:it + 1], in1=mid, op0=ALU.mult, op1=ALU.add),
                      reads=['bs', 'steps'], writes=['bs'])
                yield
            kb.op('dve', lambda v: v.tensor_tensor(out=lo, in0=mid, in1=steps[:, NBIS:NBIS + 1], op=ALU.subtract), reads=['bs', 'steps'], writes=['bs'])
            kb.op('dve', lambda v: v.tensor_tensor(out=hi, in0=mid, in1=steps[:, NBIS:NBIS + 1], op=ALU.add), reads=['bs', 'steps'], writes=['bs'])
            kb.op('dve', lambda v: v.tensor_scalar(out=junk[:, 0:hsp], in0=sc[:, 0:hsp], scalar1=hi, scalar2=None, op0=ALU.is_ge, op1=ALU.add, accum_out=cgt),
                  reads=['sc', 'bs'], writes=['junk', 'bs'])
            kb.op('dve', lambda v: v.tensor_scalar(out=junk[:, 0:wact], in0=sc[:, hsp:n], scalar1=hi, scalar2=cgt, op0=ALU.is_ge, op1=ALU.add, accum_out=cgt),
                  reads=['sc', 'bs'], writes=['junk', 'bs'])
            kb.op('dve', lambda v: v.tensor_scalar(out=need, in0=cgt, scalar1=-1.0, scalar2=float(KSEL), op0=ALU.mult, op1=ALU.add), reads=['bs'], writes=['bs'])
            yield

        def selection_b(qb):
            n = (qb + 1) * 128
            for ci_, c0 in enumerate(range(0, n, CH)):
                w = min(CH, n - c0)
                r_ = ci_ % 2
                tk, ck = ('tiec', r_), ('cumc', r_)
                kb.op('dve', lambda v, c0=c0, w=w, r_=r_: v.tensor_scalar(out=tiec[r_][:, 0:w], in0=sc[:, c0:c0 + w], scalar1=hi, scalar2=None, op0=ALU.is_lt),
                      reads=['sc', 'bs'], writes=[tk])
                kb.op('dve', lambda v, c0=c0, w=w, r_=r_: v.scalar_tensor_tensor(out=tiec[r_][:, 0:w], in0=sc[:, c0:c0 + w], scalar=lo, in1=tiec[r_][:, 0:w],
                      op0=ALU.is_ge, op1=ALU.mult), reads=['sc', 'bs', tk], writes=[tk])
                init = 0.0 if c0 == 0 else carry
                kb.op('dve', lambda v, w=w, r_=r_, init=init: v.tensor_tensor_scan(out=cumc[r_][:, 0:w], data0=onesc[:, 0:w], data1=tiec[r_][:, 0:w],
                      initial=init, op0=ALU.mult, op1=ALU.add), reads=['onesc', tk, 'bs'], writes=[ck])
                kb.op('dve', lambda v, w=w, r_=r_: v.tensor_copy(out=carry, in_=cumc[r_][:, w - 1:w]), reads=[ck], writes=['bs'])
                kb.op('dve', lambda v, w=w, r_=r_: v.scalar_tensor_tensor(out=tiec[r_][:, 0:w], in0=cumc[r_][:, 0:w], scalar=need, in1=tiec[r_][:, 0:w],
                      op0=ALU.is_le, op1=ALU.mult), reads=[ck, 'bs', tk], writes=[tk])
                kb.op('dve', lambda v, c0=c0, w=w, r_=r_: v.scalar_tensor_tensor(out=maskb[:, c0:c0 + w], in0=sc[:, c0:c0 + w], scalar=hi, in1=tiec[r_][:, 0:w],
                      op0=ALU.is_ge, op1=ALU.add), reads=['sc', 'bs', tk], writes=['maskb'])
                yield

        def attention(qb):
            mx = mixT[qb % 2]
            kb.dma('sp', lambda q: q.dma_start(out=ql[:], in_=qlat[:, :, qb * 128:(qb + 1) * 128].rearrange("h p t -> p h t")), writes=['ql'])
            kb.dma('sp', lambda q: q.dma_start(out=mx[:, 6:8, :], in_=memod[:, :, qb * 128:(qb + 1) * 128].rearrange("j p t -> p j t")),
                   writes=[('mixTm', qb % 2)])
            pO, pOk = ps[6], ('ps', 6)
            pD, pDk = ps[7], ('ps', 7)
            steps_ = [(hg, j) for hg in range(3) for j in range(qb + 1)]

            def logits(hg, j):
                qrhs = ql[:, hg * 4:(hg + 1) * 4, :].rearrange("p h t -> p (h t)")
                li = 4 + (natt[0] % 2)
                natt[0] += 1
                pL, pLk = ps[li], ('ps', li)
                dt = qb - j
                nmm = 1 + (1 if qb >= 2 else 0) + (1 if dt <= 1 else 0)
                m = 0
                kb.op('pe', lambda p: p.matmul(pL[:, :], lhsT=cT[:, j * 128:(j + 1) * 128], rhs=qrhs, start=True, stop=(nmm == 1)),
                      reads=[('cT', j), 'ql'], writes=[pLk])
                m += 1
                if qb >= 2:
                    kb.op('pe', lambda p, m=m: p.matmul(pL[:, :], lhsT=maskb[:, j * 128:(j + 1) * 128],
                          rhs=i4big[:].rearrange("p r t -> p (r t)"), start=False, stop=(m == nmm - 1)), reads=['maskb', 'i4big'], writes=[pLk])
                    m += 1
                if dt <= 1:
                    kb.op('pe', lambda p, m=m: p.matmul(pL[:, :], lhsT=ident_b[:],
                          rhs=BT[:, dt, hg * 4:(hg + 1) * 4, :].rearrange("p h t -> p (h t)"), start=False, stop=(m == nmm - 1)),
                          reads=['BT', 'ident_b'], writes=[pLk])
                    m += 1
                return pL, pLk

            cur = logits(*steps_[0])
            for idx, (hg, j) in enumerate(steps_):
                pL, pLk = cur
                pi = npt[0] % 3
                npt[0] += 1
                kb.op('act', lambda a, pL=pL, pi=pi: a.activation(out=Pt[pi][:], in_=pL[:, :], func=AF.Exp, bias=(nbias[:] if qb >= 2 else zbias[:]), scale=1.0),
                      reads=[pLk, 'nbias'], writes=[('Pt', pi)])
                if idx + 1 < len(steps_):
                    cur = logits(*steps_[idx + 1])
                kb.op('pe', lambda p, j=j, pi=pi: p.matmul(pO[:, :], lhsT=cTok[:, j, :], rhs=Pt[pi][:], start=(j == 0), stop=(j == qb)),
                      reads=[('cTok', j), ('Pt', pi)], writes=[pOk])
                kb.op('pe', lambda p, j=j, pi=pi: p.matmul(pD[:, :], lhsT=ones_b[:], rhs=Pt[pi][:], start=(j == 0), stop=(j == qb)),
                      reads=['ones_b', ('Pt', pi)], writes=[pDk])
                yield
                if j == qb:
                    oi = hg % 2
                    kb.op('act', lambda a: a.copy(out=Dsb[:], in_=pD[:, :]), reads=[pDk], writes=['Dsb'])
                    kb.op('act', lambda a: a.copy(out=Osb[:], in_=pO[:, :]), reads=[pOk], writes=['Osb'])
                    kb.op('dve', lambda v: v.reciprocal(out=Dsb[:], in_=Dsb[:]), reads=['Dsb'], writes=['Dsb'])
                    kb.op('pool', lambda g, oi=oi: g.tensor_tensor(out=olat[oi][:], in0=Osb[:], in1=Dsb[:], op=ALU.mult), reads=['Osb', 'Dsb'], writes=[('olat', oi)])
                    for pp in range(2):
                        pT, pTk = psum()
                        for hh in range(2):
                            hl = 2 * pp + hh
                            h = hg * 4 + hl
                            kb.op('pe', lambda p, pT=pT, h=h, hl=hl, hh=hh, oi=oi: p.matmul(pT[:, 0:128], lhsT=wuvpad[:, h, :], rhs=olat[oi][:, hl * 128:(hl + 1) * 128],
                                  start=(hh == 0), stop=(hh == 1)), reads=['wuvpad', ('olat', oi)], writes=[pTk])
                        kb.op('act', lambda a, pT=pT, hg=hg, pp=pp: a.copy(out=mx[:, hg * 2 + pp, :], in_=pT[:, 0:128]), reads=[pTk], writes=[('mixTt', qb % 2, hg * 2 + pp)])
                    yield

        nbias = sb("nbias", [128, 1], F32, s2)
        zbias = sb("zbias", [128, 1], F32, s2)
        kb.op('dve', lambda v: v.memset(nbias[:], -100.0), writes=['nbias'])
        kb.op('dve', lambda v: v.memset(zbias[:], 0.0), writes=['nbias'])

        nqb = ntiles

        def chain(*gens):
            for g in gens:
                yield from g

        def nsteps_sel(qb):
            n = (qb + 1) * 128
            return 2 * ((n + 511) // 512) + 3 + 2 * NBIS

        def tail_stream(qb):
            mx = mixT[qb % 2]
            mkeys = [('mixTt', qb % 2, k) for k in range(6)] + [('mixTm', qb % 2)]
            kb.dma('sp', lambda q: q.dma_start(out=x2t[:], in_=src[qb * 128:(qb + 1) * 128, :]), writes=['x2t'])
            yield
            yield from tail.run_gen(qb, lambda k, mx=mx: mx[:, k, :], mkeys, x2t, 'x2t', x1buf[qb * 128:(qb + 1) * 128, :])

        if nqb > 2:
            for _ in selection_a(2):
                pass
        gT = None
        for qb in range(nqb):
            if qb >= 2:
                gS = selection_b(qb)
                st_ = [(gS, 2 * (qb + 1))]
                if gT is not None:
                    st_.append((gT, 40))
                interleave(st_, until=gS)
            streams = [(attention(qb), 3 * (qb + 1) + 3)]
            if gT is not None:
                streams.append((gT, 12))
            if qb + 1 < nqb and qb + 1 >= 2:
                streams.append((selection_a(qb + 1), nsteps_sel(qb + 1)))
            interleave(streams)
            gT = tail_stream(qb)
        for _ in gT:
            pass
        psrot[0] = 6
        kb.barrier()
        s2.close()
        st.close()

    def phase_moe(layer, dst, ntiles=NT, nexp=NE):
        L = layer
        st = ExitStack()
        w1 = [sb(f"w1_{L}_{i}", [128, 8, 2 * D], BF16, st) for i in range(2)]
        w2 = [sb(f"w2_{L}_{i}", [128, 8, D], BF16, st) for i in range(2)]
        b2r = [sb(f"b2r_{L}_{i}", [1, D], BF16, st) for i in range(2)]
        b1a = sb(f"b1a_{L}", [128, NE, 16], F32, st)
        b1u = sb(f"b1u_{L}", [128, NE, 8], F32, st)
        with nc.allow_non_contiguous_dma(reason="bias layout"):
            kb.dma('sp', lambda q: q.dma_start(out=b1a[:], in_=exp_b1[L].rearrange("e (j p) -> p e j", p=128)), writes=['b1a'])
        kb.op('dve', lambda v: v.tensor_scalar(out=b1u[:], in0=b1a[:, :, 8:16], scalar1=1.0, scalar2=None, op0=ALU.add), reads=['b1a'], writes=['b1u'])
        xgt = [sb(f"xgt_{L}_{i}", [128, RG // 128, D], BF16, st) for i in range(2)]
        xgT = [sb(f"xgT_{L}_{i}", [128, 8, RG], BF16, st) for i in range(2)]
        actT = [sb(f"actT_{L}_{i}", [128, 8, RG], BF16, st) for i in range(2)]
        tg = [sb(f"tg_{L}_{i}", [128, RG], F32, st) for i in range(2)]
        tu = [sb(f"tu_{L}_{i}", [128, RG], F32, st) for i in range(2)]
        tsg = [sb(f"tsg_{L}_{i}", [128, RG], F32, st) for i in range(2)]
        tt = [sb(f"tt_{L}_{i}", [128, RG], F32, st) for i in range(2)]
        yev = [sb(f"yev_{L}_{i}", [128, D], F32, st) for i in range(2)]
        nyev = 0

        def load_w(e):
            i = e % 2
            load_cast(b2r[i][:], exp_b2[L, e:e + 1, :], ('b2r', i))
            load_cast(w1[i][:], exp_w1[L, e].rearrange("(k p) f -> p k f", p=128), ('w1', i))
            load_cast(w2[i][:], exp_w2[L, e].rearrange("(k p) n -> p k n", p=128), ('w2', i))

        NG = CAP // RG

        def stage_a(e, g, gi):
            wi = e % 2
            for k in range(8):
                pt, pk = psum()
                ptb = pt[:].bitcast(BF16)
                for t in range(RG // 128):
                    kb.op('pe', lambda p, ptb=ptb, t=t, k=k: p.transpose(out=ptb[:, t * 128:(t + 1) * 128],
                          in_=xgt[gi][:, t, k * 128:(k + 1) * 128], identity=ident_b[:]), reads=[('xgt', gi), 'ident_b'], writes=[pk])
                if k % 2 == 0:
                    kb.op('dve', lambda v, ptb=ptb, k=k: v.tensor_copy(out=xgT[gi][:, k, :], in_=ptb[:, 0:RG]), reads=[pk], writes=[('xgT', gi, k)])
                else:
                    kb.op('act', lambda a, ptb=ptb, k=k: a.copy(out=xgT[gi][:, k, :], in_=ptb[:, 0:RG]), reads=[pk], writes=[('xgT', gi, k)])
                if k % 2 == 1:
                    yield
            xgTk = [('xgT', gi, k) for k in range(8)]
            for j in range(8):
                ji = j % 2
                pg, pgk = psum()
                pu, puk = psum()
                for k in range(8):
                    kb.op('pe', lambda p, pg=pg, k=k, j=j: p.matmul(pg[:, 0:RG], lhsT=w1[wi][:, k, j * 128:(j + 1) * 128],
                          rhs=xgT[gi][:, k, :], start=(k == 0), stop=(k == 7)), reads=[('w1', wi)] + xgTk, writes=[pgk])
                for k in range(8):
                    kb.op('pe', lambda p, pu=pu, k=k, j=j: p.matmul(pu[:, 0:RG], lhsT=w1[wi][:, k, D + j * 128:D + (j + 1) * 128],
                          rhs=xgT[gi][:, k, :], start=(k == 0), stop=(k == 7)), reads=[('w1', wi)] + xgTk, writes=[puk])
                kb.op('dve', lambda v, pg=pg, ji=ji, j=j: v.tensor_scalar(out=tg[ji][:], in0=pg[:, 0:RG], scalar1=b1a[:, e, j:j + 1],
                      scalar2=7.0, op0=ALU.add, op1=ALU.min), reads=[pgk, 'b1a'], writes=[('tg', ji)])
                kb.op('act', lambda a, ji=ji: a.activation(out=tsg[ji][:], in_=tg[ji][:], func=AF.Sigmoid, scale=1.702),
                      reads=[('tg', ji)], writes=[('tsg', ji)])
                kb.op('dve', lambda v, pu=pu, ji=ji, j=j: v.tensor_scalar(out=tu[ji][:], in0=pu[:, 0:RG], scalar1=b1u[:, e, j:j + 1],
                      scalar2=8.0, op0=ALU.add, op1=ALU.min), reads=[puk, 'b1u'], writes=[('tu', ji)])
                kb.op('dve', lambda v, ji=ji: v.scalar_tensor_tensor(out=tt[ji][:], in0=tu[ji][:], scalar=-6.0, in1=tg[ji][:],
                      op0=ALU.max, op1=ALU.mult), reads=[('tu', ji), ('tg', ji)], writes=[('tt', ji)])
                kb.op('dve', lambda v, ji=ji, j=j: v.tensor_tensor(out=actT[gi][:, j, :], in0=tt[ji][:], in1=tsg[ji][:], op=ALU.mult),
                      reads=[('tt', ji), ('tsg', ji)], writes=[('actT', gi, j)])
                yield

        def stage_b(e, g, gi):
            nonlocal nyev
            wi = e % 2
            r0 = e * CAP + g * RG
            actk = [('actT', gi, j) for j in range(8)]
            for t in range(RG // 128):
                yi = nyev % 2
                nyev += 1
                for nh in range(2):
                    py, pyk = psum()
                    for k in range(8):
                        kb.op('pe', lambda p, py=py, k=k, t=t, nh=nh: p.matmul(py[:, :], lhsT=actT[gi][:, k, t * 128:(t + 1) * 128],
                              rhs=w2[wi][:, k, nh * 512:(nh + 1) * 512], start=(k == 0), stop=False), reads=[('w2', wi)] + actk, writes=[pyk])
                    kb.op('pe', lambda p, py=py, nh=nh: p.matmul(py[:, :], lhsT=ones_b[0:1, :], rhs=b2r[wi][0:1, nh * 512:(nh + 1) * 512],
                          start=False, stop=True), reads=[('b2r', wi), 'ones_b'], writes=[pyk])
                    kb.op('act', lambda a, py=py, nh=nh, yi=yi: a.copy(out=yev[yi][:, nh * 512:(nh + 1) * 512], in_=py[:, :]),
                          reads=[pyk], writes=[('yev', yi)])
                    yield
                rr0 = r0 + t * 128
                kb.dma('sp', lambda q, rr0=rr0, yi=yi: q.dma_start(out=yg[rr0:rr0 + 128, :], in_=yev[yi][:]), reads=[('yev', yi)], writes=['yg'])

        groups = [(e, g) for e in range(nexp) for g in range(NG)]

        def load_x(i):
            e_, g_ = groups[i]
            r0 = e_ * CAP + g_ * RG
            kb.dma('sp', lambda q: q.dma_start(out=xgt[i % 2][:], in_=xg[r0:r0 + RG, :].rearrange("(t p) d -> p t d", p=128)),
                   reads=['xg'], writes=[('xgt', i % 2)])

        load_x(0)
        if len(groups) > 1:
            load_x(1)
        load_w(0)
        if nexp > 1:
            load_w(1)
        for _ in stage_a(groups[0][0], groups[0][1], 0):
            pass
        for i, (e, g) in enumerate(groups):
            if i + 2 < len(groups):
                load_x(i + 2)
            if g == 0 and e >= 1 and e + 1 < nexp:
                load_w(e + 1)
            streams = [(stage_b(e, g, i % 2), 6)]
            if i + 1 < len(groups):
                e2, g2 = groups[i + 1]
                streams.append((stage_a(e2, g2, (i + 1) % 2), 20))
            interleave(streams)
        kb.barrier()
        st.close()
        st = ExitStack()
        lng = sb(f"ln2g{L}", [128, D], F32, st)
        lnb = sb(f"ln2b{L}", [128, D], F32, st)
        bcast_rows(lng[:], ln2_g[L], 'lng2')
        bcast_rows(lnb[:], ln2_b[L], 'lnb2')
        yk = [[sb(f"yk{L}_{i}_{k}", [128, D], F32, st) for k in range(4)] for i in range(2)]
        x1r = [sb(f"x1r{L}_{i}", [128, D], F32, st) for i in range(2)]
        zz = [sb(f"zz{L}_{i}", [128, D], F32, st) for i in range(2)]
        oo = [sb(f"oo{L}_{i}", [128, D], F32, st) for i in range(2)]
        smm = [sb(f"smm{L}_{i}", [128, 256], F32, st) for i in range(2)]
        lnh = Tail.__new__(Tail)
        for ti in range(ntiles):
            i = ti % 2
            kb.dma('sp', lambda q, ti=ti, i=i: q.dma_start(out=x1r[i][:], in_=x1buf[ti * 128:(ti + 1) * 128, :]), reads=[('x1d', ti)], writes=[('x1r', i)])
            for k in range(4):
                kb.op('act', lambda a, i=i, k=k: a.memzero(yk[i][k][:]), writes=[('yk', i, k)])
                kb.dma('pool', lambda q, ti=ti, i=i, k=k: q.indirect_dma_start(
                    out=yk[i][k][:, :], out_offset=None, in_=yg[:, :],
                    in_offset=bass.IndirectOffsetOnAxis(ap=destall[:, ti, k:k + 1], axis=0),
                    bounds_check=bc_reg, oob_is_err=False), reads=['yg', ('dest', ti)], writes=[('yk', i, k)])
            kb.op('dve', lambda v, i=i: v.tensor_scalar(out=zz[i][:], in0=x1r[i][:], scalar1=ALPHA, scalar2=None, op0=ALU.mult),
                  reads=[('x1r', i)], writes=[('zz', i)])
            for k in range(4):
                kb.op('dve', lambda v, ti=ti, i=i, k=k: v.scalar_tensor_tensor(out=zz[i][:], in0=yk[i][k][:], scalar=gall[:, ti, k:k + 1],
                      in1=zz[i][:], op0=ALU.mult, op1=ALU.add), reads=[('yk', i, k), ('gate', ti), ('zz', i)], writes=[('zz', i)])
            Tail.layernorm(lnh, zz[i], ('zz', i), oo[i], ('oo', i), smm[i], ('smm', i), lng, lnb, 'lng2', 'lnb2', e2='dve')
            kb.dma('sp', lambda q, ti=ti, i=i: q.dma_start(out=dst[ti * 128:(ti + 1) * 128, :], in_=oo[i][:]), reads=[('oo', i)], writes=[('dst', L, ti)])
        kb.barrier()
        kb.pool_depth = 2
        st.close()

    if mode.startswith("a0"):
        ntl = NT if mode == "a0" else int(mode[2:])
        phase_a0(ntiles=ntl)
        st = ExitStack()
        cp = [sb(f"cp{i}", [128, D], F32, st) for i in range(2)]
        for ti in range(ntl):
            i = ti % 2
            kb.dma('sp', lambda q, ti=ti, i=i: q.dma_start(out=cp[i][:], in_=x1buf[ti * 128:(ti + 1) * 128, :]), writes=[('cp', i)])
            kb.dma('sp', lambda q, ti=ti, i=i: q.dma_start(out=out[ti * 128:(ti + 1) * 128, :], in_=cp[i][:]), reads=[('cp', i)], writes=[('o', ti)])
        kb.barrier()
        st.close()
    elif mode == "full":
        phase_a0()
        phase_moe(0, x2buf)
        phase_a1(x2buf)
        phase_moe(1, out)
    elif mode.startswith("a1"):
        ntl = int(mode[2:])
        zt = sb("zt1", [128, 4096], BF16)
        kb.op('pool', lambda g: g.memset(zt[:], 0.0), writes=['zt'])
        for r0 in range(0, NROW, 512):
            kb.dma('sp', lambda q, r0=r0: q.dma_start(out=xg[r0:r0 + 512, :].rearrange("(p t) d -> p (t d)", t=4), in_=zt[:]),
                   reads=['zt'], writes=['xg'])
        phase_a1(x_in, ntiles=ntl)
        st = ExitStack()
        cp = [sb(f"cp{i}", [128, D], F32, st) for i in range(2)]
        for ti in range(ntl):
            i = ti % 2
            kb.dma('sp', lambda q, ti=ti, i=i: q.dma_start(out=cp[i][:], in_=x1buf[ti * 128:(ti + 1) * 128, :]), writes=[('cp', i)])
            kb.dma('sp', lambda q, ti=ti, i=i: q.dma_start(out=out[ti * 128:(ti + 1) * 128, :], in_=cp[i][:]), reads=[('cp', i)], writes=[('o', ti)])
        kb.barrier()
        st.close()
    elif mode == "l0":
        phase_a0()
        phase_moe(0, out)
    es.close()
    return nc


def host_consts():
    ident = np.eye(128, dtype=np.float32)
    tri = np.triu(np.ones((128, 128), np.float32), 1)
    iota = np.tile(np.arange(NE, dtype=np.float32)[None, :], (128, 1))
    ecap = iota * CAP
    q = np.arange(128)
    cmask = np.where(q[None, :] <= q[:, None], 0.0, -2000.0).astype(np.float32)
    caus = np.where(q[:, None] <= q[None, :], 0.0, -30000.0).astype(np.float32)
    bkt = np.zeros((128, 2, 128), np.float32)
    for dt in range(2):
        rel = np.maximum(q[None, :] - q[:, None] + 128 * dt, 0)
        large = 16 + (np.log(np.maximum(rel, 1).astype(np.float32) / 16) / np.float32(np.log(128 / 16)) * 16).astype(np.int32)
        large = np.minimum(large, 31)
        bkt[:, dt, :] = np.where(rel < 16, rel, large)
    pow2 = np.tile((2.0 ** -(np.arange(24, dtype=np.float64) + 1)).astype(np.float32)[None, :], (128, 1))
    return {"c_ident": ident, "c_tri": tri, "c_iota": iota, "c_ecap": ecap, "c_cmask": cmask, "c_caus": caus, "c_bkt": bkt,
            "c_pow2": pow2}


_PARAMS = ["rel_bias", "a_w_in", "a_conv_w", "a_conv_b", "a_wr", "a_br", "a_wi", "a_bi", "a_lambda", "b_w_in",
           "b_kv_norm_g", "b_w_uk", "b_w_uv", "b_idx_norm_g", "b_idx_norm_b", "w_mem_kv", "w_out", "ln1_g", "ln1_b",
           "router_w", "router_b", "exp_w1", "exp_b1", "exp_w2", "exp_b2", "ln2_g", "ln2_b"]
_SQUEEZE = {"a_w_in", "a_conv_w", "a_conv_b", "a_wr", "a_br", "a_wi", "a_bi", "a_lambda", "b_w_in", "b_kv_norm_g",
            "b_w_uk", "b_w_uv", "b_idx_norm_g", "b_idx_norm_b"}


def make_in_maps(inputs, cores):
    shared = {}
    for k in _PARAMS:
        v = np.ascontiguousarray(np.asarray(inputs[k], dtype=np.float32))
        if k in _SQUEEZE:
            v = v[0]
        shared[k] = v
    shared.update(host_consts())
    maps = []
    for c in cores:
        m = dict(shared)
        m["x"] = np.ascontiguousarray(inputs["x"][c])
        m["mem"] = np.ascontiguousarray(inputs["mem"][c])
        maps.append(m)
    return maps


def kernel(**inputs):
    nc = build_program("full")
    maps = make_in_maps(inputs, list(range(8)))
    res = run_bass_kernel_spmd(nc, maps, core_ids=list(range(8)))
    return np.stack([r["out"] for r in res.results], axis=0)
```
